# Optimizing a Trainium2 kernel written in Bass

```python
import jax, jax.numpy as jnp
from jax import lax
import numpy as np

D_MODEL = 2048
BATCH = 4
SEQ = 2048
DEPTH = 2
DEC_BATCH = 4
DEC_SEQ = 8192
PAST_LEN = 128

MIX_W = D_MODEL
POOL_W = MIX_W // 4
N_POOL = 4
POOL_CH = POOL_W // N_POOL
POOL_WINDOWS = (2, 4, 8, 16)
CONV_W = MIX_W // 2
CONV_K = 31
FOURIER_W = MIX_W - POOL_W - CONV_W
N_FOURIER = 4
FOURIER_CH = FOURIER_W // N_FOURIER
IN_COLS = POOL_W + 2 * CONV_W + FOURIER_W
N_GROUPS = 4
EXPERTS_PER_GROUP = 8
N_EXPERTS = N_GROUPS * EXPERTS_PER_GROUP
TOP_K = 2
D_EXPERT = D_MODEL // 4
ROUTE_BLOCK = 128
EPS = 1e-6

kernel_name = "hybrid_pool_conv_fourier_hmoe_encoder"


def rmsnorm(x, g):
    xf = x.astype(jnp.float32)
    y = xf * lax.rsqrt(jnp.mean(xf * xf, axis=-1, keepdims=True) + EPS)
    return (y * g.astype(jnp.float32)).astype(x.dtype)


def layernorm(x, g, b):
    xf = x.astype(jnp.float32)
    xc = xf - jnp.mean(xf, axis=-1, keepdims=True)
    var = jnp.mean(xc * xc, axis=-1, keepdims=True)
    y = xc * lax.rsqrt(var + EPS) * g.astype(jnp.float32) + b.astype(jnp.float32)
    return y.astype(x.dtype)


def pool_mixer(u, w_grp, scale):
    B, S, _ = u.shape
    ug = u.astype(jnp.float32).reshape(B, S, N_POOL, POOL_CH)
    cs = jnp.pad(jnp.cumsum(ug, axis=1), ((0, 0), (1, 0), (0, 0), (0, 0)))
    t = jnp.arange(S)
    diffs = []
    for gi, w in enumerate(POOL_WINDOWS):
        lo = jnp.clip(t - w // 2, 0, S)
        hi = jnp.clip(t + (w - w // 2), 0, S)
        csg = cs[:, :, gi]
        mean = (csg[:, hi] - csg[:, lo]) / (hi - lo).astype(jnp.float32)[None, :, None]
        diffs.append(mean - ug[:, :, gi])
    d = jnp.stack(diffs, axis=2).astype(u.dtype)
    y = jnp.einsum('bsgc,gce->bsge', d, w_grp).reshape(B, S, POOL_W)
    return y * scale


def conv_module(u, dw_w, dw_b, ln_g, ln_b, pw_w, pw_b):
    a, b = jnp.split(u, 2, axis=-1)
    v = a * jax.nn.sigmoid(b)
    v = lax.conv_general_dilated(
        v, dw_w[:, None, :].astype(v.dtype), window_strides=(1,),
        padding=((CONV_K // 2, CONV_K // 2),),
        dimension_numbers=('NWC', 'WIO', 'NWC'),
        feature_group_count=CONV_W) + dw_b
    v = jax.nn.silu(layernorm(v, ln_g, ln_b))
    return v @ pw_w + pw_b


def fourier_mixer(u, w):
    B, S, _ = u.shape
    z = jnp.fft.fft2(u.astype(jnp.float32).reshape(B, S, N_FOURIER, FOURIER_CH), axes=(1, 3), norm='ortho')
    return jnp.real(z).reshape(B, S, FOURIER_W).astype(u.dtype) @ w


def hier_moe(h, wc, bc, wf, bf, w1, w3, w2):
    N, D = h.shape
    A = N * TOP_K
    lc = (h @ wc).astype(jnp.float32) + bc.astype(jnp.float32)
    pc = jax.nn.softmax(lc, axis=-1)
    grp = jnp.argmax(lc, axis=-1)
    p_grp = jnp.take_along_axis(pc, grp[:, None], axis=1)
    lf = jnp.einsum('nd,gde->nge', h, wf).astype(jnp.float32) + bf.astype(jnp.float32)
    lf = jnp.take_along_axis(lf, grp[:, None, None], axis=1)[:, 0]
    top_v, top_i = lax.top_k(lf, TOP_K)
    wts = (p_grp * jax.nn.softmax(top_v, axis=-1)).reshape(A)
    eid = (grp[:, None] * EXPERTS_PER_GROUP + top_i).reshape(A)
    tok = jnp.repeat(jnp.arange(N, dtype=jnp.int32), TOP_K)
    order = jnp.argsort(eid)
    e_s = eid[order]
    counts = jnp.bincount(eid, length=N_EXPERTS)
    padded = (counts + ROUTE_BLOCK - 1) // ROUTE_BLOCK * ROUTE_BLOCK
    pad_end = jnp.cumsum(padded)
    pad_start = pad_end - padded
    start = jnp.cumsum(counts) - counts
    dest = pad_start[e_s] + jnp.arange(A) - start[e_s]
    n_blocks = -(-(A + N_EXPERTS * (ROUTE_BLOCK - 1)) // ROUTE_BLOCK)
    L = n_blocks * ROUTE_BLOCK
    buf_tok = jnp.zeros((L,), jnp.int32).at[dest].set(tok[order])
    buf_w = jnp.zeros((L,), h.dtype).at[dest].set(wts[order].astype(h.dtype))
    blk_e = jnp.minimum(jnp.searchsorted(pad_end, jnp.arange(n_blocks) * ROUTE_BLOCK, side='right'), N_EXPERTS - 1)

    def expert_block(args):
        tb, wb, e = args
        xb = h[tb]
        yb = (jax.nn.silu(xb @ w1[e]) * (xb @ w3[e])) @ w2[e]
        return yb * wb[:, None]

    ys = lax.map(expert_block, (buf_tok.reshape(n_blocks, ROUTE_BLOCK), buf_w.reshape(n_blocks, ROUTE_BLOCK), blk_e))
    return jax.ops.segment_sum(ys.reshape(L, D), buf_tok, num_segments=N)


def trunk(x, c, w_ada, b_ada, norm1_g, w_in, pool_w, pool_scale, conv_dw_w, conv_dw_b,
          conv_ln_g, conv_ln_b, conv_pw_w, conv_pw_b, fourier_w, w_out, norm2_g,
          router_coarse_w, router_coarse_b, router_fine_w, router_fine_b,
          expert_w1, expert_w3, expert_w2, final_g):
    B, S, D = x.shape
    for l in range(DEPTH):
        mod = jax.nn.silu(c) @ w_ada[l] + b_ada[l]
        sh1, sc1, g1, sh2, sc2, g2 = [m[:, None, :] for m in jnp.split(mod, 6, axis=-1)]
        h = rmsnorm(x, norm1_g[l]) * (1 + sc1) + sh1
        z = h @ w_in[l]
        za = z[..., :POOL_W]
        zb = z[..., POOL_W:POOL_W + 2 * CONV_W]
        zc = z[..., POOL_W + 2 * CONV_W:]
        mixed = jnp.concatenate([
            pool_mixer(za, pool_w[l], pool_scale[l]),
            conv_module(zb, conv_dw_w[l], conv_dw_b[l], conv_ln_g[l], conv_ln_b[l], conv_pw_w[l], conv_pw_b[l]),
            fourier_mixer(zc, fourier_w[l]),
        ], axis=-1)
        x = x + g1 * (mixed @ w_out[l])
        h = rmsnorm(x, norm2_g[l]) * (1 + sc2) + sh2
        y = hier_moe(h.reshape(B * S, D), router_coarse_w[l], router_coarse_b[l], router_fine_w[l],
                     router_fine_b[l], expert_w1[l], expert_w3[l], expert_w2[l])
        x = x + g2 * y.reshape(B, S, D)
    return rmsnorm(x, final_g)


def _normal(k, shape, std):
    return std * jax.random.normal(k, shape, jnp.float32)


def setup_inputs(seed: int = 0) -> dict:
    key = jax.random.key(seed)
    ks = jax.random.split(key, 32)
    D = D_MODEL
    return {
        'x_prompt': _normal(ks[0], (BATCH, SEQ, D), 1.0),
        'x_sample': _normal(ks[1], (DEC_BATCH, DEC_SEQ, D), 1.0),
        'c_prompt': _normal(ks[2], (BATCH, D), 1.0),
        'c_sample': _normal(ks[3], (DEC_BATCH, D), 1.0),
        'w_ada': _normal(ks[4], (DEPTH, D, 6 * D), 0.5 * D ** -0.5),
        'b_ada': _normal(ks[5], (DEPTH, 6 * D), 0.02),
        'norm1_g': 1.0 + _normal(ks[6], (DEPTH, D), 0.02),
        'w_in': _normal(ks[7], (DEPTH, D, IN_COLS), D ** -0.5),
        'pool_w': _normal(ks[8], (DEPTH, N_POOL, POOL_CH, POOL_CH), POOL_CH ** -0.5),
        'pool_scale': 1.0 + _normal(ks[9], (DEPTH, POOL_W), 0.1),
        'conv_dw_w': _normal(ks[10], (DEPTH, CONV_K, CONV_W), CONV_K ** -0.5),
        'conv_dw_b': _normal(ks[11], (DEPTH, CONV_W), 0.02),
        'conv_ln_g': 1.0 + _normal(ks[12], (DEPTH, CONV_W), 0.02),
        'conv_ln_b': _normal(ks[13], (DEPTH, CONV_W), 0.02),
        'conv_pw_w': _normal(ks[14], (DEPTH, CONV_W, CONV_W), CONV_W ** -0.5),
        'conv_pw_b': _normal(ks[15], (DEPTH, CONV_W), 0.02),
        'fourier_w': _normal(ks[16], (DEPTH, FOURIER_W, FOURIER_W), FOURIER_W ** -0.5),
        'w_out': _normal(ks[17], (DEPTH, MIX_W, D), MIX_W ** -0.5),
        'norm2_g': 1.0 + _normal(ks[18], (DEPTH, D), 0.02),
        'router_coarse_w': _normal(ks[19], (DEPTH, D, N_GROUPS), D ** -0.5),
        'router_coarse_b': _normal(ks[20], (DEPTH, N_GROUPS), 0.01),
        'router_fine_w': _normal(ks[21], (DEPTH, N_GROUPS, D, EXPERTS_PER_GROUP), D ** -0.5),
        'router_fine_b': _normal(ks[22], (DEPTH, N_GROUPS, EXPERTS_PER_GROUP), 0.01),
        'expert_w1': _normal(ks[23], (DEPTH, N_EXPERTS, D, D_EXPERT), D ** -0.5),
        'expert_w3': _normal(ks[24], (DEPTH, N_EXPERTS, D, D_EXPERT), D ** -0.5),
        'expert_w2': _normal(ks[25], (DEPTH, N_EXPERTS, D_EXPERT, D), D_EXPERT ** -0.5),
        'final_g': 1.0 + _normal(ks[26], (D,), 0.02),
    }


def reference(x_prompt, x_sample, c_prompt, c_sample, w_ada, b_ada, norm1_g, w_in, pool_w,
              pool_scale, conv_dw_w, conv_dw_b, conv_ln_g, conv_ln_b, conv_pw_w, conv_pw_b,
              fourier_w, w_out, norm2_g, router_coarse_w, router_coarse_b, router_fine_w,
              router_fine_b, expert_w1, expert_w3, expert_w2, final_g):
    params = (w_ada, b_ada, norm1_g, w_in, pool_w, pool_scale, conv_dw_w, conv_dw_b,
              conv_ln_g, conv_ln_b, conv_pw_w, conv_pw_b, fourier_w, w_out, norm2_g,
              router_coarse_w, router_coarse_b, router_fine_w, router_fine_b,
              expert_w1, expert_w3, expert_w2, final_g)
    y_prompt = trunk(x_prompt, c_prompt, *params)
    y_sample = trunk(x_sample, c_sample, *params)
    return (y_prompt, y_sample)
```

```python
import math
from contextlib import ExitStack

import numpy as np
import ml_dtypes
import concourse.bass as bass
import concourse.mybir as mybir
from concourse.bass_utils import run_bass_kernel_spmd

F32 = mybir.dt.float32
BF16 = mybir.dt.bfloat16
I32 = mybir.dt.int32
AF = mybir.ActivationFunctionType
ALU = mybir.AluOpType
AX = mybir.AxisListType

D = 2048
NG, EPG, NE = 4, 8, 32
DE = 512
CONV_K = 31
EPS = 1e-6
SAME_ENGINE_SYNC = True


class _Op:
    __slots__ = ("q", "fn", "deps", "sig", "sigval", "sem", "is_dma", "idx", "bar")


class Prog:
    def __init__(self, nc):
        self.nc = nc
        self.ops = []
        self.last_w = {}
        self.readers = {}
        self.dma_cnt = {}
        self.last_eng = {}
        self.bar = None
        self.bar_done = set()

    def barrier(self):
        deps = [(o, None) for o in self.last_eng.values()]
        dmas = dict(self.dma_cnt)
        self.bar = (deps, dmas)
        self.bar_done = set()

    def _add(self, q, fn, reads, writes, is_dma, semkey):
        o = _Op()
        o.q = q
        o.fn = fn
        o.is_dma = is_dma
        o.sig = False
        o.sigval = 0
        o.idx = len(self.ops)
        deps = {}
        for r in reads:
            w = self.last_w.get(r)
            if w is not None:
                deps[w.idx] = w
        for w_ in writes:
            w = self.last_w.get(w_)
            if w is not None:
                deps[w.idx] = w
            for rd in self.readers.get(w_, ()):
                deps[rd.idx] = rd
        o.deps = []
        for d in deps.values():
            if d.is_dma:
                o.deps.append((d, self.dma_cnt[d.sem]))
            else:
                o.deps.append((d, None))
        o.bar = None
        if self.bar is not None and q not in self.bar_done:
            self.bar_done.add(q)
            bdeps, bdmas = self.bar
            o.deps.extend(bdeps)
            o.bar = bdmas
        if is_dma:
            o.sem = ("dma", semkey)
            self.dma_cnt[o.sem] = self.dma_cnt.get(o.sem, 0) + 1
            o.sigval = self.dma_cnt[o.sem]
        else:
            o.sem = ("eng", q)
            self.last_eng[q] = o
        for w_ in writes:
            self.last_w[w_] = o
            self.readers[w_] = []
        for r in reads:
            if r not in writes:
                self.readers.setdefault(r, []).append(o)
        self.ops.append(o)
        return o

    def op(self, eng, fn, reads=(), writes=()):
        return self._add(eng, fn, tuple(reads), tuple(writes), False, None)

    def dma(self, q, fn, reads=(), writes=(), semkey=None):
        assert semkey is not None
        return self._add(q, fn, tuple(reads), tuple(writes), True, semkey)

    def emit(self):
        nc = self.nc
        ops = self.ops
        for o in ops:
            for d, _ in o.deps:
                if d.is_dma:
                    continue
                if d.q != o.q or (SAME_ENGINE_SYNC and d.q != "tensor"):
                    d.sig = True
        cnt = {}
        for o in ops:
            if not o.is_dma and o.sig:
                cnt[o.q] = cnt.get(o.q, 0) + 1
                o.sigval = cnt[o.q]
        semkeys = []
        seen = set()
        for o in ops:
            if (o.is_dma or o.sig) and o.sem not in seen:
                seen.add(o.sem)
                semkeys.append(o.sem)
        self.n_sems = len(semkeys)
        with ExitStack() as es:
            sems = {}
            for i, k in enumerate(semkeys):
                sems[k] = es.enter_context(nc.semaphore("s%d" % i))
            block = es.enter_context(nc.Block())
            queues = {}
            for o in ops:
                queues.setdefault(o.q, []).append(o)
            totals = dict(self.dma_cnt)

            def run_queue(qname, eng):
                waited = {}
                for o in queues.get(qname, ()):
                    need = {}
                    for d, n in o.deps:
                        if d.is_dma:
                            v = 16 * n
                        else:
                            if d.q == o.q and (d.q == "tensor" or not SAME_ENGINE_SYNC):
                                continue
                            v = d.sigval
                        if v > need.get(d.sem, 0):
                            need[d.sem] = v
                    if o.bar is not None:
                        for k, n in o.bar.items():
                            if 16 * n > need.get(k, 0):
                                need[k] = 16 * n
                    for k, v in need.items():
                        if waited.get(k, 0) < v:
                            eng.wait_ge(sems[k], v)
                            waited[k] = v
                    ins = o.fn(eng)
                    if o.is_dma:
                        ins.then_inc(sems[o.sem], 16)
                    elif o.sig:
                        ins.then_inc(sems[o.sem], 1)
                if qname == "sync":
                    for k, n in totals.items():
                        if waited.get(k, 0) < 16 * n:
                            eng.wait_ge(sems[k], 16 * n)

            @block.sync
            def _(e):
                run_queue("sync", e)

            @block.scalar
            def _(e):
                run_queue("scalar", e)

            @block.gpsimd
            def _(e):
                run_queue("gpsimd", e)

            @block.vector
            def _(e):
                run_queue("vector", e)

            @block.tensor
            def _(e):
                run_queue("tensor", e)


def _bf(a):
    return np.ascontiguousarray(a.astype(ml_dtypes.bfloat16))


def make_consts(SEQS, NB):
    c = {}
    t = np.arange(128)
    ang = 2 * np.pi * np.outer(t, t) / 128.0
    cs = np.zeros((128, 2, 192), np.float64)
    for h in range(2):
        sl = slice(h * 64, h * 64 + 64)
        cs[:, h, 0:64] = np.cos(ang[:, sl])
        cs[:, h, 64:128] = np.sin(ang[:, sl])
        cs[:, h, 128:192] = -np.sin(ang[:, sl])
    c["cs128"] = _bf(cs)
    ccn = np.zeros((128, 2, 128), np.float64)
    ccn[:, 0] = np.cos(ang)
    ccn[:, 1] = -np.sin(ang)
    c["ccn"] = _bf(ccn)
    for q, S in enumerate(SEQS):
        S2 = S // 128
        a = 2 * np.pi * (np.outer(np.arange(S2), np.arange(S)) % S) / S
        f = np.zeros((S2, 2, S), np.float64)
        f[:, 0] = np.cos(a)
        f[:, 1] = np.sin(a)
        c["fcs%d" % q] = _bf(f)
    pc = np.zeros((128, 4, 16), np.float32)
    for g, w in enumerate((2, 4, 8, 16)):
        for j in range(8):
            cnt = min(j + w // 2, 10 ** 9) - max(j - w // 2, 0)
            pc[:, g, j] = 1.0 / cnt
        for j in range(8):
            tt = -8 + j
            hi = min(tt + w // 2, 0)
            lo = tt - w // 2
            pc[:, g, 8 + j] = 1.0 / (hi - lo)
    c["poolc"] = pc
    ut = (np.arange(128)[:, None] <= np.arange(128)[None, :]).astype(np.float32)
    c["utinc"] = _bf(ut)
    c["iotap"] = np.arange(128, dtype=np.float32).reshape(128, 1)
    c["bstart"] = np.tile((128.0 * np.arange(NB, dtype=np.float32))[None, :], (128, 1))
    c["iota32"] = np.tile(np.arange(32, dtype=np.float32)[None, :], (128, 1))
    return c


class Arena:
    def __init__(self, t, size):
        self.t = t
        self.size = size
        self.off = 0

    def reset(self):
        self.off = 0

    def alloc(self, n, align=16):
        self.off = (self.off + align - 1) // align * align
        o = self.off
        self.off += n
        assert self.off <= self.size, ("arena overflow", self.off, self.size)
        return self.t[:, o:o + n]


def build(SEQS=(8192, 2048), DEPTH=2, dbg=False, NL=None):
    NL = DEPTH if NL is None else NL
    N = sum(SEQS)
    NT = N // 128
    NB = -(-(2 * N + NE * 127) // 128)
    L = NB * 128
    OFFS = [0]
    for S in SEQS:
        OFFS.append(OFFS[-1] + S)
    nc = bass.Bass("TRN2", target_bir_lowering=False)

    def din(name, shape, dt=F32):
        return nc.dram_tensor(name, list(shape), dt, kind="ExternalInput").ap()

    def dscr(name, shape, dt):
        kind = "ExternalOutput" if dbg else "Internal"
        return nc.dram_tensor(name, list(shape), dt, kind=kind).ap()

    x_in = din("x", [N, D])
    c_in = din("c", [2, D])
    w_ada = din("w_ada", [DEPTH, D, 6 * D])
    b_ada = din("b_ada", [DEPTH, 6 * D])
    norm1_g = din("norm1_g", [DEPTH, D])
    w_in = din("w_in", [DEPTH, D, 3072])
    pool_w = din("pool_w", [DEPTH, 4, 128, 128])
    pool_scale = din("pool_scale", [DEPTH, 512])
    conv_dw_w = din("conv_dw_w", [DEPTH, CONV_K, 1024])
    conv_dw_b = din("conv_dw_b", [DEPTH, 1024])
    conv_ln_g = din("conv_ln_g", [DEPTH, 1024])
    conv_ln_b = din("conv_ln_b", [DEPTH, 1024])
    conv_pw_w = din("conv_pw_w", [DEPTH, 1024, 1024])
    conv_pw_b = din("conv_pw_b", [DEPTH, 1024])
    fourier_w = din("fourier_w", [DEPTH, 512, 512])
    w_out = din("w_out", [DEPTH, D, D])
    norm2_g = din("norm2_g", [DEPTH, D])
    rc_w = din("router_coarse_w", [DEPTH, D, NG])
    rc_b = din("router_coarse_b", [DEPTH, NG])
    rf_w = din("router_fine_w", [DEPTH, NG, D, EPG])
    rf_b = din("router_fine_b", [DEPTH, NG, EPG])
    e_w1 = din("expert_w1", [DEPTH, NE, D, DE])
    e_w3 = din("expert_w3", [DEPTH, NE, D, DE])
    e_w2 = din("expert_w2", [DEPTH, NE, DE, D])
    final_g = din("final_g", [D])
    k_cs128 = din("cs128", [128, 2, 192], BF16)
    k_ccn = din("ccn", [128, 2, 128], BF16)
    k_fcs = [din("fcs%d" % q, [S // 128, 2, S], BF16) for q, S in enumerate(SEQS)]
    k_poolc = din("poolc", [128, 4, 16])
    k_utinc = din("utinc", [128, 128], BF16)
    k_iotap = din("iotap", [128, 1])
    k_bstart = din("bstart", [128, NB])
    k_iota32 = din("iota32", [128, 32])

    y_out = nc.dram_tensor("y", [N, D], F32, kind="ExternalOutput").ap()

    x1_d = dscr("x1", [N, D], F32)
    x2_d = dscr("x2", [N, D], F32)
    h2_d = dscr("h2", [N, D], BF16)
    xs_d = dscr("xs", [L, D], BF16)
    ys_d = dscr("ys", [L, D], F32)
    mod_d = dscr("modrow", [2, 6 * D], F32)
    zaT = [dscr("zaT%d" % q, [512, S], BF16) for q, S in enumerate(SEQS)]
    vT = [dscr("vT%d" % q, [1024, S], BF16) for q, S in enumerate(SEQS)]
    zcT = [dscr("zcT%d" % q, [512, S], BF16) for q, S in enumerate(SEQS)]
    rzT = [dscr("rzT%d" % q, [512, S], BF16) for q, S in enumerate(SEQS)]
    mixT = [dscr("mixT%d" % q, [2048, S], BF16) for q, S in enumerate(SEQS)]

    P = Prog(nc)
    es = ExitStack()
    with es:
        AR_SZ = 96 * 1024
        art = es.enter_context(nc.sbuf_tensor("arena", [128, AR_SZ], BF16))
        AR = Arena(art, AR_SZ)

        class _A16:
            def alloc(self, n):
                return AR.alloc(n)

            def reset(self):
                AR.reset()

        class _A32:
            def alloc(self, n):
                return AR.alloc(2 * n).bitcast(F32)

            def reset(self):
                pass

        A16 = _A16()
        A32 = _A32()
        ident_f = es.enter_context(nc.sbuf_tensor("ident_f", [128, 128], F32))
        ident_b = es.enter_context(nc.sbuf_tensor("ident_b", [128, 128], BF16))
        ones_b = es.enter_context(nc.sbuf_tensor("ones_b", [128, 128], BF16))
        utinc = es.enter_context(nc.sbuf_tensor("utinc_s", [128, 128], BF16))
        iotap = es.enter_context(nc.sbuf_tensor("iotap_s", [128, 1], F32))
        iota32 = es.enter_context(nc.sbuf_tensor("iota32_s", [128, 32], F32))
        eps_t = es.enter_context(nc.sbuf_tensor("eps_t", [128, 1], F32))
        r_E = es.enter_context(nc.sbuf_tensor("r_E", [128, NT, 64], BF16))
        r_f = es.enter_context(nc.sbuf_tensor("r_f", [128, NT, 4], F32))
        r_slot = es.enter_context(nc.sbuf_tensor("r_slot", [128, NT, 2], I32))
        cnt_run = es.enter_context(nc.sbuf_tensor("cnt_run", [128, 32], F32))
        widx = es.enter_context(nc.sbuf_tensor("widx", [128, NB], I32))
        pm = [es.enter_context(nc.psum_tensor("pm%d" % i, [128, 512], F32)) for i in range(4)]
        pt = es.enter_context(nc.psum_tensor("pt", [128, 2048], BF16))
        pr = [es.enter_context(nc.psum_tensor("pr%d" % i, [128, 512], F32)) for i in range(2)]
        PM = [("pm", i) for i in range(4)]
        PT = "pt"
        PR = [("pr", i) for i in range(2)]

        uid = [0]
        _bregs = {}

        def BR(e, val):
            if val not in _bregs:
                _bregs[val] = e.to_reg(val)
            return _bregs[val]

        def U(name):
            uid[0] += 1
            return (name, uid[0])

        def t16(n, shape=None):
            v = A16.alloc(n)
            return v

        def DMA(q, out, in_, reads, writes, semkey, **kw):
            P.dma(q, lambda e: e.dma_start(out=out, in_=in_, **kw), reads, writes, semkey)

        def MM(out, lhsT, rhs, start, stop, reads, writes):
            P.op("tensor", lambda e: e.matmul(out, lhsT=lhsT, rhs=rhs, start=start, stop=stop), reads, writes)

        def TR(out, in_, ident, reads, writes):
            P.op("tensor", lambda e: e.transpose(out=out, in_=in_, identity=ident), reads, writes)

        def ACT(out, in_, func, reads, writes, bias=None, scale=None, accum_out=None, eng="scalar"):
            kw = {}
            if bias is not None:
                kw["bias"] = bias
            if scale is not None:
                kw["scale"] = scale
            if accum_out is not None:
                kw["accum_out"] = accum_out
            P.op("scalar", lambda e: e.activation(out=out, in_=in_, func=func, **kw), reads, writes)

        def TT(eng, out, in0, in1, op, reads, writes):
            P.op(eng, lambda e: e.tensor_tensor(out=out, in0=in0, in1=in1, op=op), reads, writes)

        def TS(eng, out, in0, s1, s2, op0, op1, reads, writes, accum_out=None):
            if op1 is None:
                P.op(eng, lambda e: e.tensor_scalar(out=out, in0=in0, scalar1=s1, scalar2=None, op0=op0), reads, writes)
            elif accum_out is not None:
                P.op(eng, lambda e: e.tensor_scalar(out=out, in0=in0, scalar1=s1, scalar2=s2, op0=op0, op1=op1, accum_out=accum_out), reads, writes)
            else:
                P.op(eng, lambda e: e.tensor_scalar(out=out, in0=in0, scalar1=s1, scalar2=s2, op0=op0, op1=op1), reads, writes)

        def STT(out, in0, scalar, in1, op0, op1, reads, writes):
            P.op("vector", lambda e: e.scalar_tensor_tensor(out=out, in0=in0, scalar=scalar, in1=in1, op0=op0, op1=op1), reads, writes)

        def CP(eng, out, in_, reads, writes):
            if eng == "scalar":
                P.op("scalar", lambda e: e.copy(out=out, in_=in_), reads, writes)
            else:
                P.op(eng, lambda e: e.tensor_copy(out=out, in_=in_), reads, writes)

        def MEMSET(eng, ap, val, writes):
            P.op(eng, lambda e: e.memset(ap, val), (), writes)

        def RED(out, in_, op, reads, writes, axis=AX.X):
            P.op("vector", lambda e: e.tensor_reduce(out=out, in_=in_, axis=axis, op=op), reads, writes)

        def bcast_row(dram_row_ap, n):
            return dram_row_ap.partition_broadcast(128)

        MEMSET("gpsimd", ident_f[:], 0.0, ["ident_f"])
        P.op("gpsimd", lambda e: e.affine_select(out=ident_f[:], in_=ident_f[:], pattern=[[-1, 128]],
                                                 compare_op=ALU.not_equal, fill=1.0, base=0, channel_multiplier=1),
             ["ident_f"], ["ident_f"])
        CP("vector", ident_b[:], ident_f[:], ["ident_f"], ["ident_b"])
        MEMSET("gpsimd", ones_b[:], 1.0, ["ones_b"])
        MEMSET("gpsimd", eps_t[:], EPS, ["eps_t"])
        DMA("sync", utinc[:], k_utinc, [], ["utinc"], "c_utinc")
        DMA("sync", iotap[:], k_iotap, [], ["iotap"], "c_iotap")
        DMA("sync", iota32[:], k_iota32, [], ["iota32"], "c_iota32")

        cur_x = x_in
        for l in range(NL):
            last = (l == NL - 1)
            nxt_x = y_out if False else x2_d
            P.barrier()
            A16.reset(); A32.reset()
            cst = A32.alloc(2 * 16).rearrange("p (q k) -> p q k", q=2)
            csb = A16.alloc(16 * 2).rearrange("p (k q) -> p k q", q=2)
            brow = [A32.alloc(512) for _ in range(2)]
            mrow = A32.alloc(512)
            DMA("sync", cst, c_in.rearrange("q (p k) -> p q k", k=16), [], ["cst"], "cst")
            for q in range(2):
                ACT(csb[:, :, q], cst[:, q, :], AF.Silu, ["cst"], [("csb", q)])
            wa = [A16.alloc(16 * 512).rearrange("p (k n) -> p k n", k=16) for _ in range(2)]
            for cb in range(24):
                wbuf = wa[cb % 2]
                wk = ("wa", cb % 2)
                DMA("gpsimd", wbuf, w_ada[l, :, cb * 512:(cb + 1) * 512].rearrange("(p k) n -> p k n", k=16),
                    [], [wk], "wa%d" % (cb % 2))
                pk = PM[cb % 2]
                pst = pm[cb % 2]
                for kc in range(16):
                    MM(pst[0:2, :], csb[:, kc, :], wbuf[:, kc, :], kc == 0, kc == 15,
                       [wk, ("csb", 0), ("csb", 1)], [pk])
                mk = U("mrow")
                bk = ("brow", cb % 2)
                DMA("sync", brow[cb % 2][0:2, :], b_ada[l:l + 1, cb * 512:(cb + 1) * 512].broadcast_to([2, 512]),
                    [], [bk], "brow%d" % (cb % 2))
                TT("vector", mrow[0:2, :], pst[0:2, :], brow[cb % 2][0:2, :], ALU.add,
                   [pk, bk], ["mrow"])
                DMA("sync", mod_d[:, cb * 512:(cb + 1) * 512], mrow[0:2, :], ["mrow"], [("mod", cb)], "mrow")
            MODK = [("mod", cb) for cb in range(24)]

            def mod_rep(q, i):
                return mod_d[q:q + 1, i * D:(i + 1) * D].broadcast_to([128, D])

            for q, S in enumerate(SEQS):
                T0 = OFFS[q]
                NT5 = S // 512
                S2 = S // 128
                P.barrier()
                A16.reset(); A32.reset()
                wi = A16.alloc(16 * 3072).rearrange("p (k n) -> p k n", k=16)
                DMA("gpsimd", wi, w_in[l].rearrange("(p k) n -> p k n", k=16), [], ["wi"], "wi")
                a1 = A32.alloc(D)
                sh1 = A32.alloc(D)
                tmpf = A32.alloc(D)
                DMA("sync", a1, mod_rep(q, 1), MODK, ["a1"], "a1")
                DMA("sync", sh1, mod_rep(q, 0), MODK, ["sh1"], "sh1")
                DMA("sync", tmpf, norm1_g[l:l + 1, :].broadcast_to([128, D]), [], ["tmpf"], "g_rep")
                STT(a1, a1, 1.0, tmpf, ALU.add, ALU.mult, ["a1", "tmpf"], ["a1"])
                xt = [A32.alloc(D) for _ in range(2)]
                hb = [A16.alloc(D) for _ in range(2)]
                hT = [A16.alloc(16 * 512).rearrange("p (k t) -> p k t", k=16) for _ in range(1)]
                stg = [A16.alloc(16 * 512).rearrange("p (j t) -> p j t", j=16) for _ in range(1)]
                sgt = [A32.alloc(512) for _ in range(2)]
                ss = A32.alloc(8)
                for ti in range(NT5):
                    hTt = hT[0]
                    hk = ("hT", 0)
                    for sub in range(4):
                        it = ti * 4 + sub
                        b2 = it % 2
                        r0 = T0 + it * 128
                        xk = ("xt", b2)
                        DMA("sync", xt[b2], cur_x[r0:r0 + 128, :], [("xcur", r0 // 128)], [xk], "xt%d" % b2)
                        ssk = U("ss")
                        hbk = ("hb", b2)
                        ACT(hb[b2], xt[b2], AF.Square, [xk], [hbk, ssk], accum_out=ss[:, 0:1])
                        ACT(ss[:, 1:2], ss[:, 0:1], AF.Sqrt, [ssk, "eps_t"], [ssk], scale=1.0 / D, bias=eps_t[:, 0:1])
                        P.op("vector", lambda e, ss=ss: e.reciprocal(out=ss[:, 2:3], in_=ss[:, 1:2]), [ssk], [ssk])
                        STT(tmpf, xt[b2], ss[:, 2:3], a1, ALU.mult, ALU.mult, [xk, ssk, "a1"], ["tmpf"])
                        TT("gpsimd", hb[b2], tmpf, sh1, ALU.add, ["tmpf", "sh1"], [hbk])
                        for kc in range(16):
                            TR(pt[:, kc * 128:(kc + 1) * 128], hb[b2].rearrange("p (c k) -> p k c", k=16)[:, kc, :],
                               ident_b[:], [hbk, "ident_b"], [PT])
                        CP("scalar", hTt[:, :, sub * 128:(sub + 1) * 128],
                           pt[:, :].rearrange("p (k t) -> p k t", k=16), [PT], [hk])
                    st = stg[0]
                    sk = ("stg", 0)
                    c0 = ti * 512

                    def mmgrp(j, pi):
                        for kc in range(16):
                            MM(pm[pi][:, :], wi[:, kc, j * 128:(j + 1) * 128], hTt[:, kc, :], kc == 0, kc == 15,
                               ["wi", hk], [PM[pi]])

                    pi = 0
                    for j in range(4):
                        mmgrp(j, pi)
                        CP("scalar", st[:, j, :], pm[pi][:, :], [PM[pi]], [sk])
                        pi = (pi + 1) % 4
                    for j in range(8):
                        pa = pi
                        mmgrp(4 + j, pa)
                        pb = (pi + 1) % 4
                        mmgrp(12 + j, pb)
                        sg = sgt[j % 2]
                        sgk = ("sg", j % 2)
                        ACT(sg, pm[pb][:, :], AF.Sigmoid, [PM[pb]], [sgk])
                        TT("vector", st[:, 4 + j, :], pm[pa][:, :], sg, ALU.mult, [PM[pa], sgk], [sk])
                        pi = (pi + 2) % 4
                    for j in range(4):
                        mmgrp(20 + j, pi)
                        CP("scalar", st[:, 12 + j, :], pm[pi][:, :], [PM[pi]], [sk])
                        pi = (pi + 1) % 4
                    DMA("sync", zaT[q][:, c0:c0 + 512].rearrange("(j p) t -> p j t", p=128), st[:, 0:4, :],
                        [sk], [("zaT", q, ti)], "stg0")
                    DMA("sync", vT[q][:, c0:c0 + 512].rearrange("(j p) t -> p j t", p=128), st[:, 4:12, :],
                        [sk], [("vT", q, ti)], "stg0")
                    DMA("sync", zcT[q][:, c0:c0 + 512].rearrange("(j p) t -> p j t", p=128), st[:, 12:16, :],
                        [sk], [("zcT", q, ti)], "stg0")

                P.barrier()
                A16.reset(); A32.reset()
                pwf = A32.alloc(4 * 128).rearrange("p (g e) -> p g e", g=4)
                pwb = A16.alloc(4 * 128).rearrange("p (g e) -> p g e", g=4)
                DMA("sync", pwf, pool_w[l].rearrange("g c e -> c g e"), [], ["pwf"], "pwf")
                CP("vector", pwb, pwf, ["pwf"], ["pwb"])
                psc = A32.alloc(4)
                pscr = A32.alloc(4 * 128).rearrange("p (g e) -> p g e", g=4)[0:4]
                DMA("sync", pscr[:, 0, :], pool_scale[l].rearrange("(g e) -> g e", g=4), [], ["pscr"], "pscr")
                TR(pr[0][:, 0:4], pscr[:, 0, :], ident_f[0:4, 0:4], ["pscr", "ident_f"], [PR[0]])
                CP("vector", psc, pr[0][:, 0:4], [PR[0]], ["psc"])
                pcst = A32.alloc(64).rearrange("p (g j) -> p g j", g=4)
                DMA("sync", pcst, k_poolc, [], ["pcst"], "pcst")
                ub = [A16.alloc(528) for _ in range(2)]
                sa = [A32.alloc(528) for _ in range(2)]
                sb_ = [A32.alloc(528) for _ in range(2)]
                dmt = [A16.alloc(512) for _ in range(2)]
                pst2 = [A16.alloc(4 * 512).rearrange("p (g t) -> p g t", g=4) for _ in range(2)]
                it = 0
                for ti in range(NT5):
                    c0 = ti * 512
                    st = pst2[ti % 2]
                    sk = ("pst2", ti % 2)
                    for g, w in enumerate((2, 4, 8, 16)):
                        b2 = it % 2
                        it += 1
                        u = ub[b2]
                        uk = ("ub", b2)
                        lo = max(c0 - 8, 0)
                        hi = min(c0 + 520, S)
                        rd = [("zaT", q, tj) for tj in range(max(ti - 1, 0), min(ti + 2, NT5))]
                        if lo > c0 - 8:
                            MEMSET("gpsimd", u[:, 0:8], 0.0, [uk])
                        if hi < c0 + 520:
                            MEMSET("gpsimd", u[:, 520:528], 0.0, [uk])
                        DMA("sync", u[:, lo - (c0 - 8):hi - (c0 - 8)], zaT[q][g * 128:(g + 1) * 128, lo:hi], rd, [uk], "ub%d" % b2)
                        s_a, s_b = sa[b2], sb_[b2]
                        ka, kb = ("sa", b2), ("sb", b2)
                        TT("vector", s_a[:, 1:528], u[:, 0:527], u[:, 1:528], ALU.add, [uk], [ka])
                        cur, curk, oth, othk = s_a, ka, s_b, kb
                        lo_v = 1
                        hi_v = 528
                        step = 1
                        ww = 2
                        while ww < w:
                            nlo = lo_v + step
                            nhi = hi_v - step
                            TT("vector", oth[:, nlo:nhi], cur[:, nlo - step:nhi - step], cur[:, nlo + step:nhi + step],
                               ALU.add, [curk], [othk])
                            cur, curk, oth, othk = oth, othk, cur, curk
                            lo_v, hi_v = nlo, nhi
                            step *= 2
                            ww *= 2
                        dm = dmt[b2]
                        dk_ = ("dm", b2)
                        STT(dm, cur[:, 8:520], 1.0 / w, u[:, 8:520], ALU.mult, ALU.subtract, [curk, uk], [dk_])
                        if ti == 0:
                            TT("vector", oth[:, 8:16], cur[:, 8:16], pcst[:, g, 0:8], ALU.mult, [curk, "pcst"], [othk])
                            TT("vector", dm[:, 0:8], oth[:, 8:16], u[:, 8:16], ALU.subtract, [othk, uk], [dk_])
                        if ti == NT5 - 1:
                            TT("vector", oth[:, 512:520], cur[:, 512:520], pcst[:, g, 8:16], ALU.mult, [curk, "pcst"], [othk])
                            TT("vector", dm[:, 504:512], oth[:, 512:520], u[:, 512:520], ALU.subtract, [othk, uk], [dk_])
                        pi = g
                        MM(pm[pi][:, :], pwb[:, g, :], dm, True, True, ["pwb", dk_], [PM[pi]])
                        ACT(st[:, g, :], pm[pi][:, :], AF.Copy, [PM[pi], "psc"], [sk], scale=psc[:, g:g + 1])
                    DMA("sync", mixT[q][0:512, c0:c0 + 512].rearrange("(g p) t -> p g t", p=128), st, [sk],
                        [("mixT", q, ti, 0)], "pst2%d" % (ti % 2))

                P.barrier()
                A16.reset(); A32.reset()
                pww = A16.alloc(8 * 1024).rearrange("p (k n) -> p k n", k=8)
                DMA("gpsimd", pww, conv_pw_w[l].rearrange("(k p) n -> p k n", p=128), [], ["pww"], "pww")
                dwr = A32.alloc(1024)
                DMA("sync", dwr[0:CONV_K, :], conv_dw_w[l], [], ["dwr"], "dwr")
                dwT = A32.alloc(8 * 32).rearrange("p (j k) -> p j k", j=8)
                for j in range(8):
                    TR(pr[0][:, j * 32:j * 32 + CONV_K], dwr[0:CONV_K, j * 128:(j + 1) * 128],
                       ident_f[0:CONV_K, 0:CONV_K], ["dwr", "ident_f"], [PR[0]])
                CP("vector", dwT[:, :, 0:CONV_K], pr[0][:, 0:256].rearrange("p (j k) -> p j k", j=8)[:, :, 0:CONV_K],
                   [PR[0]], ["dwT"])
                vr = A32.alloc(4 * 1024).rearrange("p (v n) -> p v n", v=4)
                vecs = A32.alloc(32).rearrange("p (v j) -> p v j", v=4)
                for vi, src in enumerate((conv_dw_b, conv_ln_g, conv_ln_b, conv_pw_b)):
                    DMA("sync", vr[0:8, vi, 0:128], src[l].rearrange("(j p) -> j p", p=128), [], [("vr", vi)], "vr%d" % vi)
                    TR(pr[1][:, vi * 8:vi * 8 + 8], vr[0:8, vi, 0:128], ident_f[0:8, 0:8], [("vr", vi), "ident_f"], [PR[1]])
                CP("vector", vecs, pr[1][:, 0:32].rearrange("p (v j) -> p v j", v=4), [PR[1]], ["vecs"])
                vb = [A16.alloc(544) for _ in range(2)]
                acc = [A32.alloc(512) for _ in range(8)]
                cbf = [A16.alloc(512) for _ in range(2)]
                sqb = [A16.alloc(512) for _ in range(2)]
                sT = [A16.alloc(512) for _ in range(8)]
                mean = A32.alloc(512)
                var = A32.alloc(512)
                rstd = A32.alloc(512)
                xn = [A32.alloc(512) for _ in range(2)]
                st3 = [A16.alloc(8 * 512).rearrange("p (j t) -> p j t", j=8) for _ in range(2)]
                it = 0
                for ti in range(NT5):
                    c0 = ti * 512
                    for j in range(8):
                        b2 = it % 2
                        it += 1
                        v = vb[b2]
                        vk = ("vb", b2)
                        lo = max(c0 - 15, 0)
                        hi = min(c0 + 527, S)
                        rd = [("vT", q, tj) for tj in range(max(ti - 1, 0), min(ti + 2, NT5))]
                        if lo > c0 - 15:
                            MEMSET("gpsimd", v[:, 0:15], 0.0, [vk])
                        if hi < c0 + 527:
                            MEMSET("gpsimd", v[:, 527:542], 0.0, [vk])
                        DMA("sync", v[:, lo - (c0 - 15):hi - (c0 - 15)], vT[q][j * 128:(j + 1) * 128, lo:hi], rd, [vk], "vb%d" % b2)
                        a = acc[j]
                        ak = ("acc", j)
                        TS("vector", a, v[:, 0:512], dwT[:, j, 0:1], vecs[:, 0, j:j + 1], ALU.mult, ALU.add,
                           [vk, "dwT", "vecs"], [ak])
                        for k in range(1, CONV_K):
                            STT(a, v[:, k:k + 512], dwT[:, j, k:k + 1], a, ALU.mult, ALU.add, [vk, "dwT", ak], [ak])
                        cb_, ck = cbf[b2], ("cbf", b2)
                        sq_, sqk = sqb[b2], ("sqb", b2)
                        CP("gpsimd", cb_, a, [ak], [ck])
                        ACT(sq_, a, AF.Square, [ak], [sqk])
                        MM(pr[0][:, :], ones_b[:], cb_, j == 0, j == 7, ["ones_b", ck], [PR[0]])
                        MM(pr[1][:, :], ones_b[:], sq_, j == 0, j == 7, ["ones_b", sqk], [PR[1]])
                    TS("vector", mean, pr[0][:, :], 1.0 / 1024, None, ALU.mult, None, [PR[0]], ["mean"])
                    TS("vector", var, pr[1][:, :], 1.0 / 1024, None, ALU.mult, None, [PR[1]], ["var"])
                    TT("vector", rstd, mean, mean, ALU.mult, ["mean"], ["rstd"])
                    TT("vector", var, var, rstd, ALU.subtract, ["var", "rstd"], ["var"])
                    ACT(var, var, AF.Sqrt, ["var", "eps_t"], ["var"], bias=eps_t[:, 0:1], scale=1.0)
                    P.op("vector", lambda e, rstd=rstd, var=var: e.reciprocal(out=rstd, in_=var), ["var"], ["rstd"])
                    for j in range(8):
                        x_, xk_ = xn[j % 2], ("xn", j % 2)
                        TT("gpsimd", x_, acc[j], mean, ALU.subtract, [("acc", j), "mean"], [xk_])
                        TT("vector", x_, x_, rstd, ALU.mult, [xk_, "rstd"], [xk_])
                        ACT(sT[j], x_, AF.Silu, [xk_, "vecs"], [("sT", j)], scale=vecs[:, 1, j:j + 1], bias=vecs[:, 2, j:j + 1])
                    st = st3[ti % 2]
                    sk = ("st3", ti % 2)
                    for e_ in range(8):
                        pi = e_ % 4
                        for j in range(8):
                            MM(pm[pi][:, :], pww[:, j, e_ * 128:(e_ + 1) * 128], sT[j], j == 0, j == 7,
                               ["pww", ("sT", j)], [PM[pi]])
                        ACT(st[:, e_, :], pm[pi][:, :], AF.Identity, [PM[pi], "vecs"], [sk], bias=vecs[:, 3, e_:e_ + 1])
                    DMA("sync", mixT[q][512:1536, c0:c0 + 512].rearrange("(j p) t -> p j t", p=128), st, [sk],
                        [("mixT", q, ti, 1)], "st3%d" % (ti % 2))

                P.barrier()
                A16.reset(); A32.reset()
                cs = A16.alloc(2 * 192).rearrange("p (h n) -> p h n", h=2)
                ccn = A16.alloc(2 * 128).rearrange("p (h n) -> p h n", h=2)
                fcs = A16.alloc(2 * S).rearrange("p (h n) -> p h n", h=2)
                DMA("sync", cs, k_cs128, [], ["cs"], "cs")
                DMA("sync", ccn, k_ccn, [], ["ccn"], "ccn")
                DMA("sync", fcs[0:S2], k_fcs[q], [], ["fcs"], "fcs")
                fwf = A32.alloc(4 * 512).rearrange("p (h e) -> p h e", h=4)
                fwb = A16.alloc(4 * 512).rearrange("p (h e) -> p h e", h=4)
                DMA("sync", fwf, fourier_w[l].rearrange("(h m) e -> m h e", h=4), [], ["fwf"], "fwf")
                CP("vector", fwb, fwf, ["fwf"], ["fwb"])
                Ut = A16.alloc(128 * S2).rearrange("p (c t) -> p c t", c=128)
                Ah = A16.alloc(128 * 192).rearrange("p (c n) -> p c n", c=128)
                Xh = A16.alloc(2 * S).rearrange("p (h k) -> p h k", h=2)
                rst = [A16.alloc(512) for _ in range(2)]
                ALLZC = [("zcT", q, tj) for tj in range(NT5)]
                nrm = 1.0 / math.sqrt(S * 128.0)
                ri = 0
                for h in range(4):
                    for cq in range(8):
                        DMA("sync", Ut[:, cq * 16:(cq + 1) * 16, :],
                            zcT[q][h * 128 + cq * 16:h * 128 + (cq + 1) * 16, :].rearrange("c (a b) -> a c b", b=S2),
                            ALLZC, ["Ut"], "Ut")
                    for half in range(2):
                        pmv = [pm[i] for i in range(4)]
                        for cg in range(16):
                            for ci in range(8):
                                c_ = cg * 8 + ci
                                bank = ci // 2
                                col = (ci % 2) * 192
                                MM(pmv[bank][0:S2, col:col + 192], Ut[:, c_, :], cs[:, half, :], True, True,
                                   ["Ut", "cs"], [PM[bank]])
                            for bank in range(4):
                                eng = "vector" if bank % 2 == 0 else "scalar"
                                CP(eng, Ah[0:S2, cg * 8 + bank * 2:cg * 8 + bank * 2 + 2, :],
                                   pmv[bank][0:S2, 0:384].rearrange("p (c n) -> p c n", c=2), [PM[bank]], [("Ah", cg)])
                        AHK = [("Ah", cg) for cg in range(16)]
                        G = 512 // S2
                        G = min(G, 64)
                        for kg in range(64 // G):
                            pxr, pxi = pr[0], pr[1]
                            for gi in range(G):
                                k1l = kg * G + gi
                                k1 = half * 64 + k1l
                                fc_ = fcs[0:S2, 0, :].rearrange("p (b a) -> p a b", a=128)[:, k1, :]
                                fs_ = fcs[0:S2, 1, :].rearrange("p (b a) -> p a b", a=128)[:, k1, :]
                                ar = Ah[0:S2, :, k1l]
                                ai = Ah[0:S2, :, 64 + k1l]
                                an = Ah[0:S2, :, 128 + k1l]
                                o = gi * S2
                                MM(pxr[:, o:o + S2], ar, fc_, True, False, AHK + ["fcs"], [PR[0]])
                                MM(pxr[:, o:o + S2], an, fs_, False, True, AHK + ["fcs"], [PR[0]])
                                MM(pxi[:, o:o + S2], ar, fs_, True, False, AHK + ["fcs"], [PR[1]])
                                MM(pxi[:, o:o + S2], ai, fc_, False, True, AHK + ["fcs"], [PR[1]])
                            k1b = half * 64 + kg * G
                            for ri_, px in enumerate((pxr, pxi)):
                                dst = Xh[:, ri_, :].rearrange("p (b a) -> p a b", a=128)[:, k1b:k1b + G, :]
                                src = px[:, 0:G * S2].rearrange("p (g b) -> p g b", g=G)
                                CP("vector" if ri_ == 0 else "scalar", dst, src, [PR[ri_]], [("Xh", half, kg)])
                    XHK = [("Xh", hf, kg) for hf in range(2) for kg in range(64 // G)]
                    for kt in range(NT5):
                        pi = kt % 4
                        MM(pm[pi][:, :], ccn[:, 0, :], Xh[:, 0, kt * 512:(kt + 1) * 512], True, False, ["ccn"] + XHK, [PM[pi]])
                        MM(pm[pi][:, :], ccn[:, 1, :], Xh[:, 1, kt * 512:(kt + 1) * 512], False, True, ["ccn"] + XHK, [PM[pi]])
                        r_ = rst[ri % 2]
                        rk = ("rst", ri % 2)
                        ACT(r_, pm[pi][:, :], AF.Copy, [PM[pi]], [rk], scale=nrm)
                        DMA("sync", rzT[q][h * 128:(h + 1) * 128, kt * 512:(kt + 1) * 512], r_, [rk], [("rzT", q, h, kt)],
                            "rst%d" % (ri % 2))
                        ri += 1
                rzb = [A16.alloc(4 * 512).rearrange("p (h t) -> p h t", h=4) for _ in range(2)]
                st4 = [A16.alloc(4 * 512).rearrange("p (e t) -> p e t", e=4) for _ in range(2)]
                for ti in range(NT5):
                    c0 = ti * 512
                    rb, rbk = rzb[ti % 2], ("rzb", ti % 2)
                    DMA("sync", rb, rzT[q][:, c0:c0 + 512].rearrange("(h m) t -> m h t", h=4),
                        [("rzT", q, h, ti) for h in range(4)], [rbk], "rzb%d" % (ti % 2))
                    st, sk = st4[ti % 2], ("st4", ti % 2)
                    for e_ in range(4):
                        pi = e_
                        for h in range(4):
                            MM(pm[pi][:, :], fwb[:, h, e_ * 128:(e_ + 1) * 128], rb[:, h, :], h == 0, h == 3,
                               ["fwb", rbk], [PM[pi]])
                        CP("scalar" if e_ % 2 else "vector", st[:, e_, :], pm[pi][:, :], [PM[pi]], [sk])
                    DMA("sync", mixT[q][1536:2048, c0:c0 + 512].rearrange("(e p) t -> p e t", p=128), st, [sk],
                        [("mixT", q, ti, 2)], "st4%d" % (ti % 2))

                P.barrier()
                A16.reset(); A32.reset()
                wo = A16.alloc(16 * 2048).rearrange("p (k n) -> p k n", k=16)
                DMA("gpsimd", wo, w_out[l].rearrange("(k p) n -> p k n", p=128), [], ["wo"], "wo")
                g1 = A32.alloc(D)
                a2 = A32.alloc(D)
                sh2 = A32.alloc(D)
                tmpf = A32.alloc(D)
                TFK = [("tmpf", i) for i in range(4)]
                DMA("sync", g1, mod_rep(q, 2), MODK, ["g1"], "g1")
                DMA("sync", a2, mod_rep(q, 4), MODK, ["a2"], "a2")
                DMA("sync", sh2, mod_rep(q, 3), MODK, ["sh2"], "sh2")
                DMA("sync", tmpf, norm2_g[l:l + 1, :].broadcast_to([128, D]), [], TFK, "g_rep")
                STT(a2, a2, 1.0, tmpf, ALU.add, ALU.mult, ["a2"] + TFK, ["a2"])
                wr = A32.alloc(16 * 36).rearrange("p (k n) -> p k n", k=16)
                DMA("sync", wr[:, :, 0:4], rc_w[l].rearrange("(p k) g -> p k g", k=16), [], [("wr", 0)], "wr")
                for g in range(4):
                    DMA("sync", wr[:, :, 4 + 8 * g:12 + 8 * g], rf_w[l, g].rearrange("(p k) e -> p k e", k=16), [],
                        [("wr", 1 + g)], "wr")
                WRK = [("wr", i) for i in range(5)]
                rb_ = A32.alloc(36)
                DMA("sync", rb_[:, 0:4], rc_b[l:l + 1, :].broadcast_to([128, 4]), [], [("rb", 0)], "rb")
                DMA("sync", rb_[:, 4:36], rf_b[l:l + 1].rearrange("o g e -> o (g e)").broadcast_to([128, 32]), [], [("rb", 1)], "rb")
                RBK = [("rb", 0), ("rb", 1)]
                mx = [A16.alloc(16 * 512).rearrange("p (k t) -> p k t", k=16) for _ in range(1)]
                xt = [A32.alloc(D) for _ in range(1)]
                x1t = [A32.alloc(D) for _ in range(2)]
                h2f = A32.alloc(D)
                h2b = [A16.alloc(D) for _ in range(2)]
                h2T = A32.alloc(16 * 128).rearrange("p (k t) -> p k t", k=16)
                sm = A32.alloc(256)
                if q == 0:
                    MEMSET("vector", cnt_run[:], 0.0, ["cnt_run"])
                for ti in range(NT5):
                    c0 = ti * 512
                    m_, mk_ = mx[0], ("mx", 0)
                    DMA("sync", m_, mixT[q][:, c0:c0 + 512].rearrange("(k p) t -> p k t", p=128),
                        [("mixT", q, ti, i) for i in range(3)], [mk_], "mx0")
                    for sub in range(4):
                        it = ti * 4 + sub
                        git = (T0 // 128) + it
                        b2 = it % 2
                        r0 = T0 + it * 128
                        xk = ("xt", 0)
                        DMA("sync", xt[0], cur_x[r0:r0 + 128, :], [("xcur", r0 // 128)], [xk], "xt0")
                        x1, x1k = x1t[b2], ("x1t", b2)
                        for cbk in range(4):
                            pi = cbk
                            for kc in range(16):
                                MM(pm[pi][:, :], m_[:, kc, sub * 128:(sub + 1) * 128], wo[:, kc, cbk * 512:(cbk + 1) * 512],
                                   kc == 0, kc == 15, [mk_, "wo"], [PM[pi]])
                            sl = slice(cbk * 512, (cbk + 1) * 512)
                            TT("vector", tmpf[:, sl], pm[pi][:, :], g1[:, sl], ALU.mult, [PM[pi], "g1"], [("tmpf", cbk)])
                            TT("gpsimd", x1[:, sl], tmpf[:, sl], xt[0][:, sl], ALU.add, [("tmpf", cbk), xk], [x1k])
                        DMA("sync", x1_d[r0:r0 + 128, :], x1, [x1k], [("x1", r0 // 128)], "x1t%d" % b2)
                        ssk = U("ss")
                        ss = sm[:, 0:4]
                        hb_, hbk = h2b[b2], ("h2b", b2)
                        ACT(hb_, x1, AF.Square, [x1k], [hbk, ssk], accum_out=ss[:, 0:1])
                        ACT(ss[:, 1:2], ss[:, 0:1], AF.Sqrt, [ssk, "eps_t"], [ssk], scale=1.0 / D, bias=eps_t[:, 0:1])
                        P.op("vector", lambda e, ss=ss: e.reciprocal(out=ss[:, 2:3], in_=ss[:, 1:2]), [ssk], [ssk])
                        STT(tmpf, x1, ss[:, 2:3], a2, ALU.mult, ALU.mult, [x1k, ssk, "a2"] , [("tmpf", i) for i in range(4)])
                        TT("gpsimd", h2f, tmpf, sh2, ALU.add, [("tmpf", i) for i in range(4)] + ["sh2"], ["h2f"])
                        CP("scalar", hb_, h2f, ["h2f"], [hbk])
                        DMA("sync", h2_d[r0:r0 + 128, :], hb_, [hbk], [("h2", r0 // 128)], "h2b%d" % b2)
                        for kc in range(16):
                            pj = pr[(kc // 4) % 2]
                            TR(pj[:, (kc % 4) * 128:(kc % 4 + 1) * 128], h2f.rearrange("p (c k) -> p k c", k=16)[:, kc, :],
                               ident_f[:], ["h2f", "ident_f"], [PR[(kc // 4) % 2]])
                            if kc % 4 == 3:
                                g4 = kc // 4
                                CP("scalar" if g4 % 2 else "vector", h2T[:, g4 * 4:g4 * 4 + 4, :],
                                   pj[:, :].rearrange("p (k t) -> p k t", k=4), [PR[(kc // 4) % 2]], [("h2T", g4)])
                        for kc in range(16):
                            MM(pr[0][:, 0:36], h2T[:, kc, :], wr[:, kc, :], kc == 0, kc == 15,
                               [("h2T", kc // 4)] + WRK, [PR[0]])
                        route_tile(P, nc, sm, pr, PR, rb_, RBK, r_E, r_f, cnt_run, utinc, ones_b, git,
                                   TT, TS, STT, ACT, CP, RED, MM, U)

            P.barrier()
            A16.reset(); A32.reset()
            fin = A32.alloc(8 * 32).rearrange("p (a e) -> p a e", a=8)
            RALL = ["cnt_run"]
            TS("vector", fin[:, 0, :], cnt_run[:], 127.0, None, ALU.add, None, ["cnt_run"], ["fin"])
            fin_i = A32.alloc(32).bitcast(I32)
            CP("vector", fin_i, fin[:, 0, :], ["fin"], ["fin_i"])
            TS("vector", fin_i, fin_i, 7, 7, ALU.arith_shift_right, ALU.logical_shift_left, ["fin_i"], ["fin_i"])
            CP("vector", fin[:, 2, :], fin_i, ["fin_i"], ["fin"])
            MEMSET("vector", fin[:, 3, :], 1.0, ["fin"])
            P.op("vector", lambda e: e.tensor_tensor_scan(out=fin[:, 4, :], data0=fin[:, 3, :], data1=fin[:, 2, :],
                                                          initial=0.0, op0=ALU.mult, op1=ALU.add), ["fin"], ["fin"])
            TT("vector", fin[:, 5, :], fin[:, 4, :], fin[:, 2, :], ALU.subtract, ["fin"], ["fin"])
            big = A32.alloc(NT * 32).rearrange("p (t e) -> p t e", e=32)
            slf = A32.alloc(NT * 2).rearrange("p (t k) -> p t k", k=2)
            for k in range(2):
                TT("vector", big, r_E[:, :, k * 32:(k + 1) * 32], fin[:, 5:6, :].to_broadcast([128, NT, 32]), ALU.mult,
                   ["fin", "r_E"], ["big"])
                RED(slf[:, :, k], big, ALU.add, ["big"], ["slf"])
                TT("vector", slf[:, :, k], slf[:, :, k], r_f[:, :, k], ALU.add, ["slf", "r_f"], ["slf"])
            CP("vector", r_slot[:], slf, ["slf"], ["r_slot"])
            bst = A32.alloc(NB)
            DMA("sync", bst, k_bstart, [], ["bst"], "bst")
            ebf = A32.alloc(NB)
            CH = 32
            bigb = A32.alloc(CH * 32).rearrange("p (b e) -> p b e", e=32)
            for b0 in range(0, NB, CH):
                nb_ = min(CH, NB - b0)
                TT("vector", bigb[:, 0:nb_, :], fin[:, 4:5, :].to_broadcast([128, nb_, 32]),
                   bst[:, b0:b0 + nb_].unsqueeze(2).to_broadcast([128, nb_, 32]), ALU.is_le, ["fin", "bst"], ["bigb"])
                RED(ebf[:, b0:b0 + nb_], bigb[:, 0:nb_, :], ALU.add, ["bigb"], ["ebf"])
            TS("vector", ebf, ebf, 31.0, None, ALU.min, None, ["ebf"], ["ebf"])
            sam = A32.alloc(NB)
            MEMSET("vector", sam[:, 0:1], 0.0, ["sam"])
            TT("vector", sam[:, 1:NB], ebf[:, 1:NB], ebf[:, 0:NB - 1], ALU.is_equal, ["ebf"], ["sam"])
            wif = A32.alloc(NB)
            TS("vector", wif, ebf, 128.0, iotap[:, 0:1], ALU.mult, ALU.add, ["ebf", "iotap"], ["wif"])
            STT(wif, sam, 1.0e6, wif, ALU.mult, ALU.add, ["sam", "wif"], ["wif"])
            if l > 0:
                TS("vector", wif, wif, float(l * NE * 128), None, ALU.add, None, ["wif"], ["wif"])
            CP("vector", widx[:], wif, ["wif"], ["widx"])
            hl = [A16.alloc(D) for _ in range(3)]
            for it in range(NT):
                b3 = it % 3
                hk_ = ("hl", b3)
                DMA("sync", hl[b3], h2_d[it * 128:(it + 1) * 128, :], [("h2", it)], [hk_], "hl%d" % b3)
                for k in range(2):
                    off = r_slot[:, it, k:k + 1]
                    P.dma("gpsimd", (lambda e, off=off, src=hl[b3]: e.indirect_dma_start(
                        out=xs_d, out_offset=bass.IndirectOffsetOnAxis(ap=off, axis=0), in_=src, in_offset=None,
                        bounds_check=BR(e, L - 1), oob_is_err=False)), [hk_, "r_slot"], [("xs", it, k)], "xs_sc")
            XSK = [("xs", it, k) for it in range(NT) for k in range(2)]

            P.barrier()
            A16.reset(); A32.reset()
            W1 = A16.alloc(16 * 512)
            W3 = A16.alloc(16 * 512)
            W2 = A16.alloc(4 * 2048)
            xb = [A16.alloc(D) for _ in range(2)]
            xbT = [A16.alloc(16 * 128).rearrange("p (k t) -> p k t", k=16) for _ in range(2)]
            sgf = A32.alloc(512)
            hid = A16.alloc(512)
            hidT = A16.alloc(4 * 128).rearrange("p (k t) -> p k t", k=4)
            yb = [A32.alloc(D) for _ in range(2)]
            w1v = e_w1.rearrange("l e (p k) n -> (l e p) (k n)", k=16)
            w3v = e_w3.rearrange("l e (p k) n -> (l e p) (k n)", k=16)
            w2v = e_w2.rearrange("l e (p k) n -> (l e p) (k n)", k=4)
            for b in range(NB):
                b2 = b % 2
                off = widx[:, b:b + 1]
                for Wt, wv, wk in ((W1, w1v, "W1"), (W3, w3v, "W3"), (W2, w2v, "W2")):
                    P.dma("gpsimd", (lambda e, off=off, Wt=Wt, wv=wv, bnd=(l + 1) * NE * 128 - 1: e.indirect_dma_start(
                        out=Wt, out_offset=None, in_=wv, in_offset=bass.IndirectOffsetOnAxis(ap=off, axis=0),
                        bounds_check=BR(e, bnd), oob_is_err=False)), ["widx"], [wk], wk)
                xk = ("xb", b2)
                DMA("sync", xb[b2], xs_d[b * 128:(b + 1) * 128, :], XSK, [xk], "xb%d" % b2)
                for kc in range(16):
                    TR(pt[:, kc * 128:(kc + 1) * 128], xb[b2].rearrange("p (c k) -> p k c", k=16)[:, kc, :], ident_b[:],
                       [xk, "ident_b"], [PT])
                xT, xTk = xbT[b2], ("xbT", b2)
                CP("scalar", xT, pt[:, :].rearrange("p (k t) -> p k t", k=16), [PT], [xTk])
                for kc in range(16):
                    MM(pm[0][:, :], xT[:, kc, :], W1[:, kc * 512:(kc + 1) * 512], kc == 0, kc == 15, [xTk, "W1"], [PM[0]])
                for kc in range(16):
                    MM(pm[1][:, :], xT[:, kc, :], W3[:, kc * 512:(kc + 1) * 512], kc == 0, kc == 15, [xTk, "W3"], [PM[1]])
                ACT(sgf, pm[0][:, :], AF.Silu, [PM[0]], ["sgf"])
                TT("vector", hid, pm[1][:, :], sgf, ALU.mult, [PM[1], "sgf"], ["hid"])
                for fc in range(4):
                    TR(pt[:, fc * 128:(fc + 1) * 128], hid.rearrange("p (c k) -> p k c", k=4)[:, fc, :], ident_b[:],
                       ["hid", "ident_b"], [PT])
                CP("vector", hidT, pt[:, 0:512].rearrange("p (k t) -> p k t", k=4), [PT], ["hidT"])
                y_, yk = yb[b2], ("yb", b2)
                for cbk in range(4):
                    pi = 2 + (cbk % 2)
                    for fc in range(4):
                        MM(pm[pi][:, :], hidT[:, fc, :], W2[:, fc * 2048 + cbk * 512:fc * 2048 + (cbk + 1) * 512],
                           fc == 0, fc == 3, ["hidT", "W2"], [PM[pi]])
                    CP("scalar" if cbk % 2 else "vector", y_[:, cbk * 512:(cbk + 1) * 512], pm[pi][:, :], [PM[pi]], [yk])
                DMA("sync", ys_d[b * 128:(b + 1) * 128, :], y_, [yk], [("ys", b)], "yb%d" % b2)
            YSK = [("ys", b) for b in range(NB)]

            P.barrier()
            A16.reset(); A32.reset()
            g2 = [A32.alloc(D) for _ in range(2)]
            for q in range(2):
                DMA("sync", g2[q], mod_rep(q, 5), MODK, [("g2", q)], "g2%d" % q)
            if last:
                fg = A32.alloc(D)
                DMA("sync", fg, final_g.rearrange("(o n) -> o n", o=1).broadcast_to([128, D]), [], ["fg"], "fg")
            ya = [A32.alloc(D) for _ in range(2)]
            ybb = [A32.alloc(D) for _ in range(2)]
            x1t = [A32.alloc(D) for _ in range(2)]
            junk = A16.alloc(D)
            ss = A32.alloc(8)
            dst_x = y_out if last else x2_d
            for it in range(NT):
                b2 = it % 2
                q = 0 if it * 128 < OFFS[1] else 1
                for k, yt in enumerate((ya, ybb)):
                    off = r_slot[:, it, k:k + 1]
                    P.dma("gpsimd", (lambda e, off=off, dst=yt[b2]: e.indirect_dma_start(
                        out=dst, out_offset=None, in_=ys_d, in_offset=bass.IndirectOffsetOnAxis(ap=off, axis=0),
                        bounds_check=BR(e, L - 1), oob_is_err=False)), YSK + ["r_slot"], [("yg", k, b2)], "yg%d%d" % (k, b2))
                xk = ("x1t", b2)
                DMA("sync", x1t[b2], x1_d[it * 128:(it + 1) * 128, :], [("x1", it)], [xk], "x1l%d" % b2)
                A_, B_ = ya[b2], ybb[b2]
                TS("vector", A_, A_, r_f[:, it, 2:3], None, ALU.mult, None, [("yg", 0, b2), "r_f"], [("yg", 0, b2)])
                STT(A_, B_, r_f[:, it, 3:4], A_, ALU.mult, ALU.add, [("yg", 1, b2), ("yg", 0, b2), "r_f"], [("yg", 0, b2)])
                TT("gpsimd", A_, A_, g2[q], ALU.mult, [("yg", 0, b2), ("g2", q)], [("yg", 0, b2)])
                TT("vector", B_, A_, x1t[b2], ALU.add, [("yg", 0, b2), xk], [("yg", 1, b2)])
                if not last:
                    DMA("sync", dst_x[it * 128:(it + 1) * 128, :], B_, [("yg", 1, b2)], [("xcur", it)], "xo%d" % b2)
                else:
                    ssk = U("ss")
                    ACT(junk, B_, AF.Square, [("yg", 1, b2)], ["junk", ssk], accum_out=ss[:, 0:1])
                    ACT(ss[:, 1:2], ss[:, 0:1], AF.Sqrt, [ssk, "eps_t"], [ssk], scale=1.0 / D, bias=eps_t[:, 0:1])
                    P.op("vector", lambda e, ss=ss: e.reciprocal(out=ss[:, 2:3], in_=ss[:, 1:2]), [ssk], [ssk])
                    STT(A_, B_, ss[:, 2:3], fg, ALU.mult, ALU.mult, [("yg", 1, b2), ssk, "fg"], [("yg", 0, b2)])
                    DMA("sync", dst_x[it * 128:(it + 1) * 128, :], A_, [("yg", 0, b2)], [("yout", it)], "xo%d" % b2)
            cur_x = x2_d
        P.emit()
    return nc


def route_tile(P, nc, sm, pr, PR, rb_, RBK, r_E, r_f, cnt_run, utinc, ones_b, git,
               TT, TS, STT, ACT, CP, RED, MM, U):
    rk = U("rt")
    Lg = sm[:, 8:44]
    TT("vector", Lg, pr[0][:, 0:36], rb_, ALU.add, [PR[0]] + RBK, [rk])
    m = sm[:, 44:45]
    RED(m, Lg[:, 0:4], ALU.max, [rk], [rk])
    oh = sm[:, 48:52]
    TS("vector", oh, Lg[:, 0:4], m, None, ALU.is_equal, None, [rk], [rk])
    negm = sm[:, 45:46]
    TS("vector", negm, m, -1.0, None, ALU.mult, None, [rk], [rk])
    ex = sm[:, 52:56]
    se = sm[:, 46:47]
    ACT(ex, Lg[:, 0:4], AF.Exp, [rk], [rk], bias=negm, scale=1.0, accum_out=se)
    pg = sm[:, 47:48]
    P.op("vector", lambda e: e.reciprocal(out=pg, in_=se), [rk], [rk])
    lf = sm[:, 56:64]
    TS("vector", lf, Lg[:, 4:12], oh[:, 0:1], None, ALU.mult, None, [rk], [rk])
    for g in range(1, 4):
        STT(lf, Lg[:, 4 + 8 * g:12 + 8 * g], oh[:, g:g + 1], lf, ALU.mult, ALU.add, [rk], [rk])
    top = sm[:, 64:72]
    P.op("vector", lambda e: e.max(out=top, in_=lf), [rk], [rk])
    s1 = sm[:, 72:80]
    s2 = sm[:, 80:88]
    TS("vector", s1, lf, top[:, 0:1], None, ALU.is_equal, None, [rk], [rk])
    TS("vector", s2, lf, top[:, 1:2], None, ALU.is_equal, None, [rk], [rk])
    dv = sm[:, 88:89]
    TT("vector", dv, top[:, 0:1], top[:, 1:2], ALU.subtract, [rk], [rk])
    sg = sm[:, 89:90]
    ACT(sg, dv, AF.Sigmoid, [rk], [rk])
    TT("vector", r_f[:, git, 2:3], pg, sg, ALU.mult, [rk], ["r_f"])
    TT("vector", r_f[:, git, 3:4], pg, r_f[:, git, 2:3], ALU.subtract, [rk, "r_f"], ["r_f"])
    Ef = sm[:, 96:160]
    for g in range(4):
        TS("vector", Ef[:, 8 * g:8 * g + 8], s1, oh[:, g:g + 1], None, ALU.mult, None, [rk], [rk])
        TS("vector", Ef[:, 32 + 8 * g:40 + 8 * g], s2, oh[:, g:g + 1], None, ALU.mult, None, [rk], [rk])
    CP("vector", r_E[:, git, :], Ef, [rk], ["r_E"])
    Mb = sm[:, 160:192]
    TT("vector", Mb, Ef[:, 0:32], Ef[:, 32:64], ALU.add, [rk], [rk])
    Mbb = sm[:, 192:224].bitcast(BF16)[:, 0:32]
    CP("vector", Mbb, Mb, [rk], [rk])
    MM(pr[1][:, 0:32], utinc[:], Mbb, True, True, ["utinc", rk], [PR[1]])
    MM(pr[1][:, 32:64], ones_b[:], Mbb, True, True, ["ones_b", rk], [PR[1]])
    rank = sm[:, 224:256]
    TT("vector", rank, pr[1][:, 0:32], Mb, ALU.subtract, [PR[1], rk], [rk])
    TT("vector", rank, rank, cnt_run[:], ALU.add, [rk, "cnt_run"], [rk])
    TT("vector", cnt_run[:], cnt_run[:], pr[1][:, 32:64], ALU.add, [PR[1], "cnt_run"], ["cnt_run"])
    tmp = sm[:, 160:192]
    for k in range(2):
        TT("vector", tmp, Ef[:, 32 * k:32 * k + 32], rank, ALU.mult, [rk], [rk])
        RED(r_f[:, git, k:k + 1], tmp, ALU.add, [rk], ["r_f"])


_WNAMES = ["w_ada", "b_ada", "norm1_g", "w_in", "pool_w", "pool_scale", "conv_dw_w", "conv_dw_b",
           "conv_ln_g", "conv_ln_b", "conv_pw_w", "conv_pw_b", "fourier_w", "w_out", "norm2_g",
           "router_coarse_w", "router_coarse_b", "router_fine_w", "router_fine_b",
           "expert_w1", "expert_w3", "expert_w2", "final_g"]


def kernel(**inputs):
    xs_ = np.asarray(inputs["x_sample"], np.float32)
    xp_ = np.asarray(inputs["x_prompt"], np.float32)
    cs_ = np.asarray(inputs["c_sample"], np.float32)
    cp_ = np.asarray(inputs["c_prompt"], np.float32)
    S0, S1 = xs_.shape[1], xp_.shape[1]
    depth = inputs["w_ada"].shape[0]
    nc = build((S0, S1), depth)
    N = S0 + S1
    NB = -(-(2 * N + NE * 127) // 128)
    consts = make_consts((S0, S1), NB)
    wts = {k: np.ascontiguousarray(np.asarray(inputs[k], np.float32)) for k in _WNAMES}
    in_maps = []
    for core in range(8):
        b = core % 4
        m = {"x": np.ascontiguousarray(np.concatenate([xs_[b], xp_[b]], axis=0)),
             "c": np.ascontiguousarray(np.stack([cs_[b], cp_[b]], axis=0))}
        m.update(wts)
        m.update(consts)
        in_maps.append(m)
    res = run_bass_kernel_spmd(nc, in_maps, core_ids=list(range(8)))
    y_s = np.stack([res.results[b]["y"][:S0] for b in range(4)], axis=0)
    y_p = np.stack([res.results[b]["y"][S0:] for b in range(4)], axis=0)
    return (y_p.astype(np.float32), y_s.astype(np.float32))
```

```python
import math
import os
from contextlib import ExitStack

import numpy as np
import ml_dtypes
import concourse.bass as bass
import concourse.mybir as mybir
from concourse.bass_utils import run_bass_kernel_spmd

F32 = mybir.dt.float32
BF16 = mybir.dt.bfloat16
I32 = mybir.dt.int32
AF = mybir.ActivationFunctionType
ALU = mybir.AluOpType
AX = mybir.AxisListType

D = 2048
NG, EPG, NE = 4, 8, 32
DE = 512
CONV_K = 31
EPS = 1e-6
SAME_ENGINE_SYNC = True


class _Op:
    __slots__ = ("q", "fn", "deps", "sig", "sigval", "sem", "is_dma", "idx", "bar")


class Prog:
    def __init__(self, nc):
        self.nc = nc
        self.ops = []
        self.last_w = {}
        self.readers = {}
        self.dma_cnt = {}
        self.last_eng = {}
        self.bar = None
        self.bar_done = set()

    def barrier(self):
        deps = [(o, None) for o in self.last_eng.values()]
        dmas = dict(self.dma_cnt)
        self.bar = (deps, dmas)
        self.bar_done = set()

    def _add(self, q, fn, reads, writes, is_dma, semkey):
        o = _Op()
        o.q = q
        o.fn = fn
        o.is_dma = is_dma
        o.sig = False
        o.sigval = 0
        o.idx = len(self.ops)
        deps = {}
        for r in reads:
            w = self.last_w.get(r)
            if w is not None:
                deps[w.idx] = w
        for w_ in writes:
            w = self.last_w.get(w_)
            if w is not None:
                deps[w.idx] = w
            for rd in self.readers.get(w_, ()):
                deps[rd.idx] = rd
        o.deps = []
        for d in deps.values():
            if d.is_dma:
                o.deps.append((d, self.dma_cnt[d.sem]))
            else:
                o.deps.append((d, None))
        o.bar = None
        if self.bar is not None and q not in self.bar_done:
            self.bar_done.add(q)
            bdeps, bdmas = self.bar
            o.deps.extend(bdeps)
            o.bar = bdmas
        if is_dma:
            o.sem = ("dma", semkey)
            self.dma_cnt[o.sem] = self.dma_cnt.get(o.sem, 0) + 1
            o.sigval = self.dma_cnt[o.sem]
        else:
            o.sem = ("eng", q)
            self.last_eng[q] = o
        for w_ in writes:
            self.last_w[w_] = o
            self.readers[w_] = []
        for r in reads:
            if r not in writes:
                self.readers.setdefault(r, []).append(o)
        self.ops.append(o)
        return o

    def op(self, eng, fn, reads=(), writes=()):
        return self._add(eng, fn, tuple(reads), tuple(writes), False, None)

    def dma(self, q, fn, reads=(), writes=(), semkey=None):
        assert semkey is not None
        return self._add(q, fn, tuple(reads), tuple(writes), True, semkey)

    def emit(self):
        nc = self.nc
        ops = self.ops
        for o in ops:
            for d, _ in o.deps:
                if d.is_dma:
                    continue
                if d.q != o.q or (SAME_ENGINE_SYNC and d.q != "tensor"):
                    d.sig = True
        cnt = {}
        for o in ops:
            if not o.is_dma and o.sig:
                cnt[o.q] = cnt.get(o.q, 0) + 1
                o.sigval = cnt[o.q]
        semkeys = []
        seen = set()
        for o in ops:
            if (o.is_dma or o.sig) and o.sem not in seen:
                seen.add(o.sem)
                semkeys.append(o.sem)
        self.n_sems = len(semkeys)
        with ExitStack() as es:
            sems = {}
            for i, k in enumerate(semkeys):
                sems[k] = es.enter_context(nc.semaphore("s%d" % i))
            block = es.enter_context(nc.Block())
            queues = {}
            for o in ops:
                queues.setdefault(o.q, []).append(o)
            totals = dict(self.dma_cnt)

            def run_queue(qname, eng):
                waited = {}
                for o in queues.get(qname, ()):
                    need = {}
                    for d, n in o.deps:
                        if d.is_dma:
                            v = 16 * n
                        else:
                            if d.q == o.q and (d.q == "tensor" or not SAME_ENGINE_SYNC):
                                continue
                            v = d.sigval
                        if v > need.get(d.sem, 0):
                            need[d.sem] = v
                    if o.bar is not None:
                        for k, n in o.bar.items():
                            if 16 * n > need.get(k, 0):
                                need[k] = 16 * n
                    for k, v in need.items():
                        if waited.get(k, 0) < v:
                            eng.wait_ge(sems[k], v)
                            waited[k] = v
                    ins = o.fn(eng)
                    if o.is_dma:
                        ins.then_inc(sems[o.sem], 16)
                    elif o.sig:
                        ins.then_inc(sems[o.sem], 1)
                if qname == "sync":
                    for k, n in totals.items():
                        if waited.get(k, 0) < 16 * n:
                            eng.wait_ge(sems[k], 16 * n)

            @block.sync
            def _(e):
                run_queue("sync", e)

            @block.scalar
            def _(e):
                run_queue("scalar", e)

            @block.gpsimd
            def _(e):
                run_queue("gpsimd", e)

            @block.vector
            def _(e):
                run_queue("vector", e)

            @block.tensor
            def _(e):
                run_queue("tensor", e)


def _bf(a):
    return np.ascontiguousarray(a.astype(ml_dtypes.bfloat16))


def make_consts(SEQS, NB):
    c = {}
    t = np.arange(128)
    ang = 2 * np.pi * np.outer(t, t) / 128.0
    cs = np.zeros((128, 2, 192), np.float64)
    for h in range(2):
        sl = slice(h * 64, h * 64 + 64)
        cs[:, h, 0:64] = np.cos(ang[:, sl])
        cs[:, h, 64:128] = np.sin(ang[:, sl])
        cs[:, h, 128:192] = -np.sin(ang[:, sl])
    c["cs128"] = _bf(cs)
    ccn = np.zeros((128, 2, 128), np.float64)
    ccn[:, 0] = np.cos(ang)
    ccn[:, 1] = -np.sin(ang)
    c["ccn"] = _bf(ccn)
    for q, S in enumerate(SEQS):
        S2 = S // 128
        a = 2 * np.pi * (np.outer(np.arange(S2), np.arange(S)) % S) / S
        f = np.zeros((S2, 2, S), np.float64)
        f[:, 0] = np.cos(a)
        f[:, 1] = np.sin(a)
        c["fcs%d" % q] = _bf(f)
    pc = np.zeros((128, 4, 16), np.float32)
    for g, w in enumerate((2, 4, 8, 16)):
        for j in range(8):
            cnt = min(j + w // 2, 10 ** 9) - max(j - w // 2, 0)
            pc[:, g, j] = 1.0 / cnt
        for j in range(8):
            tt = -8 + j
            hi = min(tt + w // 2, 0)
            lo = tt - w // 2
            pc[:, g, 8 + j] = 1.0 / (hi - lo)
    c["poolc"] = pc
    ut = (np.arange(128)[:, None] <= np.arange(128)[None, :]).astype(np.float32)
    c["utinc"] = _bf(ut)
    c["iotap"] = np.arange(128, dtype=np.float32).reshape(128, 1)
    c["bstart"] = np.tile((128.0 * np.arange(NB, dtype=np.float32))[None, :], (128, 1))
    c["iota32"] = np.tile(np.arange(32, dtype=np.float32)[None, :], (128, 1))
    return c


class Arena:
    def __init__(self, t, size):
        self.t = t
        self.size = size
        self.off = 0

    def reset(self):
        self.off = 0

    def alloc(self, n, align=16):
        self.off = (self.off + align - 1) // align * align
        o = self.off
        self.off += n
        assert self.off <= self.size, ("arena overflow", self.off, self.size)
        return self.t[:, o:o + n]


def build(SEQS=(8192, 2048), DEPTH=2, dbg=False, NL=None):
    NL = DEPTH if NL is None else NL
    N = sum(SEQS)
    NT = N // 128
    NB = -(-(2 * N + NE * 127) // 128)
    L = NB * 128
    OFFS = [0]
    for S in SEQS:
        OFFS.append(OFFS[-1] + S)
    nc = bass.Bass("TRN2", target_bir_lowering=False)

    def din(name, shape, dt=F32):
        return nc.dram_tensor(name, list(shape), dt, kind="ExternalInput").ap()

    def dscr(name, shape, dt):
        kind = "ExternalOutput" if dbg else "Internal"
        return nc.dram_tensor(name, list(shape), dt, kind=kind).ap()

    x_in = din("x", [N, D])
    c_in = din("c", [2, D])
    w_ada = din("w_ada", [DEPTH, D, 6 * D])
    b_ada = din("b_ada", [DEPTH, 6 * D])
    norm1_g = din("norm1_g", [DEPTH, D])
    w_in = din("w_in", [DEPTH, D, 3072])
    pool_w = din("pool_w", [DEPTH, 4, 128, 128])
    pool_scale = din("pool_scale", [DEPTH, 512])
    conv_dw_w = din("conv_dw_w", [DEPTH, CONV_K, 1024])
    conv_dw_b = din("conv_dw_b", [DEPTH, 1024])
    conv_ln_g = din("conv_ln_g", [DEPTH, 1024])
    conv_ln_b = din("conv_ln_b", [DEPTH, 1024])
    conv_pw_w = din("conv_pw_w", [DEPTH, 1024, 1024])
    conv_pw_b = din("conv_pw_b", [DEPTH, 1024])
    fourier_w = din("fourier_w", [DEPTH, 512, 512])
    w_out = din("w_out", [DEPTH, D, D])
    norm2_g = din("norm2_g", [DEPTH, D])
    rc_w = din("router_coarse_w", [DEPTH, D, NG])
    rc_b = din("router_coarse_b", [DEPTH, NG])
    rf_w = din("router_fine_w", [DEPTH, NG, D, EPG])
    rf_b = din("router_fine_b", [DEPTH, NG, EPG])
    e_w1 = din("expert_w1", [DEPTH, NE, D, DE])
    e_w3 = din("expert_w3", [DEPTH, NE, D, DE])
    e_w2 = din("expert_w2", [DEPTH, NE, DE, D])
    final_g = din("final_g", [D])
    k_cs128 = din("cs128", [128, 2, 192], BF16)
    k_ccn = din("ccn", [128, 2, 128], BF16)
    k_fcs = [din("fcs%d" % q, [S // 128, 2, S], BF16) for q, S in enumerate(SEQS)]
    k_poolc = din("poolc", [128, 4, 16])
    k_utinc = din("utinc", [128, 128], BF16)
    k_iotap = din("iotap", [128, 1])
    k_bstart = din("bstart", [128, NB])
    k_iota32 = din("iota32", [128, 32])

    y_out = nc.dram_tensor("y", [N, D], F32, kind="ExternalOutput").ap()

    x1_d = dscr("x1", [N, D], F32)
    x2_d = dscr("x2", [N, D], F32)
    h2_d = dscr("h2", [N, D], BF16)
    xs_d = dscr("xs", [L, D], BF16)
    ys_d = dscr("ys", [L, D], F32)
    mod_d = dscr("modrow", [2, 6 * D], F32)
    zaT = [dscr("zaT%d" % q, [512, S], BF16) for q, S in enumerate(SEQS)]
    vT = [dscr("vT%d" % q, [1024, S], BF16) for q, S in enumerate(SEQS)]
    zcT = [dscr("zcT%d" % q, [512, S], BF16) for q, S in enumerate(SEQS)]
    rzT = [dscr("rzT%d" % q, [512, S], BF16) for q, S in enumerate(SEQS)]
    mixT = [dscr("mixT%d" % q, [2048, S], BF16) for q, S in enumerate(SEQS)]

    P = Prog(nc)
    es = ExitStack()
    with es:
        AR_SZ = 96 * 1024
        art = es.enter_context(nc.sbuf_tensor("arena", [128, AR_SZ], BF16))
        AR = Arena(art, AR_SZ)

        class _A16:
            def alloc(self, n):
                return AR.alloc(n)

            def reset(self):
                AR.reset()

        class _A32:
            def alloc(self, n):
                return AR.alloc(2 * n).bitcast(F32)

            def reset(self):
                pass

        A16 = _A16()
        A32 = _A32()
        ident_f = es.enter_context(nc.sbuf_tensor("ident_f", [128, 128], F32))
        ident_b = es.enter_context(nc.sbuf_tensor("ident_b", [128, 128], BF16))
        ones_b = es.enter_context(nc.sbuf_tensor("ones_b", [128, 128], BF16))
        utinc = es.enter_context(nc.sbuf_tensor("utinc_s", [128, 128], BF16))
        iotap = es.enter_context(nc.sbuf_tensor("iotap_s", [128, 1], F32))
        iota32 = es.enter_context(nc.sbuf_tensor("iota32_s", [128, 32], F32))
        eps_t = es.enter_context(nc.sbuf_tensor("eps_t", [128, 1], F32))
        r_E = es.enter_context(nc.sbuf_tensor("r_E", [128, NT, 64], BF16))
        r_f = es.enter_context(nc.sbuf_tensor("r_f", [128, NT, 4], F32))
        r_slot = es.enter_context(nc.sbuf_tensor("r_slot", [128, NT, 2], I32))
        cnt_run = es.enter_context(nc.sbuf_tensor("cnt_run", [128, 32], F32))
        widx = es.enter_context(nc.sbuf_tensor("widx", [128, NB], I32))
        pm = [es.enter_context(nc.psum_tensor("pm%d" % i, [128, 512], F32)) for i in range(4)]
        pt = es.enter_context(nc.psum_tensor("pt", [128, 2048], BF16))
        pr = [es.enter_context(nc.psum_tensor("pr%d" % i, [128, 512], F32)) for i in range(2)]
        PM = [("pm", i) for i in range(4)]
        PT = "pt"
        PR = [("pr", i) for i in range(2)]

        uid = [0]
        _bregs = {}

        def BR(e, val):
            if val not in _bregs:
                _bregs[val] = e.to_reg(val)
            return _bregs[val]

        def U(name):
            uid[0] += 1
            return (name, uid[0])

        def t16(n, shape=None):
            v = A16.alloc(n)
            return v

        def DMA(q, out, in_, reads, writes, semkey, **kw):
            P.dma(q, lambda e: e.dma_start(out=out, in_=in_, **kw), reads, writes, semkey)

        def MM(out, lhsT, rhs, start, stop, reads, writes):
            P.op("tensor", lambda e: e.matmul(out, lhsT=lhsT, rhs=rhs, start=start, stop=stop), reads, writes)

        def TR(out, in_, ident, reads, writes):
            P.op("tensor", lambda e: e.transpose(out=out, in_=in_, identity=ident), reads, writes)

        def ACT(out, in_, func, reads, writes, bias=None, scale=None, accum_out=None, eng="scalar"):
            kw = {}
            if bias is not None:
                kw["bias"] = bias
            if scale is not None:
                kw["scale"] = scale
            if accum_out is not None:
                kw["accum_out"] = accum_out
            P.op("scalar", lambda e: e.activation(out=out, in_=in_, func=func, **kw), reads, writes)

        def TT(eng, out, in0, in1, op, reads, writes):
            P.op(eng, lambda e: e.tensor_tensor(out=out, in0=in0, in1=in1, op=op), reads, writes)

        def TS(eng, out, in0, s1, s2, op0, op1, reads, writes, accum_out=None):
            if op1 is None:
                P.op(eng, lambda e: e.tensor_scalar(out=out, in0=in0, scalar1=s1, scalar2=None, op0=op0), reads, writes)
            elif accum_out is not None:
                P.op(eng, lambda e: e.tensor_scalar(out=out, in0=in0, scalar1=s1, scalar2=s2, op0=op0, op1=op1, accum_out=accum_out), reads, writes)
            else:
                P.op(eng, lambda e: e.tensor_scalar(out=out, in0=in0, scalar1=s1, scalar2=s2, op0=op0, op1=op1), reads, writes)

        def STT(out, in0, scalar, in1, op0, op1, reads, writes):
            P.op("vector", lambda e: e.scalar_tensor_tensor(out=out, in0=in0, scalar=scalar, in1=in1, op0=op0, op1=op1), reads, writes)

        def CP(eng, out, in_, reads, writes):
            if eng == "scalar":
                P.op("scalar", lambda e: e.copy(out=out, in_=in_), reads, writes)
            else:
                P.op(eng, lambda e: e.tensor_copy(out=out, in_=in_), reads, writes)

        def MEMSET(eng, ap, val, writes):
            P.op(eng, lambda e: e.memset(ap, val), (), writes)

        def RED(out, in_, op, reads, writes, axis=AX.X):
            P.op("vector", lambda e: e.tensor_reduce(out=out, in_=in_, axis=axis, op=op), reads, writes)

        def bcast_row(dram_row_ap, n):
            return dram_row_ap.partition_broadcast(128)

        MEMSET("gpsimd", ident_f[:], 0.0, ["ident_f"])
        P.op("gpsimd", lambda e: e.affine_select(out=ident_f[:], in_=ident_f[:], pattern=[[-1, 128]],
                                                 compare_op=ALU.not_equal, fill=1.0, base=0, channel_multiplier=1),
             ["ident_f"], ["ident_f"])
        CP("vector", ident_b[:], ident_f[:], ["ident_f"], ["ident_b"])
        MEMSET("gpsimd", ones_b[:], 1.0, ["ones_b"])
        MEMSET("gpsimd", eps_t[:], EPS, ["eps_t"])
        DMA("sync", utinc[:], k_utinc, [], ["utinc"], "c_utinc")
        DMA("sync", iotap[:], k_iotap, [], ["iotap"], "c_iotap")
        DMA("sync", iota32[:], k_iota32, [], ["iota32"], "c_iota32")

        class _Stop(Exception):
            pass

        def stop_here(tag):
            if os.environ.get("KSTOP") == tag:
                P.emit()
                raise _Stop()

        cur_x = x_in
        try:
          for l in range(NL):
              last = (l == NL - 1)
              nxt_x = y_out if False else x2_d
              P.barrier()
              A16.reset(); A32.reset()
              cst = A32.alloc(2 * 16).rearrange("p (q k) -> p q k", q=2)
              csb = A16.alloc(16 * 2).rearrange("p (k q) -> p k q", q=2)
              brow = [A32.alloc(512) for _ in range(2)]
              mrow = A32.alloc(512)
              DMA("sync", cst, c_in.rearrange("q (p k) -> p q k", k=16), [], ["cst"], "cst")
              for q in range(2):
                  ACT(csb[:, :, q], cst[:, q, :], AF.Silu, ["cst"], [("csb", q)])
              wa = [A16.alloc(16 * 512).rearrange("p (k n) -> p k n", k=16) for _ in range(2)]
              for cb in range(24):
                  wbuf = wa[cb % 2]
                  wk = ("wa", cb % 2)
                  DMA("gpsimd", wbuf, w_ada[l, :, cb * 512:(cb + 1) * 512].rearrange("(p k) n -> p k n", k=16),
                      [], [wk], "wa%d" % (cb % 2))
                  pk = PM[cb % 2]
                  pst = pm[cb % 2]
                  for kc in range(16):
                      MM(pst[0:2, :], csb[:, kc, :], wbuf[:, kc, :], kc == 0, kc == 15,
                         [wk, ("csb", 0), ("csb", 1)], [pk])
                  mk = U("mrow")
                  bk = ("brow", cb % 2)
                  DMA("sync", brow[cb % 2][0:2, :], b_ada[l:l + 1, cb * 512:(cb + 1) * 512].broadcast_to([2, 512]),
                      [], [bk], "brow%d" % (cb % 2))
                  TT("vector", mrow[0:2, :], pst[0:2, :], brow[cb % 2][0:2, :], ALU.add,
                     [pk, bk], ["mrow"])
                  DMA("sync", mod_d[:, cb * 512:(cb + 1) * 512], mrow[0:2, :], ["mrow"], [("mod", cb)], "mrow")
              MODK = [("mod", cb) for cb in range(24)]

              def mod_rep(q, i):
                  return mod_d[q:q + 1, i * D:(i + 1) * D].broadcast_to([128, D])

              for q, S in enumerate(SEQS):
                  T0 = OFFS[q]
                  NT5 = S // 512
                  S2 = S // 128
                  P.barrier()
                  A16.reset(); A32.reset()
                  wi = A16.alloc(16 * 3072).rearrange("p (k n) -> p k n", k=16)
                  DMA("gpsimd", wi, w_in[l].rearrange("(p k) n -> p k n", k=16), [], ["wi"], "wi")
                  a1 = A32.alloc(D)
                  sh1 = A32.alloc(D)
                  tmpf = A32.alloc(D)
                  DMA("sync", a1, mod_rep(q, 1), MODK, ["a1"], "a1")
                  DMA("sync", sh1, mod_rep(q, 0), MODK, ["sh1"], "sh1")
                  DMA("sync", tmpf, norm1_g[l:l + 1, :].broadcast_to([128, D]), [], ["tmpf"], "g_rep")
                  STT(a1, a1, 1.0, tmpf, ALU.add, ALU.mult, ["a1", "tmpf"], ["a1"])
                  xt = [A32.alloc(D) for _ in range(2)]
                  hb = [A16.alloc(D) for _ in range(4)]
                  hT = [A16.alloc(16 * 512).rearrange("p (k t) -> p k t", k=16) for _ in range(1)]
                  stg = [A16.alloc(16 * 512).rearrange("p (j t) -> p j t", j=16) for _ in range(1)]
                  sgt = [A32.alloc(512) for _ in range(2)]
                  ss = A32.alloc(8)
                  hTt = hT[0]
                  hk = ("hT", 0)

                  def p1_norm(ti):
                      for sub in range(4):
                          it = ti * 4 + sub
                          b2 = it % 2
                          r0 = T0 + it * 128
                          xk = ("xt", b2)
                          DMA("sync", xt[b2], cur_x[r0:r0 + 128, :], [("xcur", r0 // 128)], [xk], "xt%d" % b2)
                          ssk = U("ss")
                          hbk = ("hb", sub)
                          ACT(hb[sub], xt[b2], AF.Square, [xk], [hbk, ssk], accum_out=ss[:, 0:1])
                          ACT(ss[:, 1:2], ss[:, 0:1], AF.Sqrt, [ssk, "eps_t"], [ssk], scale=1.0 / D, bias=eps_t[:, 0:1])
                          P.op("vector", lambda e, ss=ss: e.reciprocal(out=ss[:, 2:3], in_=ss[:, 1:2]), [ssk], [ssk])
                          STT(tmpf, xt[b2], ss[:, 2:3], a1, ALU.mult, ALU.mult, [xk, ssk, "a1"], ["tmpf"])
                          TT("gpsimd", hb[sub], tmpf, sh1, ALU.add, ["tmpf", "sh1"], [hbk])

                  def p1_tr(ti):
                      for sub in range(4):
                          hbk = ("hb", sub)
                          for kc in range(16):
                              TR(pt[:, kc * 128:(kc + 1) * 128], hb[sub].rearrange("p (c k) -> p k c", k=16)[:, kc, :],
                                 ident_b[:], [hbk, "ident_b"], [PT])
                          CP("scalar" if sub % 2 else "vector", hTt[:, :, sub * 128:(sub + 1) * 128],
                             pt[:, :].rearrange("p (k t) -> p k t", k=16), [PT], [hk])

                  def p1_mm(ti):
                      st = stg[0]
                      sk = ("stg", 0)
                      c0 = ti * 512

                      def mmgrp(j, pi):
                          for kc in range(16):
                              MM(pm[pi][:, :], wi[:, kc, j * 128:(j + 1) * 128], hTt[:, kc, :], kc == 0, kc == 15,
                                 ["wi", hk], [PM[pi]])

                      pi = 0
                      for j in range(4):
                          mmgrp(j, pi)
                          CP("scalar", st[:, j, :], pm[pi][:, :], [PM[pi]], [sk])
                          pi = (pi + 1) % 4
                      for j in range(8):
                          pa = pi
                          mmgrp(4 + j, pa)
                          pb = (pi + 1) % 4
                          mmgrp(12 + j, pb)
                          sg = sgt[j % 2]
                          sgk = ("sg", j % 2)
                          ACT(sg, pm[pb][:, :], AF.Sigmoid, [PM[pb]], [sgk])
                          TT("vector", st[:, 4 + j, :], pm[pa][:, :], sg, ALU.mult, [PM[pa], sgk], [sk])
                          pi = (pi + 2) % 4
                      for j in range(4):
                          mmgrp(20 + j, pi)
                          CP("scalar", st[:, 12 + j, :], pm[pi][:, :], [PM[pi]], [sk])
                          pi = (pi + 1) % 4
                      DMA("sync", zaT[q][:, c0:c0 + 512].rearrange("(j p) t -> p j t", p=128), st[:, 0:4, :],
                          [sk], [("zaT", q, ti)], "stg0")
                      DMA("sync", vT[q][:, c0:c0 + 512].rearrange("(j p) t -> p j t", p=128), st[:, 4:12, :],
                          [sk], [("vT", q, ti)], "stg0")
                      DMA("sync", zcT[q][:, c0:c0 + 512].rearrange("(j p) t -> p j t", p=128), st[:, 12:16, :],
                          [sk], [("zcT", q, ti)], "stg0")

                  p1_norm(0)
                  p1_tr(0)
                  for ti in range(NT5):
                      if ti + 1 < NT5:
                          p1_norm(ti + 1)
                      p1_mm(ti)
                      if ti + 1 < NT5:
                          p1_tr(ti + 1)

                  stop_here('P1')
                  P.barrier()
                  A16.reset(); A32.reset()
                  pwf = A32.alloc(4 * 128).rearrange("p (g e) -> p g e", g=4)
                  pwb = A16.alloc(4 * 128).rearrange("p (g e) -> p g e", g=4)
                  DMA("sync", pwf, pool_w[l].rearrange("g c e -> c g e"), [], ["pwf"], "pwf")
                  CP("vector", pwb, pwf, ["pwf"], ["pwb"])
                  psc = A32.alloc(4)
                  pscr = A32.alloc(4 * 128).rearrange("p (g e) -> p g e", g=4)[0:4]
                  DMA("sync", pscr[:, 0, :], pool_scale[l].rearrange("(g e) -> g e", g=4), [], ["pscr"], "pscr")
                  TR(pr[0][:, 0:4], pscr[:, 0, :], ident_f[0:4, 0:4], ["pscr", "ident_f"], [PR[0]])
                  CP("vector", psc, pr[0][:, 0:4], [PR[0]], ["psc"])
                  pcst = A32.alloc(64).rearrange("p (g j) -> p g j", g=4)
                  DMA("sync", pcst, k_poolc, [], ["pcst"], "pcst")
                  ub = [A16.alloc(528) for _ in range(2)]
                  sa = [A32.alloc(528) for _ in range(2)]
                  sb_ = [A32.alloc(528) for _ in range(2)]
                  dmt = [A16.alloc(512) for _ in range(2)]
                  pst2 = [A16.alloc(4 * 512).rearrange("p (g t) -> p g t", g=4) for _ in range(2)]
                  it = 0
                  for ti in range(NT5):
                      c0 = ti * 512
                      st = pst2[ti % 2]
                      sk = ("pst2", ti % 2)
                      for g, w in enumerate((2, 4, 8, 16)):
                          b2 = it % 2
                          it += 1
                          u = ub[b2]
                          uk = ("ub", b2)
                          lo = max(c0 - 8, 0)
                          hi = min(c0 + 520, S)
                          rd = [("zaT", q, tj) for tj in range(max(ti - 1, 0), min(ti + 2, NT5))]
                          if lo > c0 - 8:
                              MEMSET("gpsimd", u[:, 0:8], 0.0, [uk])
                          if hi < c0 + 520:
                              MEMSET("gpsimd", u[:, 520:528], 0.0, [uk])
                          DMA("sync", u[:, lo - (c0 - 8):hi - (c0 - 8)], zaT[q][g * 128:(g + 1) * 128, lo:hi], rd, [uk], "ub%d" % b2)
                          s_a, s_b = sa[b2], sb_[b2]
                          ka, kb = ("sa", b2), ("sb", b2)
                          TT("vector", s_a[:, 1:528], u[:, 0:527], u[:, 1:528], ALU.add, [uk], [ka])
                          cur, curk, oth, othk = s_a, ka, s_b, kb
                          lo_v = 1
                          hi_v = 528
                          step = 1
                          ww = 2
                          while ww < w:
                              nlo = lo_v + step
                              nhi = hi_v - step
                              TT("vector", oth[:, nlo:nhi], cur[:, nlo - step:nhi - step], cur[:, nlo + step:nhi + step],
                                 ALU.add, [curk], [othk])
                              cur, curk, oth, othk = oth, othk, cur, curk
                              lo_v, hi_v = nlo, nhi
                              step *= 2
                              ww *= 2
                          dm = dmt[b2]
                          dk_ = ("dm", b2)
                          STT(dm, cur[:, 8:520], 1.0 / w, u[:, 8:520], ALU.mult, ALU.subtract, [curk, uk], [dk_])
                          if ti == 0:
                              TT("vector", oth[:, 8:16], cur[:, 8:16], pcst[:, g, 0:8], ALU.mult, [curk, "pcst"], [othk])
                              TT("vector", dm[:, 0:8], oth[:, 8:16], u[:, 8:16], ALU.subtract, [othk, uk], [dk_])
                          if ti == NT5 - 1:
                              TT("vector", oth[:, 512:520], cur[:, 512:520], pcst[:, g, 8:16], ALU.mult, [curk, "pcst"], [othk])
                              TT("vector", dm[:, 504:512], oth[:, 512:520], u[:, 512:520], ALU.subtract, [othk, uk], [dk_])
                          pi = g
                          MM(pm[pi][:, :], pwb[:, g, :], dm, True, True, ["pwb", dk_], [PM[pi]])
                          ACT(st[:, g, :], pm[pi][:, :], AF.Copy, [PM[pi], "psc"], [sk], scale=psc[:, g:g + 1])
                      DMA("sync", mixT[q][0:512, c0:c0 + 512].rearrange("(g p) t -> p g t", p=128), st, [sk],
                          [("mixT", q, ti, 0)], "pst2%d" % (ti % 2))

                  P.barrier()
                  A16.reset(); A32.reset()
                  pww = A16.alloc(8 * 1024).rearrange("p (k n) -> p k n", k=8)
                  DMA("gpsimd", pww, conv_pw_w[l].rearrange("(k p) n -> p k n", p=128), [], ["pww"], "pww")
                  dwr = A32.alloc(1024)
                  DMA("sync", dwr[0:CONV_K, :], conv_dw_w[l], [], ["dwr"], "dwr")
                  dwT = A32.alloc(8 * 32).rearrange("p (j k) -> p j k", j=8)
                  for j in range(8):
                      TR(pr[0][:, j * 32:j * 32 + CONV_K], dwr[0:CONV_K, j * 128:(j + 1) * 128],
                         ident_f[0:CONV_K, 0:CONV_K], ["dwr", "ident_f"], [PR[0]])
                  CP("vector", dwT[:, :, 0:CONV_K], pr[0][:, 0:256].rearrange("p (j k) -> p j k", j=8)[:, :, 0:CONV_K],
                     [PR[0]], ["dwT"])
                  vr = A32.alloc(4 * 1024).rearrange("p (v n) -> p v n", v=4)
                  vecs = A32.alloc(32).rearrange("p (v j) -> p v j", v=4)
                  for vi, src in enumerate((conv_dw_b, conv_ln_g, conv_ln_b, conv_pw_b)):
                      DMA("sync", vr[0:8, vi, 0:128], src[l].rearrange("(j p) -> j p", p=128), [], [("vr", vi)], "vr%d" % vi)
                      TR(pr[1][:, vi * 8:vi * 8 + 8], vr[0:8, vi, 0:128], ident_f[0:8, 0:8], [("vr", vi), "ident_f"], [PR[1]])
                  CP("vector", vecs, pr[1][:, 0:32].rearrange("p (v j) -> p v j", v=4), [PR[1]], ["vecs"])
                  vb = [A16.alloc(544) for _ in range(2)]
                  acc = [A32.alloc(512) for _ in range(8)]
                  cbf = [A16.alloc(512) for _ in range(2)]
                  sqb = [A16.alloc(512) for _ in range(2)]
                  sT = [A16.alloc(512) for _ in range(8)]
                  mean = A32.alloc(512)
                  var = A32.alloc(512)
                  rstd = A32.alloc(512)
                  xn = [A32.alloc(512) for _ in range(2)]
                  st3 = [A16.alloc(8 * 512).rearrange("p (j t) -> p j t", j=8) for _ in range(2)]
                  it = 0
                  for ti in range(NT5):
                      c0 = ti * 512
                      for j in range(8):
                          b2 = it % 2
                          it += 1
                          v = vb[b2]
                          vk = ("vb", b2)
                          lo = max(c0 - 15, 0)
                          hi = min(c0 + 527, S)
                          rd = [("vT", q, tj) for tj in range(max(ti - 1, 0), min(ti + 2, NT5))]
                          if lo > c0 - 15:
                              MEMSET("gpsimd", v[:, 0:15], 0.0, [vk])
                          if hi < c0 + 527:
                              MEMSET("gpsimd", v[:, 527:542], 0.0, [vk])
                          DMA("sync", v[:, lo - (c0 - 15):hi - (c0 - 15)], vT[q][j * 128:(j + 1) * 128, lo:hi], rd, [vk], "vb%d" % b2)
                          a = acc[j]
                          ak = ("acc", j)
                          TS("vector", a, v[:, 0:512], dwT[:, j, 0:1], vecs[:, 0, j:j + 1], ALU.mult, ALU.add,
                             [vk, "dwT", "vecs"], [ak])
                          for k in range(1, CONV_K):
                              STT(a, v[:, k:k + 512], dwT[:, j, k:k + 1], a, ALU.mult, ALU.add, [vk, "dwT", ak], [ak])
                          cb_, ck = cbf[b2], ("cbf", b2)
                          sq_, sqk = sqb[b2], ("sqb", b2)
                          CP("gpsimd", cb_, a, [ak], [ck])
                          ACT(sq_, a, AF.Square, [ak], [sqk])
                          MM(pr[0][:, :], ones_b[:], cb_, j == 0, j == 7, ["ones_b", ck], [PR[0]])
                          MM(pr[1][:, :], ones_b[:], sq_, j == 0, j == 7, ["ones_b", sqk], [PR[1]])
                      TS("vector", mean, pr[0][:, :], 1.0 / 1024, None, ALU.mult, None, [PR[0]], ["mean"])
                      TS("vector", var, pr[1][:, :], 1.0 / 1024, None, ALU.mult, None, [PR[1]], ["var"])
                      TT("vector", rstd, mean, mean, ALU.mult, ["mean"], ["rstd"])
                      TT("vector", var, var, rstd, ALU.subtract, ["var", "rstd"], ["var"])
                      ACT(var, var, AF.Sqrt, ["var", "eps_t"], ["var"], bias=eps_t[:, 0:1], scale=1.0)
                      P.op("vector", lambda e, rstd=rstd, var=var: e.reciprocal(out=rstd, in_=var), ["var"], ["rstd"])
                      for j in range(8):
                          x_, xk_ = xn[j % 2], ("xn", j % 2)
                          TT("gpsimd", x_, acc[j], mean, ALU.subtract, [("acc", j), "mean"], [xk_])
                          TT("vector", x_, x_, rstd, ALU.mult, [xk_, "rstd"], [xk_])
                          ACT(sT[j], x_, AF.Silu, [xk_, "vecs"], [("sT", j)], scale=vecs[:, 1, j:j + 1], bias=vecs[:, 2, j:j + 1])
                      st = st3[ti % 2]
                      sk = ("st3", ti % 2)
                      for e_ in range(8):
                          pi = e_ % 4
                          for j in range(8):
                              MM(pm[pi][:, :], pww[:, j, e_ * 128:(e_ + 1) * 128], sT[j], j == 0, j == 7,
                                 ["pww", ("sT", j)], [PM[pi]])
                          ACT(st[:, e_, :], pm[pi][:, :], AF.Identity, [PM[pi], "vecs"], [sk], bias=vecs[:, 3, e_:e_ + 1])
                      DMA("sync", mixT[q][512:1536, c0:c0 + 512].rearrange("(j p) t -> p j t", p=128), st, [sk],
                          [("mixT", q, ti, 1)], "st3%d" % (ti % 2))

                  stop_here('P3')
                  P.barrier()
                  A16.reset(); A32.reset()
                  cs = A16.alloc(2 * 192).rearrange("p (h n) -> p h n", h=2)
                  ccn = A16.alloc(2 * 128).rearrange("p (h n) -> p h n", h=2)
                  fcs = A16.alloc(2 * S).rearrange("p (h n) -> p h n", h=2)
                  DMA("sync", cs, k_cs128, [], ["cs"], "cs")
                  DMA("sync", ccn, k_ccn, [], ["ccn"], "ccn")
                  DMA("sync", fcs[0:S2], k_fcs[q], [], ["fcs"], "fcs")
                  fwf = A32.alloc(4 * 512).rearrange("p (h e) -> p h e", h=4)
                  fwb = A16.alloc(4 * 512).rearrange("p (h e) -> p h e", h=4)
                  DMA("sync", fwf, fourier_w[l].rearrange("(h m) e -> m h e", h=4), [], ["fwf"], "fwf")
                  CP("vector", fwb, fwf, ["fwf"], ["fwb"])
                  Ut = A16.alloc(128 * S2).rearrange("p (c t) -> p c t", c=128)
                  Ah = A16.alloc(128 * 192).rearrange("p (c n) -> p c n", c=128)
                  Xh = A16.alloc(2 * S).rearrange("p (h k) -> p h k", h=2)
                  rst = [A16.alloc(512) for _ in range(2)]
                  ALLZC = [("zcT", q, tj) for tj in range(NT5)]
                  nrm = 1.0 / math.sqrt(S * 128.0)
                  ri = 0
                  for h in range(4):
                      for cq in range(8):
                          DMA("sync", Ut[:, cq * 16:(cq + 1) * 16, :],
                              zcT[q][h * 128 + cq * 16:h * 128 + (cq + 1) * 16, :].rearrange("c (a b) -> a c b", b=S2),
                              ALLZC, ["Ut"], "Ut")
                      for half in range(2):
                          pmv = [pm[i] for i in range(4)]
                          for cg in range(16):
                              for ci in range(8):
                                  c_ = cg * 8 + ci
                                  bank = ci // 2
                                  col = (ci % 2) * 192
                                  MM(pmv[bank][0:S2, col:col + 192], Ut[:, c_, :], cs[:, half, :], True, True,
                                     ["Ut", "cs"], [PM[bank]])
                              for bank in range(4):
                                  eng = "vector" if bank % 2 == 0 else "scalar"
                                  CP(eng, Ah[0:S2, cg * 8 + bank * 2:cg * 8 + bank * 2 + 2, :],
                                     pmv[bank][0:S2, 0:384].rearrange("p (c n) -> p c n", c=2), [PM[bank]], [("Ah", cg)])
                          AHK = [("Ah", cg) for cg in range(16)]
                          G = 512 // S2
                          G = min(G, 64)
                          for kg in range(64 // G):
                              pxr, pxi = pr[0], pr[1]
                              for gi in range(G):
                                  k1l = kg * G + gi
                                  k1 = half * 64 + k1l
                                  fc_ = fcs[0:S2, 0, :].rearrange("p (b a) -> p a b", a=128)[:, k1, :]
                                  fs_ = fcs[0:S2, 1, :].rearrange("p (b a) -> p a b", a=128)[:, k1, :]
                                  ar = Ah[0:S2, :, k1l]
                                  ai = Ah[0:S2, :, 64 + k1l]
                                  an = Ah[0:S2, :, 128 + k1l]
                                  o = gi * S2
                                  MM(pxr[:, o:o + S2], ar, fc_, True, False, AHK + ["fcs"], [PR[0]])
                                  MM(pxr[:, o:o + S2], an, fs_, False, True, AHK + ["fcs"], [PR[0]])
                                  MM(pxi[:, o:o + S2], ar, fs_, True, False, AHK + ["fcs"], [PR[1]])
                                  MM(pxi[:, o:o + S2], ai, fc_, False, True, AHK + ["fcs"], [PR[1]])
                              k1b = half * 64 + kg * G
                              for ri_, px in enumerate((pxr, pxi)):
                                  dst = Xh[:, ri_, :].rearrange("p (b a) -> p a b", a=128)[:, k1b:k1b + G, :]
                                  src = px[:, 0:G * S2].rearrange("p (g b) -> p g b", g=G)
                                  CP("vector" if ri_ == 0 else "scalar", dst, src, [PR[ri_]], [("Xh", half, kg)])
                      XHK = [("Xh", hf, kg) for hf in range(2) for kg in range(64 // G)]
                      for kt in range(NT5):
                          pi = kt % 4
                          MM(pm[pi][:, :], ccn[:, 0, :], Xh[:, 0, kt * 512:(kt + 1) * 512], True, False, ["ccn"] + XHK, [PM[pi]])
                          MM(pm[pi][:, :], ccn[:, 1, :], Xh[:, 1, kt * 512:(kt + 1) * 512], False, True, ["ccn"] + XHK, [PM[pi]])
                          r_ = rst[ri % 2]
                          rk = ("rst", ri % 2)
                          ACT(r_, pm[pi][:, :], AF.Copy, [PM[pi]], [rk], scale=nrm)
                          DMA("sync", rzT[q][h * 128:(h + 1) * 128, kt * 512:(kt + 1) * 512], r_, [rk], [("rzT", q, h, kt)],
                              "rst%d" % (ri % 2))
                          ri += 1
                  rzb = [A16.alloc(4 * 512).rearrange("p (h t) -> p h t", h=4) for _ in range(2)]
                  st4 = [A16.alloc(4 * 512).rearrange("p (e t) -> p e t", e=4) for _ in range(2)]
                  for ti in range(NT5):
                      c0 = ti * 512
                      rb, rbk = rzb[ti % 2], ("rzb", ti % 2)
                      DMA("sync", rb, rzT[q][:, c0:c0 + 512].rearrange("(h m) t -> m h t", h=4),
                          [("rzT", q, h, ti) for h in range(4)], [rbk], "rzb%d" % (ti % 2))
                      st, sk = st4[ti % 2], ("st4", ti % 2)
                      for e_ in range(4):
                          pi = e_
                          for h in range(4):
                              MM(pm[pi][:, :], fwb[:, h, e_ * 128:(e_ + 1) * 128], rb[:, h, :], h == 0, h == 3,
                                 ["fwb", rbk], [PM[pi]])
                          CP("scalar" if e_ % 2 else "vector", st[:, e_, :], pm[pi][:, :], [PM[pi]], [sk])
                      DMA("sync", mixT[q][1536:2048, c0:c0 + 512].rearrange("(e p) t -> p e t", p=128), st, [sk],
                          [("mixT", q, ti, 2)], "st4%d" % (ti % 2))

                  stop_here('P4')
                  P.barrier()
                  A16.reset(); A32.reset()
                  wo = A16.alloc(16 * 2048).rearrange("p (k n) -> p k n", k=16)
                  DMA("gpsimd", wo, w_out[l].rearrange("(k p) n -> p k n", p=128), [], ["wo"], "wo")
                  g1 = A32.alloc(D)
                  a2 = A32.alloc(D)
                  sh2 = A32.alloc(D)
                  tmpf = A32.alloc(D)
                  TFK = [("tmpf", i) for i in range(4)]
                  DMA("sync", g1, mod_rep(q, 2), MODK, ["g1"], "g1")
                  DMA("sync", a2, mod_rep(q, 4), MODK, ["a2"], "a2")
                  DMA("sync", sh2, mod_rep(q, 3), MODK, ["sh2"], "sh2")
                  DMA("sync", tmpf, norm2_g[l:l + 1, :].broadcast_to([128, D]), [], TFK, "g_rep")
                  STT(a2, a2, 1.0, tmpf, ALU.add, ALU.mult, ["a2"] + TFK, ["a2"])
                  wr = A32.alloc(16 * 36).rearrange("p (k n) -> p k n", k=16)
                  DMA("sync", wr[:, :, 0:4], rc_w[l].rearrange("(p k) g -> p k g", k=16), [], [("wr", 0)], "wr")
                  for g in range(4):
                      DMA("sync", wr[:, :, 4 + 8 * g:12 + 8 * g], rf_w[l, g].rearrange("(p k) e -> p k e", k=16), [],
                          [("wr", 1 + g)], "wr")
                  WRK = [("wr", i) for i in range(5)]
                  rb_ = A32.alloc(36)
                  DMA("sync", rb_[:, 0:4], rc_b[l:l + 1, :].broadcast_to([128, 4]), [], [("rb", 0)], "rb")
                  DMA("sync", rb_[:, 4:36], rf_b[l:l + 1].rearrange("o g e -> o (g e)").broadcast_to([128, 32]), [], [("rb", 1)], "rb")
                  RBK = [("rb", 0), ("rb", 1)]
                  mx = [A16.alloc(16 * 512).rearrange("p (k t) -> p k t", k=16) for _ in range(2)]
                  xt = [A32.alloc(D) for _ in range(1)]
                  x1t = [A32.alloc(D) for _ in range(2)]
                  h2f2 = [A32.alloc(D) for _ in range(2)]
                  h2b = [A16.alloc(D) for _ in range(2)]
                  h2T = A32.alloc(16 * 128).rearrange("p (k t) -> p k t", k=16)
                  sm = A32.alloc(256)
                  if q == 0:
                      MEMSET("vector", cnt_run[:], 0.0, ["cnt_run"])

                  def p5_main(it):
                      ti, sub = it // 4, it % 4
                      c0 = ti * 512
                      m_, mk_ = mx[ti % 2], ("mx", ti % 2)
                      if sub == 0:
                          DMA("sync", m_, mixT[q][:, c0:c0 + 512].rearrange("(k p) t -> p k t", p=128),
                              [("mixT", q, ti, i) for i in range(3)], [mk_], "mx%d" % (ti % 2))
                      b2 = it % 2
                      r0 = T0 + it * 128
                      xk = ("xt", 0)
                      DMA("sync", xt[0], cur_x[r0:r0 + 128, :], [("xcur", r0 // 128)], [xk], "xt0")
                      x1, x1k = x1t[b2], ("x1t", b2)
                      for cbk in range(4):
                          pi = cbk
                          for kc in range(16):
                              MM(pm[pi][:, :], m_[:, kc, sub * 128:(sub + 1) * 128], wo[:, kc, cbk * 512:(cbk + 1) * 512],
                                 kc == 0, kc == 15, [mk_, "wo"], [PM[pi]])
                          sl = slice(cbk * 512, (cbk + 1) * 512)
                          TT("vector", tmpf[:, sl], pm[pi][:, :], g1[:, sl], ALU.mult, [PM[pi], "g1"], [("tmpf", cbk)])
                          TT("gpsimd", x1[:, sl], tmpf[:, sl], xt[0][:, sl], ALU.add, [("tmpf", cbk), xk], [x1k])
                      DMA("sync", x1_d[r0:r0 + 128, :], x1, [x1k], [("x1", r0 // 128)], "x1t%d" % b2)
                      ssk = U("ss")
                      ss = sm[:, 0:4]
                      hb_, hbk = h2b[b2], ("h2b", b2)
                      h2f, h2fk = h2f2[b2], ("h2f", b2)
                      ACT(hb_, x1, AF.Square, [x1k], [hbk, ssk], accum_out=ss[:, 0:1])
                      ACT(ss[:, 1:2], ss[:, 0:1], AF.Sqrt, [ssk, "eps_t"], [ssk], scale=1.0 / D, bias=eps_t[:, 0:1])
                      P.op("vector", lambda e, ss=ss: e.reciprocal(out=ss[:, 2:3], in_=ss[:, 1:2]), [ssk], [ssk])
                      STT(tmpf, x1, ss[:, 2:3], a2, ALU.mult, ALU.mult, [x1k, ssk, "a2"], [("tmpf", i) for i in range(4)])
                      TT("gpsimd", h2f, tmpf, sh2, ALU.add, [("tmpf", i) for i in range(4)] + ["sh2"], [h2fk])
                      CP("scalar", hb_, h2f, [h2fk], [hbk])
                      DMA("sync", h2_d[r0:r0 + 128, :], hb_, [hbk], [("h2", r0 // 128)], "h2b%d" % b2)

                  def p5_router(it):
                      git = (T0 // 128) + it
                      b2 = it % 2
                      h2f, h2fk = h2f2[b2], ("h2f", b2)
                      for kc in range(16):
                          pj = pr[(kc // 4) % 2]
                          TR(pj[:, (kc % 4) * 128:(kc % 4 + 1) * 128], h2f.rearrange("p (c k) -> p k c", k=16)[:, kc, :],
                             ident_f[:], [h2fk, "ident_f"], [PR[(kc // 4) % 2]])
                          if kc % 4 == 3:
                              g4 = kc // 4
                              CP("scalar" if g4 % 2 else "vector", h2T[:, g4 * 4:g4 * 4 + 4, :],
                                 pj[:, :].rearrange("p (k t) -> p k t", k=4), [PR[(kc // 4) % 2]], [("h2T", g4)])
                      for kc in range(16):
                          MM(pr[0][:, 0:36], h2T[:, kc, :], wr[:, kc, :], kc == 0, kc == 15,
                             [("h2T", kc // 4)] + WRK, [PR[0]])
                      route_tile(P, nc, sm, pr, PR, rb_, RBK, r_E, r_f, cnt_run, utinc, ones_b, git,
                                 TT, TS, STT, ACT, CP, RED, MM, U)

                  NSUB = NT5 * 4
                  for it in range(NSUB):
                      p5_main(it)
                      if it > 0:
                          p5_router(it - 1)
                  p5_router(NSUB - 1)

              stop_here('P5')
              P.barrier()
              A16.reset(); A32.reset()
              fin = A32.alloc(8 * 32).rearrange("p (a e) -> p a e", a=8)
              RALL = ["cnt_run"]
              TS("vector", fin[:, 0, :], cnt_run[:], 127.0, None, ALU.add, None, ["cnt_run"], ["fin"])
              fin_i = A32.alloc(32).bitcast(I32)
              CP("vector", fin_i, fin[:, 0, :], ["fin"], ["fin_i"])
              TS("vector", fin_i, fin_i, 7, 7, ALU.arith_shift_right, ALU.logical_shift_left, ["fin_i"], ["fin_i"])
              CP("vector", fin[:, 2, :], fin_i, ["fin_i"], ["fin"])
              MEMSET("vector", fin[:, 3, :], 1.0, ["fin"])
              P.op("vector", lambda e: e.tensor_tensor_scan(out=fin[:, 4, :], data0=fin[:, 3, :], data1=fin[:, 2, :],
                                                            initial=0.0, op0=ALU.mult, op1=ALU.add), ["fin"], ["fin"])
              TT("vector", fin[:, 5, :], fin[:, 4, :], fin[:, 2, :], ALU.subtract, ["fin"], ["fin"])
              big = A32.alloc(NT * 32).rearrange("p (t e) -> p t e", e=32)
              slf = A32.alloc(NT * 2).rearrange("p (t k) -> p t k", k=2)
              for k in range(2):
                  TT("vector", big, r_E[:, :, k * 32:(k + 1) * 32], fin[:, 5:6, :].to_broadcast([128, NT, 32]), ALU.mult,
                     ["fin", "r_E"], ["big"])
                  RED(slf[:, :, k], big, ALU.add, ["big"], ["slf"])
                  TT("vector", slf[:, :, k], slf[:, :, k], r_f[:, :, k], ALU.add, ["slf", "r_f"], ["slf"])
              CP("vector", r_slot[:], slf, ["slf"], ["r_slot"])
              bst = A32.alloc(NB)
              DMA("sync", bst, k_bstart, [], ["bst"], "bst")
              ebf = A32.alloc(NB)
              CH = 32
              bigb = A32.alloc(CH * 32).rearrange("p (b e) -> p b e", e=32)
              for b0 in range(0, NB, CH):
                  nb_ = min(CH, NB - b0)
                  TT("vector", bigb[:, 0:nb_, :], fin[:, 4:5, :].to_broadcast([128, nb_, 32]),
                     bst[:, b0:b0 + nb_].unsqueeze(2).to_broadcast([128, nb_, 32]), ALU.is_le, ["fin", "bst"], ["bigb"])
                  RED(ebf[:, b0:b0 + nb_], bigb[:, 0:nb_, :], ALU.add, ["bigb"], ["ebf"])
              TS("vector", ebf, ebf, 31.0, None, ALU.min, None, ["ebf"], ["ebf"])
              sam = A32.alloc(NB)
              MEMSET("vector", sam[:, 0:1], 0.0, ["sam"])
              TT("vector", sam[:, 1:NB], ebf[:, 1:NB], ebf[:, 0:NB - 1], ALU.is_equal, ["ebf"], ["sam"])
              wif = A32.alloc(NB)
              TS("vector", wif, ebf, 128.0, iotap[:, 0:1], ALU.mult, ALU.add, ["ebf", "iotap"], ["wif"])
              STT(wif, sam, 1.0e6, wif, ALU.mult, ALU.add, ["sam", "wif"], ["wif"])
              if l > 0:
                  TS("vector", wif, wif, float(l * NE * 128), None, ALU.add, None, ["wif"], ["wif"])
              CP("vector", widx[:], wif, ["wif"], ["widx"])
              hl = [A16.alloc(D) for _ in range(3)]
              for it in range(NT):
                  b3 = it % 3
                  hk_ = ("hl", b3)
                  DMA("sync", hl[b3], h2_d[it * 128:(it + 1) * 128, :], [("h2", it)], [hk_], "hl%d" % b3)
                  for k in range(2):
                      off = r_slot[:, it, k:k + 1]
                      P.dma("gpsimd", (lambda e, off=off, src=hl[b3]: e.indirect_dma_start(
                          out=xs_d, out_offset=bass.IndirectOffsetOnAxis(ap=off, axis=0), in_=src, in_offset=None,
                          bounds_check=BR(e, L - 1), oob_is_err=False)), [hk_, "r_slot"], [("xs", it, k)], "xs_sc")
              XSK = [("xs", it, k) for it in range(NT) for k in range(2)]

              P.barrier()
              A16.reset(); A32.reset()
              W1 = A16.alloc(16 * 512)
              W3 = A16.alloc(16 * 512)
              W2 = A16.alloc(4 * 2048)
              xb = [A16.alloc(D) for _ in range(2)]
              xbT = [A16.alloc(16 * 128).rearrange("p (k t) -> p k t", k=16) for _ in range(2)]
              sgf = [A32.alloc(512) for _ in range(2)]
              hid = [A16.alloc(512) for _ in range(2)]
              hidT = [A16.alloc(4 * 128).rearrange("p (k t) -> p k t", k=4) for _ in range(2)]
              yb = [A32.alloc(D) for _ in range(2)]
              w1v = e_w1.rearrange("l e (p k) n -> (l e p) (k n)", k=16)
              w3v = e_w3.rearrange("l e (p k) n -> (l e p) (k n)", k=16)
              w2v = e_w2.rearrange("l e (p k) n -> (l e p) (k n)", k=4)
              bnd_l = (l + 1) * NE * 128 - 1

              def wgather(b, Wt, wv, wk):
                  off = widx[:, b:b + 1]
                  P.dma("gpsimd", (lambda e, off=off, Wt=Wt, wv=wv, bnd=bnd_l: e.indirect_dma_start(
                      out=Wt, out_offset=None, in_=wv, in_offset=bass.IndirectOffsetOnAxis(ap=off, axis=0),
                      bounds_check=BR(e, bnd), oob_is_err=False)), ["widx"], [wk], wk)

              def stageA(b):
                  b2 = b % 2
                  wgather(b, W1, w1v, "W1")
                  wgather(b, W3, w3v, "W3")
                  xk = ("xb", b2)
                  DMA("sync", xb[b2], xs_d[b * 128:(b + 1) * 128, :], XSK, [xk], "xb%d" % b2)
                  for kc in range(16):
                      TR(pt[:, kc * 128:(kc + 1) * 128], xb[b2].rearrange("p (c k) -> p k c", k=16)[:, kc, :], ident_b[:],
                         [xk, "ident_b"], [PT])
                  xT, xTk = xbT[b2], ("xbT", b2)
                  CP("scalar", xT[:, 0:8, :], pt[:, 0:1024].rearrange("p (k t) -> p k t", k=8), [PT], [xTk])
                  CP("vector", xT[:, 8:16, :], pt[:, 1024:2048].rearrange("p (k t) -> p k t", k=8), [PT], [xTk])
                  p1, p3 = 2 * b2, 2 * b2 + 1
                  for kc in range(16):
                      MM(pm[p1][:, :], xT[:, kc, :], W1[:, kc * 512:(kc + 1) * 512], kc == 0, kc == 15, [xTk, "W1"], [PM[p1]])
                  for kc in range(16):
                      MM(pm[p3][:, :], xT[:, kc, :], W3[:, kc * 512:(kc + 1) * 512], kc == 0, kc == 15, [xTk, "W3"], [PM[p3]])
                  ACT(sgf[b2], pm[p1][:, :], AF.Silu, [PM[p1]], [("sgf", b2)])
                  TT("vector", hid[b2], pm[p3][:, :], sgf[b2], ALU.mult, [PM[p3], ("sgf", b2)], [("hid", b2)])

              def stageB(b):
                  b2 = b % 2
                  wgather(b, W2, w2v, "W2")
                  for fc in range(4):
                      TR(pt[:, fc * 128:(fc + 1) * 128], hid[b2].rearrange("p (c k) -> p k c", k=4)[:, fc, :], ident_b[:],
                         [("hid", b2), "ident_b"], [PT])
                  CP("vector", hidT[b2], pt[:, 0:512].rearrange("p (k t) -> p k t", k=4), [PT], [("hidT", b2)])
                  y_, yk = yb[b2], ("yb", b2)
                  for cbk in range(4):
                      pi = cbk % 2
                      for fc in range(4):
                          MM(pr[pi][:, :], hidT[b2][:, fc, :], W2[:, fc * 2048 + cbk * 512:fc * 2048 + (cbk + 1) * 512],
                             fc == 0, fc == 3, [("hidT", b2), "W2"], [PR[pi]])
                      if cbk % 2:
                          CP("scalar", y_[:, cbk * 512:(cbk + 1) * 512], pr[pi][:, :], [PR[pi]], [yk])
                      else:
                          CP("vector", y_[:, cbk * 512:(cbk + 1) * 512], pr[pi][:, :], [PR[pi]], [yk])
                  DMA("sync", ys_d[b * 128:(b + 1) * 128, :], y_, [yk], [("ys", b)], "yb%d" % b2)

              stageA(0)
              for b in range(NB):
                  if b + 1 < NB:
                      stageA(b + 1)
                  stageB(b)
              YSK = [("ys", b) for b in range(NB)]

              stop_here('P7')
              P.barrier()
              A16.reset(); A32.reset()
              g2 = [A32.alloc(D) for _ in range(2)]
              for q in range(2):
                  DMA("sync", g2[q], mod_rep(q, 5), MODK, [("g2", q)], "g2%d" % q)
              if last:
                  fg = A32.alloc(D)
                  DMA("sync", fg, final_g.rearrange("(o n) -> o n", o=1).broadcast_to([128, D]), [], ["fg"], "fg")
              ya = [A32.alloc(D) for _ in range(2)]
              ybb = [A32.alloc(D) for _ in range(2)]
              x1t = [A32.alloc(D) for _ in range(2)]
              junk = A16.alloc(D)
              ss = A32.alloc(8)
              dst_x = y_out if last else x2_d
              for it in range(NT):
                  b2 = it % 2
                  q = 0 if it * 128 < OFFS[1] else 1
                  for k, yt in enumerate((ya, ybb)):
                      off = r_slot[:, it, k:k + 1]
                      P.dma("gpsimd", (lambda e, off=off, dst=yt[b2]: e.indirect_dma_start(
                          out=dst, out_offset=None, in_=ys_d, in_offset=bass.IndirectOffsetOnAxis(ap=off, axis=0),
                          bounds_check=BR(e, L - 1), oob_is_err=False)), YSK + ["r_slot"], [("yg", k, b2)], "yg%d%d" % (k, b2))
                  xk = ("x1t", b2)
                  DMA("sync", x1t[b2], x1_d[it * 128:(it + 1) * 128, :], [("x1", it)], [xk], "x1l%d" % b2)
                  A_, B_ = ya[b2], ybb[b2]
                  TS("vector", A_, A_, r_f[:, it, 2:3], None, ALU.mult, None, [("yg", 0, b2), "r_f"], [("yg", 0, b2)])
                  STT(A_, B_, r_f[:, it, 3:4], A_, ALU.mult, ALU.add, [("yg", 1, b2), ("yg", 0, b2), "r_f"], [("yg", 0, b2)])
                  TT("gpsimd", A_, A_, g2[q], ALU.mult, [("yg", 0, b2), ("g2", q)], [("yg", 0, b2)])
                  TT("vector", B_, A_, x1t[b2], ALU.add, [("yg", 0, b2), xk], [("yg", 1, b2)])
                  if not last:
                      DMA("sync", dst_x[it * 128:(it + 1) * 128, :], B_, [("yg", 1, b2)], [("xcur", it)], "xo%d" % b2)
                  else:
                      ssk = U("ss")
                      ACT(junk, B_, AF.Square, [("yg", 1, b2)], ["junk", ssk], accum_out=ss[:, 0:1])
                      ACT(ss[:, 1:2], ss[:, 0:1], AF.Sqrt, [ssk, "eps_t"], [ssk], scale=1.0 / D, bias=eps_t[:, 0:1])
                      P.op("vector", lambda e, ss=ss: e.reciprocal(out=ss[:, 2:3], in_=ss[:, 1:2]), [ssk], [ssk])
                      STT(A_, B_, ss[:, 2:3], fg, ALU.mult, ALU.mult, [("yg", 1, b2), ssk, "fg"], [("yg", 0, b2)])
                      DMA("sync", dst_x[it * 128:(it + 1) * 128, :], A_, [("yg", 0, b2)], [("yout", it)], "xo%d" % b2)
              cur_x = x2_d

          P.emit()
        except _Stop:
            pass
    return nc


def route_tile(P, nc, sm, pr, PR, rb_, RBK, r_E, r_f, cnt_run, utinc, ones_b, git,
               TT, TS, STT, ACT, CP, RED, MM, U):
    rk = U("rt")
    Lg = sm[:, 8:44]
    TT("vector", Lg, pr[0][:, 0:36], rb_, ALU.add, [PR[0]] + RBK, [rk])
    m = sm[:, 44:45]
    RED(m, Lg[:, 0:4], ALU.max, [rk], [rk])
    oh = sm[:, 48:52]
    TS("vector", oh, Lg[:, 0:4], m, None, ALU.is_equal, None, [rk], [rk])
    negm = sm[:, 45:46]
    TS("vector", negm, m, -1.0, None, ALU.mult, None, [rk], [rk])
    ex = sm[:, 52:56]
    se = sm[:, 46:47]
    ACT(ex, Lg[:, 0:4], AF.Exp, [rk], [rk], bias=negm, scale=1.0, accum_out=se)
    pg = sm[:, 47:48]
    P.op("vector", lambda e: e.reciprocal(out=pg, in_=se), [rk], [rk])
    lf = sm[:, 56:64]
    TS("vector", lf, Lg[:, 4:12], oh[:, 0:1], None, ALU.mult, None, [rk], [rk])
    for g in range(1, 4):
        STT(lf, Lg[:, 4 + 8 * g:12 + 8 * g], oh[:, g:g + 1], lf, ALU.mult, ALU.add, [rk], [rk])
    top = sm[:, 64:72]
    P.op("vector", lambda e: e.max(out=top, in_=lf), [rk], [rk])
    s1 = sm[:, 72:80]
    s2 = sm[:, 80:88]
    TS("vector", s1, lf, top[:, 0:1], None, ALU.is_equal, None, [rk], [rk])
    TS("vector", s2, lf, top[:, 1:2], None, ALU.is_equal, None, [rk], [rk])
    dv = sm[:, 88:89]
    TT("vector", dv, top[:, 0:1], top[:, 1:2], ALU.subtract, [rk], [rk])
    sg = sm[:, 89:90]
    ACT(sg, dv, AF.Sigmoid, [rk], [rk])
    TT("vector", r_f[:, git, 2:3], pg, sg, ALU.mult, [rk], ["r_f"])
    TT("vector", r_f[:, git, 3:4], pg, r_f[:, git, 2:3], ALU.subtract, [rk, "r_f"], ["r_f"])
    Ef = sm[:, 96:160]
    for g in range(4):
        TS("vector", Ef[:, 8 * g:8 * g + 8], s1, oh[:, g:g + 1], None, ALU.mult, None, [rk], [rk])
        TS("vector", Ef[:, 32 + 8 * g:40 + 8 * g], s2, oh[:, g:g + 1], None, ALU.mult, None, [rk], [rk])
    CP("vector", r_E[:, git, :], Ef, [rk], ["r_E"])
    Mb = sm[:, 160:192]
    TT("vector", Mb, Ef[:, 0:32], Ef[:, 32:64], ALU.add, [rk], [rk])
    Mbb = sm[:, 192:224].bitcast(BF16)[:, 0:32]
    CP("vector", Mbb, Mb, [rk], [rk])
    MM(pr[1][:, 0:32], utinc[:], Mbb, True, True, ["utinc", rk], [PR[1]])
    MM(pr[1][:, 32:64], ones_b[:], Mbb, True, True, ["ones_b", rk], [PR[1]])
    rank = sm[:, 224:256]
    TT("vector", rank, pr[1][:, 0:32], Mb, ALU.subtract, [PR[1], rk], [rk])
    TT("vector", rank, rank, cnt_run[:], ALU.add, [rk, "cnt_run"], [rk])
    TT("vector", cnt_run[:], cnt_run[:], pr[1][:, 32:64], ALU.add, [PR[1], "cnt_run"], ["cnt_run"])
    tmp = sm[:, 160:192]
    for k in range(2):
        TT("vector", tmp, Ef[:, 32 * k:32 * k + 32], rank, ALU.mult, [rk], [rk])
        RED(r_f[:, git, k:k + 1], tmp, ALU.add, [rk], ["r_f"])


_WNAMES = ["w_ada", "b_ada", "norm1_g", "w_in", "pool_w", "pool_scale", "conv_dw_w", "conv_dw_b",
           "conv_ln_g", "conv_ln_b", "conv_pw_w", "conv_pw_b", "fourier_w", "w_out", "norm2_g",
           "router_coarse_w", "router_coarse_b", "router_fine_w", "router_fine_b",
           "expert_w1", "expert_w3", "expert_w2", "final_g"]


def kernel(**inputs):
    xs_ = np.asarray(inputs["x_sample"], np.float32)
    xp_ = np.asarray(inputs["x_prompt"], np.float32)
    cs_ = np.asarray(inputs["c_sample"], np.float32)
    cp_ = np.asarray(inputs["c_prompt"], np.float32)
    S0, S1 = xs_.shape[1], xp_.shape[1]
    depth = inputs["w_ada"].shape[0]
    nc = build((S0, S1), depth)
    N = S0 + S1
    NB = -(-(2 * N + NE * 127) // 128)
    consts = make_consts((S0, S1), NB)
    wts = {k: np.ascontiguousarray(np.asarray(inputs[k], np.float32)) for k in _WNAMES}
    in_maps = []
    for core in range(8):
        b = core % 4
        m = {"x": np.ascontiguousarray(np.concatenate([xs_[b], xp_[b]], axis=0)),
             "c": np.ascontiguousarray(np.stack([cs_[b], cp_[b]], axis=0))}
        m.update(wts)
        m.update(consts)
        in_maps.append(m)
    res = run_bass_kernel_spmd(nc, in_maps, core_ids=list(range(8)))
    y_s = np.stack([res.results[b]["y"][:S0] for b in range(4)], axis=0)
    y_p = np.stack([res.results[b]["y"][S0:] for b in range(4)], axis=0)
    return (y_p.astype(np.float32), y_s.astype(np.float32))
```

```python
import math
import os
from contextlib import ExitStack

import numpy as np
import ml_dtypes
import concourse.bass as bass
import concourse.mybir as mybir
from concourse.bass_utils import run_bass_kernel_spmd

F32 = mybir.dt.float32
BF16 = mybir.dt.bfloat16
I32 = mybir.dt.int32
AF = mybir.ActivationFunctionType
ALU = mybir.AluOpType
AX = mybir.AxisListType

D = 2048
NG, EPG, NE = 4, 8, 32
DE = 512
CONV_K = 31
EPS = 1e-6
SAME_ENGINE_SYNC = True
CONV_PE = os.environ.get('KCONV', 'pe') == 'pe'
NOSYNC = set(os.environ.get('KNOSYNC', 'tensor').split(','))


class _Op:
    __slots__ = ("q", "fn", "deps", "sig", "sigval", "sem", "is_dma", "idx", "bar")


class Prog:
    def __init__(self, nc):
        self.nc = nc
        self.ops = []
        self.last_w = {}
        self.readers = {}
        self.dma_cnt = {}
        self.last_eng = {}
        self.bar = None
        self.bar_done = set()

    def barrier(self):
        deps = [(o, None) for o in self.last_eng.values()]
        dmas = dict(self.dma_cnt)
        self.bar = (deps, dmas)
        self.bar_done = set()

    def _add(self, q, fn, reads, writes, is_dma, semkey):
        o = _Op()
        o.q = q
        o.fn = fn
        o.is_dma = is_dma
        o.sig = False
        o.sigval = 0
        o.idx = len(self.ops)
        deps = {}
        for r in reads:
            w = self.last_w.get(r)
            if w is not None:
                deps[w.idx] = w
        for w_ in writes:
            w = self.last_w.get(w_)
            if w is not None:
                deps[w.idx] = w
            for rd in self.readers.get(w_, ()):
                deps[rd.idx] = rd
        o.deps = []
        for d in deps.values():
            if d.is_dma:
                o.deps.append((d, self.dma_cnt[d.sem]))
            else:
                o.deps.append((d, None))
        o.bar = None
        if self.bar is not None and q not in self.bar_done:
            self.bar_done.add(q)
            bdeps, bdmas = self.bar
            o.deps.extend(bdeps)
            o.bar = bdmas
        if is_dma:
            o.sem = ("dma", semkey)
            self.dma_cnt[o.sem] = self.dma_cnt.get(o.sem, 0) + 1
            o.sigval = self.dma_cnt[o.sem]
        else:
            o.sem = ("eng", q)
            self.last_eng[q] = o
        for w_ in writes:
            self.last_w[w_] = o
            self.readers[w_] = []
        for r in reads:
            if r not in writes:
                self.readers.setdefault(r, []).append(o)
        self.ops.append(o)
        return o

    def op(self, eng, fn, reads=(), writes=()):
        return self._add(eng, fn, tuple(reads), tuple(writes), False, None)

    def dma(self, q, fn, reads=(), writes=(), semkey=None):
        assert semkey is not None
        return self._add(q, fn, tuple(reads), tuple(writes), True, semkey)

    def emit(self):
        nc = self.nc
        ops = self.ops
        for o in ops:
            for d, _ in o.deps:
                if d.is_dma:
                    continue
                if d.q != o.q or (SAME_ENGINE_SYNC and d.q not in NOSYNC):
                    d.sig = True
        cnt = {}
        for o in ops:
            if not o.is_dma and o.sig:
                cnt[o.q] = cnt.get(o.q, 0) + 1
                o.sigval = cnt[o.q]
        semkeys = []
        seen = set()
        for o in ops:
            if (o.is_dma or o.sig) and o.sem not in seen:
                seen.add(o.sem)
                semkeys.append(o.sem)
        self.n_sems = len(semkeys)
        with ExitStack() as es:
            sems = {}
            for i, k in enumerate(semkeys):
                sems[k] = es.enter_context(nc.semaphore("s%d" % i))
            block = es.enter_context(nc.Block())
            queues = {}
            for o in ops:
                queues.setdefault(o.q, []).append(o)
            totals = dict(self.dma_cnt)

            def run_queue(qname, eng):
                waited = {}
                for o in queues.get(qname, ()):
                    need = {}
                    for d, n in o.deps:
                        if d.is_dma:
                            v = 16 * n
                        else:
                            if d.q == o.q and (d.q in NOSYNC or not SAME_ENGINE_SYNC):
                                continue
                            v = d.sigval
                        if v > need.get(d.sem, 0):
                            need[d.sem] = v
                    if o.bar is not None:
                        for k, n in o.bar.items():
                            if 16 * n > need.get(k, 0):
                                need[k] = 16 * n
                    for k, v in need.items():
                        if waited.get(k, 0) < v:
                            eng.wait_ge(sems[k], v)
                            waited[k] = v
                    ins = o.fn(eng)
                    if o.is_dma:
                        ins.then_inc(sems[o.sem], 16)
                    elif o.sig:
                        ins.then_inc(sems[o.sem], 1)
                if qname == "sync":
                    for k, n in totals.items():
                        if waited.get(k, 0) < 16 * n:
                            eng.wait_ge(sems[k], 16 * n)

            @block.sync
            def _(e):
                run_queue("sync", e)

            @block.scalar
            def _(e):
                run_queue("scalar", e)

            @block.gpsimd
            def _(e):
                run_queue("gpsimd", e)

            @block.vector
            def _(e):
                run_queue("vector", e)

            @block.tensor
            def _(e):
                run_queue("tensor", e)


def _bf(a):
    return np.ascontiguousarray(a.astype(ml_dtypes.bfloat16))


def make_consts(SEQS, NB):
    c = {}
    t = np.arange(128)
    ang = 2 * np.pi * np.outer(t, t) / 128.0
    cs = np.zeros((128, 2, 192), np.float64)
    for h in range(2):
        sl = slice(h * 64, h * 64 + 64)
        cs[:, h, 0:64] = np.cos(ang[:, sl])
        cs[:, h, 64:128] = np.sin(ang[:, sl])
        cs[:, h, 128:192] = -np.sin(ang[:, sl])
    c["cs128"] = _bf(cs)
    ccn = np.zeros((128, 2, 128), np.float64)
    ccn[:, 0] = np.cos(ang)
    ccn[:, 1] = -np.sin(ang)
    c["ccn"] = _bf(ccn)
    for q, S in enumerate(SEQS):
        S2 = S // 128
        a = 2 * np.pi * (np.outer(np.arange(S2), np.arange(S)) % S) / S
        f = np.zeros((S2, 2, S), np.float64)
        f[:, 0] = np.cos(a)
        f[:, 1] = np.sin(a)
        c["fcs%d" % q] = _bf(f)
    pc = np.zeros((128, 4, 16), np.float32)
    for g, w in enumerate((2, 4, 8, 16)):
        for j in range(8):
            cnt = min(j + w // 2, 10 ** 9) - max(j - w // 2, 0)
            pc[:, g, j] = 1.0 / cnt
        for j in range(8):
            tt = -8 + j
            hi = min(tt + w // 2, 0)
            lo = tt - w // 2
            pc[:, g, 8 + j] = 1.0 / (hi - lo)
    c["poolc"] = pc
    ut = (np.arange(128)[:, None] <= np.arange(128)[None, :]).astype(np.float32)
    c["utinc"] = _bf(ut)
    c["iotap"] = np.arange(128, dtype=np.float32).reshape(128, 1)
    c["bstart"] = np.tile((128.0 * np.arange(NB, dtype=np.float32))[None, :], (128, 1))
    c["iota32"] = np.tile(np.arange(32, dtype=np.float32)[None, :], (128, 1))
    return c


class Arena:
    def __init__(self, t, size):
        self.t = t
        self.size = size
        self.off = 0

    def reset(self):
        self.off = 0

    def alloc(self, n, align=16):
        self.off = (self.off + align - 1) // align * align
        o = self.off
        self.off += n
        assert self.off <= self.size, ("arena overflow", self.off, self.size)
        return self.t[:, o:o + n]


def build(SEQS=(8192, 2048), DEPTH=2, dbg=False, NL=None):
    NL = DEPTH if NL is None else NL
    N = sum(SEQS)
    NT = N // 128
    NB = -(-(2 * N + NE * 127) // 128)
    L = NB * 128
    OFFS = [0]
    for S in SEQS:
        OFFS.append(OFFS[-1] + S)
    nc = bass.Bass("TRN2", target_bir_lowering=False)

    def din(name, shape, dt=F32):
        return nc.dram_tensor(name, list(shape), dt, kind="ExternalInput").ap()

    def dscr(name, shape, dt):
        kind = "ExternalOutput" if dbg else "Internal"
        return nc.dram_tensor(name, list(shape), dt, kind=kind).ap()

    x_in = din("x", [N, D])
    c_in = din("c", [2, D])
    w_ada = din("w_ada", [DEPTH, D, 6 * D])
    b_ada = din("b_ada", [DEPTH, 6 * D])
    norm1_g = din("norm1_g", [DEPTH, D])
    w_in = din("w_in", [DEPTH, D, 3072])
    pool_w = din("pool_w", [DEPTH, 4, 128, 128])
    pool_scale = din("pool_scale", [DEPTH, 512])
    conv_dw_w = din("conv_dw_w", [DEPTH, CONV_K, 1024])
    conv_dw_b = din("conv_dw_b", [DEPTH, 1024])
    conv_ln_g = din("conv_ln_g", [DEPTH, 1024])
    conv_ln_b = din("conv_ln_b", [DEPTH, 1024])
    conv_pw_w = din("conv_pw_w", [DEPTH, 1024, 1024])
    conv_pw_b = din("conv_pw_b", [DEPTH, 1024])
    fourier_w = din("fourier_w", [DEPTH, 512, 512])
    w_out = din("w_out", [DEPTH, D, D])
    norm2_g = din("norm2_g", [DEPTH, D])
    rc_w = din("router_coarse_w", [DEPTH, D, NG])
    rc_b = din("router_coarse_b", [DEPTH, NG])
    rf_w = din("router_fine_w", [DEPTH, NG, D, EPG])
    rf_b = din("router_fine_b", [DEPTH, NG, EPG])
    e_w1 = din("expert_w1", [DEPTH, NE, D, DE])
    e_w3 = din("expert_w3", [DEPTH, NE, D, DE])
    e_w2 = din("expert_w2", [DEPTH, NE, DE, D])
    final_g = din("final_g", [D])
    k_cs128 = din("cs128", [128, 2, 192], BF16)
    k_ccn = din("ccn", [128, 2, 128], BF16)
    k_fcs = [din("fcs%d" % q, [S // 128, 2, S], BF16) for q, S in enumerate(SEQS)]
    k_poolc = din("poolc", [128, 4, 16])
    k_utinc = din("utinc", [128, 128], BF16)
    k_iotap = din("iotap", [128, 1])
    k_bstart = din("bstart", [128, NB])
    k_iota32 = din("iota32", [128, 32])

    y_out = nc.dram_tensor("y", [N, D], F32, kind="ExternalOutput").ap()

    x1_d = dscr("x1", [N, D], F32)
    x2_d = dscr("x2", [N, D], F32)
    h2_d = dscr("h2", [N, D], BF16)
    xs_d = dscr("xs", [L, D], BF16)
    ys_d = dscr("ys", [L, D], F32)
    mod_d = dscr("modrow", [2, 6 * D], F32)
    zaT = [dscr("zaT%d" % q, [512, S], BF16) for q, S in enumerate(SEQS)]
    vT = [dscr("vT%d" % q, [1024, S], BF16) for q, S in enumerate(SEQS)]
    zcT = [dscr("zcT%d" % q, [512, S], BF16) for q, S in enumerate(SEQS)]
    rzT = [dscr("rzT%d" % q, [512, S], BF16) for q, S in enumerate(SEQS)]
    mixT = [dscr("mixT%d" % q, [2048, S], BF16) for q, S in enumerate(SEQS)]

    P = Prog(nc)
    es = ExitStack()
    with es:
        AR_SZ = 96 * 1024
        art = es.enter_context(nc.sbuf_tensor("arena", [128, AR_SZ], BF16))
        AR = Arena(art, AR_SZ)

        class _A16:
            def alloc(self, n):
                return AR.alloc(n)

            def reset(self):
                AR.reset()

        class _A32:
            def alloc(self, n):
                return AR.alloc(2 * n).bitcast(F32)

            def reset(self):
                pass

        A16 = _A16()
        A32 = _A32()
        ident_f = es.enter_context(nc.sbuf_tensor("ident_f", [128, 128], F32))
        ident_b = es.enter_context(nc.sbuf_tensor("ident_b", [128, 128], BF16))
        ones_b = es.enter_context(nc.sbuf_tensor("ones_b", [128, 128], BF16))
        utinc = es.enter_context(nc.sbuf_tensor("utinc_s", [128, 128], BF16))
        iotap = es.enter_context(nc.sbuf_tensor("iotap_s", [128, 1], F32))
        iota32 = es.enter_context(nc.sbuf_tensor("iota32_s", [128, 32], F32))
        eps_t = es.enter_context(nc.sbuf_tensor("eps_t", [128, 1], F32))
        r_E = es.enter_context(nc.sbuf_tensor("r_E", [128, NT, 64], BF16))
        r_f = es.enter_context(nc.sbuf_tensor("r_f", [128, NT, 4], F32))
        r_slot = es.enter_context(nc.sbuf_tensor("r_slot", [128, NT, 2], I32))
        cnt_run = es.enter_context(nc.sbuf_tensor("cnt_run", [128, 32], F32))
        widx = es.enter_context(nc.sbuf_tensor("widx", [128, NB], I32))
        pm = [es.enter_context(nc.psum_tensor("pm%d" % i, [128, 512], F32)) for i in range(4)]
        pt = es.enter_context(nc.psum_tensor("pt", [128, 2048], BF16))
        pr = [es.enter_context(nc.psum_tensor("pr%d" % i, [128, 512], F32)) for i in range(2)]
        PM = [("pm", i) for i in range(4)]
        PT = "pt"
        PR = [("pr", i) for i in range(2)]

        uid = [0]
        _bregs = {}

        def BR(e, val):
            if val not in _bregs:
                _bregs[val] = e.to_reg(val)
            return _bregs[val]

        def U(name):
            uid[0] += 1
            return (name, uid[0])

        def t16(n, shape=None):
            v = A16.alloc(n)
            return v

        def DMA(q, out, in_, reads, writes, semkey, **kw):
            P.dma(q, lambda e: e.dma_start(out=out, in_=in_, **kw), reads, writes, semkey)

        def MM(out, lhsT, rhs, start, stop, reads, writes):
            P.op("tensor", lambda e: e.matmul(out, lhsT=lhsT, rhs=rhs, start=start, stop=stop), reads, writes)

        def TR(out, in_, ident, reads, writes):
            P.op("tensor", lambda e: e.transpose(out=out, in_=in_, identity=ident), reads, writes)

        def ACT(out, in_, func, reads, writes, bias=None, scale=None, accum_out=None, eng="scalar"):
            kw = {}
            if bias is not None:
                kw["bias"] = bias
            if scale is not None:
                kw["scale"] = scale
            if accum_out is not None:
                kw["accum_out"] = accum_out
            P.op("scalar", lambda e: e.activation(out=out, in_=in_, func=func, **kw), reads, writes)

        def TT(eng, out, in0, in1, op, reads, writes):
            P.op(eng, lambda e: e.tensor_tensor(out=out, in0=in0, in1=in1, op=op), reads, writes)

        def TS(eng, out, in0, s1, s2, op0, op1, reads, writes, accum_out=None):
            if op1 is None:
                P.op(eng, lambda e: e.tensor_scalar(out=out, in0=in0, scalar1=s1, scalar2=None, op0=op0), reads, writes)
            elif accum_out is not None:
                P.op(eng, lambda e: e.tensor_scalar(out=out, in0=in0, scalar1=s1, scalar2=s2, op0=op0, op1=op1, accum_out=accum_out), reads, writes)
            else:
                P.op(eng, lambda e: e.tensor_scalar(out=out, in0=in0, scalar1=s1, scalar2=s2, op0=op0, op1=op1), reads, writes)

        def STT(out, in0, scalar, in1, op0, op1, reads, writes):
            P.op("vector", lambda e: e.scalar_tensor_tensor(out=out, in0=in0, scalar=scalar, in1=in1, op0=op0, op1=op1), reads, writes)

        def CP(eng, out, in_, reads, writes):
            if eng == "scalar":
                P.op("scalar", lambda e: e.copy(out=out, in_=in_), reads, writes)
            else:
                P.op(eng, lambda e: e.tensor_copy(out=out, in_=in_), reads, writes)

        def MEMSET(eng, ap, val, writes):
            P.op(eng, lambda e: e.memset(ap, val), (), writes)

        def RED(out, in_, op, reads, writes, axis=AX.X):
            P.op("vector", lambda e: e.tensor_reduce(out=out, in_=in_, axis=axis, op=op), reads, writes)

        def bcast_row(dram_row_ap, n):
            return dram_row_ap.partition_broadcast(128)

        MEMSET("gpsimd", ident_f[:], 0.0, ["ident_f"])
        P.op("gpsimd", lambda e: e.affine_select(out=ident_f[:], in_=ident_f[:], pattern=[[-1, 128]],
                                                 compare_op=ALU.not_equal, fill=1.0, base=0, channel_multiplier=1),
             ["ident_f"], ["ident_f"])
        CP("vector", ident_b[:], ident_f[:], ["ident_f"], ["ident_b"])
        MEMSET("gpsimd", ones_b[:], 1.0, ["ones_b"])
        MEMSET("gpsimd", eps_t[:], EPS, ["eps_t"])
        DMA("sync", utinc[:], k_utinc, [], ["utinc"], "c_utinc")
        DMA("sync", iotap[:], k_iotap, [], ["iotap"], "c_iotap")
        DMA("sync", iota32[:], k_iota32, [], ["iota32"], "c_iota32")

        class _Stop(Exception):
            pass

        def stop_here(tag):
            if os.environ.get("KSTOP") == tag:
                P.emit()
                raise _Stop()

        cur_x = x_in
        try:
          for l in range(NL):
              last = (l == NL - 1)
              nxt_x = y_out if False else x2_d
              P.barrier()
              A16.reset(); A32.reset()
              cst = A32.alloc(2 * 16).rearrange("p (q k) -> p q k", q=2)
              csb = A16.alloc(16 * 2).rearrange("p (k q) -> p k q", q=2)
              brow = [A32.alloc(512) for _ in range(2)]
              mrow = A32.alloc(512)
              DMA("sync", cst, c_in.rearrange("q (p k) -> p q k", k=16), [], ["cst"], "cst")
              for q in range(2):
                  ACT(csb[:, :, q], cst[:, q, :], AF.Silu, ["cst"], [("csb", q)])
              wa = [A16.alloc(16 * 512).rearrange("p (k n) -> p k n", k=16) for _ in range(2)]
              for cb in range(24):
                  wbuf = wa[cb % 2]
                  wk = ("wa", cb % 2)
                  DMA("gpsimd", wbuf, w_ada[l, :, cb * 512:(cb + 1) * 512].rearrange("(p k) n -> p k n", k=16),
                      [], [wk], "wa%d" % (cb % 2))
                  pk = PM[cb % 2]
                  pst = pm[cb % 2]
                  for kc in range(16):
                      MM(pst[0:2, :], csb[:, kc, :], wbuf[:, kc, :], kc == 0, kc == 15,
                         [wk, ("csb", 0), ("csb", 1)], [pk])
                  mk = U("mrow")
                  bk = ("brow", cb % 2)
                  DMA("sync", brow[cb % 2][0:2, :], b_ada[l:l + 1, cb * 512:(cb + 1) * 512].broadcast_to([2, 512]),
                      [], [bk], "brow%d" % (cb % 2))
                  TT("vector", mrow[0:2, :], pst[0:2, :], brow[cb % 2][0:2, :], ALU.add,
                     [pk, bk], ["mrow"])
                  DMA("sync", mod_d[:, cb * 512:(cb + 1) * 512], mrow[0:2, :], ["mrow"], [("mod", cb)], "mrow")
              MODK = [("mod", cb) for cb in range(24)]

              def mod_rep(q, i):
                  return mod_d[q:q + 1, i * D:(i + 1) * D].broadcast_to([128, D])

              for q, S in enumerate(SEQS):
                  T0 = OFFS[q]
                  NT5 = S // 512
                  S2 = S // 128
                  P.barrier()
                  A16.reset(); A32.reset()
                  wi = A16.alloc(16 * 3072).rearrange("p (k n) -> p k n", k=16)
                  DMA("gpsimd", wi, w_in[l].rearrange("(p k) n -> p k n", k=16), [], ["wi"], "wi")
                  a1 = A32.alloc(D)
                  sh1 = A32.alloc(D)
                  tmpf = A32.alloc(D)
                  DMA("sync", a1, mod_rep(q, 1), MODK, ["a1"], "a1")
                  DMA("sync", sh1, mod_rep(q, 0), MODK, ["sh1"], "sh1")
                  DMA("sync", tmpf, norm1_g[l:l + 1, :].broadcast_to([128, D]), [], ["tmpf"], "g_rep")
                  STT(a1, a1, 1.0, tmpf, ALU.add, ALU.mult, ["a1", "tmpf"], ["a1"])
                  xt = [A32.alloc(D) for _ in range(2)]
                  hb = [A16.alloc(D) for _ in range(4)]
                  hT = [A16.alloc(16 * 512).rearrange("p (k t) -> p k t", k=16) for _ in range(1)]
                  stg = [A16.alloc(16 * 512).rearrange("p (j t) -> p j t", j=16) for _ in range(1)]
                  sgt = [A32.alloc(512) for _ in range(2)]
                  ss = A32.alloc(8)
                  hTt = hT[0]
                  hk = ("hT", 0)

                  def p1_norm(ti):
                      for sub in range(4):
                          it = ti * 4 + sub
                          b2 = it % 2
                          r0 = T0 + it * 128
                          xk = ("xt", b2)
                          DMA("sync", xt[b2], cur_x[r0:r0 + 128, :], [("xcur", r0 // 128)], [xk], "xt%d" % b2)
                          ssk = U("ss")
                          hbk = ("hb", sub)
                          ACT(hb[sub], xt[b2], AF.Square, [xk], [hbk, ssk], accum_out=ss[:, 0:1])
                          ACT(ss[:, 1:2], ss[:, 0:1], AF.Sqrt, [ssk, "eps_t"], [ssk], scale=1.0 / D, bias=eps_t[:, 0:1])
                          P.op("vector", lambda e, ss=ss: e.reciprocal(out=ss[:, 2:3], in_=ss[:, 1:2]), [ssk], [ssk])
                          STT(tmpf, xt[b2], ss[:, 2:3], a1, ALU.mult, ALU.mult, [xk, ssk, "a1"], ["tmpf"])
                          TT("gpsimd", hb[sub], tmpf, sh1, ALU.add, ["tmpf", "sh1"], [hbk])

                  def p1_tr(ti):
                      for sub in range(4):
                          hbk = ("hb", sub)
                          for kc in range(16):
                              TR(pt[:, kc * 128:(kc + 1) * 128], hb[sub].rearrange("p (c k) -> p k c", k=16)[:, kc, :],
                                 ident_b[:], [hbk, "ident_b"], [PT])
                          CP("scalar" if sub % 2 else "vector", hTt[:, :, sub * 128:(sub + 1) * 128],
                             pt[:, :].rearrange("p (k t) -> p k t", k=16), [PT], [hk])

                  def p1_mm(ti):
                      st = stg[0]
                      sk = ("stg", 0)
                      c0 = ti * 512

                      def mmgrp(j, pi):
                          for kc in range(16):
                              MM(pm[pi][:, :], wi[:, kc, j * 128:(j + 1) * 128], hTt[:, kc, :], kc == 0, kc == 15,
                                 ["wi", hk], [PM[pi]])

                      pi = 0
                      for j in range(4):
                          mmgrp(j, pi)
                          CP("scalar", st[:, j, :], pm[pi][:, :], [PM[pi]], [sk])
                          pi = (pi + 1) % 4
                      for j in range(8):
                          pa = pi
                          mmgrp(4 + j, pa)
                          pb = (pi + 1) % 4
                          mmgrp(12 + j, pb)
                          sg = sgt[j % 2]
                          sgk = ("sg", j % 2)
                          ACT(sg, pm[pb][:, :], AF.Sigmoid, [PM[pb]], [sgk])
                          TT("vector", st[:, 4 + j, :], pm[pa][:, :], sg, ALU.mult, [PM[pa], sgk], [sk])
                          pi = (pi + 2) % 4
                      for j in range(4):
                          mmgrp(20 + j, pi)
                          CP("scalar", st[:, 12 + j, :], pm[pi][:, :], [PM[pi]], [sk])
                          pi = (pi + 1) % 4
                      DMA("sync", zaT[q][:, c0:c0 + 512].rearrange("(j p) t -> p j t", p=128), st[:, 0:4, :],
                          [sk], [("zaT", q, ti)], "stg0")
                      DMA("sync", vT[q][:, c0:c0 + 512].rearrange("(j p) t -> p j t", p=128), st[:, 4:12, :],
                          [sk], [("vT", q, ti)], "stg0")
                      DMA("sync", zcT[q][:, c0:c0 + 512].rearrange("(j p) t -> p j t", p=128), st[:, 12:16, :],
                          [sk], [("zcT", q, ti)], "stg0")

                  p1_norm(0)
                  p1_tr(0)
                  for ti in range(NT5):
                      if ti + 1 < NT5:
                          p1_norm(ti + 1)
                      p1_mm(ti)
                      if ti + 1 < NT5:
                          p1_tr(ti + 1)

                  stop_here('P1')
                  P.barrier()
                  A16.reset(); A32.reset()
                  pwf = A32.alloc(4 * 128).rearrange("p (g e) -> p g e", g=4)
                  pwb = A16.alloc(4 * 128).rearrange("p (g e) -> p g e", g=4)
                  DMA("sync", pwf, pool_w[l].rearrange("g c e -> c g e"), [], ["pwf"], "pwf")
                  CP("vector", pwb, pwf, ["pwf"], ["pwb"])
                  psc = A32.alloc(4)
                  pscr = A32.alloc(4 * 128).rearrange("p (g e) -> p g e", g=4)[0:4]
                  DMA("sync", pscr[:, 0, :], pool_scale[l].rearrange("(g e) -> g e", g=4), [], ["pscr"], "pscr")
                  TR(pr[0][:, 0:4], pscr[:, 0, :], ident_f[0:4, 0:4], ["pscr", "ident_f"], [PR[0]])
                  CP("vector", psc, pr[0][:, 0:4], [PR[0]], ["psc"])
                  pcst = A32.alloc(64).rearrange("p (g j) -> p g j", g=4)
                  DMA("sync", pcst, k_poolc, [], ["pcst"], "pcst")
                  ub = [A16.alloc(528) for _ in range(2)]
                  sa = [A32.alloc(528) for _ in range(2)]
                  sb_ = [A32.alloc(528) for _ in range(2)]
                  dmt = [A16.alloc(512) for _ in range(2)]
                  pst2 = [A16.alloc(4 * 512).rearrange("p (g t) -> p g t", g=4) for _ in range(2)]
                  it = 0
                  for ti in range(NT5):
                      c0 = ti * 512
                      st = pst2[ti % 2]
                      sk = ("pst2", ti % 2)
                      for g, w in enumerate((2, 4, 8, 16)):
                          b2 = it % 2
                          it += 1
                          u = ub[b2]
                          uk = ("ub", b2)
                          lo = max(c0 - 8, 0)
                          hi = min(c0 + 520, S)
                          rd = [("zaT", q, tj) for tj in range(max(ti - 1, 0), min(ti + 2, NT5))]
                          if lo > c0 - 8:
                              MEMSET("gpsimd", u[:, 0:8], 0.0, [uk])
                          if hi < c0 + 520:
                              MEMSET("gpsimd", u[:, 520:528], 0.0, [uk])
                          DMA("sync", u[:, lo - (c0 - 8):hi - (c0 - 8)], zaT[q][g * 128:(g + 1) * 128, lo:hi], rd, [uk], "ub%d" % b2)
                          s_a, s_b = sa[b2], sb_[b2]
                          ka, kb = ("sa", b2), ("sb", b2)
                          TT("vector", s_a[:, 1:528], u[:, 0:527], u[:, 1:528], ALU.add, [uk], [ka])
                          cur, curk, oth, othk = s_a, ka, s_b, kb
                          lo_v = 1
                          hi_v = 528
                          step = 1
                          ww = 2
                          while ww < w:
                              nlo = lo_v + step
                              nhi = hi_v - step
                              TT("vector", oth[:, nlo:nhi], cur[:, nlo - step:nhi - step], cur[:, nlo + step:nhi + step],
                                 ALU.add, [curk], [othk])
                              cur, curk, oth, othk = oth, othk, cur, curk
                              lo_v, hi_v = nlo, nhi
                              step *= 2
                              ww *= 2
                          dm = dmt[b2]
                          dk_ = ("dm", b2)
                          STT(dm, cur[:, 8:520], 1.0 / w, u[:, 8:520], ALU.mult, ALU.subtract, [curk, uk], [dk_])
                          if ti == 0:
                              TT("vector", oth[:, 8:16], cur[:, 8:16], pcst[:, g, 0:8], ALU.mult, [curk, "pcst"], [othk])
                              TT("vector", dm[:, 0:8], oth[:, 8:16], u[:, 8:16], ALU.subtract, [othk, uk], [dk_])
                          if ti == NT5 - 1:
                              TT("vector", oth[:, 512:520], cur[:, 512:520], pcst[:, g, 8:16], ALU.mult, [curk, "pcst"], [othk])
                              TT("vector", dm[:, 504:512], oth[:, 512:520], u[:, 512:520], ALU.subtract, [othk, uk], [dk_])
                          pi = g
                          MM(pm[pi][:, :], pwb[:, g, :], dm, True, True, ["pwb", dk_], [PM[pi]])
                          ACT(st[:, g, :], pm[pi][:, :], AF.Copy, [PM[pi], "psc"], [sk], scale=psc[:, g:g + 1])
                      DMA("sync", mixT[q][0:512, c0:c0 + 512].rearrange("(g p) t -> p g t", p=128), st, [sk],
                          [("mixT", q, ti, 0)], "pst2%d" % (ti % 2))

                  P.barrier()
                  A16.reset(); A32.reset()
                  pww = A16.alloc(8 * 1024).rearrange("p (k n) -> p k n", k=8)
                  DMA("gpsimd", pww, conv_pw_w[l].rearrange("(k p) n -> p k n", p=128), [], ["pww"], "pww")
                  dwr = A32.alloc(1024)
                  DMA("sync", dwr[0:CONV_K, :], conv_dw_w[l], [], ["dwr"], "dwr")
                  dwT = A32.alloc(8 * 32).rearrange("p (j k) -> p j k", j=8)
                  for j in range(8):
                      TR(pr[0][:, j * 32:j * 32 + CONV_K], dwr[0:CONV_K, j * 128:(j + 1) * 128],
                         ident_f[0:CONV_K, 0:CONV_K], ["dwr", "ident_f"], [PR[0]])
                  CP("vector", dwT[:, :, 0:CONV_K], pr[0][:, 0:256].rearrange("p (j k) -> p j k", j=8)[:, :, 0:CONV_K],
                     [PR[0]], ["dwT"])
                  vr = A32.alloc(4 * 1024).rearrange("p (v n) -> p v n", v=4)
                  vecs = A32.alloc(32).rearrange("p (v j) -> p v j", v=4)
                  for vi, src in enumerate((conv_dw_b, conv_ln_g, conv_ln_b, conv_pw_b)):
                      DMA("sync", vr[0:8, vi, 0:128], src[l].rearrange("(j p) -> j p", p=128), [], [("vr", vi)], "vr%d" % vi)
                      TR(pr[1][:, vi * 8:vi * 8 + 8], vr[0:8, vi, 0:128], ident_f[0:8, 0:8], [("vr", vi), "ident_f"], [PR[1]])
                  CP("vector", vecs, pr[1][:, 0:32].rearrange("p (v j) -> p v j", v=4), [PR[1]], ["vecs"])
                  acc = [A32.alloc(512) for _ in range(8)]
                  cbf = [A16.alloc(512) for _ in range(2)]
                  sqb = [A16.alloc(512) for _ in range(2)]
                  sT = [A16.alloc(512) for _ in range(8)]
                  mean = A32.alloc(512)
                  var = A32.alloc(512)
                  rstd = A32.alloc(512)
                  xn = [A32.alloc(512) for _ in range(2)]
                  st3 = [A16.alloc(8 * 512).rearrange("p (j t) -> p j t", j=8) for _ in range(2)]
                  if CONV_PE:
                      vb = [A16.alloc(544) for _ in range(3)]
                      vb1 = [A16.alloc(544) for _ in range(3)]
                      dg = A16.alloc(8 * 32 * 128).rearrange("p (j k c) -> p j k c", j=8, k=32)
                      for j in range(8):
                          for k in range(CONV_K):
                              TS("vector" if (j * CONV_K + k) % 2 else "gpsimd", dg[:, j, k, :], ident_b[:, :], dwT[:, j, k:k + 1], None,
                                 ALU.mult, None, ["ident_b", "dwT"], [("dg", j)])
                  else:
                      vb = [A16.alloc(544) for _ in range(2)]
                  it = 0
                  for ti in range(NT5):
                      c0 = ti * 512
                      for j in range(8):
                          if CONV_PE:
                              b2 = it % 2
                              b3 = it % 3
                              it += 1
                              v, vk = vb[b3], ("vb", b3)
                              v1, v1k = vb1[b3], ("vb1", b3)
                              lo = max(c0 - 15, 0)
                              hi = min(c0 + 527, S)
                              rd = [("vT", q, tj) for tj in range(max(ti - 1, 0), min(ti + 2, NT5))]
                              if lo > c0 - 15:
                                  MEMSET("gpsimd", v[:, 0:16], 0.0, [vk])
                                  MEMSET("gpsimd", v1[:, 0:16], 0.0, [v1k])
                              if hi < c0 + 527:
                                  MEMSET("gpsimd", v[:, 526:544], 0.0, [vk])
                                  MEMSET("gpsimd", v1[:, 526:544], 0.0, [v1k])
                              DMA("sync", v[:, lo - (c0 - 15):hi - (c0 - 15)], vT[q][j * 128:(j + 1) * 128, lo:hi], rd, [vk], "vb%d" % b3)
                              lo1 = max(c0 - 14, 0)
                              DMA("sync", v1[:, lo1 - (c0 - 14):hi - (c0 - 14)], vT[q][j * 128:(j + 1) * 128, lo1:hi], rd, [v1k], "vb1%d" % b3)
                              pi = it % 4
                              for k in range(CONV_K):
                                  if k % 2 == 0:
                                      MM(pm[pi][:, :], dg[:, j, k, :], v[:, k:k + 512], k == 0, k == CONV_K - 1,
                                         [("dg", j), vk], [PM[pi]])
                                  else:
                                      MM(pm[pi][:, :], dg[:, j, k, :], v1[:, k - 1:k - 1 + 512], k == 0, k == CONV_K - 1,
                                         [("dg", j), v1k], [PM[pi]])
                              a = acc[j]
                              ak = ("acc", j)
                              ACT(a, pm[pi][:, :], AF.Identity, [PM[pi], "vecs"], [ak], bias=vecs[:, 0, j:j + 1])
                              cb_, ck = cbf[b2], ("cbf", b2)
                              sq_, sqk = sqb[b2], ("sqb", b2)
                              CP("vector", cb_, a, [ak], [ck])
                              ACT(sq_, a, AF.Square, [ak], [sqk])
                              MM(pr[0][:, :], ones_b[:], cb_, j == 0, j == 7, ["ones_b", ck], [PR[0]])
                              MM(pr[1][:, :], ones_b[:], sq_, j == 0, j == 7, ["ones_b", sqk], [PR[1]])
                          else:
                              b2 = it % 2
                              it += 1
                              v = vb[b2]
                              vk = ("vb", b2)
                              lo = max(c0 - 15, 0)
                              hi = min(c0 + 527, S)
                              rd = [("vT", q, tj) for tj in range(max(ti - 1, 0), min(ti + 2, NT5))]
                              if lo > c0 - 15:
                                  MEMSET("gpsimd", v[:, 0:15], 0.0, [vk])
                              if hi < c0 + 527:
                                  MEMSET("gpsimd", v[:, 527:542], 0.0, [vk])
                              DMA("sync", v[:, lo - (c0 - 15):hi - (c0 - 15)], vT[q][j * 128:(j + 1) * 128, lo:hi], rd, [vk], "vb%d" % b2)
                              a = acc[j]
                              ak = ("acc", j)
                              TS("vector", a, v[:, 0:512], dwT[:, j, 0:1], vecs[:, 0, j:j + 1], ALU.mult, ALU.add,
                                 [vk, "dwT", "vecs"], [ak])
                              for k in range(1, CONV_K):
                                  STT(a, v[:, k:k + 512], dwT[:, j, k:k + 1], a, ALU.mult, ALU.add, [vk, "dwT", ak], [ak])
                              cb_, ck = cbf[b2], ("cbf", b2)
                              sq_, sqk = sqb[b2], ("sqb", b2)
                              CP("gpsimd", cb_, a, [ak], [ck])
                              ACT(sq_, a, AF.Square, [ak], [sqk])
                              MM(pr[0][:, :], ones_b[:], cb_, j == 0, j == 7, ["ones_b", ck], [PR[0]])
                              MM(pr[1][:, :], ones_b[:], sq_, j == 0, j == 7, ["ones_b", sqk], [PR[1]])
                      TS("vector", mean, pr[0][:, :], 1.0 / 1024, None, ALU.mult, None, [PR[0]], ["mean"])
                      TS("vector", var, pr[1][:, :], 1.0 / 1024, None, ALU.mult, None, [PR[1]], ["var"])
                      TT("vector", rstd, mean, mean, ALU.mult, ["mean"], ["rstd"])
                      TT("vector", var, var, rstd, ALU.subtract, ["var", "rstd"], ["var"])
                      ACT(var, var, AF.Sqrt, ["var", "eps_t"], ["var"], bias=eps_t[:, 0:1], scale=1.0)
                      P.op("vector", lambda e, rstd=rstd, var=var: e.reciprocal(out=rstd, in_=var), ["var"], ["rstd"])
                      for j in range(8):
                          x_, xk_ = xn[j % 2], ("xn", j % 2)
                          TT("gpsimd", x_, acc[j], mean, ALU.subtract, [("acc", j), "mean"], [xk_])
                          TT("vector", x_, x_, rstd, ALU.mult, [xk_, "rstd"], [xk_])
                          ACT(sT[j], x_, AF.Silu, [xk_, "vecs"], [("sT", j)], scale=vecs[:, 1, j:j + 1], bias=vecs[:, 2, j:j + 1])
                      st = st3[ti % 2]
                      sk = ("st3", ti % 2)
                      for e_ in range(8):
                          pi = e_ % 4
                          for j in range(8):
                              MM(pm[pi][:, :], pww[:, j, e_ * 128:(e_ + 1) * 128], sT[j], j == 0, j == 7,
                                 ["pww", ("sT", j)], [PM[pi]])
                          ACT(st[:, e_, :], pm[pi][:, :], AF.Identity, [PM[pi], "vecs"], [sk], bias=vecs[:, 3, e_:e_ + 1])
                      DMA("sync", mixT[q][512:1536, c0:c0 + 512].rearrange("(j p) t -> p j t", p=128), st, [sk],
                          [("mixT", q, ti, 1)], "st3%d" % (ti % 2))

                  stop_here('P3')
                  P.barrier()
                  A16.reset(); A32.reset()
                  cs = A16.alloc(2 * 192).rearrange("p (h n) -> p h n", h=2)
                  ccn = A16.alloc(2 * 128).rearrange("p (h n) -> p h n", h=2)
                  fcs = A16.alloc(2 * S).rearrange("p (h n) -> p h n", h=2)
                  DMA("sync", cs, k_cs128, [], ["cs"], "cs")
                  DMA("sync", ccn, k_ccn, [], ["ccn"], "ccn")
                  DMA("sync", fcs[0:S2], k_fcs[q], [], ["fcs"], "fcs")
                  fwf = A32.alloc(4 * 512).rearrange("p (h e) -> p h e", h=4)
                  fwb = A16.alloc(4 * 512).rearrange("p (h e) -> p h e", h=4)
                  DMA("sync", fwf, fourier_w[l].rearrange("(h m) e -> m h e", h=4), [], ["fwf"], "fwf")
                  CP("vector", fwb, fwf, ["fwf"], ["fwb"])
                  Ut = A16.alloc(128 * S2).rearrange("p (c t) -> p c t", c=128)
                  Ah = A16.alloc(128 * 192).rearrange("p (c n) -> p c n", c=128)
                  Xh = A16.alloc(2 * S).rearrange("p (h k) -> p h k", h=2)
                  rst = [A16.alloc(512) for _ in range(2)]
                  ALLZC = [("zcT", q, tj) for tj in range(NT5)]
                  nrm = 1.0 / math.sqrt(S * 128.0)
                  ri = 0
                  for h in range(4):
                      for cq in range(8):
                          DMA("sync", Ut[:, cq * 16:(cq + 1) * 16, :],
                              zcT[q][h * 128 + cq * 16:h * 128 + (cq + 1) * 16, :].rearrange("c (a b) -> a c b", b=S2),
                              ALLZC, ["Ut"], "Ut")
                      for half in range(2):
                          pmv = [pm[i] for i in range(4)]
                          for cg in range(16):
                              for ci in range(8):
                                  c_ = cg * 8 + ci
                                  bank = ci // 2
                                  col = (ci % 2) * 192
                                  MM(pmv[bank][0:S2, col:col + 192], Ut[:, c_, :], cs[:, half, :], True, True,
                                     ["Ut", "cs"], [PM[bank]])
                              for bank in range(4):
                                  eng = "vector" if bank % 2 == 0 else "scalar"
                                  CP(eng, Ah[0:S2, cg * 8 + bank * 2:cg * 8 + bank * 2 + 2, :],
                                     pmv[bank][0:S2, 0:384].rearrange("p (c n) -> p c n", c=2), [PM[bank]], [("Ah", cg)])
                          AHK = [("Ah", cg) for cg in range(16)]
                          G = 512 // S2
                          G = min(G, 64)
                          for kg in range(64 // G):
                              pxr, pxi = pr[0], pr[1]
                              for gi in range(G):
                                  k1l = kg * G + gi
                                  k1 = half * 64 + k1l
                                  fc_ = fcs[0:S2, 0, :].rearrange("p (b a) -> p a b", a=128)[:, k1, :]
                                  fs_ = fcs[0:S2, 1, :].rearrange("p (b a) -> p a b", a=128)[:, k1, :]
                                  ar = Ah[0:S2, :, k1l]
                                  ai = Ah[0:S2, :, 64 + k1l]
                                  an = Ah[0:S2, :, 128 + k1l]
                                  o = gi * S2
                                  MM(pxr[:, o:o + S2], ar, fc_, True, False, AHK + ["fcs"], [PR[0]])
                                  MM(pxr[:, o:o + S2], an, fs_, False, True, AHK + ["fcs"], [PR[0]])
                                  MM(pxi[:, o:o + S2], ar, fs_, True, False, AHK + ["fcs"], [PR[1]])
                                  MM(pxi[:, o:o + S2], ai, fc_, False, True, AHK + ["fcs"], [PR[1]])
                              k1b = half * 64 + kg * G
                              for ri_, px in enumerate((pxr, pxi)):
                                  dst = Xh[:, ri_, :].rearrange("p (b a) -> p a b", a=128)[:, k1b:k1b + G, :]
                                  src = px[:, 0:G * S2].rearrange("p (g b) -> p g b", g=G)
                                  CP("vector" if ri_ == 0 else "scalar", dst, src, [PR[ri_]], [("Xh", half, kg)])
                      XHK = [("Xh", hf, kg) for hf in range(2) for kg in range(64 // G)]
                      for kt in range(NT5):
                          pi = kt % 4
                          MM(pm[pi][:, :], ccn[:, 0, :], Xh[:, 0, kt * 512:(kt + 1) * 512], True, False, ["ccn"] + XHK, [PM[pi]])
                          MM(pm[pi][:, :], ccn[:, 1, :], Xh[:, 1, kt * 512:(kt + 1) * 512], False, True, ["ccn"] + XHK, [PM[pi]])
                          r_ = rst[ri % 2]
                          rk = ("rst", ri % 2)
                          ACT(r_, pm[pi][:, :], AF.Copy, [PM[pi]], [rk], scale=nrm)
                          DMA("sync", rzT[q][h * 128:(h + 1) * 128, kt * 512:(kt + 1) * 512], r_, [rk], [("rzT", q, h, kt)],
                              "rst%d" % (ri % 2))
                          ri += 1
                  rzb = [A16.alloc(4 * 512).rearrange("p (h t) -> p h t", h=4) for _ in range(2)]
                  st4 = [A16.alloc(4 * 512).rearrange("p (e t) -> p e t", e=4) for _ in range(2)]
                  for ti in range(NT5):
                      c0 = ti * 512
                      rb, rbk = rzb[ti % 2], ("rzb", ti % 2)
                      DMA("sync", rb, rzT[q][:, c0:c0 + 512].rearrange("(h m) t -> m h t", h=4),
                          [("rzT", q, h, ti) for h in range(4)], [rbk], "rzb%d" % (ti % 2))
                      st, sk = st4[ti % 2], ("st4", ti % 2)
                      for e_ in range(4):
                          pi = e_
                          for h in range(4):
                              MM(pm[pi][:, :], fwb[:, h, e_ * 128:(e_ + 1) * 128], rb[:, h, :], h == 0, h == 3,
                                 ["fwb", rbk], [PM[pi]])
                          CP("scalar" if e_ % 2 else "vector", st[:, e_, :], pm[pi][:, :], [PM[pi]], [sk])
                      DMA("sync", mixT[q][1536:2048, c0:c0 + 512].rearrange("(e p) t -> p e t", p=128), st, [sk],
                          [("mixT", q, ti, 2)], "st4%d" % (ti % 2))

                  stop_here('P4')
                  P.barrier()
                  A16.reset(); A32.reset()
                  wo = A16.alloc(16 * 2048).rearrange("p (k n) -> p k n", k=16)
                  DMA("gpsimd", wo, w_out[l].rearrange("(k p) n -> p k n", p=128), [], ["wo"], "wo")
                  g1 = A32.alloc(D)
                  a2 = A32.alloc(D)
                  sh2 = A32.alloc(D)
                  tmpf = A32.alloc(D)
                  TFK = [("tmpf", i) for i in range(4)]
                  DMA("sync", g1, mod_rep(q, 2), MODK, ["g1"], "g1")
                  DMA("sync", a2, mod_rep(q, 4), MODK, ["a2"], "a2")
                  DMA("sync", sh2, mod_rep(q, 3), MODK, ["sh2"], "sh2")
                  DMA("sync", tmpf, norm2_g[l:l + 1, :].broadcast_to([128, D]), [], TFK, "g_rep")
                  STT(a2, a2, 1.0, tmpf, ALU.add, ALU.mult, ["a2"] + TFK, ["a2"])
                  wr = A32.alloc(16 * 36).rearrange("p (k n) -> p k n", k=16)
                  DMA("sync", wr[:, :, 0:4], rc_w[l].rearrange("(p k) g -> p k g", k=16), [], [("wr", 0)], "wr")
                  for g in range(4):
                      DMA("sync", wr[:, :, 4 + 8 * g:12 + 8 * g], rf_w[l, g].rearrange("(p k) e -> p k e", k=16), [],
                          [("wr", 1 + g)], "wr")
                  WRK = [("wr", i) for i in range(5)]
                  rb_ = A32.alloc(36)
                  DMA("sync", rb_[:, 0:4], rc_b[l:l + 1, :].broadcast_to([128, 4]), [], [("rb", 0)], "rb")
                  DMA("sync", rb_[:, 4:36], rf_b[l:l + 1].rearrange("o g e -> o (g e)").broadcast_to([128, 32]), [], [("rb", 1)], "rb")
                  RBK = [("rb", 0), ("rb", 1)]
                  mx = [A16.alloc(16 * 512).rearrange("p (k t) -> p k t", k=16) for _ in range(2)]
                  xt = [A32.alloc(D) for _ in range(1)]
                  x1t = [A32.alloc(D) for _ in range(2)]
                  h2f2 = [A32.alloc(D) for _ in range(2)]
                  h2b = [A16.alloc(D) for _ in range(2)]
                  h2T = A32.alloc(16 * 128).rearrange("p (k t) -> p k t", k=16)
                  sm = A32.alloc(256)
                  if q == 0:
                      MEMSET("vector", cnt_run[:], 0.0, ["cnt_run"])

                  def p5_main(it):
                      ti, sub = it // 4, it % 4
                      c0 = ti * 512
                      m_, mk_ = mx[ti % 2], ("mx", ti % 2)
                      if sub == 0:
                          DMA("sync", m_, mixT[q][:, c0:c0 + 512].rearrange("(k p) t -> p k t", p=128),
                              [("mixT", q, ti, i) for i in range(3)], [mk_], "mx%d" % (ti % 2))
                      b2 = it % 2
                      r0 = T0 + it * 128
                      xk = ("xt", 0)
                      DMA("sync", xt[0], cur_x[r0:r0 + 128, :], [("xcur", r0 // 128)], [xk], "xt0")
                      x1, x1k = x1t[b2], ("x1t", b2)
                      for cbk in range(4):
                          pi = cbk
                          for kc in range(16):
                              MM(pm[pi][:, :], m_[:, kc, sub * 128:(sub + 1) * 128], wo[:, kc, cbk * 512:(cbk + 1) * 512],
                                 kc == 0, kc == 15, [mk_, "wo"], [PM[pi]])
                          sl = slice(cbk * 512, (cbk + 1) * 512)
                          TT("vector", tmpf[:, sl], pm[pi][:, :], g1[:, sl], ALU.mult, [PM[pi], "g1"], [("tmpf", cbk)])
                          TT("gpsimd", x1[:, sl], tmpf[:, sl], xt[0][:, sl], ALU.add, [("tmpf", cbk), xk], [x1k])
                      DMA("sync", x1_d[r0:r0 + 128, :], x1, [x1k], [("x1", r0 // 128)], "x1t%d" % b2)
                      ssk = U("ss")
                      ss = sm[:, 0:4]
                      hb_, hbk = h2b[b2], ("h2b", b2)
                      h2f, h2fk = h2f2[b2], ("h2f", b2)
                      ACT(hb_, x1, AF.Square, [x1k], [hbk, ssk], accum_out=ss[:, 0:1])
                      ACT(ss[:, 1:2], ss[:, 0:1], AF.Sqrt, [ssk, "eps_t"], [ssk], scale=1.0 / D, bias=eps_t[:, 0:1])
                      P.op("vector", lambda e, ss=ss: e.reciprocal(out=ss[:, 2:3], in_=ss[:, 1:2]), [ssk], [ssk])
                      STT(tmpf, x1, ss[:, 2:3], a2, ALU.mult, ALU.mult, [x1k, ssk, "a2"], [("tmpf", i) for i in range(4)])
                      TT("gpsimd", h2f, tmpf, sh2, ALU.add, [("tmpf", i) for i in range(4)] + ["sh2"], [h2fk])
                      CP("scalar", hb_, h2f, [h2fk], [hbk])
                      DMA("sync", h2_d[r0:r0 + 128, :], hb_, [hbk], [("h2", r0 // 128)], "h2b%d" % b2)

                  def p5_router(it):
                      git = (T0 // 128) + it
                      b2 = it % 2
                      h2f, h2fk = h2f2[b2], ("h2f", b2)
                      for kc in range(16):
                          pj = pr[(kc // 4) % 2]
                          TR(pj[:, (kc % 4) * 128:(kc % 4 + 1) * 128], h2f.rearrange("p (c k) -> p k c", k=16)[:, kc, :],
                             ident_f[:], [h2fk, "ident_f"], [PR[(kc // 4) % 2]])
                          if kc % 4 == 3:
                              g4 = kc // 4
                              CP("scalar" if g4 % 2 else "vector", h2T[:, g4 * 4:g4 * 4 + 4, :],
                                 pj[:, :].rearrange("p (k t) -> p k t", k=4), [PR[(kc // 4) % 2]], [("h2T", g4)])
                      for kc in range(16):
                          MM(pr[0][:, 0:36], h2T[:, kc, :], wr[:, kc, :], kc == 0, kc == 15,
                             [("h2T", kc // 4)] + WRK, [PR[0]])
                      route_tile(P, nc, sm, pr, PR, rb_, RBK, r_E, r_f, cnt_run, utinc, ones_b, git,
                                 TT, TS, STT, ACT, CP, RED, MM, U)

                  NSUB = NT5 * 4
                  for it in range(NSUB):
                      p5_main(it)
                      if it > 0:
                          p5_router(it - 1)
                  p5_router(NSUB - 1)

              stop_here('P5')
              P.barrier()
              A16.reset(); A32.reset()
              fin = A32.alloc(8 * 32).rearrange("p (a e) -> p a e", a=8)
              RALL = ["cnt_run"]
              TS("vector", fin[:, 0, :], cnt_run[:], 127.0, None, ALU.add, None, ["cnt_run"], ["fin"])
              fin_i = A32.alloc(32).bitcast(I32)
              CP("vector", fin_i, fin[:, 0, :], ["fin"], ["fin_i"])
              TS("vector", fin_i, fin_i, 7, 7, ALU.arith_shift_right, ALU.logical_shift_left, ["fin_i"], ["fin_i"])
              CP("vector", fin[:, 2, :], fin_i, ["fin_i"], ["fin"])
              MEMSET("vector", fin[:, 3, :], 1.0, ["fin"])
              P.op("vector", lambda e: e.tensor_tensor_scan(out=fin[:, 4, :], data0=fin[:, 3, :], data1=fin[:, 2, :],
                                                            initial=0.0, op0=ALU.mult, op1=ALU.add), ["fin"], ["fin"])
              TT("vector", fin[:, 5, :], fin[:, 4, :], fin[:, 2, :], ALU.subtract, ["fin"], ["fin"])
              big = A32.alloc(NT * 32).rearrange("p (t e) -> p t e", e=32)
              slf = A32.alloc(NT * 2).rearrange("p (t k) -> p t k", k=2)
              for k in range(2):
                  TT("vector", big, r_E[:, :, k * 32:(k + 1) * 32], fin[:, 5:6, :].to_broadcast([128, NT, 32]), ALU.mult,
                     ["fin", "r_E"], ["big"])
                  RED(slf[:, :, k], big, ALU.add, ["big"], ["slf"])
                  TT("vector", slf[:, :, k], slf[:, :, k], r_f[:, :, k], ALU.add, ["slf", "r_f"], ["slf"])
              CP("vector", r_slot[:], slf, ["slf"], ["r_slot"])
              bst = A32.alloc(NB)
              DMA("sync", bst, k_bstart, [], ["bst"], "bst")
              ebf = A32.alloc(NB)
              CH = 32
              bigb = A32.alloc(CH * 32).rearrange("p (b e) -> p b e", e=32)
              for b0 in range(0, NB, CH):
                  nb_ = min(CH, NB - b0)
                  TT("vector", bigb[:, 0:nb_, :], fin[:, 4:5, :].to_broadcast([128, nb_, 32]),
                     bst[:, b0:b0 + nb_].unsqueeze(2).to_broadcast([128, nb_, 32]), ALU.is_le, ["fin", "bst"], ["bigb"])
                  RED(ebf[:, b0:b0 + nb_], bigb[:, 0:nb_, :], ALU.add, ["bigb"], ["ebf"])
              TS("vector", ebf, ebf, 31.0, None, ALU.min, None, ["ebf"], ["ebf"])
              sam = A32.alloc(NB)
              MEMSET("vector", sam[:, 0:1], 0.0, ["sam"])
              TT("vector", sam[:, 1:NB], ebf[:, 1:NB], ebf[:, 0:NB - 1], ALU.is_equal, ["ebf"], ["sam"])
              wif = A32.alloc(NB)
              TS("vector", wif, ebf, 128.0, iotap[:, 0:1], ALU.mult, ALU.add, ["ebf", "iotap"], ["wif"])
              STT(wif, sam, 1.0e6, wif, ALU.mult, ALU.add, ["sam", "wif"], ["wif"])
              if l > 0:
                  TS("vector", wif, wif, float(l * NE * 128), None, ALU.add, None, ["wif"], ["wif"])
              CP("vector", widx[:], wif, ["wif"], ["widx"])
              hl = [A16.alloc(D) for _ in range(3)]
              for it in range(NT):
                  b3 = it % 3
                  hk_ = ("hl", b3)
                  DMA("sync", hl[b3], h2_d[it * 128:(it + 1) * 128, :], [("h2", it)], [hk_], "hl%d" % b3)
                  for k in range(2):
                      off = r_slot[:, it, k:k + 1]
                      P.dma("gpsimd", (lambda e, off=off, src=hl[b3]: e.indirect_dma_start(
                          out=xs_d, out_offset=bass.IndirectOffsetOnAxis(ap=off, axis=0), in_=src, in_offset=None,
                          bounds_check=BR(e, L - 1), oob_is_err=False)), [hk_, "r_slot"], [("xs", it, k)], "xs_sc")
              XSK = [("xs", it, k) for it in range(NT) for k in range(2)]

              P.barrier()
              A16.reset(); A32.reset()
              W1 = A16.alloc(16 * 512)
              W3 = A16.alloc(16 * 512)
              W2 = A16.alloc(4 * 2048)
              xb = [A16.alloc(D) for _ in range(2)]
              xbT = [A16.alloc(16 * 128).rearrange("p (k t) -> p k t", k=16) for _ in range(2)]
              sgf = [A32.alloc(512) for _ in range(2)]
              hid = [A16.alloc(512) for _ in range(2)]
              hidT = [A16.alloc(4 * 128).rearrange("p (k t) -> p k t", k=4) for _ in range(2)]
              yb = [A32.alloc(D) for _ in range(2)]
              w1v = e_w1.rearrange("l e (p k) n -> (l e p) (k n)", k=16)
              w3v = e_w3.rearrange("l e (p k) n -> (l e p) (k n)", k=16)
              w2v = e_w2.rearrange("l e (p k) n -> (l e p) (k n)", k=4)
              bnd_l = (l + 1) * NE * 128 - 1

              def wgather(b, Wt, wv, wk):
                  off = widx[:, b:b + 1]
                  P.dma("gpsimd", (lambda e, off=off, Wt=Wt, wv=wv, bnd=bnd_l: e.indirect_dma_start(
                      out=Wt, out_offset=None, in_=wv, in_offset=bass.IndirectOffsetOnAxis(ap=off, axis=0),
                      bounds_check=BR(e, bnd), oob_is_err=False)), ["widx"], [wk], wk)

              def stageA(b):
                  b2 = b % 2
                  wgather(b, W1, w1v, "W1")
                  wgather(b, W3, w3v, "W3")
                  xk = ("xb", b2)
                  DMA("sync", xb[b2], xs_d[b * 128:(b + 1) * 128, :], XSK, [xk], "xb%d" % b2)
                  for kc in range(16):
                      TR(pt[:, kc * 128:(kc + 1) * 128], xb[b2].rearrange("p (c k) -> p k c", k=16)[:, kc, :], ident_b[:],
                         [xk, "ident_b"], [PT])
                  xT, xTk = xbT[b2], ("xbT", b2)
                  CP("scalar", xT[:, 0:8, :], pt[:, 0:1024].rearrange("p (k t) -> p k t", k=8), [PT], [xTk])
                  CP("vector", xT[:, 8:16, :], pt[:, 1024:2048].rearrange("p (k t) -> p k t", k=8), [PT], [xTk])
                  p1, p3 = 2 * b2, 2 * b2 + 1
                  for kc in range(16):
                      MM(pm[p1][:, :], xT[:, kc, :], W1[:, kc * 512:(kc + 1) * 512], kc == 0, kc == 15, [xTk, "W1"], [PM[p1]])
                  for kc in range(16):
                      MM(pm[p3][:, :], xT[:, kc, :], W3[:, kc * 512:(kc + 1) * 512], kc == 0, kc == 15, [xTk, "W3"], [PM[p3]])
                  ACT(sgf[b2], pm[p1][:, :], AF.Silu, [PM[p1]], [("sgf", b2)])
                  TT("vector", hid[b2], pm[p3][:, :], sgf[b2], ALU.mult, [PM[p3], ("sgf", b2)], [("hid", b2)])

              def stageB(b):
                  b2 = b % 2
                  wgather(b, W2, w2v, "W2")
                  for fc in range(4):
                      TR(pt[:, fc * 128:(fc + 1) * 128], hid[b2].rearrange("p (c k) -> p k c", k=4)[:, fc, :], ident_b[:],
                         [("hid", b2), "ident_b"], [PT])
                  CP("vector", hidT[b2], pt[:, 0:512].rearrange("p (k t) -> p k t", k=4), [PT], [("hidT", b2)])
                  y_, yk = yb[b2], ("yb", b2)
                  for cbk in range(4):
                      pi = cbk % 2
                      for fc in range(4):
                          MM(pr[pi][:, :], hidT[b2][:, fc, :], W2[:, fc * 2048 + cbk * 512:fc * 2048 + (cbk + 1) * 512],
                             fc == 0, fc == 3, [("hidT", b2), "W2"], [PR[pi]])
                      if cbk % 2:
                          CP("scalar", y_[:, cbk * 512:(cbk + 1) * 512], pr[pi][:, :], [PR[pi]], [yk])
                      else:
                          CP("vector", y_[:, cbk * 512:(cbk + 1) * 512], pr[pi][:, :], [PR[pi]], [yk])
                  DMA("sync", ys_d[b * 128:(b + 1) * 128, :], y_, [yk], [("ys", b)], "yb%d" % b2)

              stageA(0)
              for b in range(NB):
                  if b + 1 < NB:
                      stageA(b + 1)
                  stageB(b)
              YSK = [("ys", b) for b in range(NB)]

              stop_here('P7')
              P.barrier()
              A16.reset(); A32.reset()
              g2 = [A32.alloc(D) for _ in range(2)]
              for q in range(2):
                  DMA("sync", g2[q], mod_rep(q, 5), MODK, [("g2", q)], "g2%d" % q)
              if last:
                  fg = A32.alloc(D)
                  DMA("sync", fg, final_g.rearrange("(o n) -> o n", o=1).broadcast_to([128, D]), [], ["fg"], "fg")
              ya = [A32.alloc(D) for _ in range(2)]
              ybb = [A32.alloc(D) for _ in range(2)]
              x1t = [A32.alloc(D) for _ in range(2)]
              junk = A16.alloc(D)
              ss = A32.alloc(8)
              dst_x = y_out if last else x2_d
              for it in range(NT):
                  b2 = it % 2
                  q = 0 if it * 128 < OFFS[1] else 1
                  for k, yt in enumerate((ya, ybb)):
                      off = r_slot[:, it, k:k + 1]
                      P.dma("gpsimd", (lambda e, off=off, dst=yt[b2]: e.indirect_dma_start(
                          out=dst, out_offset=None, in_=ys_d, in_offset=bass.IndirectOffsetOnAxis(ap=off, axis=0),
                          bounds_check=BR(e, L - 1), oob_is_err=False)), YSK + ["r_slot"], [("yg", k, b2)], "yg%d%d" % (k, b2))
                  xk = ("x1t", b2)
                  DMA("sync", x1t[b2], x1_d[it * 128:(it + 1) * 128, :], [("x1", it)], [xk], "x1l%d" % b2)
                  A_, B_ = ya[b2], ybb[b2]
                  TS("vector", A_, A_, r_f[:, it, 2:3], None, ALU.mult, None, [("yg", 0, b2), "r_f"], [("yg", 0, b2)])
                  STT(A_, B_, r_f[:, it, 3:4], A_, ALU.mult, ALU.add, [("yg", 1, b2), ("yg", 0, b2), "r_f"], [("yg", 0, b2)])
                  TT("gpsimd", A_, A_, g2[q], ALU.mult, [("yg", 0, b2), ("g2", q)], [("yg", 0, b2)])
                  TT("vector", B_, A_, x1t[b2], ALU.add, [("yg", 0, b2), xk], [("yg", 1, b2)])
                  if not last:
                      DMA("sync", dst_x[it * 128:(it + 1) * 128, :], B_, [("yg", 1, b2)], [("xcur", it)], "xo%d" % b2)
                  else:
                      ssk = U("ss")
                      ACT(junk, B_, AF.Square, [("yg", 1, b2)], ["junk", ssk], accum_out=ss[:, 0:1])
                      ACT(ss[:, 1:2], ss[:, 0:1], AF.Sqrt, [ssk, "eps_t"], [ssk], scale=1.0 / D, bias=eps_t[:, 0:1])
                      P.op("vector", lambda e, ss=ss: e.reciprocal(out=ss[:, 2:3], in_=ss[:, 1:2]), [ssk], [ssk])
                      STT(A_, B_, ss[:, 2:3], fg, ALU.mult, ALU.mult, [("yg", 1, b2), ssk, "fg"], [("yg", 0, b2)])
                      DMA("sync", dst_x[it * 128:(it + 1) * 128, :], A_, [("yg", 0, b2)], [("yout", it)], "xo%d" % b2)
              cur_x = x2_d

          P.emit()
        except _Stop:
            pass
    return nc


def route_tile(P, nc, sm, pr, PR, rb_, RBK, r_E, r_f, cnt_run, utinc, ones_b, git,
               TT, TS, STT, ACT, CP, RED, MM, U):
    rk = U("rt")
    Lg = sm[:, 8:44]
    TT("vector", Lg, pr[0][:, 0:36], rb_, ALU.add, [PR[0]] + RBK, [rk])
    m = sm[:, 44:45]
    RED(m, Lg[:, 0:4], ALU.max, [rk], [rk])
    oh = sm[:, 48:52]
    TS("vector", oh, Lg[:, 0:4], m, None, ALU.is_equal, None, [rk], [rk])
    negm = sm[:, 45:46]
    TS("vector", negm, m, -1.0, None, ALU.mult, None, [rk], [rk])
    ex = sm[:, 52:56]
    se = sm[:, 46:47]
    ACT(ex, Lg[:, 0:4], AF.Exp, [rk], [rk], bias=negm, scale=1.0, accum_out=se)
    pg = sm[:, 47:48]
    P.op("vector", lambda e: e.reciprocal(out=pg, in_=se), [rk], [rk])
    lf = sm[:, 56:64]
    TS("vector", lf, Lg[:, 4:12], oh[:, 0:1], None, ALU.mult, None, [rk], [rk])
    for g in range(1, 4):
        STT(lf, Lg[:, 4 + 8 * g:12 + 8 * g], oh[:, g:g + 1], lf, ALU.mult, ALU.add, [rk], [rk])
    top = sm[:, 64:72]
    P.op("vector", lambda e: e.max(out=top, in_=lf), [rk], [rk])
    s1 = sm[:, 72:80]
    s2 = sm[:, 80:88]
    TS("vector", s1, lf, top[:, 0:1], None, ALU.is_equal, None, [rk], [rk])
    TS("vector", s2, lf, top[:, 1:2], None, ALU.is_equal, None, [rk], [rk])
    dv = sm[:, 88:89]
    TT("vector", dv, top[:, 0:1], top[:, 1:2], ALU.subtract, [rk], [rk])
    sg = sm[:, 89:90]
    ACT(sg, dv, AF.Sigmoid, [rk], [rk])
    TT("vector", r_f[:, git, 2:3], pg, sg, ALU.mult, [rk], ["r_f"])
    TT("vector", r_f[:, git, 3:4], pg, r_f[:, git, 2:3], ALU.subtract, [rk, "r_f"], ["r_f"])
    Ef = sm[:, 96:160]
    for g in range(4):
        TS("vector", Ef[:, 8 * g:8 * g + 8], s1, oh[:, g:g + 1], None, ALU.mult, None, [rk], [rk])
        TS("vector", Ef[:, 32 + 8 * g:40 + 8 * g], s2, oh[:, g:g + 1], None, ALU.mult, None, [rk], [rk])
    CP("vector", r_E[:, git, :], Ef, [rk], ["r_E"])
    Mb = sm[:, 160:192]
    TT("vector", Mb, Ef[:, 0:32], Ef[:, 32:64], ALU.add, [rk], [rk])
    Mbb = sm[:, 192:224].bitcast(BF16)[:, 0:32]
    CP("vector", Mbb, Mb, [rk], [rk])
    MM(pr[1][:, 0:32], utinc[:], Mbb, True, True, ["utinc", rk], [PR[1]])
    MM(pr[1][:, 32:64], ones_b[:], Mbb, True, True, ["ones_b", rk], [PR[1]])
    rank = sm[:, 224:256]
    TT("vector", rank, pr[1][:, 0:32], Mb, ALU.subtract, [PR[1], rk], [rk])
    TT("vector", rank, rank, cnt_run[:], ALU.add, [rk, "cnt_run"], [rk])
    TT("vector", cnt_run[:], cnt_run[:], pr[1][:, 32:64], ALU.add, [PR[1], "cnt_run"], ["cnt_run"])
    tmp = sm[:, 160:192]
    for k in range(2):
        TT("vector", tmp, Ef[:, 32 * k:32 * k + 32], rank, ALU.mult, [rk], [rk])
        RED(r_f[:, git, k:k + 1], tmp, ALU.add, [rk], ["r_f"])


_WNAMES = ["w_ada", "b_ada", "norm1_g", "w_in", "pool_w", "pool_scale", "conv_dw_w", "conv_dw_b",
           "conv_ln_g", "conv_ln_b", "conv_pw_w", "conv_pw_b", "fourier_w", "w_out", "norm2_g",
           "router_coarse_w", "router_coarse_b", "router_fine_w", "router_fine_b",
           "expert_w1", "expert_w3", "expert_w2", "final_g"]


def kernel(**inputs):
    xs_ = np.asarray(inputs["x_sample"], np.float32)
    xp_ = np.asarray(inputs["x_prompt"], np.float32)
    cs_ = np.asarray(inputs["c_sample"], np.float32)
    cp_ = np.asarray(inputs["c_prompt"], np.float32)
    S0, S1 = xs_.shape[1], xp_.shape[1]
    depth = inputs["w_ada"].shape[0]
    nc = build((S0, S1), depth)
    N = S0 + S1
    NB = -(-(2 * N + NE * 127) // 128)
    consts = make_consts((S0, S1), NB)
    wts = {k: np.ascontiguousarray(np.asarray(inputs[k], np.float32)) for k in _WNAMES}
    in_maps = []
    for core in range(8):
        b = core % 4
        m = {"x": np.ascontiguousarray(np.concatenate([xs_[b], xp_[b]], axis=0)),
             "c": np.ascontiguousarray(np.stack([cs_[b], cp_[b]], axis=0))}
        m.update(wts)
        m.update(consts)
        in_maps.append(m)
    res = run_bass_kernel_spmd(nc, in_maps, core_ids=list(range(8)))
    y_s = np.stack([res.results[b]["y"][:S0] for b in range(4)], axis=0)
    y_p = np.stack([res.results[b]["y"][S0:] for b in range(4)], axis=0)
    return (y_p.astype(np.float32), y_s.astype(np.float32))
```

```python
import math
import os
from contextlib import ExitStack

import numpy as np
import ml_dtypes
import concourse.bass as bass
import concourse.mybir as mybir
from concourse.bass_utils import run_bass_kernel_spmd

F32 = mybir.dt.float32
BF16 = mybir.dt.bfloat16
I32 = mybir.dt.int32
AF = mybir.ActivationFunctionType
ALU = mybir.AluOpType
AX = mybir.AxisListType

D = 2048
NG, EPG, NE = 4, 8, 32
DE = 512
CONV_K = 31
EPS = 1e-6
SAME_ENGINE_SYNC = True
CONV_PE = os.environ.get('KCONV', 'pe') == 'pe'
NOSYNC = set(os.environ.get('KNOSYNC', 'tensor').split(','))


class _Op:
    __slots__ = ("q", "fn", "deps", "sig", "sigval", "sem", "is_dma", "idx", "bar")


class Prog:
    def __init__(self, nc):
        self.nc = nc
        self.ops = []
        self.last_w = {}
        self.readers = {}
        self.dma_cnt = {}
        self.last_eng = {}
        self.bar = None
        self.bar_done = set()

    def barrier(self):
        deps = [(o, None) for o in self.last_eng.values()]
        dmas = dict(self.dma_cnt)
        self.bar = (deps, dmas)
        self.bar_done = set()

    def _add(self, q, fn, reads, writes, is_dma, semkey):
        o = _Op()
        o.q = q
        o.fn = fn
        o.is_dma = is_dma
        o.sig = False
        o.sigval = 0
        o.idx = len(self.ops)
        deps = {}
        for r in reads:
            w = self.last_w.get(r)
            if w is not None:
                deps[w.idx] = w
        for w_ in writes:
            w = self.last_w.get(w_)
            if w is not None:
                deps[w.idx] = w
            for rd in self.readers.get(w_, ()):
                deps[rd.idx] = rd
        o.deps = []
        for d in deps.values():
            if d.is_dma:
                o.deps.append((d, self.dma_cnt[d.sem]))
            else:
                o.deps.append((d, None))
        o.bar = None
        if self.bar is not None and q not in self.bar_done:
            self.bar_done.add(q)
            bdeps, bdmas = self.bar
            o.deps.extend(bdeps)
            o.bar = bdmas
        if is_dma:
            o.sem = ("dma", semkey)
            self.dma_cnt[o.sem] = self.dma_cnt.get(o.sem, 0) + 1
            o.sigval = self.dma_cnt[o.sem]
        else:
            o.sem = ("eng", q)
            self.last_eng[q] = o
        for w_ in writes:
            self.last_w[w_] = o
            self.readers[w_] = []
        for r in reads:
            if r not in writes:
                self.readers.setdefault(r, []).append(o)
        self.ops.append(o)
        return o

    def op(self, eng, fn, reads=(), writes=()):
        return self._add(eng, fn, tuple(reads), tuple(writes), False, None)

    def dma(self, q, fn, reads=(), writes=(), semkey=None):
        assert semkey is not None
        return self._add(q, fn, tuple(reads), tuple(writes), True, semkey)

    def emit(self):
        nc = self.nc
        ops = self.ops
        for o in ops:
            for d, _ in o.deps:
                if d.is_dma:
                    continue
                if d.q != o.q or (SAME_ENGINE_SYNC and d.q not in NOSYNC):
                    d.sig = True
        cnt = {}
        for o in ops:
            if not o.is_dma and o.sig:
                cnt[o.q] = cnt.get(o.q, 0) + 1
                o.sigval = cnt[o.q]
        semkeys = []
        seen = set()
        for o in ops:
            if (o.is_dma or o.sig) and o.sem not in seen:
                seen.add(o.sem)
                semkeys.append(o.sem)
        self.n_sems = len(semkeys)
        with ExitStack() as es:
            sems = {}
            for i, k in enumerate(semkeys):
                sems[k] = es.enter_context(nc.semaphore("s%d" % i))
            block = es.enter_context(nc.Block())
            queues = {}
            for o in ops:
                queues.setdefault(o.q, []).append(o)
            totals = dict(self.dma_cnt)

            def run_queue(qname, eng):
                waited = {}
                for o in queues.get(qname, ()):
                    need = {}
                    for d, n in o.deps:
                        if d.is_dma:
                            v = 16 * n
                        else:
                            if d.q == o.q and (d.q in NOSYNC or not SAME_ENGINE_SYNC):
                                continue
                            v = d.sigval
                        if v > need.get(d.sem, 0):
                            need[d.sem] = v
                    if o.bar is not None:
                        for k, n in o.bar.items():
                            if 16 * n > need.get(k, 0):
                                need[k] = 16 * n
                    for k, v in need.items():
                        if waited.get(k, 0) < v:
                            eng.wait_ge(sems[k], v)
                            waited[k] = v
                    ins = o.fn(eng)
                    if o.is_dma:
                        ins.then_inc(sems[o.sem], 16)
                    elif o.sig:
                        ins.then_inc(sems[o.sem], 1)
                if qname == "sync":
                    for k, n in totals.items():
                        if waited.get(k, 0) < 16 * n:
                            eng.wait_ge(sems[k], 16 * n)

            @block.sync
            def _(e):
                run_queue("sync", e)

            @block.scalar
            def _(e):
                run_queue("scalar", e)

            @block.gpsimd
            def _(e):
                run_queue("gpsimd", e)

            @block.vector
            def _(e):
                run_queue("vector", e)

            @block.tensor
            def _(e):
                run_queue("tensor", e)


def _bf(a):
    return np.ascontiguousarray(a.astype(ml_dtypes.bfloat16))


def make_consts(SEQS, NB):
    c = {}
    t = np.arange(128)
    ang = 2 * np.pi * np.outer(t, t) / 128.0
    cs = np.zeros((128, 2, 192), np.float64)
    for h in range(2):
        sl = slice(h * 64, h * 64 + 64)
        cs[:, h, 0:64] = np.cos(ang[:, sl])
        cs[:, h, 64:128] = np.sin(ang[:, sl])
        cs[:, h, 128:192] = -np.sin(ang[:, sl])
    c["cs128"] = _bf(cs)
    ccn = np.zeros((128, 2, 128), np.float64)
    ccn[:, 0] = np.cos(ang)
    ccn[:, 1] = -np.sin(ang)
    c["ccn"] = _bf(ccn)
    for q, S in enumerate(SEQS):
        S2 = S // 128
        a = 2 * np.pi * (np.outer(np.arange(S2), np.arange(S)) % S) / S
        f = np.zeros((S2, 2, S), np.float64)
        f[:, 0] = np.cos(a)
        f[:, 1] = np.sin(a)
        c["fcs%d" % q] = _bf(f)
    pc = np.zeros((128, 4, 16), np.float32)
    for g, w in enumerate((2, 4, 8, 16)):
        for j in range(8):
            cnt = min(j + w // 2, 10 ** 9) - max(j - w // 2, 0)
            pc[:, g, j] = 1.0 / cnt
        for j in range(8):
            tt = -8 + j
            hi = min(tt + w // 2, 0)
            lo = tt - w // 2
            pc[:, g, 8 + j] = 1.0 / (hi - lo)
    c["poolc"] = pc
    ut = (np.arange(128)[:, None] <= np.arange(128)[None, :]).astype(np.float32)
    c["utinc"] = _bf(ut)
    c["iotap"] = np.arange(128, dtype=np.float32).reshape(128, 1)
    c["bstart"] = np.tile((128.0 * np.arange(NB, dtype=np.float32))[None, :], (128, 1))
    c["iota32"] = np.tile(np.arange(32, dtype=np.float32)[None, :], (128, 1))
    return c


class Arena:
    def __init__(self, t, size):
        self.t = t
        self.size = size
        self.off = 0

    def reset(self):
        self.off = 0

    def alloc(self, n, align=16):
        self.off = (self.off + align - 1) // align * align
        o = self.off
        self.off += n
        assert self.off <= self.size, ("arena overflow", self.off, self.size)
        return self.t[:, o:o + n]


def build(SEQS=(8192, 2048), DEPTH=2, dbg=False, NL=None):
    NL = DEPTH if NL is None else NL
    N = sum(SEQS)
    NT = N // 128
    NB = -(-(2 * N + NE * 127) // 128)
    L = NB * 128
    OFFS = [0]
    for S in SEQS:
        OFFS.append(OFFS[-1] + S)
    nc = bass.Bass("TRN2", target_bir_lowering=False)

    def din(name, shape, dt=F32):
        return nc.dram_tensor(name, list(shape), dt, kind="ExternalInput").ap()

    def dscr(name, shape, dt):
        kind = "ExternalOutput" if dbg else "Internal"
        return nc.dram_tensor(name, list(shape), dt, kind=kind).ap()

    x_in = din("x", [N, D])
    c_in = din("c", [2, D])
    w_ada = din("w_ada", [DEPTH, D, 6 * D])
    b_ada = din("b_ada", [DEPTH, 6 * D])
    norm1_g = din("norm1_g", [DEPTH, D])
    w_in = din("w_in", [DEPTH, D, 3072])
    pool_w = din("pool_w", [DEPTH, 4, 128, 128])
    pool_scale = din("pool_scale", [DEPTH, 512])
    conv_dw_w = din("conv_dw_w", [DEPTH, CONV_K, 1024])
    conv_dw_b = din("conv_dw_b", [DEPTH, 1024])
    conv_ln_g = din("conv_ln_g", [DEPTH, 1024])
    conv_ln_b = din("conv_ln_b", [DEPTH, 1024])
    conv_pw_w = din("conv_pw_w", [DEPTH, 1024, 1024])
    conv_pw_b = din("conv_pw_b", [DEPTH, 1024])
    fourier_w = din("fourier_w", [DEPTH, 512, 512])
    w_out = din("w_out", [DEPTH, D, D])
    norm2_g = din("norm2_g", [DEPTH, D])
    rc_w = din("router_coarse_w", [DEPTH, D, NG])
    rc_b = din("router_coarse_b", [DEPTH, NG])
    rf_w = din("router_fine_w", [DEPTH, NG, D, EPG])
    rf_b = din("router_fine_b", [DEPTH, NG, EPG])
    e_w1 = din("expert_w1", [DEPTH, NE, D, DE])
    e_w3 = din("expert_w3", [DEPTH, NE, D, DE])
    e_w2 = din("expert_w2", [DEPTH, NE, DE, D])
    final_g = din("final_g", [D])
    k_cs128 = din("cs128", [128, 2, 192], BF16)
    k_ccn = din("ccn", [128, 2, 128], BF16)
    k_fcs = [din("fcs%d" % q, [S // 128, 2, S], BF16) for q, S in enumerate(SEQS)]
    k_poolc = din("poolc", [128, 4, 16])
    k_utinc = din("utinc", [128, 128], BF16)
    k_iotap = din("iotap", [128, 1])
    k_bstart = din("bstart", [128, NB])
    k_iota32 = din("iota32", [128, 32])

    y_out = nc.dram_tensor("y", [N, D], F32, kind="ExternalOutput").ap()

    x1_d = dscr("x1", [N, D], F32)
    x2_d = dscr("x2", [N, D], F32)
    h2_d = dscr("h2", [N, D], BF16)
    xs_d = dscr("xs", [L, D], BF16)
    ys_d = dscr("ys", [L, D], F32)
    mod_d = dscr("modrow", [2, 6 * D], F32)
    zaT = [dscr("zaT%d" % q, [512, S], BF16) for q, S in enumerate(SEQS)]
    vT = [dscr("vT%d" % q, [1024, S], BF16) for q, S in enumerate(SEQS)]
    zcT = [dscr("zcT%d" % q, [512, S], BF16) for q, S in enumerate(SEQS)]
    rzT = [dscr("rzT%d" % q, [512, S], BF16) for q, S in enumerate(SEQS)]
    mixT = [dscr("mixT%d" % q, [2048, S], BF16) for q, S in enumerate(SEQS)]

    P = Prog(nc)
    es = ExitStack()
    with es:
        AR_SZ = 96 * 1024
        art = es.enter_context(nc.sbuf_tensor("arena", [128, AR_SZ], BF16))
        AR = Arena(art, AR_SZ)

        class _A16:
            def alloc(self, n):
                return AR.alloc(n)

            def reset(self):
                AR.reset()

        class _A32:
            def alloc(self, n):
                return AR.alloc(2 * n).bitcast(F32)

            def reset(self):
                pass

        A16 = _A16()
        A32 = _A32()
        ident_f = es.enter_context(nc.sbuf_tensor("ident_f", [128, 128], F32))
        ident_b = es.enter_context(nc.sbuf_tensor("ident_b", [128, 128], BF16))
        ones_b = es.enter_context(nc.sbuf_tensor("ones_b", [128, 128], BF16))
        utinc = es.enter_context(nc.sbuf_tensor("utinc_s", [128, 128], BF16))
        iotap = es.enter_context(nc.sbuf_tensor("iotap_s", [128, 1], F32))
        iota32 = es.enter_context(nc.sbuf_tensor("iota32_s", [128, 32], F32))
        eps_t = es.enter_context(nc.sbuf_tensor("eps_t", [128, 1], F32))
        r_E = es.enter_context(nc.sbuf_tensor("r_E", [128, NT, 64], BF16))
        r_f = es.enter_context(nc.sbuf_tensor("r_f", [128, NT, 4], F32))
        r_slot = es.enter_context(nc.sbuf_tensor("r_slot", [128, NT, 2], I32))
        cnt_run = es.enter_context(nc.sbuf_tensor("cnt_run", [128, 32], F32))
        widx = es.enter_context(nc.sbuf_tensor("widx", [128, NB], I32))
        pm = [es.enter_context(nc.psum_tensor("pm%d" % i, [128, 512], F32)) for i in range(4)]
        pt = es.enter_context(nc.psum_tensor("pt", [128, 2048], BF16))
        pr = [es.enter_context(nc.psum_tensor("pr%d" % i, [128, 512], F32)) for i in range(2)]
        PM = [("pm", i) for i in range(4)]
        PT = "pt"
        PR = [("pr", i) for i in range(2)]

        STQ = os.environ.get('KSTQ', 'sync')
        uid = [0]
        _bregs = {}

        def BR(e, val):
            if val not in _bregs:
                _bregs[val] = e.to_reg(val)
            return _bregs[val]

        def U(name):
            uid[0] += 1
            return (name, uid[0])

        def t16(n, shape=None):
            v = A16.alloc(n)
            return v

        def DMA(q, out, in_, reads, writes, semkey, **kw):
            P.dma(q, lambda e: e.dma_start(out=out, in_=in_, **kw), reads, writes, semkey)

        def MM(out, lhsT, rhs, start, stop, reads, writes):
            P.op("tensor", lambda e: e.matmul(out, lhsT=lhsT, rhs=rhs, start=start, stop=stop), reads, writes)

        def TR(out, in_, ident, reads, writes):
            P.op("tensor", lambda e: e.transpose(out=out, in_=in_, identity=ident), reads, writes)

        def ACT(out, in_, func, reads, writes, bias=None, scale=None, accum_out=None, eng="scalar"):
            kw = {}
            if bias is not None:
                kw["bias"] = bias
            if scale is not None:
                kw["scale"] = scale
            if accum_out is not None:
                kw["accum_out"] = accum_out
            P.op("scalar", lambda e: e.activation(out=out, in_=in_, func=func, **kw), reads, writes)

        def TT(eng, out, in0, in1, op, reads, writes):
            P.op(eng, lambda e: e.tensor_tensor(out=out, in0=in0, in1=in1, op=op), reads, writes)

        def TS(eng, out, in0, s1, s2, op0, op1, reads, writes, accum_out=None):
            if op1 is None:
                P.op(eng, lambda e: e.tensor_scalar(out=out, in0=in0, scalar1=s1, scalar2=None, op0=op0), reads, writes)
            elif accum_out is not None:
                P.op(eng, lambda e: e.tensor_scalar(out=out, in0=in0, scalar1=s1, scalar2=s2, op0=op0, op1=op1, accum_out=accum_out), reads, writes)
            else:
                P.op(eng, lambda e: e.tensor_scalar(out=out, in0=in0, scalar1=s1, scalar2=s2, op0=op0, op1=op1), reads, writes)

        def STT(out, in0, scalar, in1, op0, op1, reads, writes):
            P.op("vector", lambda e: e.scalar_tensor_tensor(out=out, in0=in0, scalar=scalar, in1=in1, op0=op0, op1=op1), reads, writes)

        def CP(eng, out, in_, reads, writes):
            if eng == "scalar":
                P.op("scalar", lambda e: e.copy(out=out, in_=in_), reads, writes)
            else:
                P.op(eng, lambda e: e.tensor_copy(out=out, in_=in_), reads, writes)

        def MEMSET(eng, ap, val, writes):
            P.op(eng, lambda e: e.memset(ap, val), (), writes)

        def RED(out, in_, op, reads, writes, axis=AX.X):
            P.op("vector", lambda e: e.tensor_reduce(out=out, in_=in_, axis=axis, op=op), reads, writes)

        def bcast_row(dram_row_ap, n):
            return dram_row_ap.partition_broadcast(128)

        MEMSET("gpsimd", ident_f[:], 0.0, ["ident_f"])
        P.op("gpsimd", lambda e: e.affine_select(out=ident_f[:], in_=ident_f[:], pattern=[[-1, 128]],
                                                 compare_op=ALU.not_equal, fill=1.0, base=0, channel_multiplier=1),
             ["ident_f"], ["ident_f"])
        CP("vector", ident_b[:], ident_f[:], ["ident_f"], ["ident_b"])
        MEMSET("gpsimd", ones_b[:], 1.0, ["ones_b"])
        MEMSET("gpsimd", eps_t[:], EPS, ["eps_t"])
        DMA("sync", utinc[:], k_utinc, [], ["utinc"], "c_utinc")
        DMA("sync", iotap[:], k_iotap, [], ["iotap"], "c_iotap")
        DMA("sync", iota32[:], k_iota32, [], ["iota32"], "c_iota32")

        class _Stop(Exception):
            pass

        def stop_here(tag):
            if os.environ.get("KSTOP") == tag:
                P.emit()
                raise _Stop()

        cur_x = x_in
        try:
          for l in range(NL):
              last = (l == NL - 1)
              nxt_x = y_out if False else x2_d
              P.barrier()
              A16.reset(); A32.reset()
              cst = A32.alloc(2 * 16).rearrange("p (q k) -> p q k", q=2)
              csb = A16.alloc(16 * 2).rearrange("p (k q) -> p k q", q=2)
              brow = [A32.alloc(512) for _ in range(2)]
              mrow = A32.alloc(512)
              DMA("sync", cst, c_in.rearrange("q (p k) -> p q k", k=16), [], ["cst"], "cst")
              for q in range(2):
                  ACT(csb[:, :, q], cst[:, q, :], AF.Silu, ["cst"], [("csb", q)])
              wa = [A16.alloc(16 * 512).rearrange("p (k n) -> p k n", k=16) for _ in range(2)]
              for cb in range(24):
                  wbuf = wa[cb % 2]
                  wk = ("wa", cb % 2)
                  DMA("gpsimd", wbuf, w_ada[l, :, cb * 512:(cb + 1) * 512].rearrange("(p k) n -> p k n", k=16),
                      [], [wk], "wa%d" % (cb % 2))
                  pk = PM[cb % 2]
                  pst = pm[cb % 2]
                  for kc in range(16):
                      MM(pst[0:2, :], csb[:, kc, :], wbuf[:, kc, :], kc == 0, kc == 15,
                         [wk, ("csb", 0), ("csb", 1)], [pk])
                  mk = U("mrow")
                  bk = ("brow", cb % 2)
                  DMA("sync", brow[cb % 2][0:2, :], b_ada[l:l + 1, cb * 512:(cb + 1) * 512].broadcast_to([2, 512]),
                      [], [bk], "brow%d" % (cb % 2))
                  TT("vector", mrow[0:2, :], pst[0:2, :], brow[cb % 2][0:2, :], ALU.add,
                     [pk, bk], ["mrow"])
                  DMA(STQ, mod_d[:, cb * 512:(cb + 1) * 512], mrow[0:2, :], ["mrow"], [("mod", cb)], "mrow")
              MODK = [("mod", cb) for cb in range(24)]

              def mod_rep(q, i):
                  return mod_d[q:q + 1, i * D:(i + 1) * D].broadcast_to([128, D])

              for q, S in enumerate(SEQS):
                  T0 = OFFS[q]
                  NT5 = S // 512
                  S2 = S // 128
                  P.barrier()
                  A16.reset(); A32.reset()
                  wi = A16.alloc(16 * 3072).rearrange("p (k n) -> p k n", k=16)
                  DMA("gpsimd", wi, w_in[l].rearrange("(p k) n -> p k n", k=16), [], ["wi"], "wi")
                  a1 = A32.alloc(D)
                  sh1 = A32.alloc(D)
                  tmpf = A32.alloc(D)
                  DMA("sync", a1, mod_rep(q, 1), MODK, ["a1"], "a1")
                  DMA("sync", sh1, mod_rep(q, 0), MODK, ["sh1"], "sh1")
                  DMA("sync", tmpf, norm1_g[l:l + 1, :].broadcast_to([128, D]), [], ["tmpf"], "g_rep")
                  STT(a1, a1, 1.0, tmpf, ALU.add, ALU.mult, ["a1", "tmpf"], ["a1"])
                  xt = [A32.alloc(D) for _ in range(2)]
                  hb = [A16.alloc(D) for _ in range(4)]
                  hT = [A16.alloc(16 * 512).rearrange("p (k t) -> p k t", k=16) for _ in range(1)]
                  stg = [A16.alloc(16 * 512).rearrange("p (j t) -> p j t", j=16) for _ in range(1)]
                  sgt = [A32.alloc(512) for _ in range(2)]
                  ss = A32.alloc(8)
                  hTt = hT[0]
                  hk = ("hT", 0)

                  def p1_norm(ti):
                      for sub in range(4):
                          it = ti * 4 + sub
                          b2 = it % 2
                          r0 = T0 + it * 128
                          xk = ("xt", b2)
                          DMA("sync", xt[b2], cur_x[r0:r0 + 128, :], [("xcur", r0 // 128)], [xk], "xt%d" % b2)
                          ssk = U("ss")
                          hbk = ("hb", sub)
                          ACT(hb[sub], xt[b2], AF.Square, [xk], [hbk, ssk], accum_out=ss[:, 0:1])
                          ACT(ss[:, 1:2], ss[:, 0:1], AF.Sqrt, [ssk, "eps_t"], [ssk], scale=1.0 / D, bias=eps_t[:, 0:1])
                          P.op("vector", lambda e, ss=ss: e.reciprocal(out=ss[:, 2:3], in_=ss[:, 1:2]), [ssk], [ssk])
                          STT(tmpf, xt[b2], ss[:, 2:3], a1, ALU.mult, ALU.mult, [xk, ssk, "a1"], ["tmpf"])
                          TT("gpsimd", hb[sub], tmpf, sh1, ALU.add, ["tmpf", "sh1"], [hbk])

                  def p1_tr(ti):
                      for sub in range(4):
                          hbk = ("hb", sub)
                          for kc in range(16):
                              TR(pt[:, kc * 128:(kc + 1) * 128], hb[sub].rearrange("p (c k) -> p k c", k=16)[:, kc, :],
                                 ident_b[:], [hbk, "ident_b"], [PT])
                          CP("scalar" if sub % 2 else "vector", hTt[:, :, sub * 128:(sub + 1) * 128],
                             pt[:, :].rearrange("p (k t) -> p k t", k=16), [PT], [hk])

                  def p1_mm(ti):
                      st = stg[0]
                      sk = ("stg", 0)
                      c0 = ti * 512

                      def mmgrp(j, pi):
                          for kc in range(16):
                              MM(pm[pi][:, :], wi[:, kc, j * 128:(j + 1) * 128], hTt[:, kc, :], kc == 0, kc == 15,
                                 ["wi", hk], [PM[pi]])

                      pi = 0
                      for j in range(4):
                          mmgrp(j, pi)
                          CP("scalar", st[:, j, :], pm[pi][:, :], [PM[pi]], [sk])
                          pi = (pi + 1) % 4
                      for j in range(8):
                          pa = pi
                          mmgrp(4 + j, pa)
                          pb = (pi + 1) % 4
                          mmgrp(12 + j, pb)
                          sg = sgt[j % 2]
                          sgk = ("sg", j % 2)
                          ACT(sg, pm[pb][:, :], AF.Sigmoid, [PM[pb]], [sgk])
                          TT("vector", st[:, 4 + j, :], pm[pa][:, :], sg, ALU.mult, [PM[pa], sgk], [sk])
                          pi = (pi + 2) % 4
                      for j in range(4):
                          mmgrp(20 + j, pi)
                          CP("scalar", st[:, 12 + j, :], pm[pi][:, :], [PM[pi]], [sk])
                          pi = (pi + 1) % 4
                      DMA(STQ, zaT[q][:, c0:c0 + 512].rearrange("(j p) t -> p j t", p=128), st[:, 0:4, :],
                          [sk], [("zaT", q, ti)], "stg0")
                      DMA(STQ, vT[q][:, c0:c0 + 512].rearrange("(j p) t -> p j t", p=128), st[:, 4:12, :],
                          [sk], [("vT", q, ti)], "stg0")
                      DMA(STQ, zcT[q][:, c0:c0 + 512].rearrange("(j p) t -> p j t", p=128), st[:, 12:16, :],
                          [sk], [("zcT", q, ti)], "stg0")

                  p1_norm(0)
                  p1_tr(0)
                  for ti in range(NT5):
                      if ti + 1 < NT5:
                          p1_norm(ti + 1)
                      p1_mm(ti)
                      if ti + 1 < NT5:
                          p1_tr(ti + 1)

                  stop_here('P1')
                  P.barrier()
                  A16.reset(); A32.reset()
                  pwf = A32.alloc(4 * 128).rearrange("p (g e) -> p g e", g=4)
                  pwb = A16.alloc(4 * 128).rearrange("p (g e) -> p g e", g=4)
                  DMA("sync", pwf, pool_w[l].rearrange("g c e -> c g e"), [], ["pwf"], "pwf")
                  CP("vector", pwb, pwf, ["pwf"], ["pwb"])
                  psc = A32.alloc(4)
                  pscr = A32.alloc(4 * 128).rearrange("p (g e) -> p g e", g=4)[0:4]
                  DMA("sync", pscr[:, 0, :], pool_scale[l].rearrange("(g e) -> g e", g=4), [], ["pscr"], "pscr")
                  TR(pr[0][:, 0:4], pscr[:, 0, :], ident_f[0:4, 0:4], ["pscr", "ident_f"], [PR[0]])
                  CP("vector", psc, pr[0][:, 0:4], [PR[0]], ["psc"])
                  pcst = A32.alloc(64).rearrange("p (g j) -> p g j", g=4)
                  DMA("sync", pcst, k_poolc, [], ["pcst"], "pcst")
                  ub = [A16.alloc(528) for _ in range(2)]
                  sa = [A32.alloc(528) for _ in range(2)]
                  sb_ = [A32.alloc(528) for _ in range(2)]
                  dmt = [A16.alloc(512) for _ in range(2)]
                  pst2 = [A16.alloc(4 * 512).rearrange("p (g t) -> p g t", g=4) for _ in range(2)]
                  it = 0
                  for ti in range(NT5):
                      c0 = ti * 512
                      st = pst2[ti % 2]
                      sk = ("pst2", ti % 2)
                      for g, w in enumerate((2, 4, 8, 16)):
                          b2 = it % 2
                          it += 1
                          u = ub[b2]
                          uk = ("ub", b2)
                          lo = max(c0 - 8, 0)
                          hi = min(c0 + 520, S)
                          rd = [("zaT", q, tj) for tj in range(max(ti - 1, 0), min(ti + 2, NT5))]
                          if lo > c0 - 8:
                              MEMSET("gpsimd", u[:, 0:8], 0.0, [uk])
                          if hi < c0 + 520:
                              MEMSET("gpsimd", u[:, 520:528], 0.0, [uk])
                          DMA("sync", u[:, lo - (c0 - 8):hi - (c0 - 8)], zaT[q][g * 128:(g + 1) * 128, lo:hi], rd, [uk], "ub%d" % b2)
                          s_a, s_b = sa[b2], sb_[b2]
                          ka, kb = ("sa", b2), ("sb", b2)
                          TT("vector", s_a[:, 1:528], u[:, 0:527], u[:, 1:528], ALU.add, [uk], [ka])
                          cur, curk, oth, othk = s_a, ka, s_b, kb
                          lo_v = 1
                          hi_v = 528
                          step = 1
                          ww = 2
                          while ww < w:
                              nlo = lo_v + step
                              nhi = hi_v - step
                              TT("vector", oth[:, nlo:nhi], cur[:, nlo - step:nhi - step], cur[:, nlo + step:nhi + step],
                                 ALU.add, [curk], [othk])
                              cur, curk, oth, othk = oth, othk, cur, curk
                              lo_v, hi_v = nlo, nhi
                              step *= 2
                              ww *= 2
                          dm = dmt[b2]
                          dk_ = ("dm", b2)
                          STT(dm, cur[:, 8:520], 1.0 / w, u[:, 8:520], ALU.mult, ALU.subtract, [curk, uk], [dk_])
                          if ti == 0:
                              TT("vector", oth[:, 8:16], cur[:, 8:16], pcst[:, g, 0:8], ALU.mult, [curk, "pcst"], [othk])
                              TT("vector", dm[:, 0:8], oth[:, 8:16], u[:, 8:16], ALU.subtract, [othk, uk], [dk_])
                          if ti == NT5 - 1:
                              TT("vector", oth[:, 512:520], cur[:, 512:520], pcst[:, g, 8:16], ALU.mult, [curk, "pcst"], [othk])
                              TT("vector", dm[:, 504:512], oth[:, 512:520], u[:, 512:520], ALU.subtract, [othk, uk], [dk_])
                          pi = g
                          MM(pm[pi][:, :], pwb[:, g, :], dm, True, True, ["pwb", dk_], [PM[pi]])
                          ACT(st[:, g, :], pm[pi][:, :], AF.Copy, [PM[pi], "psc"], [sk], scale=psc[:, g:g + 1])
                      DMA(STQ, mixT[q][0:512, c0:c0 + 512].rearrange("(g p) t -> p g t", p=128), st, [sk],
                          [("mixT", q, ti, 0)], "pst2%d" % (ti % 2))

                  P.barrier()
                  A16.reset(); A32.reset()
                  pww = A16.alloc(8 * 1024).rearrange("p (k n) -> p k n", k=8)
                  DMA("gpsimd", pww, conv_pw_w[l].rearrange("(k p) n -> p k n", p=128), [], ["pww"], "pww")
                  dwr = A32.alloc(1024)
                  DMA("sync", dwr[0:CONV_K, :], conv_dw_w[l], [], ["dwr"], "dwr")
                  dwT = A32.alloc(8 * 32).rearrange("p (j k) -> p j k", j=8)
                  for j in range(8):
                      TR(pr[0][:, j * 32:j * 32 + CONV_K], dwr[0:CONV_K, j * 128:(j + 1) * 128],
                         ident_f[0:CONV_K, 0:CONV_K], ["dwr", "ident_f"], [PR[0]])
                  CP("vector", dwT[:, :, 0:CONV_K], pr[0][:, 0:256].rearrange("p (j k) -> p j k", j=8)[:, :, 0:CONV_K],
                     [PR[0]], ["dwT"])
                  vr = A32.alloc(4 * 1024).rearrange("p (v n) -> p v n", v=4)
                  vecs = A32.alloc(32).rearrange("p (v j) -> p v j", v=4)
                  for vi, src in enumerate((conv_dw_b, conv_ln_g, conv_ln_b, conv_pw_b)):
                      DMA("sync", vr[0:8, vi, 0:128], src[l].rearrange("(j p) -> j p", p=128), [], [("vr", vi)], "vr%d" % vi)
                      TR(pr[1][:, vi * 8:vi * 8 + 8], vr[0:8, vi, 0:128], ident_f[0:8, 0:8], [("vr", vi), "ident_f"], [PR[1]])
                  CP("vector", vecs, pr[1][:, 0:32].rearrange("p (v j) -> p v j", v=4), [PR[1]], ["vecs"])
                  acc = [A32.alloc(512) for _ in range(8)]
                  cbf = [A16.alloc(512) for _ in range(2)]
                  sqb = [A16.alloc(512) for _ in range(2)]
                  sT = [A16.alloc(512) for _ in range(8)]
                  mean = A32.alloc(512)
                  var = A32.alloc(512)
                  rstd = A32.alloc(512)
                  xn = [A32.alloc(512) for _ in range(2)]
                  st3 = [A16.alloc(8 * 512).rearrange("p (j t) -> p j t", j=8) for _ in range(2)]
                  if CONV_PE:
                      vb = [A16.alloc(544) for _ in range(3)]
                      vb1 = [A16.alloc(544) for _ in range(3)]
                      dg = A16.alloc(8 * 32 * 128).rearrange("p (j k c) -> p j k c", j=8, k=32)
                      for j in range(8):
                          for k in range(CONV_K):
                              TS("vector" if (j * CONV_K + k) % 2 else "gpsimd", dg[:, j, k, :], ident_b[:, :], dwT[:, j, k:k + 1], None,
                                 ALU.mult, None, ["ident_b", "dwT"], [("dg", j)])
                  else:
                      vb = [A16.alloc(544) for _ in range(2)]
                  it = 0
                  for ti in range(NT5):
                      c0 = ti * 512
                      for j in range(8):
                          if CONV_PE:
                              b2 = it % 2
                              b3 = it % 3
                              it += 1
                              v, vk = vb[b3], ("vb", b3)
                              v1, v1k = vb1[b3], ("vb1", b3)
                              lo = max(c0 - 15, 0)
                              hi = min(c0 + 527, S)
                              rd = [("vT", q, tj) for tj in range(max(ti - 1, 0), min(ti + 2, NT5))]
                              if lo > c0 - 15:
                                  MEMSET("gpsimd", v[:, 0:16], 0.0, [vk])
                                  MEMSET("gpsimd", v1[:, 0:16], 0.0, [v1k])
                              if hi < c0 + 527:
                                  MEMSET("gpsimd", v[:, 526:544], 0.0, [vk])
                                  MEMSET("gpsimd", v1[:, 526:544], 0.0, [v1k])
                              DMA("sync", v[:, lo - (c0 - 15):hi - (c0 - 15)], vT[q][j * 128:(j + 1) * 128, lo:hi], rd, [vk], "vb%d" % b3)
                              lo1 = max(c0 - 14, 0)
                              DMA("sync", v1[:, lo1 - (c0 - 14):hi - (c0 - 14)], vT[q][j * 128:(j + 1) * 128, lo1:hi], rd, [v1k], "vb1%d" % b3)
                              pi = it % 4
                              for k in range(CONV_K):
                                  if k % 2 == 0:
                                      MM(pm[pi][:, :], dg[:, j, k, :], v[:, k:k + 512], k == 0, k == CONV_K - 1,
                                         [("dg", j), vk], [PM[pi]])
                                  else:
                                      MM(pm[pi][:, :], dg[:, j, k, :], v1[:, k - 1:k - 1 + 512], k == 0, k == CONV_K - 1,
                                         [("dg", j), v1k], [PM[pi]])
                              a = acc[j]
                              ak = ("acc", j)
                              ACT(a, pm[pi][:, :], AF.Identity, [PM[pi], "vecs"], [ak], bias=vecs[:, 0, j:j + 1])
                              cb_, ck = cbf[b2], ("cbf", b2)
                              sq_, sqk = sqb[b2], ("sqb", b2)
                              CP("vector", cb_, a, [ak], [ck])
                              ACT(sq_, a, AF.Square, [ak], [sqk])
                              MM(pr[0][:, :], ones_b[:], cb_, j == 0, j == 7, ["ones_b", ck], [PR[0]])
                              MM(pr[1][:, :], ones_b[:], sq_, j == 0, j == 7, ["ones_b", sqk], [PR[1]])
                          else:
                              b2 = it % 2
                              it += 1
                              v = vb[b2]
                              vk = ("vb", b2)
                              lo = max(c0 - 15, 0)
                              hi = min(c0 + 527, S)
                              rd = [("vT", q, tj) for tj in range(max(ti - 1, 0), min(ti + 2, NT5))]
                              if lo > c0 - 15:
                                  MEMSET("gpsimd", v[:, 0:15], 0.0, [vk])
                              if hi < c0 + 527:
                                  MEMSET("gpsimd", v[:, 527:542], 0.0, [vk])
                              DMA("sync", v[:, lo - (c0 - 15):hi - (c0 - 15)], vT[q][j * 128:(j + 1) * 128, lo:hi], rd, [vk], "vb%d" % b2)
                              a = acc[j]
                              ak = ("acc", j)
                              TS("vector", a, v[:, 0:512], dwT[:, j, 0:1], vecs[:, 0, j:j + 1], ALU.mult, ALU.add,
                                 [vk, "dwT", "vecs"], [ak])
                              for k in range(1, CONV_K):
                                  STT(a, v[:, k:k + 512], dwT[:, j, k:k + 1], a, ALU.mult, ALU.add, [vk, "dwT", ak], [ak])
                              cb_, ck = cbf[b2], ("cbf", b2)
                              sq_, sqk = sqb[b2], ("sqb", b2)
                              CP("gpsimd", cb_, a, [ak], [ck])
                              ACT(sq_, a, AF.Square, [ak], [sqk])
                              MM(pr[0][:, :], ones_b[:], cb_, j == 0, j == 7, ["ones_b", ck], [PR[0]])
                              MM(pr[1][:, :], ones_b[:], sq_, j == 0, j == 7, ["ones_b", sqk], [PR[1]])
                      TS("vector", mean, pr[0][:, :], 1.0 / 1024, None, ALU.mult, None, [PR[0]], ["mean"])
                      TS("vector", var, pr[1][:, :], 1.0 / 1024, None, ALU.mult, None, [PR[1]], ["var"])
                      TT("vector", rstd, mean, mean, ALU.mult, ["mean"], ["rstd"])
                      TT("vector", var, var, rstd, ALU.subtract, ["var", "rstd"], ["var"])
                      ACT(var, var, AF.Sqrt, ["var", "eps_t"], ["var"], bias=eps_t[:, 0:1], scale=1.0)
                      P.op("vector", lambda e, rstd=rstd, var=var: e.reciprocal(out=rstd, in_=var), ["var"], ["rstd"])
                      for j in range(8):
                          x_, xk_ = xn[j % 2], ("xn", j % 2)
                          TT("gpsimd", x_, acc[j], mean, ALU.subtract, [("acc", j), "mean"], [xk_])
                          TT("vector", x_, x_, rstd, ALU.mult, [xk_, "rstd"], [xk_])
                          ACT(sT[j], x_, AF.Silu, [xk_, "vecs"], [("sT", j)], scale=vecs[:, 1, j:j + 1], bias=vecs[:, 2, j:j + 1])
                      st = st3[ti % 2]
                      sk = ("st3", ti % 2)
                      for e_ in range(8):
                          pi = e_ % 4
                          for j in range(8):
                              MM(pm[pi][:, :], pww[:, j, e_ * 128:(e_ + 1) * 128], sT[j], j == 0, j == 7,
                                 ["pww", ("sT", j)], [PM[pi]])
                          ACT(st[:, e_, :], pm[pi][:, :], AF.Identity, [PM[pi], "vecs"], [sk], bias=vecs[:, 3, e_:e_ + 1])
                      DMA(STQ, mixT[q][512:1536, c0:c0 + 512].rearrange("(j p) t -> p j t", p=128), st, [sk],
                          [("mixT", q, ti, 1)], "st3%d" % (ti % 2))

                  stop_here('P3')
                  P.barrier()
                  A16.reset(); A32.reset()
                  cs = A16.alloc(2 * 192).rearrange("p (h n) -> p h n", h=2)
                  ccn = A16.alloc(2 * 128).rearrange("p (h n) -> p h n", h=2)
                  fcs = A16.alloc(2 * S).rearrange("p (h n) -> p h n", h=2)
                  DMA("sync", cs, k_cs128, [], ["cs"], "cs")
                  DMA("sync", ccn, k_ccn, [], ["ccn"], "ccn")
                  DMA("sync", fcs[0:S2], k_fcs[q], [], ["fcs"], "fcs")
                  fwf = A32.alloc(4 * 512).rearrange("p (h e) -> p h e", h=4)
                  fwb = A16.alloc(4 * 512).rearrange("p (h e) -> p h e", h=4)
                  DMA("sync", fwf, fourier_w[l].rearrange("(h m) e -> m h e", h=4), [], ["fwf"], "fwf")
                  CP("vector", fwb, fwf, ["fwf"], ["fwb"])
                  Ut = A16.alloc(128 * S2).rearrange("p (c t) -> p c t", c=128)
                  Ah = A16.alloc(128 * 192).rearrange("p (c n) -> p c n", c=128)
                  Xh = A16.alloc(2 * S).rearrange("p (h k) -> p h k", h=2)
                  rst = [A16.alloc(512) for _ in range(2)]
                  ALLZC = [("zcT", q, tj) for tj in range(NT5)]
                  nrm = 1.0 / math.sqrt(S * 128.0)
                  ri = 0
                  for h in range(4):
                      for cq in range(8):
                          DMA("sync", Ut[:, cq * 16:(cq + 1) * 16, :],
                              zcT[q][h * 128 + cq * 16:h * 128 + (cq + 1) * 16, :].rearrange("c (a b) -> a c b", b=S2),
                              ALLZC, ["Ut"], "Ut")
                      for half in range(2):
                          pmv = [pm[i] for i in range(4)]
                          for cg in range(16):
                              for ci in range(8):
                                  c_ = cg * 8 + ci
                                  bank = ci // 2
                                  col = (ci % 2) * 192
                                  MM(pmv[bank][0:S2, col:col + 192], Ut[:, c_, :], cs[:, half, :], True, True,
                                     ["Ut", "cs"], [PM[bank]])
                              for bank in range(4):
                                  eng = "vector" if bank % 2 == 0 else "scalar"
                                  CP(eng, Ah[0:S2, cg * 8 + bank * 2:cg * 8 + bank * 2 + 2, :],
                                     pmv[bank][0:S2, 0:384].rearrange("p (c n) -> p c n", c=2), [PM[bank]], [("Ah", cg)])
                          AHK = [("Ah", cg) for cg in range(16)]
                          G = 512 // S2
                          G = min(G, 64)
                          for kg in range(64 // G):
                              pxr, pxi = pr[0], pr[1]
                              for gi in range(G):
                                  k1l = kg * G + gi
                                  k1 = half * 64 + k1l
                                  fc_ = fcs[0:S2, 0, :].rearrange("p (b a) -> p a b", a=128)[:, k1, :]
                                  fs_ = fcs[0:S2, 1, :].rearrange("p (b a) -> p a b", a=128)[:, k1, :]
                                  ar = Ah[0:S2, :, k1l]
                                  ai = Ah[0:S2, :, 64 + k1l]
                                  an = Ah[0:S2, :, 128 + k1l]
                                  o = gi * S2
                                  MM(pxr[:, o:o + S2], ar, fc_, True, False, AHK + ["fcs"], [PR[0]])
                                  MM(pxr[:, o:o + S2], an, fs_, False, True, AHK + ["fcs"], [PR[0]])
                                  MM(pxi[:, o:o + S2], ar, fs_, True, False, AHK + ["fcs"], [PR[1]])
                                  MM(pxi[:, o:o + S2], ai, fc_, False, True, AHK + ["fcs"], [PR[1]])
                              k1b = half * 64 + kg * G
                              for ri_, px in enumerate((pxr, pxi)):
                                  dst = Xh[:, ri_, :].rearrange("p (b a) -> p a b", a=128)[:, k1b:k1b + G, :]
                                  src = px[:, 0:G * S2].rearrange("p (g b) -> p g b", g=G)
                                  CP("vector" if ri_ == 0 else "scalar", dst, src, [PR[ri_]], [("Xh", half, kg)])
                      XHK = [("Xh", hf, kg) for hf in range(2) for kg in range(64 // G)]
                      for kt in range(NT5):
                          pi = kt % 4
                          MM(pm[pi][:, :], ccn[:, 0, :], Xh[:, 0, kt * 512:(kt + 1) * 512], True, False, ["ccn"] + XHK, [PM[pi]])
                          MM(pm[pi][:, :], ccn[:, 1, :], Xh[:, 1, kt * 512:(kt + 1) * 512], False, True, ["ccn"] + XHK, [PM[pi]])
                          r_ = rst[ri % 2]
                          rk = ("rst", ri % 2)
                          ACT(r_, pm[pi][:, :], AF.Copy, [PM[pi]], [rk], scale=nrm)
                          DMA(STQ, rzT[q][h * 128:(h + 1) * 128, kt * 512:(kt + 1) * 512], r_, [rk], [("rzT", q, h, kt)],
                              "rst%d" % (ri % 2))
                          ri += 1
                  rzb = [A16.alloc(4 * 512).rearrange("p (h t) -> p h t", h=4) for _ in range(2)]
                  st4 = [A16.alloc(4 * 512).rearrange("p (e t) -> p e t", e=4) for _ in range(2)]
                  for ti in range(NT5):
                      c0 = ti * 512
                      rb, rbk = rzb[ti % 2], ("rzb", ti % 2)
                      DMA("sync", rb, rzT[q][:, c0:c0 + 512].rearrange("(h m) t -> m h t", h=4),
                          [("rzT", q, h, ti) for h in range(4)], [rbk], "rzb%d" % (ti % 2))
                      st, sk = st4[ti % 2], ("st4", ti % 2)
                      for e_ in range(4):
                          pi = e_
                          for h in range(4):
                              MM(pm[pi][:, :], fwb[:, h, e_ * 128:(e_ + 1) * 128], rb[:, h, :], h == 0, h == 3,
                                 ["fwb", rbk], [PM[pi]])
                          CP("scalar" if e_ % 2 else "vector", st[:, e_, :], pm[pi][:, :], [PM[pi]], [sk])
                      DMA(STQ, mixT[q][1536:2048, c0:c0 + 512].rearrange("(e p) t -> p e t", p=128), st, [sk],
                          [("mixT", q, ti, 2)], "st4%d" % (ti % 2))

                  stop_here('P4')
                  P.barrier()
                  A16.reset(); A32.reset()
                  wo = A16.alloc(16 * 2048).rearrange("p (k n) -> p k n", k=16)
                  DMA("gpsimd", wo, w_out[l].rearrange("(k p) n -> p k n", p=128), [], ["wo"], "wo")
                  g1 = A32.alloc(D)
                  a2 = A32.alloc(D)
                  sh2 = A32.alloc(D)
                  tmpf = A32.alloc(D)
                  TFK = [("tmpf", i) for i in range(4)]
                  DMA("sync", g1, mod_rep(q, 2), MODK, ["g1"], "g1")
                  DMA("sync", a2, mod_rep(q, 4), MODK, ["a2"], "a2")
                  DMA("sync", sh2, mod_rep(q, 3), MODK, ["sh2"], "sh2")
                  DMA("sync", tmpf, norm2_g[l:l + 1, :].broadcast_to([128, D]), [], TFK, "g_rep")
                  STT(a2, a2, 1.0, tmpf, ALU.add, ALU.mult, ["a2"] + TFK, ["a2"])
                  wr = A32.alloc(16 * 36).rearrange("p (k n) -> p k n", k=16)
                  DMA("sync", wr[:, :, 0:4], rc_w[l].rearrange("(p k) g -> p k g", k=16), [], [("wr", 0)], "wr")
                  for g in range(4):
                      DMA("sync", wr[:, :, 4 + 8 * g:12 + 8 * g], rf_w[l, g].rearrange("(p k) e -> p k e", k=16), [],
                          [("wr", 1 + g)], "wr")
                  WRK = [("wr", i) for i in range(5)]
                  rb_ = A32.alloc(36)
                  DMA("sync", rb_[:, 0:4], rc_b[l:l + 1, :].broadcast_to([128, 4]), [], [("rb", 0)], "rb")
                  DMA("sync", rb_[:, 4:36], rf_b[l:l + 1].rearrange("o g e -> o (g e)").broadcast_to([128, 32]), [], [("rb", 1)], "rb")
                  RBK = [("rb", 0), ("rb", 1)]
                  mx = [A16.alloc(16 * 512).rearrange("p (k t) -> p k t", k=16) for _ in range(2)]
                  xt = [A32.alloc(D) for _ in range(1)]
                  x1t = [A32.alloc(D) for _ in range(2)]
                  h2f2 = [A32.alloc(D) for _ in range(2)]
                  h2b = [A16.alloc(D) for _ in range(2)]
                  h2T = A32.alloc(16 * 128).rearrange("p (k t) -> p k t", k=16)
                  sm = A32.alloc(256)
                  if q == 0:
                      MEMSET("vector", cnt_run[:], 0.0, ["cnt_run"])

                  def p5_main(it):
                      ti, sub = it // 4, it % 4
                      c0 = ti * 512
                      m_, mk_ = mx[ti % 2], ("mx", ti % 2)
                      if sub == 0:
                          DMA("sync", m_, mixT[q][:, c0:c0 + 512].rearrange("(k p) t -> p k t", p=128),
                              [("mixT", q, ti, i) for i in range(3)], [mk_], "mx%d" % (ti % 2))
                      b2 = it % 2
                      r0 = T0 + it * 128
                      xk = ("xt", 0)
                      DMA("sync", xt[0], cur_x[r0:r0 + 128, :], [("xcur", r0 // 128)], [xk], "xt0")
                      x1, x1k = x1t[b2], ("x1t", b2)
                      for cbk in range(4):
                          pi = cbk
                          for kc in range(16):
                              MM(pm[pi][:, :], m_[:, kc, sub * 128:(sub + 1) * 128], wo[:, kc, cbk * 512:(cbk + 1) * 512],
                                 kc == 0, kc == 15, [mk_, "wo"], [PM[pi]])
                          sl = slice(cbk * 512, (cbk + 1) * 512)
                          TT("vector", tmpf[:, sl], pm[pi][:, :], g1[:, sl], ALU.mult, [PM[pi], "g1"], [("tmpf", cbk)])
                          TT("gpsimd", x1[:, sl], tmpf[:, sl], xt[0][:, sl], ALU.add, [("tmpf", cbk), xk], [x1k])
                      DMA(STQ, x1_d[r0:r0 + 128, :], x1, [x1k], [("x1", r0 // 128)], "x1t%d" % b2)
                      ssk = U("ss")
                      ss = sm[:, 0:4]
                      hb_, hbk = h2b[b2], ("h2b", b2)
                      h2f, h2fk = h2f2[b2], ("h2f", b2)
                      ACT(hb_, x1, AF.Square, [x1k], [hbk, ssk], accum_out=ss[:, 0:1])
                      ACT(ss[:, 1:2], ss[:, 0:1], AF.Sqrt, [ssk, "eps_t"], [ssk], scale=1.0 / D, bias=eps_t[:, 0:1])
                      P.op("vector", lambda e, ss=ss: e.reciprocal(out=ss[:, 2:3], in_=ss[:, 1:2]), [ssk], [ssk])
                      STT(tmpf, x1, ss[:, 2:3], a2, ALU.mult, ALU.mult, [x1k, ssk, "a2"], [("tmpf", i) for i in range(4)])
                      TT("gpsimd", h2f, tmpf, sh2, ALU.add, [("tmpf", i) for i in range(4)] + ["sh2"], [h2fk])
                      CP("scalar", hb_, h2f, [h2fk], [hbk])
                      DMA(STQ, h2_d[r0:r0 + 128, :], hb_, [hbk], [("h2", r0 // 128)], "h2b%d" % b2)

                  def p5_router(it):
                      git = (T0 // 128) + it
                      b2 = it % 2
                      h2f, h2fk = h2f2[b2], ("h2f", b2)
                      for kc in range(16):
                          pj = pr[(kc // 4) % 2]
                          TR(pj[:, (kc % 4) * 128:(kc % 4 + 1) * 128], h2f.rearrange("p (c k) -> p k c", k=16)[:, kc, :],
                             ident_f[:], [h2fk, "ident_f"], [PR[(kc // 4) % 2]])
                          if kc % 4 == 3:
                              g4 = kc // 4
                              CP("scalar" if g4 % 2 else "vector", h2T[:, g4 * 4:g4 * 4 + 4, :],
                                 pj[:, :].rearrange("p (k t) -> p k t", k=4), [PR[(kc // 4) % 2]], [("h2T", g4)])
                      for kc in range(16):
                          MM(pr[0][:, 0:36], h2T[:, kc, :], wr[:, kc, :], kc == 0, kc == 15,
                             [("h2T", kc // 4)] + WRK, [PR[0]])
                      route_tile(P, nc, sm, pr, PR, rb_, RBK, r_E, r_f, cnt_run, utinc, ones_b, git,
                                 TT, TS, STT, ACT, CP, RED, MM, U)

                  NSUB = NT5 * 4
                  for it in range(NSUB):
                      p5_main(it)
                      if it > 0:
                          p5_router(it - 1)
                  p5_router(NSUB - 1)

              stop_here('P5')
              P.barrier()
              A16.reset(); A32.reset()
              fin = A32.alloc(8 * 32).rearrange("p (a e) -> p a e", a=8)
              RALL = ["cnt_run"]
              TS("vector", fin[:, 0, :], cnt_run[:], 127.0, None, ALU.add, None, ["cnt_run"], ["fin"])
              fin_i = A32.alloc(32).bitcast(I32)
              CP("vector", fin_i, fin[:, 0, :], ["fin"], ["fin_i"])
              TS("vector", fin_i, fin_i, 7, 7, ALU.arith_shift_right, ALU.logical_shift_left, ["fin_i"], ["fin_i"])
              CP("vector", fin[:, 2, :], fin_i, ["fin_i"], ["fin"])
              MEMSET("vector", fin[:, 3, :], 1.0, ["fin"])
              P.op("vector", lambda e: e.tensor_tensor_scan(out=fin[:, 4, :], data0=fin[:, 3, :], data1=fin[:, 2, :],
                                                            initial=0.0, op0=ALU.mult, op1=ALU.add), ["fin"], ["fin"])
              TT("vector", fin[:, 5, :], fin[:, 4, :], fin[:, 2, :], ALU.subtract, ["fin"], ["fin"])
              big = A32.alloc(NT * 32).rearrange("p (t e) -> p t e", e=32)
              slf = A32.alloc(NT * 2).rearrange("p (t k) -> p t k", k=2)
              for k in range(2):
                  TT("vector", big, r_E[:, :, k * 32:(k + 1) * 32], fin[:, 5:6, :].to_broadcast([128, NT, 32]), ALU.mult,
                     ["fin", "r_E"], ["big"])
                  RED(slf[:, :, k], big, ALU.add, ["big"], ["slf"])
                  TT("vector", slf[:, :, k], slf[:, :, k], r_f[:, :, k], ALU.add, ["slf", "r_f"], ["slf"])
              CP("vector", r_slot[:], slf, ["slf"], ["r_slot"])
              bst = A32.alloc(NB)
              DMA("sync", bst, k_bstart, [], ["bst"], "bst")
              ebf = A32.alloc(NB)
              CH = 32
              bigb = A32.alloc(CH * 32).rearrange("p (b e) -> p b e", e=32)
              for b0 in range(0, NB, CH):
                  nb_ = min(CH, NB - b0)
                  TT("vector", bigb[:, 0:nb_, :], fin[:, 4:5, :].to_broadcast([128, nb_, 32]),
                     bst[:, b0:b0 + nb_].unsqueeze(2).to_broadcast([128, nb_, 32]), ALU.is_le, ["fin", "bst"], ["bigb"])
                  RED(ebf[:, b0:b0 + nb_], bigb[:, 0:nb_, :], ALU.add, ["bigb"], ["ebf"])
              TS("vector", ebf, ebf, 31.0, None, ALU.min, None, ["ebf"], ["ebf"])
              sam = A32.alloc(NB)
              MEMSET("vector", sam[:, 0:1], 0.0, ["sam"])
              TT("vector", sam[:, 1:NB], ebf[:, 1:NB], ebf[:, 0:NB - 1], ALU.is_equal, ["ebf"], ["sam"])
              wif = A32.alloc(NB)
              TS("vector", wif, ebf, 128.0, iotap[:, 0:1], ALU.mult, ALU.add, ["ebf", "iotap"], ["wif"])
              STT(wif, sam, 1.0e6, wif, ALU.mult, ALU.add, ["sam", "wif"], ["wif"])
              if l > 0:
                  TS("vector", wif, wif, float(l * NE * 128), None, ALU.add, None, ["wif"], ["wif"])
              CP("vector", widx[:], wif, ["wif"], ["widx"])
              hl = [A16.alloc(D) for _ in range(3)]
              for it in range(NT):
                  b3 = it % 3
                  hk_ = ("hl", b3)
                  DMA("sync", hl[b3], h2_d[it * 128:(it + 1) * 128, :], [("h2", it)], [hk_], "hl%d" % b3)
                  for k in range(2):
                      off = r_slot[:, it, k:k + 1]
                      P.dma("gpsimd", (lambda e, off=off, src=hl[b3]: e.indirect_dma_start(
                          out=xs_d, out_offset=bass.IndirectOffsetOnAxis(ap=off, axis=0), in_=src, in_offset=None,
                          bounds_check=BR(e, L - 1), oob_is_err=False)), [hk_, "r_slot"], [("xs", it, k)], "xs_sc")
              XSK = [("xs", it, k) for it in range(NT) for k in range(2)]

              P.barrier()
              A16.reset(); A32.reset()
              W1 = A16.alloc(16 * 512)
              W3 = A16.alloc(16 * 512)
              W2 = A16.alloc(4 * 2048)
              xb = [A16.alloc(D) for _ in range(3)]
              xbT = [A16.alloc(16 * 128).rearrange("p (k t) -> p k t", k=16) for _ in range(2)]

              def xload(b):
                  DMA("sync", xb[b % 3], xs_d[b * 128:(b + 1) * 128, :], XSK, [("xb", b % 3)], "xb%d" % (b % 3))

              sgf = [A32.alloc(512) for _ in range(2)]
              hid = [A16.alloc(512) for _ in range(2)]
              hidT = [A16.alloc(4 * 128).rearrange("p (k t) -> p k t", k=4) for _ in range(2)]
              yb = [A32.alloc(D) for _ in range(2)]
              w1v = e_w1.rearrange("l e (p k) n -> (l e p) (k n)", k=16)
              w3v = e_w3.rearrange("l e (p k) n -> (l e p) (k n)", k=16)
              w2v = e_w2.rearrange("l e (p k) n -> (l e p) (k n)", k=4)
              bnd_l = (l + 1) * NE * 128 - 1

              def wgather(b, Wt, wv, wk):
                  off = widx[:, b:b + 1]
                  P.dma("gpsimd", (lambda e, off=off, Wt=Wt, wv=wv, bnd=bnd_l: e.indirect_dma_start(
                      out=Wt, out_offset=None, in_=wv, in_offset=bass.IndirectOffsetOnAxis(ap=off, axis=0),
                      bounds_check=BR(e, bnd), oob_is_err=False)), ["widx"], [wk], wk)

              def stageA(b):
                  b2 = b % 2
                  wgather(b, W1, w1v, "W1")
                  wgather(b, W3, w3v, "W3")
                  xk = ("xb", b % 3)
                  if b + 1 < NB:
                      xload(b + 1)
                  for kc in range(16):
                      TR(pt[:, kc * 128:(kc + 1) * 128], xb[b % 3].rearrange("p (c k) -> p k c", k=16)[:, kc, :], ident_b[:],
                         [xk, "ident_b"], [PT])
                  xT, xTk = xbT[b2], ("xbT", b2)
                  CP("scalar", xT[:, 0:8, :], pt[:, 0:1024].rearrange("p (k t) -> p k t", k=8), [PT], [xTk])
                  CP("vector", xT[:, 8:16, :], pt[:, 1024:2048].rearrange("p (k t) -> p k t", k=8), [PT], [xTk])
                  p1, p3 = 2 * b2, 2 * b2 + 1
                  for kc in range(16):
                      MM(pm[p1][:, :], xT[:, kc, :], W1[:, kc * 512:(kc + 1) * 512], kc == 0, kc == 15, [xTk, "W1"], [PM[p1]])
                  for kc in range(16):
                      MM(pm[p3][:, :], xT[:, kc, :], W3[:, kc * 512:(kc + 1) * 512], kc == 0, kc == 15, [xTk, "W3"], [PM[p3]])
                  ACT(sgf[b2], pm[p1][:, :], AF.Silu, [PM[p1]], [("sgf", b2)])
                  TT("vector", hid[b2], pm[p3][:, :], sgf[b2], ALU.mult, [PM[p3], ("sgf", b2)], [("hid", b2)])

              def stageB(b):
                  b2 = b % 2
                  wgather(b, W2, w2v, "W2")
                  for fc in range(4):
                      TR(pt[:, fc * 128:(fc + 1) * 128], hid[b2].rearrange("p (c k) -> p k c", k=4)[:, fc, :], ident_b[:],
                         [("hid", b2), "ident_b"], [PT])
                  CP("vector", hidT[b2], pt[:, 0:512].rearrange("p (k t) -> p k t", k=4), [PT], [("hidT", b2)])
                  y_, yk = yb[b2], ("yb", b2)
                  for cbk in range(4):
                      pi = cbk % 2
                      for fc in range(4):
                          MM(pr[pi][:, :], hidT[b2][:, fc, :], W2[:, fc * 2048 + cbk * 512:fc * 2048 + (cbk + 1) * 512],
                             fc == 0, fc == 3, [("hidT", b2), "W2"], [PR[pi]])
                      if cbk % 2:
                          CP("scalar", y_[:, cbk * 512:(cbk + 1) * 512], pr[pi][:, :], [PR[pi]], [yk])
                      else:
                          CP("vector", y_[:, cbk * 512:(cbk + 1) * 512], pr[pi][:, :], [PR[pi]], [yk])
                  DMA(STQ, ys_d[b * 128:(b + 1) * 128, :], y_, [yk], [("ys", b)], "yb%d" % b2)

              xload(0)
              stageA(0)
              for b in range(NB):
                  if b + 1 < NB:
                      stageA(b + 1)
                  stageB(b)
              YSK = [("ys", b) for b in range(NB)]

              stop_here('P7')
              P.barrier()
              A16.reset(); A32.reset()
              g2 = [A32.alloc(D) for _ in range(2)]
              for q in range(2):
                  DMA("sync", g2[q], mod_rep(q, 5), MODK, [("g2", q)], "g2%d" % q)
              if last:
                  fg = A32.alloc(D)
                  DMA("sync", fg, final_g.rearrange("(o n) -> o n", o=1).broadcast_to([128, D]), [], ["fg"], "fg")
              ya = [A32.alloc(D) for _ in range(2)]
              ybb = [A32.alloc(D) for _ in range(2)]
              x1t = [A32.alloc(D) for _ in range(2)]
              junk = A16.alloc(D)
              ss = A32.alloc(8)
              dst_x = y_out if last else x2_d
              for it in range(NT):
                  b2 = it % 2
                  q = 0 if it * 128 < OFFS[1] else 1
                  for k, yt in enumerate((ya, ybb)):
                      off = r_slot[:, it, k:k + 1]
                      P.dma("gpsimd", (lambda e, off=off, dst=yt[b2]: e.indirect_dma_start(
                          out=dst, out_offset=None, in_=ys_d, in_offset=bass.IndirectOffsetOnAxis(ap=off, axis=0),
                          bounds_check=BR(e, L - 1), oob_is_err=False)), YSK + ["r_slot"], [("yg", k, b2)], "yg%d%d" % (k, b2))
                  xk = ("x1t", b2)
                  DMA("sync", x1t[b2], x1_d[it * 128:(it + 1) * 128, :], [("x1", it)], [xk], "x1l%d" % b2)
                  A_, B_ = ya[b2], ybb[b2]
                  TS("vector", A_, A_, r_f[:, it, 2:3], None, ALU.mult, None, [("yg", 0, b2), "r_f"], [("yg", 0, b2)])
                  STT(A_, B_, r_f[:, it, 3:4], A_, ALU.mult, ALU.add, [("yg", 1, b2), ("yg", 0, b2), "r_f"], [("yg", 0, b2)])
                  TT("gpsimd", A_, A_, g2[q], ALU.mult, [("yg", 0, b2), ("g2", q)], [("yg", 0, b2)])
                  TT("vector", B_, A_, x1t[b2], ALU.add, [("yg", 0, b2), xk], [("yg", 1, b2)])
                  if not last:
                      DMA(STQ, dst_x[it * 128:(it + 1) * 128, :], B_, [("yg", 1, b2)], [("xcur", it)], "xo%d" % b2)
                  else:
                      ssk = U("ss")
                      ACT(junk, B_, AF.Square, [("yg", 1, b2)], ["junk", ssk], accum_out=ss[:, 0:1])
                      ACT(ss[:, 1:2], ss[:, 0:1], AF.Sqrt, [ssk, "eps_t"], [ssk], scale=1.0 / D, bias=eps_t[:, 0:1])
                      P.op("vector", lambda e, ss=ss: e.reciprocal(out=ss[:, 2:3], in_=ss[:, 1:2]), [ssk], [ssk])
                      STT(A_, B_, ss[:, 2:3], fg, ALU.mult, ALU.mult, [("yg", 1, b2), ssk, "fg"], [("yg", 0, b2)])
                      DMA(STQ, dst_x[it * 128:(it + 1) * 128, :], A_, [("yg", 0, b2)], [("yout", it)], "xo%d" % b2)
              cur_x = x2_d

          P.emit()
        except _Stop:
            pass
    return nc


def route_tile(P, nc, sm, pr, PR, rb_, RBK, r_E, r_f, cnt_run, utinc, ones_b, git,
               TT, TS, STT, ACT, CP, RED, MM, U):
    rk = U("rt")
    Lg = sm[:, 8:44]
    TT("vector", Lg, pr[0][:, 0:36], rb_, ALU.add, [PR[0]] + RBK, [rk])
    m = sm[:, 44:45]
    RED(m, Lg[:, 0:4], ALU.max, [rk], [rk])
    oh = sm[:, 48:52]
    TS("vector", oh, Lg[:, 0:4], m, None, ALU.is_equal, None, [rk], [rk])
    negm = sm[:, 45:46]
    TS("vector", negm, m, -1.0, None, ALU.mult, None, [rk], [rk])
    ex = sm[:, 52:56]
    se = sm[:, 46:47]
    ACT(ex, Lg[:, 0:4], AF.Exp, [rk], [rk], bias=negm, scale=1.0, accum_out=se)
    pg = sm[:, 47:48]
    P.op("vector", lambda e: e.reciprocal(out=pg, in_=se), [rk], [rk])
    lf = sm[:, 56:64]
    TS("vector", lf, Lg[:, 4:12], oh[:, 0:1], None, ALU.mult, None, [rk], [rk])
    for g in range(1, 4):
        STT(lf, Lg[:, 4 + 8 * g:12 + 8 * g], oh[:, g:g + 1], lf, ALU.mult, ALU.add, [rk], [rk])
    top = sm[:, 64:72]
    P.op("vector", lambda e: e.max(out=top, in_=lf), [rk], [rk])
    s1 = sm[:, 72:80]
    s2 = sm[:, 80:88]
    TS("vector", s1, lf, top[:, 0:1], None, ALU.is_equal, None, [rk], [rk])
    TS("vector", s2, lf, top[:, 1:2], None, ALU.is_equal, None, [rk], [rk])
    dv = sm[:, 88:89]
    TT("vector", dv, top[:, 0:1], top[:, 1:2], ALU.subtract, [rk], [rk])
    sg = sm[:, 89:90]
    ACT(sg, dv, AF.Sigmoid, [rk], [rk])
    TT("vector", r_f[:, git, 2:3], pg, sg, ALU.mult, [rk], ["r_f"])
    TT("vector", r_f[:, git, 3:4], pg, r_f[:, git, 2:3], ALU.subtract, [rk, "r_f"], ["r_f"])
    Ef = sm[:, 96:160]
    for g in range(4):
        TS("vector", Ef[:, 8 * g:8 * g + 8], s1, oh[:, g:g + 1], None, ALU.mult, None, [rk], [rk])
        TS("vector", Ef[:, 32 + 8 * g:40 + 8 * g], s2, oh[:, g:g + 1], None, ALU.mult, None, [rk], [rk])
    CP("vector", r_E[:, git, :], Ef, [rk], ["r_E"])
    Mb = sm[:, 160:192]
    TT("vector", Mb, Ef[:, 0:32], Ef[:, 32:64], ALU.add, [rk], [rk])
    Mbb = sm[:, 192:224].bitcast(BF16)[:, 0:32]
    CP("vector", Mbb, Mb, [rk], [rk])
    MM(pr[1][:, 0:32], utinc[:], Mbb, True, True, ["utinc", rk], [PR[1]])
    MM(pr[1][:, 32:64], ones_b[:], Mbb, True, True, ["ones_b", rk], [PR[1]])
    rank = sm[:, 224:256]
    TT("vector", rank, pr[1][:, 0:32], Mb, ALU.subtract, [PR[1], rk], [rk])
    TT("vector", rank, rank, cnt_run[:], ALU.add, [rk, "cnt_run"], [rk])
    TT("vector", cnt_run[:], cnt_run[:], pr[1][:, 32:64], ALU.add, [PR[1], "cnt_run"], ["cnt_run"])
    tmp = sm[:, 160:192]
    for k in range(2):
        TT("vector", tmp, Ef[:, 32 * k:32 * k + 32], rank, ALU.mult, [rk], [rk])
        RED(r_f[:, git, k:k + 1], tmp, ALU.add, [rk], ["r_f"])


_WNAMES = ["w_ada", "b_ada", "norm1_g", "w_in", "pool_w", "pool_scale", "conv_dw_w", "conv_dw_b",
           "conv_ln_g", "conv_ln_b", "conv_pw_w", "conv_pw_b", "fourier_w", "w_out", "norm2_g",
           "router_coarse_w", "router_coarse_b", "router_fine_w", "router_fine_b",
           "expert_w1", "expert_w3", "expert_w2", "final_g"]


def kernel(**inputs):
    xs_ = np.asarray(inputs["x_sample"], np.float32)
    xp_ = np.asarray(inputs["x_prompt"], np.float32)
    cs_ = np.asarray(inputs["c_sample"], np.float32)
    cp_ = np.asarray(inputs["c_prompt"], np.float32)
    S0, S1 = xs_.shape[1], xp_.shape[1]
    depth = inputs["w_ada"].shape[0]
    nc = build((S0, S1), depth)
    N = S0 + S1
    NB = -(-(2 * N + NE * 127) // 128)
    consts = make_consts((S0, S1), NB)
    wts = {k: np.ascontiguousarray(np.asarray(inputs[k], np.float32)) for k in _WNAMES}
    in_maps = []
    for core in range(8):
        b = core % 4
        m = {"x": np.ascontiguousarray(np.concatenate([xs_[b], xp_[b]], axis=0)),
             "c": np.ascontiguousarray(np.stack([cs_[b], cp_[b]], axis=0))}
        m.update(wts)
        m.update(consts)
        in_maps.append(m)
    res = run_bass_kernel_spmd(nc, in_maps, core_ids=list(range(8)))
    y_s = np.stack([res.results[b]["y"][:S0] for b in range(4)], axis=0)
    y_p = np.stack([res.results[b]["y"][S0:] for b in range(4)], axis=0)
    return (y_p.astype(np.float32), y_s.astype(np.float32))
```

```python
import math
import os
from contextlib import ExitStack

import numpy as np
import ml_dtypes
import concourse.bass as bass
import concourse.mybir as mybir
from concourse.bass_utils import run_bass_kernel_spmd

F32 = mybir.dt.float32
BF16 = mybir.dt.bfloat16
I32 = mybir.dt.int32
AF = mybir.ActivationFunctionType
ALU = mybir.AluOpType
AX = mybir.AxisListType

D = 2048
NG, EPG, NE = 4, 8, 32
DE = 512
CONV_K = 31
EPS = 1e-6
SAME_ENGINE_SYNC = True
CONV_PE = os.environ.get('KCONV', 'pe') == 'pe'
NOSYNC = set(os.environ.get('KNOSYNC', 'tensor').split(','))


class _Op:
    __slots__ = ("q", "fn", "deps", "sig", "sigval", "sem", "is_dma", "idx", "bar")


class Prog:
    def __init__(self, nc):
        self.nc = nc
        self.ops = []
        self.last_w = {}
        self.readers = {}
        self.dma_cnt = {}
        self.last_eng = {}
        self.bar = None
        self.bar_done = set()

    def barrier(self):
        deps = [(o, None) for o in self.last_eng.values()]
        dmas = dict(self.dma_cnt)
        self.bar = (deps, dmas)
        self.bar_done = set()

    def _add(self, q, fn, reads, writes, is_dma, semkey):
        o = _Op()
        o.q = q
        o.fn = fn
        o.is_dma = is_dma
        o.sig = False
        o.sigval = 0
        o.idx = len(self.ops)
        deps = {}
        for r in reads:
            w = self.last_w.get(r)
            if w is not None:
                deps[w.idx] = w
        for w_ in writes:
            w = self.last_w.get(w_)
            if w is not None:
                deps[w.idx] = w
            for rd in self.readers.get(w_, ()):
                deps[rd.idx] = rd
        o.deps = []
        for d in deps.values():
            if d.is_dma:
                o.deps.append((d, self.dma_cnt[d.sem]))
            else:
                o.deps.append((d, None))
        o.bar = None
        if self.bar is not None and q not in self.bar_done:
            self.bar_done.add(q)
            bdeps, bdmas = self.bar
            o.deps.extend(bdeps)
            o.bar = bdmas
        if is_dma:
            o.sem = ("dma", semkey)
            self.dma_cnt[o.sem] = self.dma_cnt.get(o.sem, 0) + 1
            o.sigval = self.dma_cnt[o.sem]
        else:
            o.sem = ("eng", q)
            self.last_eng[q] = o
        for w_ in writes:
            self.last_w[w_] = o
            self.readers[w_] = []
        for r in reads:
            if r not in writes:
                self.readers.setdefault(r, []).append(o)
        self.ops.append(o)
        return o

    def op(self, eng, fn, reads=(), writes=()):
        return self._add(eng, fn, tuple(reads), tuple(writes), False, None)

    def dma(self, q, fn, reads=(), writes=(), semkey=None):
        assert semkey is not None
        return self._add(q, fn, tuple(reads), tuple(writes), True, semkey)

    def emit(self):
        nc = self.nc
        ops = self.ops
        for o in ops:
            for d, _ in o.deps:
                if d.is_dma:
                    continue
                if d.q != o.q or (SAME_ENGINE_SYNC and d.q not in NOSYNC):
                    d.sig = True
        cnt = {}
        for o in ops:
            if not o.is_dma and o.sig:
                cnt[o.q] = cnt.get(o.q, 0) + 1
                o.sigval = cnt[o.q]
        semkeys = []
        seen = set()
        for o in ops:
            if (o.is_dma or o.sig) and o.sem not in seen:
                seen.add(o.sem)
                semkeys.append(o.sem)
        self.n_sems = len(semkeys)
        with ExitStack() as es:
            sems = {}
            for i, k in enumerate(semkeys):
                sems[k] = es.enter_context(nc.semaphore("s%d" % i))
            block = es.enter_context(nc.Block())
            queues = {}
            for o in ops:
                queues.setdefault(o.q, []).append(o)
            totals = dict(self.dma_cnt)

            def run_queue(qname, eng):
                waited = {}
                for o in queues.get(qname, ()):
                    need = {}
                    for d, n in o.deps:
                        if d.is_dma:
                            v = 16 * n
                        else:
                            if d.q == o.q and (d.q in NOSYNC or not SAME_ENGINE_SYNC):
                                continue
                            v = d.sigval
                        if v > need.get(d.sem, 0):
                            need[d.sem] = v
                    if o.bar is not None:
                        for k, n in o.bar.items():
                            if 16 * n > need.get(k, 0):
                                need[k] = 16 * n
                    for k, v in need.items():
                        if waited.get(k, 0) < v:
                            eng.wait_ge(sems[k], v)
                            waited[k] = v
                    ins = o.fn(eng)
                    if o.is_dma:
                        ins.then_inc(sems[o.sem], 16)
                    elif o.sig:
                        ins.then_inc(sems[o.sem], 1)
                if qname == "sync":
                    for k, n in totals.items():
                        if waited.get(k, 0) < 16 * n:
                            eng.wait_ge(sems[k], 16 * n)

            @block.sync
            def _(e):
                run_queue("sync", e)

            @block.scalar
            def _(e):
                run_queue("scalar", e)

            @block.gpsimd
            def _(e):
                run_queue("gpsimd", e)

            @block.vector
            def _(e):
                run_queue("vector", e)

            @block.tensor
            def _(e):
                run_queue("tensor", e)


def _bf(a):
    return np.ascontiguousarray(a.astype(ml_dtypes.bfloat16))


def make_consts(SEQS, NB):
    c = {}
    t = np.arange(128)
    ang = 2 * np.pi * np.outer(t, t) / 128.0
    cs = np.zeros((128, 2, 192), np.float64)
    for h in range(2):
        sl = slice(h * 64, h * 64 + 64)
        cs[:, h, 0:64] = np.cos(ang[:, sl])
        cs[:, h, 64:128] = np.sin(ang[:, sl])
        cs[:, h, 128:192] = -np.sin(ang[:, sl])
    c["cs128"] = _bf(cs)
    ccn = np.zeros((128, 2, 128), np.float64)
    ccn[:, 0] = np.cos(ang)
    ccn[:, 1] = -np.sin(ang)
    c["ccn"] = _bf(ccn)
    for q, S in enumerate(SEQS):
        S2 = S // 128
        a = 2 * np.pi * (np.outer(np.arange(S2), np.arange(S)) % S) / S
        f = np.zeros((S2, 2, S), np.float64)
        f[:, 0] = np.cos(a)
        f[:, 1] = np.sin(a)
        c["fcs%d" % q] = _bf(f)
    pc = np.zeros((128, 4, 16), np.float32)
    for g, w in enumerate((2, 4, 8, 16)):
        for j in range(8):
            cnt = min(j + w // 2, 10 ** 9) - max(j - w // 2, 0)
            pc[:, g, j] = 1.0 / cnt
        for j in range(8):
            tt = -8 + j
            hi = min(tt + w // 2, 0)
            lo = tt - w // 2
            pc[:, g, 8 + j] = 1.0 / (hi - lo)
    c["poolc"] = pc
    ut = (np.arange(128)[:, None] <= np.arange(128)[None, :]).astype(np.float32)
    c["utinc"] = _bf(ut)
    c["iotap"] = np.arange(128, dtype=np.float32).reshape(128, 1)
    c["bstart"] = np.tile((128.0 * np.arange(NB, dtype=np.float32))[None, :], (128, 1))
    c["iota32"] = np.tile(np.arange(32, dtype=np.float32)[None, :], (128, 1))
    return c


class Arena:
    def __init__(self, t, size):
        self.t = t
        self.size = size
        self.off = 0

    def reset(self):
        self.off = 0

    def alloc(self, n, align=16):
        self.off = (self.off + align - 1) // align * align
        o = self.off
        self.off += n
        assert self.off <= self.size, ("arena overflow", self.off, self.size)
        return self.t[:, o:o + n]


def build(SEQS=(8192, 2048), DEPTH=2, dbg=False, NL=None):
    NL = DEPTH if NL is None else NL
    N = sum(SEQS)
    NT = N // 128
    NB = -(-(2 * N + NE * 127) // 128)
    L = NB * 128
    OFFS = [0]
    for S in SEQS:
        OFFS.append(OFFS[-1] + S)
    nc = bass.Bass("TRN2", target_bir_lowering=False)

    def din(name, shape, dt=F32):
        return nc.dram_tensor(name, list(shape), dt, kind="ExternalInput").ap()

    def dscr(name, shape, dt):
        kind = "ExternalOutput" if dbg else "Internal"
        return nc.dram_tensor(name, list(shape), dt, kind=kind).ap()

    x_in = din("x", [N, D])
    c_in = din("c", [2, D])
    w_ada = din("w_ada", [DEPTH, D, 6 * D])
    b_ada = din("b_ada", [DEPTH, 6 * D])
    norm1_g = din("norm1_g", [DEPTH, D])
    w_in = din("w_in", [DEPTH, D, 3072])
    pool_w = din("pool_w", [DEPTH, 4, 128, 128])
    pool_scale = din("pool_scale", [DEPTH, 512])
    conv_dw_w = din("conv_dw_w", [DEPTH, CONV_K, 1024])
    conv_dw_b = din("conv_dw_b", [DEPTH, 1024])
    conv_ln_g = din("conv_ln_g", [DEPTH, 1024])
    conv_ln_b = din("conv_ln_b", [DEPTH, 1024])
    conv_pw_w = din("conv_pw_w", [DEPTH, 1024, 1024])
    conv_pw_b = din("conv_pw_b", [DEPTH, 1024])
    fourier_w = din("fourier_w", [DEPTH, 512, 512])
    w_out = din("w_out", [DEPTH, D, D])
    norm2_g = din("norm2_g", [DEPTH, D])
    rc_w = din("router_coarse_w", [DEPTH, D, NG])
    rc_b = din("router_coarse_b", [DEPTH, NG])
    rf_w = din("router_fine_w", [DEPTH, NG, D, EPG])
    rf_b = din("router_fine_b", [DEPTH, NG, EPG])
    e_w1 = din("expert_w1", [DEPTH, NE, D, DE])
    e_w3 = din("expert_w3", [DEPTH, NE, D, DE])
    e_w2 = din("expert_w2", [DEPTH, NE, DE, D])
    final_g = din("final_g", [D])
    k_cs128 = din("cs128", [128, 2, 192], BF16)
    k_ccn = din("ccn", [128, 2, 128], BF16)
    k_fcs = [din("fcs%d" % q, [S // 128, 2, S], BF16) for q, S in enumerate(SEQS)]
    k_poolc = din("poolc", [128, 4, 16])
    k_utinc = din("utinc", [128, 128], BF16)
    k_iotap = din("iotap", [128, 1])
    k_bstart = din("bstart", [128, NB])
    k_iota32 = din("iota32", [128, 32])

    y_out = nc.dram_tensor("y", [N, D], F32, kind="ExternalOutput").ap()

    x1_d = dscr("x1", [N, D], F32)
    x2_d = dscr("x2", [N, D], F32)
    h2_d = dscr("h2", [N, D], BF16)
    xs_d = dscr("xs", [L, D], BF16)
    ys_d = dscr("ys", [L, D], F32)
    mod_d = dscr("modrow", [2, 6 * D], F32)
    zaT = [dscr("zaT%d" % q, [512, S], BF16) for q, S in enumerate(SEQS)]
    vT = [dscr("vT%d" % q, [1024, S], BF16) for q, S in enumerate(SEQS)]
    zcT = [dscr("zcT%d" % q, [512, S], BF16) for q, S in enumerate(SEQS)]
    rzT = [dscr("rzT%d" % q, [512, S], BF16) for q, S in enumerate(SEQS)]
    mixT = [dscr("mixT%d" % q, [2048, S], BF16) for q, S in enumerate(SEQS)]

    P = Prog(nc)
    es = ExitStack()
    with es:
        AR_SZ = 96 * 1024
        art = es.enter_context(nc.sbuf_tensor("arena", [128, AR_SZ], BF16))
        AR = Arena(art, AR_SZ)

        class _A16:
            def alloc(self, n):
                return AR.alloc(n)

            def reset(self):
                AR.reset()

        class _A32:
            def alloc(self, n):
                return AR.alloc(2 * n).bitcast(F32)

            def reset(self):
                pass

        A16 = _A16()
        A32 = _A32()
        ident_f = es.enter_context(nc.sbuf_tensor("ident_f", [128, 128], F32))
        ident_b = es.enter_context(nc.sbuf_tensor("ident_b", [128, 128], BF16))
        ones_b = es.enter_context(nc.sbuf_tensor("ones_b", [128, 128], BF16))
        utinc = es.enter_context(nc.sbuf_tensor("utinc_s", [128, 128], BF16))
        iotap = es.enter_context(nc.sbuf_tensor("iotap_s", [128, 1], F32))
        iota32 = es.enter_context(nc.sbuf_tensor("iota32_s", [128, 32], F32))
        eps_t = es.enter_context(nc.sbuf_tensor("eps_t", [128, 1], F32))
        r_E = es.enter_context(nc.sbuf_tensor("r_E", [128, NT, 64], BF16))
        r_f = es.enter_context(nc.sbuf_tensor("r_f", [128, NT, 4], F32))
        r_slot = es.enter_context(nc.sbuf_tensor("r_slot", [128, NT, 2], I32))
        cnt_run = es.enter_context(nc.sbuf_tensor("cnt_run", [128, 32], F32))
        widx = es.enter_context(nc.sbuf_tensor("widx", [128, NB], I32))
        pm = [es.enter_context(nc.psum_tensor("pm%d" % i, [128, 512], F32)) for i in range(4)]
        pt = es.enter_context(nc.psum_tensor("pt", [128, 2048], BF16))
        pr = [es.enter_context(nc.psum_tensor("pr%d" % i, [128, 512], F32)) for i in range(2)]
        PM = [("pm", i) for i in range(4)]
        PT = "pt"
        PR = [("pr", i) for i in range(2)]

        STQ = os.environ.get('KSTQ', 'sync')
        uid = [0]
        _bregs = {}

        def BR(e, val):
            if val not in _bregs:
                _bregs[val] = e.to_reg(val)
            return _bregs[val]

        def U(name):
            uid[0] += 1
            return (name, uid[0])

        def t16(n, shape=None):
            v = A16.alloc(n)
            return v

        def DMA(q, out, in_, reads, writes, semkey, **kw):
            P.dma(q, lambda e: e.dma_start(out=out, in_=in_, **kw), reads, writes, semkey)

        def MM(out, lhsT, rhs, start, stop, reads, writes):
            P.op("tensor", lambda e: e.matmul(out, lhsT=lhsT, rhs=rhs, start=start, stop=stop), reads, writes)

        def TR(out, in_, ident, reads, writes):
            P.op("tensor", lambda e: e.transpose(out=out, in_=in_, identity=ident), reads, writes)

        def ACT(out, in_, func, reads, writes, bias=None, scale=None, accum_out=None, eng="scalar"):
            kw = {}
            if bias is not None:
                kw["bias"] = bias
            if scale is not None:
                kw["scale"] = scale
            if accum_out is not None:
                kw["accum_out"] = accum_out
            P.op("scalar", lambda e: e.activation(out=out, in_=in_, func=func, **kw), reads, writes)

        def TT(eng, out, in0, in1, op, reads, writes):
            P.op(eng, lambda e: e.tensor_tensor(out=out, in0=in0, in1=in1, op=op), reads, writes)

        def TS(eng, out, in0, s1, s2, op0, op1, reads, writes, accum_out=None):
            if op1 is None:
                P.op(eng, lambda e: e.tensor_scalar(out=out, in0=in0, scalar1=s1, scalar2=None, op0=op0), reads, writes)
            elif accum_out is not None:
                P.op(eng, lambda e: e.tensor_scalar(out=out, in0=in0, scalar1=s1, scalar2=s2, op0=op0, op1=op1, accum_out=accum_out), reads, writes)
            else:
                P.op(eng, lambda e: e.tensor_scalar(out=out, in0=in0, scalar1=s1, scalar2=s2, op0=op0, op1=op1), reads, writes)

        def STT(out, in0, scalar, in1, op0, op1, reads, writes):
            P.op("vector", lambda e: e.scalar_tensor_tensor(out=out, in0=in0, scalar=scalar, in1=in1, op0=op0, op1=op1), reads, writes)

        def CP(eng, out, in_, reads, writes):
            if eng == "scalar":
                P.op("scalar", lambda e: e.copy(out=out, in_=in_), reads, writes)
            else:
                P.op(eng, lambda e: e.tensor_copy(out=out, in_=in_), reads, writes)

        def MEMSET(eng, ap, val, writes):
            P.op(eng, lambda e: e.memset(ap, val), (), writes)

        def RED(out, in_, op, reads, writes, axis=AX.X):
            P.op("vector", lambda e: e.tensor_reduce(out=out, in_=in_, axis=axis, op=op), reads, writes)

        def bcast_row(dram_row_ap, n):
            return dram_row_ap.partition_broadcast(128)

        MEMSET("gpsimd", ident_f[:], 0.0, ["ident_f"])
        P.op("gpsimd", lambda e: e.affine_select(out=ident_f[:], in_=ident_f[:], pattern=[[-1, 128]],
                                                 compare_op=ALU.not_equal, fill=1.0, base=0, channel_multiplier=1),
             ["ident_f"], ["ident_f"])
        CP("vector", ident_b[:], ident_f[:], ["ident_f"], ["ident_b"])
        MEMSET("gpsimd", ones_b[:], 1.0, ["ones_b"])
        MEMSET("gpsimd", eps_t[:], EPS, ["eps_t"])
        DMA("sync", utinc[:], k_utinc, [], ["utinc"], "c_utinc")
        DMA("sync", iotap[:], k_iotap, [], ["iotap"], "c_iotap")
        DMA("sync", iota32[:], k_iota32, [], ["iota32"], "c_iota32")

        class _Stop(Exception):
            pass

        def stop_here(tag):
            if os.environ.get("KSTOP") == tag:
                P.emit()
                raise _Stop()

        cur_x = x_in
        try:
          for l in range(NL):
              last = (l == NL - 1)
              nxt_x = y_out if False else x2_d
              P.barrier()
              A16.reset(); A32.reset()
              cst = A32.alloc(2 * 16).rearrange("p (q k) -> p q k", q=2)
              csb = A16.alloc(16 * 2).rearrange("p (k q) -> p k q", q=2)
              brow = [A32.alloc(512) for _ in range(2)]
              mrow = A32.alloc(512)
              DMA("sync", cst, c_in.rearrange("q (p k) -> p q k", k=16), [], ["cst"], "cst")
              for q in range(2):
                  ACT(csb[:, :, q], cst[:, q, :], AF.Silu, ["cst"], [("csb", q)])
              wa = [A16.alloc(16 * 512).rearrange("p (k n) -> p k n", k=16) for _ in range(2)]
              for cb in range(24):
                  wbuf = wa[cb % 2]
                  wk = ("wa", cb % 2)
                  DMA("gpsimd", wbuf, w_ada[l, :, cb * 512:(cb + 1) * 512].rearrange("(p k) n -> p k n", k=16),
                      [], [wk], "wa%d" % (cb % 2))
                  pk = PM[cb % 2]
                  pst = pm[cb % 2]
                  for kc in range(16):
                      MM(pst[0:2, :], csb[:, kc, :], wbuf[:, kc, :], kc == 0, kc == 15,
                         [wk, ("csb", 0), ("csb", 1)], [pk])
                  mk = U("mrow")
                  bk = ("brow", cb % 2)
                  DMA("sync", brow[cb % 2][0:2, :], b_ada[l:l + 1, cb * 512:(cb + 1) * 512].broadcast_to([2, 512]),
                      [], [bk], "brow%d" % (cb % 2))
                  TT("vector", mrow[0:2, :], pst[0:2, :], brow[cb % 2][0:2, :], ALU.add,
                     [pk, bk], ["mrow"])
                  DMA(STQ, mod_d[:, cb * 512:(cb + 1) * 512], mrow[0:2, :], ["mrow"], [("mod", cb)], "mrow")
              MODK = [("mod", cb) for cb in range(24)]

              def mod_rep(q, i):
                  return mod_d[q:q + 1, i * D:(i + 1) * D].broadcast_to([128, D])

              for q, S in enumerate(SEQS):
                  T0 = OFFS[q]
                  NT5 = S // 512
                  S2 = S // 128
                  P.barrier()
                  A16.reset(); A32.reset()
                  wi = A16.alloc(16 * 3072).rearrange("p (k n) -> p k n", k=16)
                  DMA("gpsimd", wi, w_in[l].rearrange("(p k) n -> p k n", k=16), [], ["wi"], "wi")
                  a1 = A32.alloc(D)
                  sh1 = A32.alloc(D)
                  tmpf = A32.alloc(D)
                  DMA("sync", a1, mod_rep(q, 1), MODK, ["a1"], "a1")
                  DMA("sync", sh1, mod_rep(q, 0), MODK, ["sh1"], "sh1")
                  DMA("sync", tmpf, norm1_g[l:l + 1, :].broadcast_to([128, D]), [], ["tmpf"], "g_rep")
                  STT(a1, a1, 1.0, tmpf, ALU.add, ALU.mult, ["a1", "tmpf"], ["a1"])
                  xt = [A32.alloc(D) for _ in range(2)]
                  hb = [A16.alloc(D) for _ in range(4)]
                  hT = [A16.alloc(16 * 512).rearrange("p (k t) -> p k t", k=16) for _ in range(1)]
                  stg = [A16.alloc(16 * 512).rearrange("p (j t) -> p j t", j=16) for _ in range(1)]
                  sgt = [A32.alloc(512) for _ in range(2)]
                  ss = A32.alloc(8)
                  hTt = hT[0]
                  hk = ("hT", 0)

                  def p1_norm(ti):
                      for sub in range(4):
                          it = ti * 4 + sub
                          b2 = it % 2
                          r0 = T0 + it * 128
                          xk = ("xt", b2)
                          DMA("sync", xt[b2], cur_x[r0:r0 + 128, :], [("xcur", r0 // 128)], [xk], "xt%d" % b2)
                          ssk = U("ss")
                          hbk = ("hb", sub)
                          ACT(hb[sub], xt[b2], AF.Square, [xk], [hbk, ssk], accum_out=ss[:, 0:1])
                          ACT(ss[:, 1:2], ss[:, 0:1], AF.Sqrt, [ssk, "eps_t"], [ssk], scale=1.0 / D, bias=eps_t[:, 0:1])
                          P.op("vector", lambda e, ss=ss: e.reciprocal(out=ss[:, 2:3], in_=ss[:, 1:2]), [ssk], [ssk])
                          STT(tmpf, xt[b2], ss[:, 2:3], a1, ALU.mult, ALU.mult, [xk, ssk, "a1"], ["tmpf"])
                          TT("gpsimd", hb[sub], tmpf, sh1, ALU.add, ["tmpf", "sh1"], [hbk])

                  def p1_tr(ti):
                      for sub in range(4):
                          hbk = ("hb", sub)
                          for kc in range(16):
                              TR(pt[:, kc * 128:(kc + 1) * 128], hb[sub].rearrange("p (c k) -> p k c", k=16)[:, kc, :],
                                 ident_b[:], [hbk, "ident_b"], [PT])
                          CP("scalar" if sub % 2 else "vector", hTt[:, :, sub * 128:(sub + 1) * 128],
                             pt[:, :].rearrange("p (k t) -> p k t", k=16), [PT], [hk])

                  def p1_mm(ti):
                      st = stg[0]
                      sk = ("stg", 0)
                      c0 = ti * 512

                      def mmgrp(j, pi):
                          for kc in range(16):
                              MM(pm[pi][:, :], wi[:, kc, j * 128:(j + 1) * 128], hTt[:, kc, :], kc == 0, kc == 15,
                                 ["wi", hk], [PM[pi]])

                      pi = 0
                      for j in range(4):
                          mmgrp(j, pi)
                          CP("scalar", st[:, j, :], pm[pi][:, :], [PM[pi]], [sk])
                          pi = (pi + 1) % 4
                      for j in range(8):
                          pa = pi
                          mmgrp(4 + j, pa)
                          pb = (pi + 1) % 4
                          mmgrp(12 + j, pb)
                          sg = sgt[j % 2]
                          sgk = ("sg", j % 2)
                          ACT(sg, pm[pb][:, :], AF.Sigmoid, [PM[pb]], [sgk])
                          TT("vector", st[:, 4 + j, :], pm[pa][:, :], sg, ALU.mult, [PM[pa], sgk], [sk])
                          pi = (pi + 2) % 4
                      for j in range(4):
                          mmgrp(20 + j, pi)
                          CP("scalar", st[:, 12 + j, :], pm[pi][:, :], [PM[pi]], [sk])
                          pi = (pi + 1) % 4
                      DMA(STQ, zaT[q][:, c0:c0 + 512].rearrange("(j p) t -> p j t", p=128), st[:, 0:4, :],
                          [sk], [("zaT", q, ti)], "stg0")
                      DMA(STQ, vT[q][:, c0:c0 + 512].rearrange("(j p) t -> p j t", p=128), st[:, 4:12, :],
                          [sk], [("vT", q, ti)], "stg0")
                      DMA(STQ, zcT[q][:, c0:c0 + 512].rearrange("(j p) t -> p j t", p=128), st[:, 12:16, :],
                          [sk], [("zcT", q, ti)], "stg0")

                  p1_norm(0)
                  p1_tr(0)
                  for ti in range(NT5):
                      if ti + 1 < NT5:
                          p1_norm(ti + 1)
                      p1_mm(ti)
                      if ti + 1 < NT5:
                          p1_tr(ti + 1)

                  stop_here('P1')
                  P.barrier()
                  A16.reset(); A32.reset()
                  pwf = A32.alloc(4 * 128).rearrange("p (g e) -> p g e", g=4)
                  pwb = A16.alloc(4 * 128).rearrange("p (g e) -> p g e", g=4)
                  DMA("sync", pwf, pool_w[l].rearrange("g c e -> c g e"), [], ["pwf"], "pwf")
                  CP("vector", pwb, pwf, ["pwf"], ["pwb"])
                  psc = A32.alloc(4)
                  pscr = A32.alloc(4 * 128).rearrange("p (g e) -> p g e", g=4)[0:4]
                  DMA("sync", pscr[:, 0, :], pool_scale[l].rearrange("(g e) -> g e", g=4), [], ["pscr"], "pscr")
                  TR(pr[0][:, 0:4], pscr[:, 0, :], ident_f[0:4, 0:4], ["pscr", "ident_f"], [PR[0]])
                  CP("vector", psc, pr[0][:, 0:4], [PR[0]], ["psc"])
                  pcst = A32.alloc(64).rearrange("p (g j) -> p g j", g=4)
                  DMA("sync", pcst, k_poolc, [], ["pcst"], "pcst")
                  ub = [A16.alloc(528) for _ in range(2)]
                  sa = [A32.alloc(528) for _ in range(2)]
                  sb_ = [A32.alloc(528) for _ in range(2)]
                  dmt = [A16.alloc(512) for _ in range(2)]
                  pst2 = [A16.alloc(4 * 512).rearrange("p (g t) -> p g t", g=4) for _ in range(2)]
                  it = 0
                  for ti in range(NT5):
                      c0 = ti * 512
                      st = pst2[ti % 2]
                      sk = ("pst2", ti % 2)
                      for g, w in enumerate((2, 4, 8, 16)):
                          b2 = it % 2
                          it += 1
                          u = ub[b2]
                          uk = ("ub", b2)
                          lo = max(c0 - 8, 0)
                          hi = min(c0 + 520, S)
                          rd = [("zaT", q, tj) for tj in range(max(ti - 1, 0), min(ti + 2, NT5))]
                          if lo > c0 - 8:
                              MEMSET("gpsimd", u[:, 0:8], 0.0, [uk])
                          if hi < c0 + 520:
                              MEMSET("gpsimd", u[:, 520:528], 0.0, [uk])
                          DMA("sync", u[:, lo - (c0 - 8):hi - (c0 - 8)], zaT[q][g * 128:(g + 1) * 128, lo:hi], rd, [uk], "ub%d" % b2)
                          s_a, s_b = sa[b2], sb_[b2]
                          ka, kb = ("sa", b2), ("sb", b2)
                          TT("vector", s_a[:, 1:528], u[:, 0:527], u[:, 1:528], ALU.add, [uk], [ka])
                          cur, curk, oth, othk = s_a, ka, s_b, kb
                          lo_v = 1
                          hi_v = 528
                          step = 1
                          ww = 2
                          while ww < w:
                              nlo = lo_v + step
                              nhi = hi_v - step
                              TT("vector", oth[:, nlo:nhi], cur[:, nlo - step:nhi - step], cur[:, nlo + step:nhi + step],
                                 ALU.add, [curk], [othk])
                              cur, curk, oth, othk = oth, othk, cur, curk
                              lo_v, hi_v = nlo, nhi
                              step *= 2
                              ww *= 2
                          dm = dmt[b2]
                          dk_ = ("dm", b2)
                          STT(dm, cur[:, 8:520], 1.0 / w, u[:, 8:520], ALU.mult, ALU.subtract, [curk, uk], [dk_])
                          if ti == 0:
                              TT("vector", oth[:, 8:16], cur[:, 8:16], pcst[:, g, 0:8], ALU.mult, [curk, "pcst"], [othk])
                              TT("vector", dm[:, 0:8], oth[:, 8:16], u[:, 8:16], ALU.subtract, [othk, uk], [dk_])
                          if ti == NT5 - 1:
                              TT("vector", oth[:, 512:520], cur[:, 512:520], pcst[:, g, 8:16], ALU.mult, [curk, "pcst"], [othk])
                              TT("vector", dm[:, 504:512], oth[:, 512:520], u[:, 512:520], ALU.subtract, [othk, uk], [dk_])
                          pi = g
                          MM(pm[pi][:, :], pwb[:, g, :], dm, True, True, ["pwb", dk_], [PM[pi]])
                          ACT(st[:, g, :], pm[pi][:, :], AF.Copy, [PM[pi], "psc"], [sk], scale=psc[:, g:g + 1])
                      DMA(STQ, mixT[q][0:512, c0:c0 + 512].rearrange("(g p) t -> p g t", p=128), st, [sk],
                          [("mixT", q, ti, 0)], "pst2%d" % (ti % 2))

                  P.barrier()
                  A16.reset(); A32.reset()
                  pww = A16.alloc(8 * 1024).rearrange("p (k n) -> p k n", k=8)
                  DMA("gpsimd", pww, conv_pw_w[l].rearrange("(k p) n -> p k n", p=128), [], ["pww"], "pww")
                  dwr = A32.alloc(1024)
                  DMA("sync", dwr[0:CONV_K, :], conv_dw_w[l], [], ["dwr"], "dwr")
                  dwT = A32.alloc(8 * 32).rearrange("p (j k) -> p j k", j=8)
                  for j in range(8):
                      TR(pr[0][:, j * 32:j * 32 + CONV_K], dwr[0:CONV_K, j * 128:(j + 1) * 128],
                         ident_f[0:CONV_K, 0:CONV_K], ["dwr", "ident_f"], [PR[0]])
                  CP("vector", dwT[:, :, 0:CONV_K], pr[0][:, 0:256].rearrange("p (j k) -> p j k", j=8)[:, :, 0:CONV_K],
                     [PR[0]], ["dwT"])
                  vr = A32.alloc(4 * 1024).rearrange("p (v n) -> p v n", v=4)
                  vecs = A32.alloc(32).rearrange("p (v j) -> p v j", v=4)
                  for vi, src in enumerate((conv_dw_b, conv_ln_g, conv_ln_b, conv_pw_b)):
                      DMA("sync", vr[0:8, vi, 0:128], src[l].rearrange("(j p) -> j p", p=128), [], [("vr", vi)], "vr%d" % vi)
                      TR(pr[1][:, vi * 8:vi * 8 + 8], vr[0:8, vi, 0:128], ident_f[0:8, 0:8], [("vr", vi), "ident_f"], [PR[1]])
                  CP("vector", vecs, pr[1][:, 0:32].rearrange("p (v j) -> p v j", v=4), [PR[1]], ["vecs"])
                  acc = [A32.alloc(512) for _ in range(8)]
                  cbf = [A16.alloc(512) for _ in range(2)]
                  sqb = [A16.alloc(512) for _ in range(2)]
                  sT = [A16.alloc(512) for _ in range(8)]
                  mean = A32.alloc(512)
                  var = A32.alloc(512)
                  rstd = A32.alloc(512)
                  xn = [A32.alloc(512) for _ in range(2)]
                  st3 = [A16.alloc(8 * 512).rearrange("p (j t) -> p j t", j=8) for _ in range(2)]
                  if CONV_PE:
                      vb = [A16.alloc(544) for _ in range(3)]
                      vb1 = [A16.alloc(544) for _ in range(3)]
                      dg = A16.alloc(8 * 32 * 128).rearrange("p (j k c) -> p j k c", j=8, k=32)
                      for j in range(8):
                          for k in range(CONV_K):
                              TS("vector" if (j * CONV_K + k) % 2 else "gpsimd", dg[:, j, k, :], ident_b[:, :], dwT[:, j, k:k + 1], None,
                                 ALU.mult, None, ["ident_b", "dwT"], [("dg", j)])
                  else:
                      vb = [A16.alloc(544) for _ in range(2)]
                  it = 0
                  for ti in range(NT5):
                      c0 = ti * 512
                      for j in range(8):
                          if CONV_PE:
                              b2 = it % 2
                              b3 = it % 3
                              it += 1
                              v, vk = vb[b3], ("vb", b3)
                              v1, v1k = vb1[b3], ("vb1", b3)
                              lo = max(c0 - 15, 0)
                              hi = min(c0 + 527, S)
                              rd = [("vT", q, tj) for tj in range(max(ti - 1, 0), min(ti + 2, NT5))]
                              if lo > c0 - 15:
                                  MEMSET("gpsimd", v[:, 0:16], 0.0, [vk])
                                  MEMSET("gpsimd", v1[:, 0:16], 0.0, [v1k])
                              if hi < c0 + 527:
                                  MEMSET("gpsimd", v[:, 526:544], 0.0, [vk])
                                  MEMSET("gpsimd", v1[:, 526:544], 0.0, [v1k])
                              DMA("sync", v[:, lo - (c0 - 15):hi - (c0 - 15)], vT[q][j * 128:(j + 1) * 128, lo:hi], rd, [vk], "vb%d" % b3)
                              lo1 = max(c0 - 14, 0)
                              DMA("sync", v1[:, lo1 - (c0 - 14):hi - (c0 - 14)], vT[q][j * 128:(j + 1) * 128, lo1:hi], rd, [v1k], "vb1%d" % b3)
                              pi = it % 4
                              for k in range(CONV_K):
                                  if k % 2 == 0:
                                      MM(pm[pi][:, :], dg[:, j, k, :], v[:, k:k + 512], k == 0, k == CONV_K - 1,
                                         [("dg", j), vk], [PM[pi]])
                                  else:
                                      MM(pm[pi][:, :], dg[:, j, k, :], v1[:, k - 1:k - 1 + 512], k == 0, k == CONV_K - 1,
                                         [("dg", j), v1k], [PM[pi]])
                              a = acc[j]
                              ak = ("acc", j)
                              ACT(a, pm[pi][:, :], AF.Identity, [PM[pi], "vecs"], [ak], bias=vecs[:, 0, j:j + 1])
                              cb_, ck = cbf[b2], ("cbf", b2)
                              sq_, sqk = sqb[b2], ("sqb", b2)
                              CP("vector", cb_, a, [ak], [ck])
                              ACT(sq_, a, AF.Square, [ak], [sqk])
                              MM(pr[0][:, :], ones_b[:], cb_, j == 0, j == 7, ["ones_b", ck], [PR[0]])
                              MM(pr[1][:, :], ones_b[:], sq_, j == 0, j == 7, ["ones_b", sqk], [PR[1]])
                          else:
                              b2 = it % 2
                              it += 1
                              v = vb[b2]
                              vk = ("vb", b2)
                              lo = max(c0 - 15, 0)
                              hi = min(c0 + 527, S)
                              rd = [("vT", q, tj) for tj in range(max(ti - 1, 0), min(ti + 2, NT5))]
                              if lo > c0 - 15:
                                  MEMSET("gpsimd", v[:, 0:15], 0.0, [vk])
                              if hi < c0 + 527:
                                  MEMSET("gpsimd", v[:, 527:542], 0.0, [vk])
                              DMA("sync", v[:, lo - (c0 - 15):hi - (c0 - 15)], vT[q][j * 128:(j + 1) * 128, lo:hi], rd, [vk], "vb%d" % b2)
                              a = acc[j]
                              ak = ("acc", j)
                              TS("vector", a, v[:, 0:512], dwT[:, j, 0:1], vecs[:, 0, j:j + 1], ALU.mult, ALU.add,
                                 [vk, "dwT", "vecs"], [ak])
                              for k in range(1, CONV_K):
                                  STT(a, v[:, k:k + 512], dwT[:, j, k:k + 1], a, ALU.mult, ALU.add, [vk, "dwT", ak], [ak])
                              cb_, ck = cbf[b2], ("cbf", b2)
                              sq_, sqk = sqb[b2], ("sqb", b2)
                              CP("gpsimd", cb_, a, [ak], [ck])
                              ACT(sq_, a, AF.Square, [ak], [sqk])
                              MM(pr[0][:, :], ones_b[:], cb_, j == 0, j == 7, ["ones_b", ck], [PR[0]])
                              MM(pr[1][:, :], ones_b[:], sq_, j == 0, j == 7, ["ones_b", sqk], [PR[1]])
                      TS("vector", mean, pr[0][:, :], 1.0 / 1024, None, ALU.mult, None, [PR[0]], ["mean"])
                      TS("vector", var, pr[1][:, :], 1.0 / 1024, None, ALU.mult, None, [PR[1]], ["var"])
                      TT("vector", rstd, mean, mean, ALU.mult, ["mean"], ["rstd"])
                      TT("vector", var, var, rstd, ALU.subtract, ["var", "rstd"], ["var"])
                      ACT(var, var, AF.Sqrt, ["var", "eps_t"], ["var"], bias=eps_t[:, 0:1], scale=1.0)
                      P.op("vector", lambda e, rstd=rstd, var=var: e.reciprocal(out=rstd, in_=var), ["var"], ["rstd"])
                      for j in range(8):
                          x_, xk_ = xn[j % 2], ("xn", j % 2)
                          TT("gpsimd", x_, acc[j], mean, ALU.subtract, [("acc", j), "mean"], [xk_])
                          TT("vector", x_, x_, rstd, ALU.mult, [xk_, "rstd"], [xk_])
                          ACT(sT[j], x_, AF.Silu, [xk_, "vecs"], [("sT", j)], scale=vecs[:, 1, j:j + 1], bias=vecs[:, 2, j:j + 1])
                      st = st3[ti % 2]
                      sk = ("st3", ti % 2)
                      for e_ in range(8):
                          pi = e_ % 4
                          for j in range(8):
                              MM(pm[pi][:, :], pww[:, j, e_ * 128:(e_ + 1) * 128], sT[j], j == 0, j == 7,
                                 ["pww", ("sT", j)], [PM[pi]])
                          ACT(st[:, e_, :], pm[pi][:, :], AF.Identity, [PM[pi], "vecs"], [sk], bias=vecs[:, 3, e_:e_ + 1])
                      DMA(STQ, mixT[q][512:1536, c0:c0 + 512].rearrange("(j p) t -> p j t", p=128), st, [sk],
                          [("mixT", q, ti, 1)], "st3%d" % (ti % 2))

                  stop_here('P3')
                  P.barrier()
                  A16.reset(); A32.reset()
                  cs = A16.alloc(2 * 192).rearrange("p (h n) -> p h n", h=2)
                  ccn = A16.alloc(2 * 128).rearrange("p (h n) -> p h n", h=2)
                  fcs = A16.alloc(2 * S).rearrange("p (h n) -> p h n", h=2)
                  DMA("sync", cs, k_cs128, [], ["cs"], "cs")
                  DMA("sync", ccn, k_ccn, [], ["ccn"], "ccn")
                  DMA("sync", fcs[0:S2], k_fcs[q], [], ["fcs"], "fcs")
                  fwf = A32.alloc(4 * 512).rearrange("p (h e) -> p h e", h=4)
                  fwb = A16.alloc(4 * 512).rearrange("p (h e) -> p h e", h=4)
                  DMA("sync", fwf, fourier_w[l].rearrange("(h m) e -> m h e", h=4), [], ["fwf"], "fwf")
                  CP("vector", fwb, fwf, ["fwf"], ["fwb"])
                  Ut = A16.alloc(128 * S2).rearrange("p (c t) -> p c t", c=128)
                  Ah = A16.alloc(128 * 192).rearrange("p (c n) -> p c n", c=128)
                  Xh = A16.alloc(2 * S).rearrange("p (h k) -> p h k", h=2)
                  rst = [A16.alloc(512) for _ in range(2)]
                  ALLZC = [("zcT", q, tj) for tj in range(NT5)]
                  nrm = 1.0 / math.sqrt(S * 128.0)
                  ri = 0
                  for h in range(4):
                      for cq in range(8):
                          DMA("sync", Ut[:, cq * 16:(cq + 1) * 16, :],
                              zcT[q][h * 128 + cq * 16:h * 128 + (cq + 1) * 16, :].rearrange("c (a b) -> a c b", b=S2),
                              ALLZC, ["Ut"], "Ut")
                      for half in range(2):
                          pmv = [pm[i] for i in range(4)]
                          for cg in range(16):
                              for ci in range(8):
                                  c_ = cg * 8 + ci
                                  bank = ci // 2
                                  col = (ci % 2) * 192
                                  MM(pmv[bank][0:S2, col:col + 192], Ut[:, c_, :], cs[:, half, :], True, True,
                                     ["Ut", "cs"], [PM[bank]])
                              for bank in range(4):
                                  eng = "vector" if bank % 2 == 0 else "scalar"
                                  CP(eng, Ah[0:S2, cg * 8 + bank * 2:cg * 8 + bank * 2 + 2, :],
                                     pmv[bank][0:S2, 0:384].rearrange("p (c n) -> p c n", c=2), [PM[bank]], [("Ah", cg)])
                          AHK = [("Ah", cg) for cg in range(16)]
                          G = 512 // S2
                          G = min(G, 64)
                          for kg in range(64 // G):
                              pxr, pxi = pr[0], pr[1]
                              for gi in range(G):
                                  k1l = kg * G + gi
                                  k1 = half * 64 + k1l
                                  fc_ = fcs[0:S2, 0, :].rearrange("p (b a) -> p a b", a=128)[:, k1, :]
                                  fs_ = fcs[0:S2, 1, :].rearrange("p (b a) -> p a b", a=128)[:, k1, :]
                                  ar = Ah[0:S2, :, k1l]
                                  ai = Ah[0:S2, :, 64 + k1l]
                                  an = Ah[0:S2, :, 128 + k1l]
                                  o = gi * S2
                                  MM(pxr[:, o:o + S2], ar, fc_, True, False, AHK + ["fcs"], [PR[0]])
                                  MM(pxr[:, o:o + S2], an, fs_, False, True, AHK + ["fcs"], [PR[0]])
                                  MM(pxi[:, o:o + S2], ar, fs_, True, False, AHK + ["fcs"], [PR[1]])
                                  MM(pxi[:, o:o + S2], ai, fc_, False, True, AHK + ["fcs"], [PR[1]])
                              k1b = half * 64 + kg * G
                              for ri_, px in enumerate((pxr, pxi)):
                                  dst = Xh[:, ri_, :].rearrange("p (b a) -> p a b", a=128)[:, k1b:k1b + G, :]
                                  src = px[:, 0:G * S2].rearrange("p (g b) -> p g b", g=G)
                                  CP("vector" if ri_ == 0 else "scalar", dst, src, [PR[ri_]], [("Xh", half, kg)])
                      XHK = [("Xh", hf, kg) for hf in range(2) for kg in range(64 // G)]
                      for kt in range(NT5):
                          pi = kt % 4
                          MM(pm[pi][:, :], ccn[:, 0, :], Xh[:, 0, kt * 512:(kt + 1) * 512], True, False, ["ccn"] + XHK, [PM[pi]])
                          MM(pm[pi][:, :], ccn[:, 1, :], Xh[:, 1, kt * 512:(kt + 1) * 512], False, True, ["ccn"] + XHK, [PM[pi]])
                          r_ = rst[ri % 2]
                          rk = ("rst", ri % 2)
                          ACT(r_, pm[pi][:, :], AF.Copy, [PM[pi]], [rk], scale=nrm)
                          DMA(STQ, rzT[q][h * 128:(h + 1) * 128, kt * 512:(kt + 1) * 512], r_, [rk], [("rzT", q, h, kt)],
                              "rst%d" % (ri % 2))
                          ri += 1
                  rzb = [A16.alloc(4 * 512).rearrange("p (h t) -> p h t", h=4) for _ in range(2)]
                  st4 = [A16.alloc(4 * 512).rearrange("p (e t) -> p e t", e=4) for _ in range(2)]
                  for ti in range(NT5):
                      c0 = ti * 512
                      rb, rbk = rzb[ti % 2], ("rzb", ti % 2)
                      DMA("sync", rb, rzT[q][:, c0:c0 + 512].rearrange("(h m) t -> m h t", h=4),
                          [("rzT", q, h, ti) for h in range(4)], [rbk], "rzb%d" % (ti % 2))
                      st, sk = st4[ti % 2], ("st4", ti % 2)
                      for e_ in range(4):
                          pi = e_
                          for h in range(4):
                              MM(pm[pi][:, :], fwb[:, h, e_ * 128:(e_ + 1) * 128], rb[:, h, :], h == 0, h == 3,
                                 ["fwb", rbk], [PM[pi]])
                          CP("scalar" if e_ % 2 else "vector", st[:, e_, :], pm[pi][:, :], [PM[pi]], [sk])
                      DMA(STQ, mixT[q][1536:2048, c0:c0 + 512].rearrange("(e p) t -> p e t", p=128), st, [sk],
                          [("mixT", q, ti, 2)], "st4%d" % (ti % 2))

                  stop_here('P4')
                  P.barrier()
                  A16.reset(); A32.reset()
                  wo = A16.alloc(16 * 2048).rearrange("p (k n) -> p k n", k=16)
                  DMA("gpsimd", wo, w_out[l].rearrange("(k p) n -> p k n", p=128), [], ["wo"], "wo")
                  g1 = A32.alloc(D)
                  a2 = A32.alloc(D)
                  sh2 = A32.alloc(D)
                  tmpf = A32.alloc(D)
                  TFK = [("tmpf", i) for i in range(4)]
                  DMA("sync", g1, mod_rep(q, 2), MODK, ["g1"], "g1")
                  DMA("sync", a2, mod_rep(q, 4), MODK, ["a2"], "a2")
                  DMA("sync", sh2, mod_rep(q, 3), MODK, ["sh2"], "sh2")
                  DMA("sync", tmpf, norm2_g[l:l + 1, :].broadcast_to([128, D]), [], TFK, "g_rep")
                  STT(a2, a2, 1.0, tmpf, ALU.add, ALU.mult, ["a2"] + TFK, ["a2"])
                  wr = A32.alloc(16 * 36).rearrange("p (k n) -> p k n", k=16)
                  DMA("sync", wr[:, :, 0:4], rc_w[l].rearrange("(p k) g -> p k g", k=16), [], [("wr", 0)], "wr")
                  for g in range(4):
                      DMA("sync", wr[:, :, 4 + 8 * g:12 + 8 * g], rf_w[l, g].rearrange("(p k) e -> p k e", k=16), [],
                          [("wr", 1 + g)], "wr")
                  WRK = [("wr", i) for i in range(5)]
                  rb_ = A32.alloc(36)
                  DMA("sync", rb_[:, 0:4], rc_b[l:l + 1, :].broadcast_to([128, 4]), [], [("rb", 0)], "rb")
                  DMA("sync", rb_[:, 4:36], rf_b[l:l + 1].rearrange("o g e -> o (g e)").broadcast_to([128, 32]), [], [("rb", 1)], "rb")
                  RBK = [("rb", 0), ("rb", 1)]
                  mx = [A16.alloc(16 * 512).rearrange("p (k t) -> p k t", k=16) for _ in range(2)]
                  xt = [A32.alloc(D) for _ in range(1)]
                  x1t = [A32.alloc(D) for _ in range(2)]
                  h2f2 = [A32.alloc(D) for _ in range(2)]
                  h2b = [A16.alloc(D) for _ in range(2)]
                  h2T = A32.alloc(16 * 128).rearrange("p (k t) -> p k t", k=16)
                  sm2 = [A32.alloc(256) for _ in range(3)]
                  ssn = A32.alloc(8)
                  if q == 0:
                      MEMSET("vector", cnt_run[:], 0.0, ["cnt_run"])

                  def p5_main(it):
                      ti, sub = it // 4, it % 4
                      c0 = ti * 512
                      m_, mk_ = mx[ti % 2], ("mx", ti % 2)
                      if sub == 0:
                          DMA("sync", m_, mixT[q][:, c0:c0 + 512].rearrange("(k p) t -> p k t", p=128),
                              [("mixT", q, ti, i) for i in range(3)], [mk_], "mx%d" % (ti % 2))
                      b2 = it % 2
                      r0 = T0 + it * 128
                      xk = ("xt", 0)
                      DMA("sync", xt[0], cur_x[r0:r0 + 128, :], [("xcur", r0 // 128)], [xk], "xt0")
                      x1, x1k = x1t[b2], ("x1t", b2)
                      for cbk in range(4):
                          pi = cbk
                          for kc in range(16):
                              MM(pm[pi][:, :], m_[:, kc, sub * 128:(sub + 1) * 128], wo[:, kc, cbk * 512:(cbk + 1) * 512],
                                 kc == 0, kc == 15, [mk_, "wo"], [PM[pi]])
                          sl = slice(cbk * 512, (cbk + 1) * 512)
                          TT("vector", tmpf[:, sl], pm[pi][:, :], g1[:, sl], ALU.mult, [PM[pi], "g1"], [("tmpf", cbk)])
                          TT("gpsimd", x1[:, sl], tmpf[:, sl], xt[0][:, sl], ALU.add, [("tmpf", cbk), xk], [x1k])
                      DMA(STQ, x1_d[r0:r0 + 128, :], x1, [x1k], [("x1", r0 // 128)], "x1t%d" % b2)
                      ssk = U("ss")
                      ss = ssn[:, 0:4]
                      hb_, hbk = h2b[b2], ("h2b", b2)
                      h2f, h2fk = h2f2[b2], ("h2f", b2)
                      ACT(hb_, x1, AF.Square, [x1k], [hbk, ssk], accum_out=ss[:, 0:1])
                      ACT(ss[:, 1:2], ss[:, 0:1], AF.Sqrt, [ssk, "eps_t"], [ssk], scale=1.0 / D, bias=eps_t[:, 0:1])
                      P.op("vector", lambda e, ss=ss: e.reciprocal(out=ss[:, 2:3], in_=ss[:, 1:2]), [ssk], [ssk])
                      STT(tmpf, x1, ss[:, 2:3], a2, ALU.mult, ALU.mult, [x1k, ssk, "a2"], [("tmpf", i) for i in range(4)])
                      TT("gpsimd", h2f, tmpf, sh2, ALU.add, [("tmpf", i) for i in range(4)] + ["sh2"], [h2fk])
                      CP("scalar", hb_, h2f, [h2fk], [hbk])
                      DMA(STQ, h2_d[r0:r0 + 128, :], hb_, [hbk], [("h2", r0 // 128)], "h2b%d" % b2)

                  def p5_router(it):
                      git = (T0 // 128) + it
                      b2 = it % 2
                      h2f, h2fk = h2f2[b2], ("h2f", b2)
                      for kc in range(16):
                          pj = pr[(kc // 4) % 2]
                          TR(pj[:, (kc % 4) * 128:(kc % 4 + 1) * 128], h2f.rearrange("p (c k) -> p k c", k=16)[:, kc, :],
                             ident_f[:], [h2fk, "ident_f"], [PR[(kc // 4) % 2]])
                          if kc % 4 == 3:
                              g4 = kc // 4
                              CP("scalar" if g4 % 2 else "vector", h2T[:, g4 * 4:g4 * 4 + 4, :],
                                 pj[:, :].rearrange("p (k t) -> p k t", k=4), [PR[(kc // 4) % 2]], [("h2T", g4)])
                      for kc in range(16):
                          MM(pr[0][:, 0:36], h2T[:, kc, :], wr[:, kc, :], kc == 0, kc == 15,
                             [("h2T", kc // 4)] + WRK, [PR[0]])
                      rkeys[it] = U("rt")
                      route_tile(P, nc, sm2[it % 3], pr, PR, rb_, RBK, r_E, r_f, cnt_run, utinc, ones_b, git,
                                 TT, TS, STT, ACT, CP, RED, MM, U, part="A", rk=rkeys[it])

                  def p5_routerB(it):
                      git = (T0 // 128) + it
                      route_tile(P, nc, sm2[it % 3], pr, PR, rb_, RBK, r_E, r_f, cnt_run, utinc, ones_b, git,
                                 TT, TS, STT, ACT, CP, RED, MM, U, part="B", rk=rkeys[it])

                  rkeys = {}
                  NSUB = NT5 * 4
                  for it in range(NSUB):
                      p5_main(it)
                      if it > 0:
                          p5_router(it - 1)
                      if it > 1:
                          p5_routerB(it - 2)
                  p5_router(NSUB - 1)
                  if NSUB > 1:
                      p5_routerB(NSUB - 2)
                  p5_routerB(NSUB - 1)

              stop_here('P5')
              P.barrier()
              A16.reset(); A32.reset()
              fin = A32.alloc(8 * 32).rearrange("p (a e) -> p a e", a=8)
              RALL = ["cnt_run"]
              TS("vector", fin[:, 0, :], cnt_run[:], 127.0, None, ALU.add, None, ["cnt_run"], ["fin"])
              fin_i = A32.alloc(32).bitcast(I32)
              CP("vector", fin_i, fin[:, 0, :], ["fin"], ["fin_i"])
              TS("vector", fin_i, fin_i, 7, 7, ALU.arith_shift_right, ALU.logical_shift_left, ["fin_i"], ["fin_i"])
              CP("vector", fin[:, 2, :], fin_i, ["fin_i"], ["fin"])
              MEMSET("vector", fin[:, 3, :], 1.0, ["fin"])
              P.op("vector", lambda e: e.tensor_tensor_scan(out=fin[:, 4, :], data0=fin[:, 3, :], data1=fin[:, 2, :],
                                                            initial=0.0, op0=ALU.mult, op1=ALU.add), ["fin"], ["fin"])
              TT("vector", fin[:, 5, :], fin[:, 4, :], fin[:, 2, :], ALU.subtract, ["fin"], ["fin"])
              big = A32.alloc(NT * 32).rearrange("p (t e) -> p t e", e=32)
              slf = A32.alloc(NT * 2).rearrange("p (t k) -> p t k", k=2)
              for k in range(2):
                  TT("vector", big, r_E[:, :, k * 32:(k + 1) * 32], fin[:, 5:6, :].to_broadcast([128, NT, 32]), ALU.mult,
                     ["fin", "r_E"], ["big"])
                  RED(slf[:, :, k], big, ALU.add, ["big"], ["slf"])
                  TT("vector", slf[:, :, k], slf[:, :, k], r_f[:, :, k], ALU.add, ["slf", "r_f"], ["slf"])
              CP("vector", r_slot[:], slf, ["slf"], ["r_slot"])
              bst = A32.alloc(NB)
              DMA("sync", bst, k_bstart, [], ["bst"], "bst")
              ebf = A32.alloc(NB)
              CH = 32
              bigb = A32.alloc(CH * 32).rearrange("p (b e) -> p b e", e=32)
              for b0 in range(0, NB, CH):
                  nb_ = min(CH, NB - b0)
                  TT("vector", bigb[:, 0:nb_, :], fin[:, 4:5, :].to_broadcast([128, nb_, 32]),
                     bst[:, b0:b0 + nb_].unsqueeze(2).to_broadcast([128, nb_, 32]), ALU.is_le, ["fin", "bst"], ["bigb"])
                  RED(ebf[:, b0:b0 + nb_], bigb[:, 0:nb_, :], ALU.add, ["bigb"], ["ebf"])
              TS("vector", ebf, ebf, 31.0, None, ALU.min, None, ["ebf"], ["ebf"])
              sam = A32.alloc(NB)
              MEMSET("vector", sam[:, 0:1], 0.0, ["sam"])
              TT("vector", sam[:, 1:NB], ebf[:, 1:NB], ebf[:, 0:NB - 1], ALU.is_equal, ["ebf"], ["sam"])
              wif = A32.alloc(NB)
              TS("vector", wif, ebf, 128.0, iotap[:, 0:1], ALU.mult, ALU.add, ["ebf", "iotap"], ["wif"])
              STT(wif, sam, 1.0e6, wif, ALU.mult, ALU.add, ["sam", "wif"], ["wif"])
              if l > 0:
                  TS("vector", wif, wif, float(l * NE * 128), None, ALU.add, None, ["wif"], ["wif"])
              CP("vector", widx[:], wif, ["wif"], ["widx"])
              hl = [A16.alloc(D) for _ in range(3)]
              for it in range(NT):
                  b3 = it % 3
                  hk_ = ("hl", b3)
                  DMA("sync", hl[b3], h2_d[it * 128:(it + 1) * 128, :], [("h2", it)], [hk_], "hl%d" % b3)
                  for k in range(2):
                      off = r_slot[:, it, k:k + 1]
                      P.dma("gpsimd", (lambda e, off=off, src=hl[b3]: e.indirect_dma_start(
                          out=xs_d, out_offset=bass.IndirectOffsetOnAxis(ap=off, axis=0), in_=src, in_offset=None,
                          bounds_check=BR(e, L - 1), oob_is_err=False)), [hk_, "r_slot"], [("xs", it, k)], "xs_sc")
              XSK = [("xs", it, k) for it in range(NT) for k in range(2)]

              P.barrier()
              A16.reset(); A32.reset()
              W1 = A16.alloc(16 * 512)
              W3 = A16.alloc(16 * 512)
              W2 = A16.alloc(4 * 2048)
              xb = [A16.alloc(D) for _ in range(3)]
              xbT = [A16.alloc(16 * 128).rearrange("p (k t) -> p k t", k=16) for _ in range(2)]

              def xload(b):
                  DMA("sync", xb[b % 3], xs_d[b * 128:(b + 1) * 128, :], XSK, [("xb", b % 3)], "xb%d" % (b % 3))

              sgf = [A32.alloc(512) for _ in range(2)]
              hid = [A16.alloc(512) for _ in range(2)]
              hidT = [A16.alloc(4 * 128).rearrange("p (k t) -> p k t", k=4) for _ in range(2)]
              yb = [A32.alloc(D) for _ in range(2)]
              w1v = e_w1.rearrange("l e (p k) n -> (l e p) (k n)", k=16)
              w3v = e_w3.rearrange("l e (p k) n -> (l e p) (k n)", k=16)
              w2v = e_w2.rearrange("l e (p k) n -> (l e p) (k n)", k=4)
              bnd_l = (l + 1) * NE * 128 - 1

              def wgather(b, Wt, wv, wk):
                  off = widx[:, b:b + 1]
                  P.dma("gpsimd", (lambda e, off=off, Wt=Wt, wv=wv, bnd=bnd_l: e.indirect_dma_start(
                      out=Wt, out_offset=None, in_=wv, in_offset=bass.IndirectOffsetOnAxis(ap=off, axis=0),
                      bounds_check=BR(e, bnd), oob_is_err=False)), ["widx"], [wk], wk)

              def stageA(b):
                  b2 = b % 2
                  wgather(b, W1, w1v, "W1")
                  wgather(b, W3, w3v, "W3")
                  xk = ("xb", b % 3)
                  if b + 1 < NB:
                      xload(b + 1)
                  for kc in range(16):
                      TR(pt[:, kc * 128:(kc + 1) * 128], xb[b % 3].rearrange("p (c k) -> p k c", k=16)[:, kc, :], ident_b[:],
                         [xk, "ident_b"], [PT])
                  xT, xTk = xbT[b2], ("xbT", b2)
                  CP("scalar", xT[:, 0:8, :], pt[:, 0:1024].rearrange("p (k t) -> p k t", k=8), [PT], [xTk])
                  CP("vector", xT[:, 8:16, :], pt[:, 1024:2048].rearrange("p (k t) -> p k t", k=8), [PT], [xTk])
                  p1, p3 = 2 * b2, 2 * b2 + 1
                  for kc in range(16):
                      MM(pm[p1][:, :], xT[:, kc, :], W1[:, kc * 512:(kc + 1) * 512], kc == 0, kc == 15, [xTk, "W1"], [PM[p1]])
                  for kc in range(16):
                      MM(pm[p3][:, :], xT[:, kc, :], W3[:, kc * 512:(kc + 1) * 512], kc == 0, kc == 15, [xTk, "W3"], [PM[p3]])
                  ACT(sgf[b2], pm[p1][:, :], AF.Silu, [PM[p1]], [("sgf", b2)])
                  TT("vector", hid[b2], pm[p3][:, :], sgf[b2], ALU.mult, [PM[p3], ("sgf", b2)], [("hid", b2)])

              def stageB(b):
                  b2 = b % 2
                  wgather(b, W2, w2v, "W2")
                  for fc in range(4):
                      TR(pt[:, fc * 128:(fc + 1) * 128], hid[b2].rearrange("p (c k) -> p k c", k=4)[:, fc, :], ident_b[:],
                         [("hid", b2), "ident_b"], [PT])
                  CP("vector", hidT[b2], pt[:, 0:512].rearrange("p (k t) -> p k t", k=4), [PT], [("hidT", b2)])
                  y_, yk = yb[b2], ("yb", b2)
                  for cbk in range(4):
                      pi = cbk % 2
                      for fc in range(4):
                          MM(pr[pi][:, :], hidT[b2][:, fc, :], W2[:, fc * 2048 + cbk * 512:fc * 2048 + (cbk + 1) * 512],
                             fc == 0, fc == 3, [("hidT", b2), "W2"], [PR[pi]])
                      if cbk % 2:
                          CP("scalar", y_[:, cbk * 512:(cbk + 1) * 512], pr[pi][:, :], [PR[pi]], [yk])
                      else:
                          CP("vector", y_[:, cbk * 512:(cbk + 1) * 512], pr[pi][:, :], [PR[pi]], [yk])
                  DMA(STQ, ys_d[b * 128:(b + 1) * 128, :], y_, [yk], [("ys", b)], "yb%d" % b2)

              xload(0)
              stageA(0)
              for b in range(NB):
                  if b + 1 < NB:
                      stageA(b + 1)
                  stageB(b)
              YSK = [("ys", b) for b in range(NB)]

              stop_here('P7')
              P.barrier()
              A16.reset(); A32.reset()
              g2 = [A32.alloc(D) for _ in range(2)]
              for q in range(2):
                  DMA("sync", g2[q], mod_rep(q, 5), MODK, [("g2", q)], "g2%d" % q)
              if last:
                  fg = A32.alloc(D)
                  DMA("sync", fg, final_g.rearrange("(o n) -> o n", o=1).broadcast_to([128, D]), [], ["fg"], "fg")
              ya = [A32.alloc(D) for _ in range(2)]
              ybb = [A32.alloc(D) for _ in range(2)]
              x1t = [A32.alloc(D) for _ in range(2)]
              junk = A16.alloc(D)
              ss = A32.alloc(8)
              dst_x = y_out if last else x2_d
              for it in range(NT):
                  b2 = it % 2
                  q = 0 if it * 128 < OFFS[1] else 1
                  for k, yt in enumerate((ya, ybb)):
                      off = r_slot[:, it, k:k + 1]
                      P.dma("gpsimd", (lambda e, off=off, dst=yt[b2]: e.indirect_dma_start(
                          out=dst, out_offset=None, in_=ys_d, in_offset=bass.IndirectOffsetOnAxis(ap=off, axis=0),
                          bounds_check=BR(e, L - 1), oob_is_err=False)), YSK + ["r_slot"], [("yg", k, b2)], "yg%d%d" % (k, b2))
                  xk = ("x1t", b2)
                  DMA("sync", x1t[b2], x1_d[it * 128:(it + 1) * 128, :], [("x1", it)], [xk], "x1l%d" % b2)
                  A_, B_ = ya[b2], ybb[b2]
                  TS("vector", A_, A_, r_f[:, it, 2:3], None, ALU.mult, None, [("yg", 0, b2), "r_f"], [("yg", 0, b2)])
                  STT(A_, B_, r_f[:, it, 3:4], A_, ALU.mult, ALU.add, [("yg", 1, b2), ("yg", 0, b2), "r_f"], [("yg", 0, b2)])
                  TT("gpsimd", A_, A_, g2[q], ALU.mult, [("yg", 0, b2), ("g2", q)], [("yg", 0, b2)])
                  TT("vector", B_, A_, x1t[b2], ALU.add, [("yg", 0, b2), xk], [("yg", 1, b2)])
                  if not last:
                      DMA(STQ, dst_x[it * 128:(it + 1) * 128, :], B_, [("yg", 1, b2)], [("xcur", it)], "xo%d" % b2)
                  else:
                      ssk = U("ss")
                      ACT(junk, B_, AF.Square, [("yg", 1, b2)], ["junk", ssk], accum_out=ss[:, 0:1])
                      ACT(ss[:, 1:2], ss[:, 0:1], AF.Sqrt, [ssk, "eps_t"], [ssk], scale=1.0 / D, bias=eps_t[:, 0:1])
                      P.op("vector", lambda e, ss=ss: e.reciprocal(out=ss[:, 2:3], in_=ss[:, 1:2]), [ssk], [ssk])
                      STT(A_, B_, ss[:, 2:3], fg, ALU.mult, ALU.mult, [("yg", 1, b2), ssk, "fg"], [("yg", 0, b2)])
                      DMA(STQ, dst_x[it * 128:(it + 1) * 128, :], A_, [("yg", 0, b2)], [("yout", it)], "xo%d" % b2)
              cur_x = x2_d

          P.emit()
        except _Stop:
            pass
    return nc


def route_tile(P, nc, sm, pr, PR, rb_, RBK, r_E, r_f, cnt_run, utinc, ones_b, git,
               TT, TS, STT, ACT, CP, RED, MM, U, part="AB", rk=None):
    if "A" not in part:
        Ef = sm[:, 96:160]
        Mb = sm[:, 160:192]
        Mbb = sm[:, 192:224].bitcast(BF16)[:, 0:32]
        return route_tile_b(P, sm, pr, PR, r_f, cnt_run, utinc, ones_b, git, TT, RED, MM, rk, Ef, Mb, Mbb)
    rk = rk if rk is not None else U("rt")
    Lg = sm[:, 8:44]
    TT("vector", Lg, pr[0][:, 0:36], rb_, ALU.add, [PR[0]] + RBK, [rk])
    m = sm[:, 44:45]
    RED(m, Lg[:, 0:4], ALU.max, [rk], [rk])
    oh = sm[:, 48:52]
    TS("vector", oh, Lg[:, 0:4], m, None, ALU.is_equal, None, [rk], [rk])
    negm = sm[:, 45:46]
    TS("vector", negm, m, -1.0, None, ALU.mult, None, [rk], [rk])
    ex = sm[:, 52:56]
    se = sm[:, 46:47]
    ACT(ex, Lg[:, 0:4], AF.Exp, [rk], [rk], bias=negm, scale=1.0, accum_out=se)
    pg = sm[:, 47:48]
    P.op("vector", lambda e: e.reciprocal(out=pg, in_=se), [rk], [rk])
    lf = sm[:, 56:64]
    TS("vector", lf, Lg[:, 4:12], oh[:, 0:1], None, ALU.mult, None, [rk], [rk])
    for g in range(1, 4):
        STT(lf, Lg[:, 4 + 8 * g:12 + 8 * g], oh[:, g:g + 1], lf, ALU.mult, ALU.add, [rk], [rk])
    top = sm[:, 64:72]
    P.op("vector", lambda e: e.max(out=top, in_=lf), [rk], [rk])
    s1 = sm[:, 72:80]
    s2 = sm[:, 80:88]
    TS("vector", s1, lf, top[:, 0:1], None, ALU.is_equal, None, [rk], [rk])
    TS("vector", s2, lf, top[:, 1:2], None, ALU.is_equal, None, [rk], [rk])
    dv = sm[:, 88:89]
    TT("vector", dv, top[:, 0:1], top[:, 1:2], ALU.subtract, [rk], [rk])
    sg = sm[:, 89:90]
    ACT(sg, dv, AF.Sigmoid, [rk], [rk])
    TT("vector", r_f[:, git, 2:3], pg, sg, ALU.mult, [rk], ["r_f"])
    TT("vector", r_f[:, git, 3:4], pg, r_f[:, git, 2:3], ALU.subtract, [rk, "r_f"], ["r_f"])
    Ef = sm[:, 96:160]
    for g in range(4):
        TS("vector", Ef[:, 8 * g:8 * g + 8], s1, oh[:, g:g + 1], None, ALU.mult, None, [rk], [rk])
        TS("vector", Ef[:, 32 + 8 * g:40 + 8 * g], s2, oh[:, g:g + 1], None, ALU.mult, None, [rk], [rk])
    CP("vector", r_E[:, git, :], Ef, [rk], ["r_E"])
    Mb = sm[:, 160:192]
    TT("vector", Mb, Ef[:, 0:32], Ef[:, 32:64], ALU.add, [rk], [rk])
    Mbb = sm[:, 192:224].bitcast(BF16)[:, 0:32]
    CP("vector", Mbb, Mb, [rk], [rk])
    if "B" in part:
        route_tile_b(P, sm, pr, PR, r_f, cnt_run, utinc, ones_b, git, TT, RED, MM, rk, Ef, Mb, Mbb)


def route_tile_b(P, sm, pr, PR, r_f, cnt_run, utinc, ones_b, git, TT, RED, MM, rk, Ef, Mb, Mbb):
    MM(pr[1][:, 0:32], utinc[:], Mbb, True, True, ["utinc", rk], [PR[1]])
    MM(pr[1][:, 32:64], ones_b[:], Mbb, True, True, ["ones_b", rk], [PR[1]])
    rank = sm[:, 224:256]
    TT("vector", rank, pr[1][:, 0:32], Mb, ALU.subtract, [PR[1], rk], [rk])
    TT("vector", rank, rank, cnt_run[:], ALU.add, [rk, "cnt_run"], [rk])
    TT("vector", cnt_run[:], cnt_run[:], pr[1][:, 32:64], ALU.add, [PR[1], "cnt_run"], ["cnt_run"])
    tmp = sm[:, 160:192]
    for k in range(2):
        TT("vector", tmp, Ef[:, 32 * k:32 * k + 32], rank, ALU.mult, [rk], [rk])
        RED(r_f[:, git, k:k + 1], tmp, ALU.add, [rk], ["r_f"])


_WNAMES = ["w_ada", "b_ada", "norm1_g", "w_in", "pool_w", "pool_scale", "conv_dw_w", "conv_dw_b",
           "conv_ln_g", "conv_ln_b", "conv_pw_w", "conv_pw_b", "fourier_w", "w_out", "norm2_g",
           "router_coarse_w", "router_coarse_b", "router_fine_w", "router_fine_b",
           "expert_w1", "expert_w3", "expert_w2", "final_g"]


def kernel(**inputs):
    xs_ = np.asarray(inputs["x_sample"], np.float32)
    xp_ = np.asarray(inputs["x_prompt"], np.float32)
    cs_ = np.asarray(inputs["c_sample"], np.float32)
    cp_ = np.asarray(inputs["c_prompt"], np.float32)
    S0, S1 = xs_.shape[1], xp_.shape[1]
    depth = inputs["w_ada"].shape[0]
    nc = build((S0, S1), depth)
    N = S0 + S1
    NB = -(-(2 * N + NE * 127) // 128)
    consts = make_consts((S0, S1), NB)
    wts = {k: np.ascontiguousarray(np.asarray(inputs[k], np.float32)) for k in _WNAMES}
    in_maps = []
    for core in range(8):
        b = core % 4
        m = {"x": np.ascontiguousarray(np.concatenate([xs_[b], xp_[b]], axis=0)),
             "c": np.ascontiguousarray(np.stack([cs_[b], cp_[b]], axis=0))}
        m.update(wts)
        m.update(consts)
        in_maps.append(m)
    res = run_bass_kernel_spmd(nc, in_maps, core_ids=list(range(8)))
    y_s = np.stack([res.results[b]["y"][:S0] for b in range(4)], axis=0)
    y_p = np.stack([res.results[b]["y"][S0:] for b in range(4)], axis=0)
    return (y_p.astype(np.float32), y_s.astype(np.float32))
```

```python
import math
import os
from contextlib import ExitStack

import numpy as np
import ml_dtypes
import concourse.bass as bass
import concourse.mybir as mybir
from concourse.bass_utils import run_bass_kernel_spmd

F32 = mybir.dt.float32
BF16 = mybir.dt.bfloat16
I32 = mybir.dt.int32
AF = mybir.ActivationFunctionType
ALU = mybir.AluOpType
AX = mybir.AxisListType

D = 2048
NG, EPG, NE = 4, 8, 32
DE = 512
CONV_K = 31
EPS = 1e-6
SAME_ENGINE_SYNC = True
CONV_PE = os.environ.get('KCONV', 'pe') == 'pe'
NOSYNC = set(os.environ.get('KNOSYNC', 'tensor').split(','))


class _Op:
    __slots__ = ("q", "fn", "deps", "sig", "sigval", "sem", "is_dma", "idx", "bar")


class Prog:
    def __init__(self, nc):
        self.nc = nc
        self.ops = []
        self.last_w = {}
        self.readers = {}
        self.dma_cnt = {}
        self.last_eng = {}
        self.bar = None
        self.bar_done = set()

    def barrier(self):
        deps = [(o, None) for o in self.last_eng.values()]
        dmas = dict(self.dma_cnt)
        self.bar = (deps, dmas)
        self.bar_done = set()

    def _add(self, q, fn, reads, writes, is_dma, semkey):
        o = _Op()
        o.q = q
        o.fn = fn
        o.is_dma = is_dma
        o.sig = False
        o.sigval = 0
        o.idx = len(self.ops)
        deps = {}
        for r in reads:
            w = self.last_w.get(r)
            if w is not None:
                deps[w.idx] = w
        for w_ in writes:
            w = self.last_w.get(w_)
            if w is not None:
                deps[w.idx] = w
            for rd in self.readers.get(w_, ()):
                deps[rd.idx] = rd
        o.deps = []
        for d in deps.values():
            if d.is_dma:
                o.deps.append((d, self.dma_cnt[d.sem]))
            else:
                o.deps.append((d, None))
        o.bar = None
        if self.bar is not None and q not in self.bar_done:
            self.bar_done.add(q)
            bdeps, bdmas = self.bar
            o.deps.extend(bdeps)
            o.bar = bdmas
        if is_dma:
            o.sem = ("dma", semkey)
            self.dma_cnt[o.sem] = self.dma_cnt.get(o.sem, 0) + 1
            o.sigval = self.dma_cnt[o.sem]
        else:
            o.sem = ("eng", q)
            self.last_eng[q] = o
        for w_ in writes:
            self.last_w[w_] = o
            self.readers[w_] = []
        for r in reads:
            if r not in writes:
                self.readers.setdefault(r, []).append(o)
        self.ops.append(o)
        return o

    def op(self, eng, fn, reads=(), writes=()):
        return self._add(eng, fn, tuple(reads), tuple(writes), False, None)

    def dma(self, q, fn, reads=(), writes=(), semkey=None):
        assert semkey is not None
        return self._add(q, fn, tuple(reads), tuple(writes), True, semkey)

    def emit(self):
        nc = self.nc
        ops = self.ops
        for o in ops:
            for d, _ in o.deps:
                if d.is_dma:
                    continue
                if d.q != o.q or (SAME_ENGINE_SYNC and d.q not in NOSYNC):
                    d.sig = True
        cnt = {}
        for o in ops:
            if not o.is_dma and o.sig:
                cnt[o.q] = cnt.get(o.q, 0) + 1
                o.sigval = cnt[o.q]
        semkeys = []
        seen = set()
        for o in ops:
            if (o.is_dma or o.sig) and o.sem not in seen:
                seen.add(o.sem)
                semkeys.append(o.sem)
        self.n_sems = len(semkeys)
        with ExitStack() as es:
            sems = {}
            for i, k in enumerate(semkeys):
                sems[k] = es.enter_context(nc.semaphore("s%d" % i))
            block = es.enter_context(nc.Block())
            queues = {}
            for o in ops:
                queues.setdefault(o.q, []).append(o)
            totals = dict(self.dma_cnt)

            def run_queue(qname, eng):
                waited = {}
                for o in queues.get(qname, ()):
                    need = {}
                    for d, n in o.deps:
                        if d.is_dma:
                            v = 16 * n
                        else:
                            if d.q == o.q and (d.q in NOSYNC or not SAME_ENGINE_SYNC):
                                continue
                            v = d.sigval
                        if v > need.get(d.sem, 0):
                            need[d.sem] = v
                    if o.bar is not None:
                        for k, n in o.bar.items():
                            if 16 * n > need.get(k, 0):
                                need[k] = 16 * n
                    for k, v in need.items():
                        if waited.get(k, 0) < v:
                            eng.wait_ge(sems[k], v)
                            waited[k] = v
                    ins = o.fn(eng)
                    if o.is_dma:
                        ins.then_inc(sems[o.sem], 16)
                    elif o.sig:
                        ins.then_inc(sems[o.sem], 1)
                if qname == "sync":
                    for k, n in totals.items():
                        if waited.get(k, 0) < 16 * n:
                            eng.wait_ge(sems[k], 16 * n)

            @block.sync
            def _(e):
                run_queue("sync", e)

            @block.scalar
            def _(e):
                run_queue("scalar", e)

            @block.gpsimd
            def _(e):
                run_queue("gpsimd", e)

            @block.vector
            def _(e):
                run_queue("vector", e)

            @block.tensor
            def _(e):
                run_queue("tensor", e)


def _bf(a):
    return np.ascontiguousarray(a.astype(ml_dtypes.bfloat16))


def make_consts(SEQS, NB):
    c = {}
    t = np.arange(128)
    ang = 2 * np.pi * np.outer(t, t) / 128.0
    cs = np.zeros((128, 2, 192), np.float64)
    for h in range(2):
        sl = slice(h * 64, h * 64 + 64)
        cs[:, h, 0:64] = np.cos(ang[:, sl])
        cs[:, h, 64:128] = np.sin(ang[:, sl])
        cs[:, h, 128:192] = -np.sin(ang[:, sl])
    c["cs128"] = _bf(cs)
    ccn = np.zeros((128, 2, 128), np.float64)
    ccn[:, 0] = np.cos(ang)
    ccn[:, 1] = -np.sin(ang)
    c["ccn"] = _bf(ccn)
    for q, S in enumerate(SEQS):
        S2 = S // 128
        a = 2 * np.pi * (np.outer(np.arange(S2), np.arange(S)) % S) / S
        f = np.zeros((S2, 2, S), np.float64)
        f[:, 0] = np.cos(a)
        f[:, 1] = np.sin(a)
        c["fcs%d" % q] = _bf(f)
    pc = np.zeros((128, 4, 16), np.float32)
    for g, w in enumerate((2, 4, 8, 16)):
        for j in range(8):
            cnt = min(j + w // 2, 10 ** 9) - max(j - w // 2, 0)
            pc[:, g, j] = 1.0 / cnt
        for j in range(8):
            tt = -8 + j
            hi = min(tt + w // 2, 0)
            lo = tt - w // 2
            pc[:, g, 8 + j] = 1.0 / (hi - lo)
    c["poolc"] = pc
    ut = (np.arange(128)[:, None] <= np.arange(128)[None, :]).astype(np.float32)
    c["utinc"] = _bf(ut)
    c["iotap"] = np.arange(128, dtype=np.float32).reshape(128, 1)
    c["bstart"] = np.tile((128.0 * np.arange(NB, dtype=np.float32))[None, :], (128, 1))
    c["iota32"] = np.tile(np.arange(32, dtype=np.float32)[None, :], (128, 1))
    return c


class Arena:
    def __init__(self, t, size):
        self.t = t
        self.size = size
        self.off = 0

    def reset(self):
        self.off = 0

    def alloc(self, n, align=16):
        self.off = (self.off + align - 1) // align * align
        o = self.off
        self.off += n
        assert self.off <= self.size, ("arena overflow", self.off, self.size)
        return self.t[:, o:o + n]


def build(SEQS=(8192, 2048), DEPTH=2, dbg=False, NL=None):
    NL = DEPTH if NL is None else NL
    N = sum(SEQS)
    NT = N // 128
    NB = -(-(2 * N + NE * 127) // 128)
    L = NB * 128
    OFFS = [0]
    for S in SEQS:
        OFFS.append(OFFS[-1] + S)
    nc = bass.Bass("TRN2", target_bir_lowering=False)

    def din(name, shape, dt=F32):
        return nc.dram_tensor(name, list(shape), dt, kind="ExternalInput").ap()

    def dscr(name, shape, dt):
        kind = "ExternalOutput" if dbg else "Internal"
        return nc.dram_tensor(name, list(shape), dt, kind=kind).ap()

    x_in = din("x", [N, D])
    c_in = din("c", [2, D])
    w_ada = din("w_ada", [DEPTH, D, 6 * D])
    b_ada = din("b_ada", [DEPTH, 6 * D])
    norm1_g = din("norm1_g", [DEPTH, D])
    w_in = din("w_in", [DEPTH, D, 3072])
    pool_w = din("pool_w", [DEPTH, 4, 128, 128])
    pool_scale = din("pool_scale", [DEPTH, 512])
    conv_dw_w = din("conv_dw_w", [DEPTH, CONV_K, 1024])
    conv_dw_b = din("conv_dw_b", [DEPTH, 1024])
    conv_ln_g = din("conv_ln_g", [DEPTH, 1024])
    conv_ln_b = din("conv_ln_b", [DEPTH, 1024])
    conv_pw_w = din("conv_pw_w", [DEPTH, 1024, 1024])
    conv_pw_b = din("conv_pw_b", [DEPTH, 1024])
    fourier_w = din("fourier_w", [DEPTH, 512, 512])
    w_out = din("w_out", [DEPTH, D, D])
    norm2_g = din("norm2_g", [DEPTH, D])
    rc_w = din("router_coarse_w", [DEPTH, D, NG])
    rc_b = din("router_coarse_b", [DEPTH, NG])
    rf_w = din("router_fine_w", [DEPTH, NG, D, EPG])
    rf_b = din("router_fine_b", [DEPTH, NG, EPG])
    e_w1 = din("expert_w1", [DEPTH, NE, D, DE])
    e_w3 = din("expert_w3", [DEPTH, NE, D, DE])
    e_w2 = din("expert_w2", [DEPTH, NE, DE, D])
    final_g = din("final_g", [D])
    k_cs128 = din("cs128", [128, 2, 192], BF16)
    k_ccn = din("ccn", [128, 2, 128], BF16)
    k_fcs = [din("fcs%d" % q, [S // 128, 2, S], BF16) for q, S in enumerate(SEQS)]
    k_poolc = din("poolc", [128, 4, 16])
    k_utinc = din("utinc", [128, 128], BF16)
    k_iotap = din("iotap", [128, 1])
    k_bstart = din("bstart", [128, NB])
    k_iota32 = din("iota32", [128, 32])

    y_out = nc.dram_tensor("y", [N, D], F32, kind="ExternalOutput").ap()

    x1_d = dscr("x1", [N, D], F32)
    x2_d = dscr("x2", [N, D], F32)
    h2_d = dscr("h2", [N, D], BF16)
    xs_d = dscr("xs", [L, D], BF16)
    ys_d = dscr("ys", [L, D], F32)
    mod_d = dscr("modrow", [2, 6 * D], F32)
    zaT = [dscr("zaT%d" % q, [512, S], BF16) for q, S in enumerate(SEQS)]
    vT = [dscr("vT%d" % q, [1024, S], BF16) for q, S in enumerate(SEQS)]
    zcT = [dscr("zcT%d" % q, [512, S], BF16) for q, S in enumerate(SEQS)]
    rzT = [dscr("rzT%d" % q, [512, S], BF16) for q, S in enumerate(SEQS)]
    mixT = [dscr("mixT%d" % q, [2048, S], BF16) for q, S in enumerate(SEQS)]

    P = Prog(nc)
    es = ExitStack()
    with es:
        AR_SZ = 96 * 1024
        art = es.enter_context(nc.sbuf_tensor("arena", [128, AR_SZ], BF16))
        AR = Arena(art, AR_SZ)

        class _A16:
            def alloc(self, n):
                return AR.alloc(n)

            def reset(self):
                AR.reset()

        class _A32:
            def alloc(self, n):
                return AR.alloc(2 * n).bitcast(F32)

            def reset(self):
                pass

        A16 = _A16()
        A32 = _A32()
        ident_f = es.enter_context(nc.sbuf_tensor("ident_f", [128, 128], F32))
        ident_b = es.enter_context(nc.sbuf_tensor("ident_b", [128, 128], BF16))
        ones_b = es.enter_context(nc.sbuf_tensor("ones_b", [128, 128], BF16))
        utinc = es.enter_context(nc.sbuf_tensor("utinc_s", [128, 128], BF16))
        iotap = es.enter_context(nc.sbuf_tensor("iotap_s", [128, 1], F32))
        iota32 = es.enter_context(nc.sbuf_tensor("iota32_s", [128, 32], F32))
        eps_t = es.enter_context(nc.sbuf_tensor("eps_t", [128, 1], F32))
        r_E = es.enter_context(nc.sbuf_tensor("r_E", [128, NT, 64], BF16))
        r_f = es.enter_context(nc.sbuf_tensor("r_f", [128, NT, 4], F32))
        r_slot = es.enter_context(nc.sbuf_tensor("r_slot", [128, NT, 2], I32))
        cnt_run = es.enter_context(nc.sbuf_tensor("cnt_run", [128, 32], F32))
        widx = es.enter_context(nc.sbuf_tensor("widx", [128, NB], I32))
        pm = [es.enter_context(nc.psum_tensor("pm%d" % i, [128, 512], F32)) for i in range(4)]
        pt = es.enter_context(nc.psum_tensor("pt", [128, 2048], BF16))
        pr = [es.enter_context(nc.psum_tensor("pr%d" % i, [128, 512], F32)) for i in range(2)]
        PM = [("pm", i) for i in range(4)]
        PT = "pt"
        PR = [("pr", i) for i in range(2)]

        STQ = os.environ.get('KSTQ', 'sync')
        uid = [0]
        _bregs = {}

        def BR(e, val):
            if val not in _bregs:
                _bregs[val] = e.to_reg(val)
            return _bregs[val]

        def U(name):
            uid[0] += 1
            return (name, uid[0])

        def t16(n, shape=None):
            v = A16.alloc(n)
            return v

        def DMA(q, out, in_, reads, writes, semkey, **kw):
            P.dma(q, lambda e: e.dma_start(out=out, in_=in_, **kw), reads, writes, semkey)

        def MM(out, lhsT, rhs, start, stop, reads, writes):
            P.op("tensor", lambda e: e.matmul(out, lhsT=lhsT, rhs=rhs, start=start, stop=stop), reads, writes)

        def TR(out, in_, ident, reads, writes):
            P.op("tensor", lambda e: e.transpose(out=out, in_=in_, identity=ident), reads, writes)

        def ACT(out, in_, func, reads, writes, bias=None, scale=None, accum_out=None, eng="scalar"):
            kw = {}
            if bias is not None:
                kw["bias"] = bias
            if scale is not None:
                kw["scale"] = scale
            if accum_out is not None:
                kw["accum_out"] = accum_out
            P.op("scalar", lambda e: e.activation(out=out, in_=in_, func=func, **kw), reads, writes)

        def TT(eng, out, in0, in1, op, reads, writes):
            P.op(eng, lambda e: e.tensor_tensor(out=out, in0=in0, in1=in1, op=op), reads, writes)

        def TS(eng, out, in0, s1, s2, op0, op1, reads, writes, accum_out=None):
            if op1 is None:
                P.op(eng, lambda e: e.tensor_scalar(out=out, in0=in0, scalar1=s1, scalar2=None, op0=op0), reads, writes)
            elif accum_out is not None:
                P.op(eng, lambda e: e.tensor_scalar(out=out, in0=in0, scalar1=s1, scalar2=s2, op0=op0, op1=op1, accum_out=accum_out), reads, writes)
            else:
                P.op(eng, lambda e: e.tensor_scalar(out=out, in0=in0, scalar1=s1, scalar2=s2, op0=op0, op1=op1), reads, writes)

        def STT(out, in0, scalar, in1, op0, op1, reads, writes):
            P.op("vector", lambda e: e.scalar_tensor_tensor(out=out, in0=in0, scalar=scalar, in1=in1, op0=op0, op1=op1), reads, writes)

        def CP(eng, out, in_, reads, writes):
            if eng == "scalar":
                P.op("scalar", lambda e: e.copy(out=out, in_=in_), reads, writes)
            else:
                P.op(eng, lambda e: e.tensor_copy(out=out, in_=in_), reads, writes)

        def MEMSET(eng, ap, val, writes):
            P.op(eng, lambda e: e.memset(ap, val), (), writes)

        def RED(out, in_, op, reads, writes, axis=AX.X):
            P.op("vector", lambda e: e.tensor_reduce(out=out, in_=in_, axis=axis, op=op), reads, writes)

        def bcast_row(dram_row_ap, n):
            return dram_row_ap.partition_broadcast(128)

        MEMSET("gpsimd", ident_f[:], 0.0, ["ident_f"])
        P.op("gpsimd", lambda e: e.affine_select(out=ident_f[:], in_=ident_f[:], pattern=[[-1, 128]],
                                                 compare_op=ALU.not_equal, fill=1.0, base=0, channel_multiplier=1),
             ["ident_f"], ["ident_f"])
        CP("vector", ident_b[:], ident_f[:], ["ident_f"], ["ident_b"])
        MEMSET("gpsimd", ones_b[:], 1.0, ["ones_b"])
        MEMSET("gpsimd", eps_t[:], EPS, ["eps_t"])
        DMA("sync", utinc[:], k_utinc, [], ["utinc"], "c_utinc")
        DMA("sync", iotap[:], k_iotap, [], ["iotap"], "c_iotap")
        DMA("sync", iota32[:], k_iota32, [], ["iota32"], "c_iota32")

        class _Stop(Exception):
            pass

        def stop_here(tag):
            if os.environ.get("KSTOP") == tag:
                P.emit()
                raise _Stop()

        cur_x = x_in
        try:
          for l in range(NL):
              last = (l == NL - 1)
              nxt_x = y_out if False else x2_d
              P.barrier()
              A16.reset(); A32.reset()
              cst = A32.alloc(2 * 16).rearrange("p (q k) -> p q k", q=2)
              csb = A16.alloc(16 * 2).rearrange("p (k q) -> p k q", q=2)
              brow = [A32.alloc(512) for _ in range(2)]
              mrow = A32.alloc(512)
              DMA("sync", cst, c_in.rearrange("q (p k) -> p q k", k=16), [], ["cst"], "cst")
              for q in range(2):
                  ACT(csb[:, :, q], cst[:, q, :], AF.Silu, ["cst"], [("csb", q)])
              wa = [A16.alloc(16 * 512).rearrange("p (k n) -> p k n", k=16) for _ in range(2)]
              for cb in range(24):
                  wbuf = wa[cb % 2]
                  wk = ("wa", cb % 2)
                  DMA("gpsimd", wbuf, w_ada[l, :, cb * 512:(cb + 1) * 512].rearrange("(p k) n -> p k n", k=16),
                      [], [wk], "wa%d" % (cb % 2))
                  pk = PM[cb % 2]
                  pst = pm[cb % 2]
                  for kc in range(16):
                      MM(pst[0:2, :], csb[:, kc, :], wbuf[:, kc, :], kc == 0, kc == 15,
                         [wk, ("csb", 0), ("csb", 1)], [pk])
                  mk = U("mrow")
                  bk = ("brow", cb % 2)
                  DMA("sync", brow[cb % 2][0:2, :], b_ada[l:l + 1, cb * 512:(cb + 1) * 512].broadcast_to([2, 512]),
                      [], [bk], "brow%d" % (cb % 2))
                  TT("vector", mrow[0:2, :], pst[0:2, :], brow[cb % 2][0:2, :], ALU.add,
                     [pk, bk], ["mrow"])
                  DMA(STQ, mod_d[:, cb * 512:(cb + 1) * 512], mrow[0:2, :], ["mrow"], [("mod", cb)], "mrow")
              MODK = [("mod", cb) for cb in range(24)]

              def mod_rep(q, i):
                  return mod_d[q:q + 1, i * D:(i + 1) * D].broadcast_to([128, D])

              for q, S in enumerate(SEQS):
                  T0 = OFFS[q]
                  NT5 = S // 512
                  S2 = S // 128
                  P.barrier()
                  A16.reset(); A32.reset()
                  wi = A16.alloc(16 * 3072).rearrange("p (k n) -> p k n", k=16)
                  DMA("gpsimd", wi, w_in[l].rearrange("(p k) n -> p k n", k=16), [], ["wi"], "wi")
                  a1 = A32.alloc(D)
                  sh1 = A32.alloc(D)
                  tmpf = A32.alloc(D)
                  DMA("sync", a1, mod_rep(q, 1), MODK, ["a1"], "a1")
                  DMA("sync", sh1, mod_rep(q, 0), MODK, ["sh1"], "sh1")
                  DMA("sync", tmpf, norm1_g[l:l + 1, :].broadcast_to([128, D]), [], ["tmpf"], "g_rep")
                  STT(a1, a1, 1.0, tmpf, ALU.add, ALU.mult, ["a1", "tmpf"], ["a1"])
                  xt = [A32.alloc(D) for _ in range(2)]
                  hb = [A16.alloc(D) for _ in range(4)]
                  hT = [A16.alloc(16 * 512).rearrange("p (k t) -> p k t", k=16) for _ in range(1)]
                  stg = [A16.alloc(16 * 512).rearrange("p (j t) -> p j t", j=16) for _ in range(1)]
                  sgt = [A32.alloc(512) for _ in range(2)]
                  ss = A32.alloc(8)
                  hTt = hT[0]
                  hk = ("hT", 0)

                  def p1_norm(ti):
                      for sub in range(4):
                          it = ti * 4 + sub
                          b2 = it % 2
                          r0 = T0 + it * 128
                          xk = ("xt", b2)
                          DMA("sync", xt[b2], cur_x[r0:r0 + 128, :], [("xcur", r0 // 128)], [xk], "xt%d" % b2)
                          ssk = U("ss")
                          hbk = ("hb", sub)
                          ACT(hb[sub], xt[b2], AF.Square, [xk], [hbk, ssk], accum_out=ss[:, 0:1])
                          ACT(ss[:, 1:2], ss[:, 0:1], AF.Sqrt, [ssk, "eps_t"], [ssk], scale=1.0 / D, bias=eps_t[:, 0:1])
                          P.op("vector", lambda e, ss=ss: e.reciprocal(out=ss[:, 2:3], in_=ss[:, 1:2]), [ssk], [ssk])
                          STT(tmpf, xt[b2], ss[:, 2:3], a1, ALU.mult, ALU.mult, [xk, ssk, "a1"], ["tmpf"])
                          TT("gpsimd", hb[sub], tmpf, sh1, ALU.add, ["tmpf", "sh1"], [hbk])

                  def p1_tr(ti):
                      for sub in range(4):
                          hbk = ("hb", sub)
                          for kc in range(16):
                              TR(pt[:, kc * 128:(kc + 1) * 128], hb[sub].rearrange("p (c k) -> p k c", k=16)[:, kc, :],
                                 ident_b[:], [hbk, "ident_b"], [PT])
                          CP("scalar" if sub % 2 else "vector", hTt[:, :, sub * 128:(sub + 1) * 128],
                             pt[:, :].rearrange("p (k t) -> p k t", k=16), [PT], [hk])

                  def p1_mm(ti):
                      st = stg[0]
                      sk = ("stg", 0)
                      c0 = ti * 512

                      def mmgrp(j, pi):
                          for kc in range(16):
                              MM(pm[pi][:, :], wi[:, kc, j * 128:(j + 1) * 128], hTt[:, kc, :], kc == 0, kc == 15,
                                 ["wi", hk], [PM[pi]])

                      pi = 0
                      for j in range(4):
                          mmgrp(j, pi)
                          CP("scalar", st[:, j, :], pm[pi][:, :], [PM[pi]], [sk])
                          pi = (pi + 1) % 4
                      for j in range(8):
                          pa = pi
                          mmgrp(4 + j, pa)
                          pb = (pi + 1) % 4
                          mmgrp(12 + j, pb)
                          sg = sgt[j % 2]
                          sgk = ("sg", j % 2)
                          ACT(sg, pm[pb][:, :], AF.Sigmoid, [PM[pb]], [sgk])
                          TT("vector", st[:, 4 + j, :], pm[pa][:, :], sg, ALU.mult, [PM[pa], sgk], [sk])
                          pi = (pi + 2) % 4
                      for j in range(4):
                          mmgrp(20 + j, pi)
                          CP("scalar", st[:, 12 + j, :], pm[pi][:, :], [PM[pi]], [sk])
                          pi = (pi + 1) % 4
                      DMA(STQ, zaT[q][:, c0:c0 + 512].rearrange("(j p) t -> p j t", p=128), st[:, 0:4, :],
                          [sk], [("zaT", q, ti)], "stg0")
                      DMA(STQ, vT[q][:, c0:c0 + 512].rearrange("(j p) t -> p j t", p=128), st[:, 4:12, :],
                          [sk], [("vT", q, ti)], "stg0")
                      DMA(STQ, zcT[q][:, c0:c0 + 512].rearrange("(j p) t -> p j t", p=128), st[:, 12:16, :],
                          [sk], [("zcT", q, ti)], "stg0")

                  p1_norm(0)
                  p1_tr(0)
                  for ti in range(NT5):
                      if ti + 1 < NT5:
                          p1_norm(ti + 1)
                      p1_mm(ti)
                      if ti + 1 < NT5:
                          p1_tr(ti + 1)

                  stop_here('P1')
                  P.barrier()
                  A16.reset(); A32.reset()
                  pwf = A32.alloc(4 * 128).rearrange("p (g e) -> p g e", g=4)
                  pwb = A16.alloc(4 * 128).rearrange("p (g e) -> p g e", g=4)
                  DMA("sync", pwf, pool_w[l].rearrange("g c e -> c g e"), [], ["pwf"], "pwf")
                  CP("vector", pwb, pwf, ["pwf"], ["pwb"])
                  psc = A32.alloc(4)
                  pscr = A32.alloc(4 * 128).rearrange("p (g e) -> p g e", g=4)[0:4]
                  DMA("sync", pscr[:, 0, :], pool_scale[l].rearrange("(g e) -> g e", g=4), [], ["pscr"], "pscr")
                  TR(pr[0][:, 0:4], pscr[:, 0, :], ident_f[0:4, 0:4], ["pscr", "ident_f"], [PR[0]])
                  CP("vector", psc, pr[0][:, 0:4], [PR[0]], ["psc"])
                  pcst = A32.alloc(64).rearrange("p (g j) -> p g j", g=4)
                  DMA("sync", pcst, k_poolc, [], ["pcst"], "pcst")
                  ub = [A16.alloc(528) for _ in range(2)]
                  sa = [A32.alloc(528) for _ in range(2)]
                  sb_ = [A32.alloc(528) for _ in range(2)]
                  dmt = [A16.alloc(512) for _ in range(2)]
                  pst2 = [A16.alloc(4 * 512).rearrange("p (g t) -> p g t", g=4) for _ in range(2)]
                  itc2 = [0]

                  def p2_tile(ti):
                      c0 = ti * 512
                      st = pst2[ti % 2]
                      sk = ("pst2", ti % 2)
                      for g, w in enumerate((2, 4, 8, 16)):
                          b2 = itc2[0] % 2
                          itc2[0] += 1
                          u = ub[b2]
                          uk = ("ub", b2)
                          lo = max(c0 - 8, 0)
                          hi = min(c0 + 520, S)
                          rd = [("zaT", q, tj) for tj in range(max(ti - 1, 0), min(ti + 2, NT5))]
                          if lo > c0 - 8:
                              MEMSET("gpsimd", u[:, 0:8], 0.0, [uk])
                          if hi < c0 + 520:
                              MEMSET("gpsimd", u[:, 520:528], 0.0, [uk])
                          DMA("sync", u[:, lo - (c0 - 8):hi - (c0 - 8)], zaT[q][g * 128:(g + 1) * 128, lo:hi], rd, [uk], "ub%d" % b2)
                          s_a, s_b = sa[b2], sb_[b2]
                          ka, kb = ("sa", b2), ("sb", b2)
                          TT("vector", s_a[:, 1:528], u[:, 0:527], u[:, 1:528], ALU.add, [uk], [ka])
                          cur, curk, oth, othk = s_a, ka, s_b, kb
                          lo_v = 1
                          hi_v = 528
                          step = 1
                          ww = 2
                          while ww < w:
                              nlo = lo_v + step
                              nhi = hi_v - step
                              TT("vector", oth[:, nlo:nhi], cur[:, nlo - step:nhi - step], cur[:, nlo + step:nhi + step],
                                 ALU.add, [curk], [othk])
                              cur, curk, oth, othk = oth, othk, cur, curk
                              lo_v, hi_v = nlo, nhi
                              step *= 2
                              ww *= 2
                          dm = dmt[b2]
                          dk_ = ("dm", b2)
                          STT(dm, cur[:, 8:520], 1.0 / w, u[:, 8:520], ALU.mult, ALU.subtract, [curk, uk], [dk_])
                          if ti == 0:
                              TT("vector", oth[:, 8:16], cur[:, 8:16], pcst[:, g, 0:8], ALU.mult, [curk, "pcst"], [othk])
                              TT("vector", dm[:, 0:8], oth[:, 8:16], u[:, 8:16], ALU.subtract, [othk, uk], [dk_])
                          if ti == NT5 - 1:
                              TT("vector", oth[:, 512:520], cur[:, 512:520], pcst[:, g, 8:16], ALU.mult, [curk, "pcst"], [othk])
                              TT("vector", dm[:, 504:512], oth[:, 512:520], u[:, 512:520], ALU.subtract, [othk, uk], [dk_])
                          pi = g
                          MM(pm[pi][:, :], pwb[:, g, :], dm, True, True, ["pwb", dk_], [PM[pi]])
                          ACT(st[:, g, :], pm[pi][:, :], AF.Copy, [PM[pi], "psc"], [sk], scale=psc[:, g:g + 1])
                      DMA(STQ, mixT[q][0:512, c0:c0 + 512].rearrange("(g p) t -> p g t", p=128), st, [sk],
                          [("mixT", q, ti, 0)], "pst2%d" % (ti % 2))

                  pww = A16.alloc(8 * 1024).rearrange("p (k n) -> p k n", k=8)
                  DMA("gpsimd", pww, conv_pw_w[l].rearrange("(k p) n -> p k n", p=128), [], ["pww"], "pww")
                  dwr = A32.alloc(1024)
                  DMA("sync", dwr[0:CONV_K, :], conv_dw_w[l], [], ["dwr"], "dwr")
                  dwT = A32.alloc(8 * 32).rearrange("p (j k) -> p j k", j=8)
                  for j in range(8):
                      TR(pr[0][:, j * 32:j * 32 + CONV_K], dwr[0:CONV_K, j * 128:(j + 1) * 128],
                         ident_f[0:CONV_K, 0:CONV_K], ["dwr", "ident_f"], [PR[0]])
                  CP("vector", dwT[:, :, 0:CONV_K], pr[0][:, 0:256].rearrange("p (j k) -> p j k", j=8)[:, :, 0:CONV_K],
                     [PR[0]], ["dwT"])
                  vr = A32.alloc(4 * 1024).rearrange("p (v n) -> p v n", v=4)
                  vecs = A32.alloc(32).rearrange("p (v j) -> p v j", v=4)
                  for vi, src in enumerate((conv_dw_b, conv_ln_g, conv_ln_b, conv_pw_b)):
                      DMA("sync", vr[0:8, vi, 0:128], src[l].rearrange("(j p) -> j p", p=128), [], [("vr", vi)], "vr%d" % vi)
                      TR(pr[1][:, vi * 8:vi * 8 + 8], vr[0:8, vi, 0:128], ident_f[0:8, 0:8], [("vr", vi), "ident_f"], [PR[1]])
                  CP("vector", vecs, pr[1][:, 0:32].rearrange("p (v j) -> p v j", v=4), [PR[1]], ["vecs"])
                  acc = [A32.alloc(512) for _ in range(8)]
                  cbf = [A16.alloc(512) for _ in range(2)]
                  sqb = [A16.alloc(512) for _ in range(2)]
                  sT = [A16.alloc(512) for _ in range(8)]
                  mean = A32.alloc(512)
                  var = A32.alloc(512)
                  rstd = A32.alloc(512)
                  xn = [A32.alloc(512) for _ in range(2)]
                  st3 = [A16.alloc(8 * 512).rearrange("p (j t) -> p j t", j=8) for _ in range(2)]
                  if CONV_PE:
                      vb = [A16.alloc(544) for _ in range(3)]
                      vb1 = [A16.alloc(544) for _ in range(3)]
                      dg = A16.alloc(8 * 32 * 128).rearrange("p (j k c) -> p j k c", j=8, k=32)
                      for j in range(8):
                          for k in range(CONV_K):
                              TS("vector" if (j * CONV_K + k) % 2 else "gpsimd", dg[:, j, k, :], ident_b[:, :], dwT[:, j, k:k + 1], None,
                                 ALU.mult, None, ["ident_b", "dwT"], [("dg", j)])
                  else:
                      vb = [A16.alloc(544) for _ in range(2)]
                  itc3 = [0]

                  def p3_tile(ti):
                      c0 = ti * 512
                      for j in range(8):
                          if CONV_PE:
                              b2 = itc3[0] % 2
                              b3 = itc3[0] % 3
                              itc3[0] += 1
                              v, vk = vb[b3], ("vb", b3)
                              v1, v1k = vb1[b3], ("vb1", b3)
                              lo = max(c0 - 15, 0)
                              hi = min(c0 + 527, S)
                              rd = [("vT", q, tj) for tj in range(max(ti - 1, 0), min(ti + 2, NT5))]
                              if lo > c0 - 15:
                                  MEMSET("gpsimd", v[:, 0:16], 0.0, [vk])
                                  MEMSET("gpsimd", v1[:, 0:16], 0.0, [v1k])
                              if hi < c0 + 527:
                                  MEMSET("gpsimd", v[:, 526:544], 0.0, [vk])
                                  MEMSET("gpsimd", v1[:, 526:544], 0.0, [v1k])
                              DMA("sync", v[:, lo - (c0 - 15):hi - (c0 - 15)], vT[q][j * 128:(j + 1) * 128, lo:hi], rd, [vk], "vb%d" % b3)
                              lo1 = max(c0 - 14, 0)
                              DMA("sync", v1[:, lo1 - (c0 - 14):hi - (c0 - 14)], vT[q][j * 128:(j + 1) * 128, lo1:hi], rd, [v1k], "vb1%d" % b3)
                              pi = itc3[0] % 4
                              for k in range(CONV_K):
                                  if k % 2 == 0:
                                      MM(pm[pi][:, :], dg[:, j, k, :], v[:, k:k + 512], k == 0, k == CONV_K - 1,
                                         [("dg", j), vk], [PM[pi]])
                                  else:
                                      MM(pm[pi][:, :], dg[:, j, k, :], v1[:, k - 1:k - 1 + 512], k == 0, k == CONV_K - 1,
                                         [("dg", j), v1k], [PM[pi]])
                              a = acc[j]
                              ak = ("acc", j)
                              ACT(a, pm[pi][:, :], AF.Identity, [PM[pi], "vecs"], [ak], bias=vecs[:, 0, j:j + 1])
                              cb_, ck = cbf[b2], ("cbf", b2)
                              sq_, sqk = sqb[b2], ("sqb", b2)
                              CP("vector", cb_, a, [ak], [ck])
                              ACT(sq_, a, AF.Square, [ak], [sqk])
                              MM(pr[0][:, :], ones_b[:], cb_, j == 0, j == 7, ["ones_b", ck], [PR[0]])
                              MM(pr[1][:, :], ones_b[:], sq_, j == 0, j == 7, ["ones_b", sqk], [PR[1]])
                          else:
                              b2 = itc3[0] % 2
                              itc3[0] += 1
                              v = vb[b2]
                              vk = ("vb", b2)
                              lo = max(c0 - 15, 0)
                              hi = min(c0 + 527, S)
                              rd = [("vT", q, tj) for tj in range(max(ti - 1, 0), min(ti + 2, NT5))]
                              if lo > c0 - 15:
                                  MEMSET("gpsimd", v[:, 0:15], 0.0, [vk])
                              if hi < c0 + 527:
                                  MEMSET("gpsimd", v[:, 527:542], 0.0, [vk])
                              DMA("sync", v[:, lo - (c0 - 15):hi - (c0 - 15)], vT[q][j * 128:(j + 1) * 128, lo:hi], rd, [vk], "vb%d" % b2)
                              a = acc[j]
                              ak = ("acc", j)
                              TS("vector", a, v[:, 0:512], dwT[:, j, 0:1], vecs[:, 0, j:j + 1], ALU.mult, ALU.add,
                                 [vk, "dwT", "vecs"], [ak])
                              for k in range(1, CONV_K):
                                  STT(a, v[:, k:k + 512], dwT[:, j, k:k + 1], a, ALU.mult, ALU.add, [vk, "dwT", ak], [ak])
                              cb_, ck = cbf[b2], ("cbf", b2)
                              sq_, sqk = sqb[b2], ("sqb", b2)
                              CP("gpsimd", cb_, a, [ak], [ck])
                              ACT(sq_, a, AF.Square, [ak], [sqk])
                              MM(pr[0][:, :], ones_b[:], cb_, j == 0, j == 7, ["ones_b", ck], [PR[0]])
                              MM(pr[1][:, :], ones_b[:], sq_, j == 0, j == 7, ["ones_b", sqk], [PR[1]])
                      TS("vector", mean, pr[0][:, :], 1.0 / 1024, None, ALU.mult, None, [PR[0]], ["mean"])
                      TS("vector", var, pr[1][:, :], 1.0 / 1024, None, ALU.mult, None, [PR[1]], ["var"])
                      TT("vector", rstd, mean, mean, ALU.mult, ["mean"], ["rstd"])
                      TT("vector", var, var, rstd, ALU.subtract, ["var", "rstd"], ["var"])
                      ACT(var, var, AF.Sqrt, ["var", "eps_t"], ["var"], bias=eps_t[:, 0:1], scale=1.0)
                      P.op("vector", lambda e, rstd=rstd, var=var: e.reciprocal(out=rstd, in_=var), ["var"], ["rstd"])
                      for j in range(8):
                          x_, xk_ = xn[j % 2], ("xn", j % 2)
                          TT("gpsimd", x_, acc[j], mean, ALU.subtract, [("acc", j), "mean"], [xk_])
                          TT("vector", x_, x_, rstd, ALU.mult, [xk_, "rstd"], [xk_])
                          ACT(sT[j], x_, AF.Silu, [xk_, "vecs"], [("sT", j)], scale=vecs[:, 1, j:j + 1], bias=vecs[:, 2, j:j + 1])
                      st = st3[ti % 2]
                      sk = ("st3", ti % 2)
                      for e_ in range(8):
                          pi = e_ % 4
                          for j in range(8):
                              MM(pm[pi][:, :], pww[:, j, e_ * 128:(e_ + 1) * 128], sT[j], j == 0, j == 7,
                                 ["pww", ("sT", j)], [PM[pi]])
                          ACT(st[:, e_, :], pm[pi][:, :], AF.Identity, [PM[pi], "vecs"], [sk], bias=vecs[:, 3, e_:e_ + 1])
                      DMA(STQ, mixT[q][512:1536, c0:c0 + 512].rearrange("(j p) t -> p j t", p=128), st, [sk],
                          [("mixT", q, ti, 1)], "st3%d" % (ti % 2))

                  for ti in range(NT5):
                      p2_tile(ti)
                      p3_tile(ti)
                  stop_here('P3')
                  P.barrier()
                  A16.reset(); A32.reset()
                  cs = A16.alloc(2 * 192).rearrange("p (h n) -> p h n", h=2)
                  ccn = A16.alloc(2 * 128).rearrange("p (h n) -> p h n", h=2)
                  fcs = A16.alloc(2 * S).rearrange("p (h n) -> p h n", h=2)
                  DMA("sync", cs, k_cs128, [], ["cs"], "cs")
                  DMA("sync", ccn, k_ccn, [], ["ccn"], "ccn")
                  DMA("sync", fcs[0:S2], k_fcs[q], [], ["fcs"], "fcs")
                  fwf = A32.alloc(4 * 512).rearrange("p (h e) -> p h e", h=4)
                  fwb = A16.alloc(4 * 512).rearrange("p (h e) -> p h e", h=4)
                  DMA("sync", fwf, fourier_w[l].rearrange("(h m) e -> m h e", h=4), [], ["fwf"], "fwf")
                  CP("vector", fwb, fwf, ["fwf"], ["fwb"])
                  Ut = A16.alloc(128 * S2).rearrange("p (c t) -> p c t", c=128)
                  Ah = A16.alloc(128 * 192).rearrange("p (c n) -> p c n", c=128)
                  Xh = A16.alloc(2 * S).rearrange("p (h k) -> p h k", h=2)
                  rst = [A16.alloc(512) for _ in range(2)]
                  ALLZC = [("zcT", q, tj) for tj in range(NT5)]
                  nrm = 1.0 / math.sqrt(S * 128.0)
                  ri = 0
                  for h in range(4):
                      for cq in range(8):
                          DMA("sync", Ut[:, cq * 16:(cq + 1) * 16, :],
                              zcT[q][h * 128 + cq * 16:h * 128 + (cq + 1) * 16, :].rearrange("c (a b) -> a c b", b=S2),
                              ALLZC, ["Ut"], "Ut")
                      for half in range(2):
                          pmv = [pm[i] for i in range(4)]
                          for cg in range(16):
                              for ci in range(8):
                                  c_ = cg * 8 + ci
                                  bank = ci // 2
                                  col = (ci % 2) * 192
                                  MM(pmv[bank][0:S2, col:col + 192], Ut[:, c_, :], cs[:, half, :], True, True,
                                     ["Ut", "cs"], [PM[bank]])
                              for bank in range(4):
                                  eng = "vector" if bank % 2 == 0 else "scalar"
                                  CP(eng, Ah[0:S2, cg * 8 + bank * 2:cg * 8 + bank * 2 + 2, :],
                                     pmv[bank][0:S2, 0:384].rearrange("p (c n) -> p c n", c=2), [PM[bank]], [("Ah", cg)])
                          AHK = [("Ah", cg) for cg in range(16)]
                          G = 512 // S2
                          G = min(G, 64)
                          for kg in range(64 // G):
                              pxr, pxi = pr[0], pr[1]
                              for gi in range(G):
                                  k1l = kg * G + gi
                                  k1 = half * 64 + k1l
                                  fc_ = fcs[0:S2, 0, :].rearrange("p (b a) -> p a b", a=128)[:, k1, :]
                                  fs_ = fcs[0:S2, 1, :].rearrange("p (b a) -> p a b", a=128)[:, k1, :]
                                  ar = Ah[0:S2, :, k1l]
                                  ai = Ah[0:S2, :, 64 + k1l]
                                  an = Ah[0:S2, :, 128 + k1l]
                                  o = gi * S2
                                  MM(pxr[:, o:o + S2], ar, fc_, True, False, AHK + ["fcs"], [PR[0]])
                                  MM(pxr[:, o:o + S2], an, fs_, False, True, AHK + ["fcs"], [PR[0]])
                                  MM(pxi[:, o:o + S2], ar, fs_, True, False, AHK + ["fcs"], [PR[1]])
                                  MM(pxi[:, o:o + S2], ai, fc_, False, True, AHK + ["fcs"], [PR[1]])
                              k1b = half * 64 + kg * G
                              for ri_, px in enumerate((pxr, pxi)):
                                  dst = Xh[:, ri_, :].rearrange("p (b a) -> p a b", a=128)[:, k1b:k1b + G, :]
                                  src = px[:, 0:G * S2].rearrange("p (g b) -> p g b", g=G)
                                  CP("vector" if ri_ == 0 else "scalar", dst, src, [PR[ri_]], [("Xh", half, kg)])
                      XHK = [("Xh", hf, kg) for hf in range(2) for kg in range(64 // G)]
                      for kt in range(NT5):
                          pi = kt % 4
                          MM(pm[pi][:, :], ccn[:, 0, :], Xh[:, 0, kt * 512:(kt + 1) * 512], True, False, ["ccn"] + XHK, [PM[pi]])
                          MM(pm[pi][:, :], ccn[:, 1, :], Xh[:, 1, kt * 512:(kt + 1) * 512], False, True, ["ccn"] + XHK, [PM[pi]])
                          r_ = rst[ri % 2]
                          rk = ("rst", ri % 2)
                          ACT(r_, pm[pi][:, :], AF.Copy, [PM[pi]], [rk], scale=nrm)
                          DMA(STQ, rzT[q][h * 128:(h + 1) * 128, kt * 512:(kt + 1) * 512], r_, [rk], [("rzT", q, h, kt)],
                              "rst%d" % (ri % 2))
                          ri += 1
                  rzb = [A16.alloc(4 * 512).rearrange("p (h t) -> p h t", h=4) for _ in range(2)]
                  st4 = [A16.alloc(4 * 512).rearrange("p (e t) -> p e t", e=4) for _ in range(2)]
                  for ti in range(NT5):
                      c0 = ti * 512
                      rb, rbk = rzb[ti % 2], ("rzb", ti % 2)
                      DMA("sync", rb, rzT[q][:, c0:c0 + 512].rearrange("(h m) t -> m h t", h=4),
                          [("rzT", q, h, ti) for h in range(4)], [rbk], "rzb%d" % (ti % 2))
                      st, sk = st4[ti % 2], ("st4", ti % 2)
                      for e_ in range(4):
                          pi = e_
                          for h in range(4):
                              MM(pm[pi][:, :], fwb[:, h, e_ * 128:(e_ + 1) * 128], rb[:, h, :], h == 0, h == 3,
                                 ["fwb", rbk], [PM[pi]])
                          CP("scalar" if e_ % 2 else "vector", st[:, e_, :], pm[pi][:, :], [PM[pi]], [sk])
                      DMA(STQ, mixT[q][1536:2048, c0:c0 + 512].rearrange("(e p) t -> p e t", p=128), st, [sk],
                          [("mixT", q, ti, 2)], "st4%d" % (ti % 2))

                  stop_here('P4')
                  P.barrier()
                  A16.reset(); A32.reset()
                  wo = A16.alloc(16 * 2048).rearrange("p (k n) -> p k n", k=16)
                  DMA("gpsimd", wo, w_out[l].rearrange("(k p) n -> p k n", p=128), [], ["wo"], "wo")
                  g1 = A32.alloc(D)
                  a2 = A32.alloc(D)
                  sh2 = A32.alloc(D)
                  tmpf = A32.alloc(D)
                  TFK = [("tmpf", i) for i in range(4)]
                  DMA("sync", g1, mod_rep(q, 2), MODK, ["g1"], "g1")
                  DMA("sync", a2, mod_rep(q, 4), MODK, ["a2"], "a2")
                  DMA("sync", sh2, mod_rep(q, 3), MODK, ["sh2"], "sh2")
                  DMA("sync", tmpf, norm2_g[l:l + 1, :].broadcast_to([128, D]), [], TFK, "g_rep")
                  STT(a2, a2, 1.0, tmpf, ALU.add, ALU.mult, ["a2"] + TFK, ["a2"])
                  wr = A32.alloc(16 * 36).rearrange("p (k n) -> p k n", k=16)
                  DMA("sync", wr[:, :, 0:4], rc_w[l].rearrange("(p k) g -> p k g", k=16), [], [("wr", 0)], "wr")
                  for g in range(4):
                      DMA("sync", wr[:, :, 4 + 8 * g:12 + 8 * g], rf_w[l, g].rearrange("(p k) e -> p k e", k=16), [],
                          [("wr", 1 + g)], "wr")
                  WRK = [("wr", i) for i in range(5)]
                  rb_ = A32.alloc(36)
                  DMA("sync", rb_[:, 0:4], rc_b[l:l + 1, :].broadcast_to([128, 4]), [], [("rb", 0)], "rb")
                  DMA("sync", rb_[:, 4:36], rf_b[l:l + 1].rearrange("o g e -> o (g e)").broadcast_to([128, 32]), [], [("rb", 1)], "rb")
                  RBK = [("rb", 0), ("rb", 1)]
                  mx = [A16.alloc(16 * 512).rearrange("p (k t) -> p k t", k=16) for _ in range(2)]
                  xt = [A32.alloc(D) for _ in range(1)]
                  x1t = [A32.alloc(D) for _ in range(2)]
                  h2f2 = [A32.alloc(D) for _ in range(2)]
                  h2b = [A16.alloc(D) for _ in range(2)]
                  h2T = A32.alloc(16 * 128).rearrange("p (k t) -> p k t", k=16)
                  sm2 = [A32.alloc(256) for _ in range(3)]
                  ssn = A32.alloc(8)
                  if q == 0:
                      MEMSET("vector", cnt_run[:], 0.0, ["cnt_run"])

                  def p5_main(it):
                      ti, sub = it // 4, it % 4
                      c0 = ti * 512
                      m_, mk_ = mx[ti % 2], ("mx", ti % 2)
                      if sub == 0:
                          DMA("sync", m_, mixT[q][:, c0:c0 + 512].rearrange("(k p) t -> p k t", p=128),
                              [("mixT", q, ti, i) for i in range(3)], [mk_], "mx%d" % (ti % 2))
                      b2 = it % 2
                      r0 = T0 + it * 128
                      xk = ("xt", 0)
                      DMA("sync", xt[0], cur_x[r0:r0 + 128, :], [("xcur", r0 // 128)], [xk], "xt0")
                      x1, x1k = x1t[b2], ("x1t", b2)
                      for cbk in range(4):
                          pi = cbk
                          for kc in range(16):
                              MM(pm[pi][:, :], m_[:, kc, sub * 128:(sub + 1) * 128], wo[:, kc, cbk * 512:(cbk + 1) * 512],
                                 kc == 0, kc == 15, [mk_, "wo"], [PM[pi]])
                          sl = slice(cbk * 512, (cbk + 1) * 512)
                          TT("vector", tmpf[:, sl], pm[pi][:, :], g1[:, sl], ALU.mult, [PM[pi], "g1"], [("tmpf", cbk)])
                          TT("gpsimd", x1[:, sl], tmpf[:, sl], xt[0][:, sl], ALU.add, [("tmpf", cbk), xk], [x1k])
                      DMA(STQ, x1_d[r0:r0 + 128, :], x1, [x1k], [("x1", r0 // 128)], "x1t%d" % b2)
                      ssk = U("ss")
                      ss = ssn[:, 0:4]
                      hb_, hbk = h2b[b2], ("h2b", b2)
                      h2f, h2fk = h2f2[b2], ("h2f", b2)
                      ACT(hb_, x1, AF.Square, [x1k], [hbk, ssk], accum_out=ss[:, 0:1])
                      ACT(ss[:, 1:2], ss[:, 0:1], AF.Sqrt, [ssk, "eps_t"], [ssk], scale=1.0 / D, bias=eps_t[:, 0:1])
                      P.op("vector", lambda e, ss=ss: e.reciprocal(out=ss[:, 2:3], in_=ss[:, 1:2]), [ssk], [ssk])
                      STT(tmpf, x1, ss[:, 2:3], a2, ALU.mult, ALU.mult, [x1k, ssk, "a2"], [("tmpf", i) for i in range(4)])
                      TT("gpsimd", h2f, tmpf, sh2, ALU.add, [("tmpf", i) for i in range(4)] + ["sh2"], [h2fk])
                      CP("scalar", hb_, h2f, [h2fk], [hbk])
                      DMA(STQ, h2_d[r0:r0 + 128, :], hb_, [hbk], [("h2", r0 // 128)], "h2b%d" % b2)

                  def p5_router(it):
                      git = (T0 // 128) + it
                      b2 = it % 2
                      h2f, h2fk = h2f2[b2], ("h2f", b2)
                      for kc in range(16):
                          pj = pr[(kc // 4) % 2]
                          TR(pj[:, (kc % 4) * 128:(kc % 4 + 1) * 128], h2f.rearrange("p (c k) -> p k c", k=16)[:, kc, :],
                             ident_f[:], [h2fk, "ident_f"], [PR[(kc // 4) % 2]])
                          if kc % 4 == 3:
                              g4 = kc // 4
                              CP("scalar" if g4 % 2 else "vector", h2T[:, g4 * 4:g4 * 4 + 4, :],
                                 pj[:, :].rearrange("p (k t) -> p k t", k=4), [PR[(kc // 4) % 2]], [("h2T", g4)])
                      for kc in range(16):
                          MM(pr[0][:, 0:36], h2T[:, kc, :], wr[:, kc, :], kc == 0, kc == 15,
                             [("h2T", kc // 4)] + WRK, [PR[0]])
                      rkeys[it] = U("rt")
                      route_tile(P, nc, sm2[it % 3], pr, PR, rb_, RBK, r_E, r_f, cnt_run, utinc, ones_b, git,
                                 TT, TS, STT, ACT, CP, RED, MM, U, part="A", rk=rkeys[it])

                  def p5_routerB(it):
                      git = (T0 // 128) + it
                      route_tile(P, nc, sm2[it % 3], pr, PR, rb_, RBK, r_E, r_f, cnt_run, utinc, ones_b, git,
                                 TT, TS, STT, ACT, CP, RED, MM, U, part="B", rk=rkeys[it])

                  rkeys = {}
                  NSUB = NT5 * 4
                  for it in range(NSUB):
                      p5_main(it)
                      if it > 0:
                          p5_router(it - 1)
                      if it > 1:
                          p5_routerB(it - 2)
                  p5_router(NSUB - 1)
                  if NSUB > 1:
                      p5_routerB(NSUB - 2)
                  p5_routerB(NSUB - 1)

              stop_here('P5')
              P.barrier()
              A16.reset(); A32.reset()
              fin = A32.alloc(8 * 32).rearrange("p (a e) -> p a e", a=8)
              RALL = ["cnt_run"]
              TS("vector", fin[:, 0, :], cnt_run[:], 127.0, None, ALU.add, None, ["cnt_run"], ["fin"])
              fin_i = A32.alloc(32).bitcast(I32)
              CP("vector", fin_i, fin[:, 0, :], ["fin"], ["fin_i"])
              TS("vector", fin_i, fin_i, 7, 7, ALU.arith_shift_right, ALU.logical_shift_left, ["fin_i"], ["fin_i"])
              CP("vector", fin[:, 2, :], fin_i, ["fin_i"], ["fin"])
              MEMSET("vector", fin[:, 3, :], 1.0, ["fin"])
              P.op("vector", lambda e: e.tensor_tensor_scan(out=fin[:, 4, :], data0=fin[:, 3, :], data1=fin[:, 2, :],
                                                            initial=0.0, op0=ALU.mult, op1=ALU.add), ["fin"], ["fin"])
              TT("vector", fin[:, 5, :], fin[:, 4, :], fin[:, 2, :], ALU.subtract, ["fin"], ["fin"])
              big = A32.alloc(NT * 32).rearrange("p (t e) -> p t e", e=32)
              slf = A32.alloc(NT * 2).rearrange("p (t k) -> p t k", k=2)
              for k in range(2):
                  TT("vector", big, r_E[:, :, k * 32:(k + 1) * 32], fin[:, 5:6, :].to_broadcast([128, NT, 32]), ALU.mult,
                     ["fin", "r_E"], ["big"])
                  RED(slf[:, :, k], big, ALU.add, ["big"], ["slf"])
                  TT("vector", slf[:, :, k], slf[:, :, k], r_f[:, :, k], ALU.add, ["slf", "r_f"], ["slf"])
              CP("vector", r_slot[:], slf, ["slf"], ["r_slot"])
              bst = A32.alloc(NB)
              DMA("sync", bst, k_bstart, [], ["bst"], "bst")
              ebf = A32.alloc(NB)
              CH = 32
              bigb = A32.alloc(CH * 32).rearrange("p (b e) -> p b e", e=32)
              for b0 in range(0, NB, CH):
                  nb_ = min(CH, NB - b0)
                  TT("vector", bigb[:, 0:nb_, :], fin[:, 4:5, :].to_broadcast([128, nb_, 32]),
                     bst[:, b0:b0 + nb_].unsqueeze(2).to_broadcast([128, nb_, 32]), ALU.is_le, ["fin", "bst"], ["bigb"])
                  RED(ebf[:, b0:b0 + nb_], bigb[:, 0:nb_, :], ALU.add, ["bigb"], ["ebf"])
              TS("vector", ebf, ebf, 31.0, None, ALU.min, None, ["ebf"], ["ebf"])
              sam = A32.alloc(NB)
              MEMSET("vector", sam[:, 0:1], 0.0, ["sam"])
              TT("vector", sam[:, 1:NB], ebf[:, 1:NB], ebf[:, 0:NB - 1], ALU.is_equal, ["ebf"], ["sam"])
              wif = A32.alloc(NB)
              TS("vector", wif, ebf, 128.0, iotap[:, 0:1], ALU.mult, ALU.add, ["ebf", "iotap"], ["wif"])
              STT(wif, sam, 1.0e6, wif, ALU.mult, ALU.add, ["sam", "wif"], ["wif"])
              if l > 0:
                  TS("vector", wif, wif, float(l * NE * 128), None, ALU.add, None, ["wif"], ["wif"])
              CP("vector", widx[:], wif, ["wif"], ["widx"])
              hl = [A16.alloc(D) for _ in range(3)]
              for it in range(NT):
                  b3 = it % 3
                  hk_ = ("hl", b3)
                  DMA("sync", hl[b3], h2_d[it * 128:(it + 1) * 128, :], [("h2", it)], [hk_], "hl%d" % b3)
                  for k in range(2):
                      off = r_slot[:, it, k:k + 1]
                      P.dma("gpsimd", (lambda e, off=off, src=hl[b3]: e.indirect_dma_start(
                          out=xs_d, out_offset=bass.IndirectOffsetOnAxis(ap=off, axis=0), in_=src, in_offset=None,
                          bounds_check=BR(e, L - 1), oob_is_err=False)), [hk_, "r_slot"], [("xs", it, k)], "xs_sc")
              XSK = [("xs", it, k) for it in range(NT) for k in range(2)]

              P.barrier()
              A16.reset(); A32.reset()
              W1 = A16.alloc(16 * 512)
              W3 = A16.alloc(16 * 512)
              W2 = A16.alloc(4 * 2048)
              xb = [A16.alloc(D) for _ in range(3)]
              xbT = [A16.alloc(16 * 128).rearrange("p (k t) -> p k t", k=16) for _ in range(2)]

              def xload(b):
                  DMA("sync", xb[b % 3], xs_d[b * 128:(b + 1) * 128, :], XSK, [("xb", b % 3)], "xb%d" % (b % 3))

              sgf = [A32.alloc(512) for _ in range(2)]
              hid = [A16.alloc(512) for _ in range(2)]
              hidT = [A16.alloc(4 * 128).rearrange("p (k t) -> p k t", k=4) for _ in range(2)]
              yb = [A32.alloc(D) for _ in range(2)]
              w1v = e_w1.rearrange("l e (p k) n -> (l e p) (k n)", k=16)
              w3v = e_w3.rearrange("l e (p k) n -> (l e p) (k n)", k=16)
              w2v = e_w2.rearrange("l e (p k) n -> (l e p) (k n)", k=4)
              bnd_l = (l + 1) * NE * 128 - 1

              def wgather(b, Wt, wv, wk):
                  off = widx[:, b:b + 1]
                  P.dma("gpsimd", (lambda e, off=off, Wt=Wt, wv=wv, bnd=bnd_l: e.indirect_dma_start(
                      out=Wt, out_offset=None, in_=wv, in_offset=bass.IndirectOffsetOnAxis(ap=off, axis=0),
                      bounds_check=BR(e, bnd), oob_is_err=False)), ["widx"], [wk], wk)

              def stageA(b):
                  b2 = b % 2
                  wgather(b, W1, w1v, "W1")
                  wgather(b, W3, w3v, "W3")
                  xk = ("xb", b % 3)
                  if b + 1 < NB:
                      xload(b + 1)
                  for kc in range(16):
                      TR(pt[:, kc * 128:(kc + 1) * 128], xb[b % 3].rearrange("p (c k) -> p k c", k=16)[:, kc, :], ident_b[:],
                         [xk, "ident_b"], [PT])
                  xT, xTk = xbT[b2], ("xbT", b2)
                  CP("scalar", xT[:, 0:8, :], pt[:, 0:1024].rearrange("p (k t) -> p k t", k=8), [PT], [xTk])
                  CP("vector", xT[:, 8:16, :], pt[:, 1024:2048].rearrange("p (k t) -> p k t", k=8), [PT], [xTk])
                  p1, p3 = 2 * b2, 2 * b2 + 1
                  for kc in range(16):
                      MM(pm[p1][:, :], xT[:, kc, :], W1[:, kc * 512:(kc + 1) * 512], kc == 0, kc == 15, [xTk, "W1"], [PM[p1]])
                  for kc in range(16):
                      MM(pm[p3][:, :], xT[:, kc, :], W3[:, kc * 512:(kc + 1) * 512], kc == 0, kc == 15, [xTk, "W3"], [PM[p3]])
                  ACT(sgf[b2], pm[p1][:, :], AF.Silu, [PM[p1]], [("sgf", b2)])
                  TT("vector", hid[b2], pm[p3][:, :], sgf[b2], ALU.mult, [PM[p3], ("sgf", b2)], [("hid", b2)])

              def stageB(b):
                  b2 = b % 2
                  wgather(b, W2, w2v, "W2")
                  for fc in range(4):
                      TR(pt[:, fc * 128:(fc + 1) * 128], hid[b2].rearrange("p (c k) -> p k c", k=4)[:, fc, :], ident_b[:],
                         [("hid", b2), "ident_b"], [PT])
                  CP("vector", hidT[b2], pt[:, 0:512].rearrange("p (k t) -> p k t", k=4), [PT], [("hidT", b2)])
                  y_, yk = yb[b2], ("yb", b2)
                  for cbk in range(4):
                      pi = cbk % 2
                      for fc in range(4):
                          MM(pr[pi][:, :], hidT[b2][:, fc, :], W2[:, fc * 2048 + cbk * 512:fc * 2048 + (cbk + 1) * 512],
                             fc == 0, fc == 3, [("hidT", b2), "W2"], [PR[pi]])
                      if cbk % 2:
                          CP("scalar", y_[:, cbk * 512:(cbk + 1) * 512], pr[pi][:, :], [PR[pi]], [yk])
                      else:
                          CP("vector", y_[:, cbk * 512:(cbk + 1) * 512], pr[pi][:, :], [PR[pi]], [yk])
                  DMA(STQ, ys_d[b * 128:(b + 1) * 128, :], y_, [yk], [("ys", b)], "yb%d" % b2)

              xload(0)
              stageA(0)
              for b in range(NB):
                  if b + 1 < NB:
                      stageA(b + 1)
                  stageB(b)
              YSK = [("ys", b) for b in range(NB)]

              stop_here('P7')
              P.barrier()
              A16.reset(); A32.reset()
              g2 = [A32.alloc(D) for _ in range(2)]
              for q in range(2):
                  DMA("sync", g2[q], mod_rep(q, 5), MODK, [("g2", q)], "g2%d" % q)
              if last:
                  fg = A32.alloc(D)
                  DMA("sync", fg, final_g.rearrange("(o n) -> o n", o=1).broadcast_to([128, D]), [], ["fg"], "fg")
              ya = [A32.alloc(D) for _ in range(2)]
              ybb = [A32.alloc(D) for _ in range(2)]
              x1t = [A32.alloc(D) for _ in range(2)]
              junk = A16.alloc(D)
              ss = A32.alloc(8)
              dst_x = y_out if last else x2_d
              for it in range(NT):
                  b2 = it % 2
                  q = 0 if it * 128 < OFFS[1] else 1
                  for k, yt in enumerate((ya, ybb)):
                      off = r_slot[:, it, k:k + 1]
                      P.dma("gpsimd", (lambda e, off=off, dst=yt[b2]: e.indirect_dma_start(
                          out=dst, out_offset=None, in_=ys_d, in_offset=bass.IndirectOffsetOnAxis(ap=off, axis=0),
                          bounds_check=BR(e, L - 1), oob_is_err=False)), YSK + ["r_slot"], [("yg", k, b2)], "yg%d%d" % (k, b2))
                  xk = ("x1t", b2)
                  DMA("sync", x1t[b2], x1_d[it * 128:(it + 1) * 128, :], [("x1", it)], [xk], "x1l%d" % b2)
                  A_, B_ = ya[b2], ybb[b2]
                  TS("vector", A_, A_, r_f[:, it, 2:3], None, ALU.mult, None, [("yg", 0, b2), "r_f"], [("yg", 0, b2)])
                  STT(A_, B_, r_f[:, it, 3:4], A_, ALU.mult, ALU.add, [("yg", 1, b2), ("yg", 0, b2), "r_f"], [("yg", 0, b2)])
                  TT("gpsimd", A_, A_, g2[q], ALU.mult, [("yg", 0, b2), ("g2", q)], [("yg", 0, b2)])
                  TT("vector", B_, A_, x1t[b2], ALU.add, [("yg", 0, b2), xk], [("yg", 1, b2)])
                  if not last:
                      DMA(STQ, dst_x[it * 128:(it + 1) * 128, :], B_, [("yg", 1, b2)], [("xcur", it)], "xo%d" % b2)
                  else:
                      ssk = U("ss")
                      ACT(junk, B_, AF.Square, [("yg", 1, b2)], ["junk", ssk], accum_out=ss[:, 0:1])
                      ACT(ss[:, 1:2], ss[:, 0:1], AF.Sqrt, [ssk, "eps_t"], [ssk], scale=1.0 / D, bias=eps_t[:, 0:1])
                      P.op("vector", lambda e, ss=ss: e.reciprocal(out=ss[:, 2:3], in_=ss[:, 1:2]), [ssk], [ssk])
                      STT(A_, B_, ss[:, 2:3], fg, ALU.mult, ALU.mult, [("yg", 1, b2), ssk, "fg"], [("yg", 0, b2)])
                      DMA(STQ, dst_x[it * 128:(it + 1) * 128, :], A_, [("yg", 0, b2)], [("yout", it)], "xo%d" % b2)
              cur_x = x2_d

          P.emit()
        except _Stop:
            pass
    return nc


def route_tile(P, nc, sm, pr, PR, rb_, RBK, r_E, r_f, cnt_run, utinc, ones_b, git,
               TT, TS, STT, ACT, CP, RED, MM, U, part="AB", rk=None):
    if "A" not in part:
        Ef = sm[:, 96:160]
        Mb = sm[:, 160:192]
        Mbb = sm[:, 192:224].bitcast(BF16)[:, 0:32]
        return route_tile_b(P, sm, pr, PR, r_f, cnt_run, utinc, ones_b, git, TT, RED, MM, rk, Ef, Mb, Mbb)
    rk = rk if rk is not None else U("rt")
    Lg = sm[:, 8:44]
    TT("vector", Lg, pr[0][:, 0:36], rb_, ALU.add, [PR[0]] + RBK, [rk])
    m = sm[:, 44:45]
    RED(m, Lg[:, 0:4], ALU.max, [rk], [rk])
    oh = sm[:, 48:52]
    TS("vector", oh, Lg[:, 0:4], m, None, ALU.is_equal, None, [rk], [rk])
    negm = sm[:, 45:46]
    TS("vector", negm, m, -1.0, None, ALU.mult, None, [rk], [rk])
    ex = sm[:, 52:56]
    se = sm[:, 46:47]
    ACT(ex, Lg[:, 0:4], AF.Exp, [rk], [rk], bias=negm, scale=1.0, accum_out=se)
    pg = sm[:, 47:48]
    P.op("vector", lambda e: e.reciprocal(out=pg, in_=se), [rk], [rk])
    lf = sm[:, 56:64]
    TS("vector", lf, Lg[:, 4:12], oh[:, 0:1], None, ALU.mult, None, [rk], [rk])
    for g in range(1, 4):
        STT(lf, Lg[:, 4 + 8 * g:12 + 8 * g], oh[:, g:g + 1], lf, ALU.mult, ALU.add, [rk], [rk])
    top = sm[:, 64:72]
    P.op("vector", lambda e: e.max(out=top, in_=lf), [rk], [rk])
    s1 = sm[:, 72:80]
    s2 = sm[:, 80:88]
    TS("vector", s1, lf, top[:, 0:1], None, ALU.is_equal, None, [rk], [rk])
    TS("vector", s2, lf, top[:, 1:2], None, ALU.is_equal, None, [rk], [rk])
    dv = sm[:, 88:89]
    TT("vector", dv, top[:, 0:1], top[:, 1:2], ALU.subtract, [rk], [rk])
    sg = sm[:, 89:90]
    ACT(sg, dv, AF.Sigmoid, [rk], [rk])
    TT("vector", r_f[:, git, 2:3], pg, sg, ALU.mult, [rk], ["r_f"])
    TT("vector", r_f[:, git, 3:4], pg, r_f[:, git, 2:3], ALU.subtract, [rk, "r_f"], ["r_f"])
    Ef = sm[:, 96:160]
    for g in range(4):
        TS("vector", Ef[:, 8 * g:8 * g + 8], s1, oh[:, g:g + 1], None, ALU.mult, None, [rk], [rk])
        TS("vector", Ef[:, 32 + 8 * g:40 + 8 * g], s2, oh[:, g:g + 1], None, ALU.mult, None, [rk], [rk])
    CP("vector", r_E[:, git, :], Ef, [rk], ["r_E"])
    Mb = sm[:, 160:192]
    TT("vector", Mb, Ef[:, 0:32], Ef[:, 32:64], ALU.add, [rk], [rk])
    Mbb = sm[:, 192:224].bitcast(BF16)[:, 0:32]
    CP("vector", Mbb, Mb, [rk], [rk])
    if "B" in part:
        route_tile_b(P, sm, pr, PR, r_f, cnt_run, utinc, ones_b, git, TT, RED, MM, rk, Ef, Mb, Mbb)


def route_tile_b(P, sm, pr, PR, r_f, cnt_run, utinc, ones_b, git, TT, RED, MM, rk, Ef, Mb, Mbb):
    MM(pr[1][:, 0:32], utinc[:], Mbb, True, True, ["utinc", rk], [PR[1]])
    MM(pr[1][:, 32:64], ones_b[:], Mbb, True, True, ["ones_b", rk], [PR[1]])
    rank = sm[:, 224:256]
    TT("vector", rank, pr[1][:, 0:32], Mb, ALU.subtract, [PR[1], rk], [rk])
    TT("vector", rank, rank, cnt_run[:], ALU.add, [rk, "cnt_run"], [rk])
    TT("vector", cnt_run[:], cnt_run[:], pr[1][:, 32:64], ALU.add, [PR[1], "cnt_run"], ["cnt_run"])
    tmp = sm[:, 160:192]
    for k in range(2):
        TT("vector", tmp, Ef[:, 32 * k:32 * k + 32], rank, ALU.mult, [rk], [rk])
        RED(r_f[:, git, k:k + 1], tmp, ALU.add, [rk], ["r_f"])


_WNAMES = ["w_ada", "b_ada", "norm1_g", "w_in", "pool_w", "pool_scale", "conv_dw_w", "conv_dw_b",
           "conv_ln_g", "conv_ln_b", "conv_pw_w", "conv_pw_b", "fourier_w", "w_out", "norm2_g",
           "router_coarse_w", "router_coarse_b", "router_fine_w", "router_fine_b",
           "expert_w1", "expert_w3", "expert_w2", "final_g"]


def kernel(**inputs):
    xs_ = np.asarray(inputs["x_sample"], np.float32)
    xp_ = np.asarray(inputs["x_prompt"], np.float32)
    cs_ = np.asarray(inputs["c_sample"], np.float32)
    cp_ = np.asarray(inputs["c_prompt"], np.float32)
    S0, S1 = xs_.shape[1], xp_.shape[1]
    depth = inputs["w_ada"].shape[0]
    nc = build((S0, S1), depth)
    N = S0 + S1
    NB = -(-(2 * N + NE * 127) // 128)
    consts = make_consts((S0, S1), NB)
    wts = {k: np.ascontiguousarray(np.asarray(inputs[k], np.float32)) for k in _WNAMES}
    in_maps = []
    for core in range(8):
        b = core % 4
        m = {"x": np.ascontiguousarray(np.concatenate([xs_[b], xp_[b]], axis=0)),
             "c": np.ascontiguousarray(np.stack([cs_[b], cp_[b]], axis=0))}
        m.update(wts)
        m.update(consts)
        in_maps.append(m)
    res = run_bass_kernel_spmd(nc, in_maps, core_ids=list(range(8)))
    y_s = np.stack([res.results[b]["y"][:S0] for b in range(4)], axis=0)
    y_p = np.stack([res.results[b]["y"][S0:] for b in range(4)], axis=0)
    return (y_p.astype(np.float32), y_s.astype(np.float32))
```

```python
import math
import os
from contextlib import ExitStack

import numpy as np
import ml_dtypes
import concourse.bass as bass
import concourse.mybir as mybir
from concourse.bass_utils import run_bass_kernel_spmd

F32 = mybir.dt.float32
BF16 = mybir.dt.bfloat16
I32 = mybir.dt.int32
AF = mybir.ActivationFunctionType
ALU = mybir.AluOpType
AX = mybir.AxisListType

D = 2048
NG, EPG, NE = 4, 8, 32
DE = 512
CONV_K = 31
EPS = 1e-6
SAME_ENGINE_SYNC = True
CONV_PE = os.environ.get('KCONV', 'pe') == 'pe'
NOSYNC = set(os.environ.get('KNOSYNC', 'tensor').split(','))


class _Op:
    __slots__ = ("q", "fn", "deps", "sig", "sigval", "sem", "is_dma", "idx", "bar")


class Prog:
    def __init__(self, nc):
        self.nc = nc
        self.ops = []
        self.last_w = {}
        self.readers = {}
        self.dma_cnt = {}
        self.last_eng = {}
        self.bar = None
        self.bar_done = set()

    def barrier(self):
        deps = [(o, None) for o in self.last_eng.values()]
        dmas = dict(self.dma_cnt)
        self.bar = (deps, dmas)
        self.bar_done = set()

    def _add(self, q, fn, reads, writes, is_dma, semkey):
        o = _Op()
        o.q = q
        o.fn = fn
        o.is_dma = is_dma
        o.sig = False
        o.sigval = 0
        o.idx = len(self.ops)
        deps = {}
        for r in reads:
            w = self.last_w.get(r)
            if w is not None:
                deps[w.idx] = w
        for w_ in writes:
            w = self.last_w.get(w_)
            if w is not None:
                deps[w.idx] = w
            for rd in self.readers.get(w_, ()):
                deps[rd.idx] = rd
        o.deps = []
        for d in deps.values():
            if d.is_dma:
                o.deps.append((d, self.dma_cnt[d.sem]))
            else:
                o.deps.append((d, None))
        o.bar = None
        if self.bar is not None and q not in self.bar_done:
            self.bar_done.add(q)
            bdeps, bdmas = self.bar
            o.deps.extend(bdeps)
            o.bar = bdmas
        if is_dma:
            o.sem = ("dma", semkey)
            self.dma_cnt[o.sem] = self.dma_cnt.get(o.sem, 0) + 1
            o.sigval = self.dma_cnt[o.sem]
        else:
            o.sem = ("eng", q)
            self.last_eng[q] = o
        for w_ in writes:
            self.last_w[w_] = o
            self.readers[w_] = []
        for r in reads:
            if r not in writes:
                self.readers.setdefault(r, []).append(o)
        self.ops.append(o)
        return o

    def op(self, eng, fn, reads=(), writes=()):
        return self._add(eng, fn, tuple(reads), tuple(writes), False, None)

    def dma(self, q, fn, reads=(), writes=(), semkey=None):
        assert semkey is not None
        return self._add(q, fn, tuple(reads), tuple(writes), True, semkey)

    def emit(self):
        nc = self.nc
        ops = self.ops
        for o in ops:
            for d, _ in o.deps:
                if d.is_dma:
                    continue
                if d.q != o.q or (SAME_ENGINE_SYNC and d.q not in NOSYNC):
                    d.sig = True
        cnt = {}
        for o in ops:
            if not o.is_dma and o.sig:
                cnt[o.q] = cnt.get(o.q, 0) + 1
                o.sigval = cnt[o.q]
        semkeys = []
        seen = set()
        for o in ops:
            if (o.is_dma or o.sig) and o.sem not in seen:
                seen.add(o.sem)
                semkeys.append(o.sem)
        self.n_sems = len(semkeys)
        with ExitStack() as es:
            sems = {}
            for i, k in enumerate(semkeys):
                sems[k] = es.enter_context(nc.semaphore("s%d" % i))
            block = es.enter_context(nc.Block())
            queues = {}
            for o in ops:
                queues.setdefault(o.q, []).append(o)
            totals = dict(self.dma_cnt)

            def run_queue(qname, eng):
                waited = {}
                for o in queues.get(qname, ()):
                    need = {}
                    for d, n in o.deps:
                        if d.is_dma:
                            v = 16 * n
                        else:
                            if d.q == o.q and (d.q in NOSYNC or not SAME_ENGINE_SYNC):
                                continue
                            v = d.sigval
                        if v > need.get(d.sem, 0):
                            need[d.sem] = v
                    if o.bar is not None:
                        for k, n in o.bar.items():
                            if 16 * n > need.get(k, 0):
                                need[k] = 16 * n
                    for k, v in need.items():
                        if waited.get(k, 0) < v:
                            eng.wait_ge(sems[k], v)
                            waited[k] = v
                    ins = o.fn(eng)
                    if o.is_dma:
                        ins.then_inc(sems[o.sem], 16)
                    elif o.sig:
                        ins.then_inc(sems[o.sem], 1)
                if qname == "sync":
                    for k, n in totals.items():
                        if waited.get(k, 0) < 16 * n:
                            eng.wait_ge(sems[k], 16 * n)

            @block.sync
            def _(e):
                run_queue("sync", e)

            @block.scalar
            def _(e):
                run_queue("scalar", e)

            @block.gpsimd
            def _(e):
                run_queue("gpsimd", e)

            @block.vector
            def _(e):
                run_queue("vector", e)

            @block.tensor
            def _(e):
                run_queue("tensor", e)


def _bf(a):
    return np.ascontiguousarray(a.astype(ml_dtypes.bfloat16))


def make_consts(SEQS, NB):
    c = {}
    t = np.arange(128)
    ang = 2 * np.pi * np.outer(t, t) / 128.0
    cs = np.zeros((128, 2, 192), np.float64)
    for h in range(2):
        sl = slice(h * 64, h * 64 + 64)
        cs[:, h, 0:64] = np.cos(ang[:, sl])
        cs[:, h, 64:128] = np.sin(ang[:, sl])
        cs[:, h, 128:192] = -np.sin(ang[:, sl])
    c["cs128"] = _bf(cs)
    ccn = np.zeros((128, 2, 128), np.float64)
    ccn[:, 0] = np.cos(ang)
    ccn[:, 1] = -np.sin(ang)
    c["ccn"] = _bf(ccn)
    for q, S in enumerate(SEQS):
        S2 = S // 128
        a = 2 * np.pi * (np.outer(np.arange(S2), np.arange(S)) % S) / S
        f = np.zeros((S2, 2, S), np.float64)
        f[:, 0] = np.cos(a)
        f[:, 1] = np.sin(a)
        c["fcs%d" % q] = _bf(f)
    pc = np.zeros((128, 4, 16), np.float32)
    for g, w in enumerate((2, 4, 8, 16)):
        for j in range(8):
            cnt = min(j + w // 2, 10 ** 9) - max(j - w // 2, 0)
            pc[:, g, j] = 1.0 / cnt
        for j in range(8):
            tt = -8 + j
            hi = min(tt + w // 2, 0)
            lo = tt - w // 2
            pc[:, g, 8 + j] = 1.0 / (hi - lo)
    c["poolc"] = pc
    ut = (np.arange(128)[:, None] <= np.arange(128)[None, :]).astype(np.float32)
    c["utinc"] = _bf(ut)
    c["iotap"] = np.arange(128, dtype=np.float32).reshape(128, 1)
    c["bstart"] = np.tile((128.0 * np.arange(NB, dtype=np.float32))[None, :], (128, 1))
    c["iota32"] = np.tile(np.arange(32, dtype=np.float32)[None, :], (128, 1))
    return c


class Arena:
    def __init__(self, t, size):
        self.t = t
        self.size = size
        self.off = 0

    def reset(self):
        self.off = 0

    def alloc(self, n, align=16):
        self.off = (self.off + align - 1) // align * align
        o = self.off
        self.off += n
        assert self.off <= self.size, ("arena overflow", self.off, self.size)
        return self.t[:, o:o + n]


def build(SEQS=(8192, 2048), DEPTH=2, dbg=False, NL=None):
    NL = DEPTH if NL is None else NL
    N = sum(SEQS)
    NT = N // 128
    NB = -(-(2 * N + NE * 127) // 128)
    L = NB * 128
    OFFS = [0]
    for S in SEQS:
        OFFS.append(OFFS[-1] + S)
    nc = bass.Bass("TRN2", target_bir_lowering=False)

    def din(name, shape, dt=F32):
        return nc.dram_tensor(name, list(shape), dt, kind="ExternalInput").ap()

    def dscr(name, shape, dt):
        kind = "ExternalOutput" if dbg else "Internal"
        return nc.dram_tensor(name, list(shape), dt, kind=kind).ap()

    x_in = din("x", [N, D])
    c_in = din("c", [2, D])
    w_ada = din("w_ada", [DEPTH, D, 6 * D])
    b_ada = din("b_ada", [DEPTH, 6 * D])
    norm1_g = din("norm1_g", [DEPTH, D])
    w_in = din("w_in", [DEPTH, D, 3072])
    pool_w = din("pool_w", [DEPTH, 4, 128, 128])
    pool_scale = din("pool_scale", [DEPTH, 512])
    conv_dw_w = din("conv_dw_w", [DEPTH, CONV_K, 1024])
    conv_dw_b = din("conv_dw_b", [DEPTH, 1024])
    conv_ln_g = din("conv_ln_g", [DEPTH, 1024])
    conv_ln_b = din("conv_ln_b", [DEPTH, 1024])
    conv_pw_w = din("conv_pw_w", [DEPTH, 1024, 1024])
    conv_pw_b = din("conv_pw_b", [DEPTH, 1024])
    fourier_w = din("fourier_w", [DEPTH, 512, 512])
    w_out = din("w_out", [DEPTH, D, D])
    norm2_g = din("norm2_g", [DEPTH, D])
    rc_w = din("router_coarse_w", [DEPTH, D, NG])
    rc_b = din("router_coarse_b", [DEPTH, NG])
    rf_w = din("router_fine_w", [DEPTH, NG, D, EPG])
    rf_b = din("router_fine_b", [DEPTH, NG, EPG])
    e_w1 = din("expert_w1", [DEPTH, NE, D, DE])
    e_w3 = din("expert_w3", [DEPTH, NE, D, DE])
    e_w2 = din("expert_w2", [DEPTH, NE, DE, D])
    final_g = din("final_g", [D])
    k_cs128 = din("cs128", [128, 2, 192], BF16)
    k_ccn = din("ccn", [128, 2, 128], BF16)
    k_fcs = [din("fcs%d" % q, [S // 128, 2, S], BF16) for q, S in enumerate(SEQS)]
    k_poolc = din("poolc", [128, 4, 16])
    k_utinc = din("utinc", [128, 128], BF16)
    k_iotap = din("iotap", [128, 1])
    k_bstart = din("bstart", [128, NB])
    k_iota32 = din("iota32", [128, 32])

    y_out = nc.dram_tensor("y", [N, D], F32, kind="ExternalOutput").ap()

    x1_d = dscr("x1", [N, D], F32)
    x2_d = dscr("x2", [N, D], F32)
    h2_d = dscr("h2", [N, D], BF16)
    xs_d = dscr("xs", [L, D], BF16)
    ys_d = dscr("ys", [L, D], F32)
    mod_d = dscr("modrow", [2, 6 * D], F32)
    zaT = [dscr("zaT%d" % q, [512, S], BF16) for q, S in enumerate(SEQS)]
    vT = [dscr("vT%d" % q, [1024, S], BF16) for q, S in enumerate(SEQS)]
    zcT = [dscr("zcT%d" % q, [512, S], BF16) for q, S in enumerate(SEQS)]
    rzT = [dscr("rzT%d" % q, [512, S], BF16) for q, S in enumerate(SEQS)]
    mixT = [dscr("mixT%d" % q, [2048, S], BF16) for q, S in enumerate(SEQS)]

    P = Prog(nc)
    es = ExitStack()
    with es:
        AR_SZ = 96 * 1024
        art = es.enter_context(nc.sbuf_tensor("arena", [128, AR_SZ], BF16))
        AR = Arena(art, AR_SZ)

        class _A16:
            def alloc(self, n):
                return AR.alloc(n)

            def reset(self):
                AR.reset()

        class _A32:
            def alloc(self, n):
                return AR.alloc(2 * n).bitcast(F32)

            def reset(self):
                pass

        A16 = _A16()
        A32 = _A32()
        ident_f = es.enter_context(nc.sbuf_tensor("ident_f", [128, 128], F32))
        ident_b = es.enter_context(nc.sbuf_tensor("ident_b", [128, 128], BF16))
        ones_b = es.enter_context(nc.sbuf_tensor("ones_b", [128, 128], BF16))
        utinc = es.enter_context(nc.sbuf_tensor("utinc_s", [128, 128], BF16))
        iotap = es.enter_context(nc.sbuf_tensor("iotap_s", [128, 1], F32))
        iota32 = es.enter_context(nc.sbuf_tensor("iota32_s", [128, 32], F32))
        eps_t = es.enter_context(nc.sbuf_tensor("eps_t", [128, 1], F32))
        r_E = es.enter_context(nc.sbuf_tensor("r_E", [128, NT, 64], BF16))
        r_f = es.enter_context(nc.sbuf_tensor("r_f", [128, NT, 4], F32))
        r_slot = es.enter_context(nc.sbuf_tensor("r_slot", [128, NT, 2], I32))
        cnt_run = es.enter_context(nc.sbuf_tensor("cnt_run", [128, 32], F32))
        widx = es.enter_context(nc.sbuf_tensor("widx", [128, NB], I32))
        pm = [es.enter_context(nc.psum_tensor("pm%d" % i, [128, 512], F32)) for i in range(4)]
        pt = es.enter_context(nc.psum_tensor("pt", [128, 2048], BF16))
        pr = [es.enter_context(nc.psum_tensor("pr%d" % i, [128, 512], F32)) for i in range(2)]
        PM = [("pm", i) for i in range(4)]
        PT = "pt"
        PR = [("pr", i) for i in range(2)]

        STQ = os.environ.get('KSTQ', 'sync')
        uid = [0]
        _bregs = {}

        def BR(e, val):
            if val not in _bregs:
                _bregs[val] = e.to_reg(val)
            return _bregs[val]

        def U(name):
            uid[0] += 1
            return (name, uid[0])

        def t16(n, shape=None):
            v = A16.alloc(n)
            return v

        def DMA(q, out, in_, reads, writes, semkey, **kw):
            P.dma(q, lambda e: e.dma_start(out=out, in_=in_, **kw), reads, writes, semkey)

        def MM(out, lhsT, rhs, start, stop, reads, writes):
            P.op("tensor", lambda e: e.matmul(out, lhsT=lhsT, rhs=rhs, start=start, stop=stop), reads, writes)

        def TR(out, in_, ident, reads, writes):
            P.op("tensor", lambda e: e.transpose(out=out, in_=in_, identity=ident), reads, writes)

        def ACT(out, in_, func, reads, writes, bias=None, scale=None, accum_out=None, eng="scalar"):
            kw = {}
            if bias is not None:
                kw["bias"] = bias
            if scale is not None:
                kw["scale"] = scale
            if accum_out is not None:
                kw["accum_out"] = accum_out
            P.op("scalar", lambda e: e.activation(out=out, in_=in_, func=func, **kw), reads, writes)

        def TT(eng, out, in0, in1, op, reads, writes):
            P.op(eng, lambda e: e.tensor_tensor(out=out, in0=in0, in1=in1, op=op), reads, writes)

        def TS(eng, out, in0, s1, s2, op0, op1, reads, writes, accum_out=None):
            if op1 is None:
                P.op(eng, lambda e: e.tensor_scalar(out=out, in0=in0, scalar1=s1, scalar2=None, op0=op0), reads, writes)
            elif accum_out is not None:
                P.op(eng, lambda e: e.tensor_scalar(out=out, in0=in0, scalar1=s1, scalar2=s2, op0=op0, op1=op1, accum_out=accum_out), reads, writes)
            else:
                P.op(eng, lambda e: e.tensor_scalar(out=out, in0=in0, scalar1=s1, scalar2=s2, op0=op0, op1=op1), reads, writes)

        def STT(out, in0, scalar, in1, op0, op1, reads, writes):
            P.op("vector", lambda e: e.scalar_tensor_tensor(out=out, in0=in0, scalar=scalar, in1=in1, op0=op0, op1=op1), reads, writes)

        def CP(eng, out, in_, reads, writes):
            if eng == "scalar":
                P.op("scalar", lambda e: e.copy(out=out, in_=in_), reads, writes)
            else:
                P.op(eng, lambda e: e.tensor_copy(out=out, in_=in_), reads, writes)

        def MEMSET(eng, ap, val, writes):
            P.op(eng, lambda e: e.memset(ap, val), (), writes)

        def RED(out, in_, op, reads, writes, axis=AX.X):
            P.op("vector", lambda e: e.tensor_reduce(out=out, in_=in_, axis=axis, op=op), reads, writes)

        def bcast_row(dram_row_ap, n):
            return dram_row_ap.partition_broadcast(128)

        MEMSET("gpsimd", ident_f[:], 0.0, ["ident_f"])
        P.op("gpsimd", lambda e: e.affine_select(out=ident_f[:], in_=ident_f[:], pattern=[[-1, 128]],
                                                 compare_op=ALU.not_equal, fill=1.0, base=0, channel_multiplier=1),
             ["ident_f"], ["ident_f"])
        CP("vector", ident_b[:], ident_f[:], ["ident_f"], ["ident_b"])
        MEMSET("gpsimd", ones_b[:], 1.0, ["ones_b"])
        MEMSET("gpsimd", eps_t[:], EPS, ["eps_t"])
        DMA("sync", utinc[:], k_utinc, [], ["utinc"], "c_utinc")
        DMA("sync", iotap[:], k_iotap, [], ["iotap"], "c_iotap")
        DMA("sync", iota32[:], k_iota32, [], ["iota32"], "c_iota32")

        class _Stop(Exception):
            pass

        def stop_here(tag):
            if os.environ.get("KSTOP") == tag:
                P.emit()
                raise _Stop()

        cur_x = x_in
        try:
          for l in range(NL):
              last = (l == NL - 1)
              nxt_x = y_out if False else x2_d
              P.barrier()
              A16.reset(); A32.reset()
              cst = A32.alloc(2 * 16).rearrange("p (q k) -> p q k", q=2)
              csb = A16.alloc(16 * 2).rearrange("p (k q) -> p k q", q=2)
              brow = [A32.alloc(512) for _ in range(2)]
              mrow = A32.alloc(512)
              DMA("sync", cst, c_in.rearrange("q (p k) -> p q k", k=16), [], ["cst"], "cst")
              for q in range(2):
                  ACT(csb[:, :, q], cst[:, q, :], AF.Silu, ["cst"], [("csb", q)])
              wa = [A16.alloc(16 * 512).rearrange("p (k n) -> p k n", k=16) for _ in range(2)]
              for cb in range(24):
                  wbuf = wa[cb % 2]
                  wk = ("wa", cb % 2)
                  DMA("gpsimd", wbuf, w_ada[l, :, cb * 512:(cb + 1) * 512].rearrange("(p k) n -> p k n", k=16),
                      [], [wk], "wa%d" % (cb % 2))
                  pk = PM[cb % 2]
                  pst = pm[cb % 2]
                  for kc in range(16):
                      MM(pst[0:2, :], csb[:, kc, :], wbuf[:, kc, :], kc == 0, kc == 15,
                         [wk, ("csb", 0), ("csb", 1)], [pk])
                  mk = U("mrow")
                  bk = ("brow", cb % 2)
                  DMA("sync", brow[cb % 2][0:2, :], b_ada[l:l + 1, cb * 512:(cb + 1) * 512].broadcast_to([2, 512]),
                      [], [bk], "brow%d" % (cb % 2))
                  TT("vector", mrow[0:2, :], pst[0:2, :], brow[cb % 2][0:2, :], ALU.add,
                     [pk, bk], ["mrow"])
                  DMA(STQ, mod_d[:, cb * 512:(cb + 1) * 512], mrow[0:2, :], ["mrow"], [("mod", cb)], "mrow")
              MODK = [("mod", cb) for cb in range(24)]

              def mod_rep(q, i):
                  return mod_d[q:q + 1, i * D:(i + 1) * D].broadcast_to([128, D])

              for q, S in enumerate(SEQS):
                  T0 = OFFS[q]
                  NT5 = S // 512
                  S2 = S // 128
                  P.barrier()
                  A16.reset(); A32.reset()
                  wi = A16.alloc(16 * 3072).rearrange("p (k n) -> p k n", k=16)
                  DMA("gpsimd", wi, w_in[l].rearrange("(p k) n -> p k n", k=16), [], ["wi"], "wi")
                  a1 = A32.alloc(D)
                  sh1 = A32.alloc(D)
                  tmpf = A32.alloc(D)
                  DMA("sync", a1, mod_rep(q, 1), MODK, ["a1"], "a1")
                  DMA("sync", sh1, mod_rep(q, 0), MODK, ["sh1"], "sh1")
                  DMA("sync", tmpf, norm1_g[l:l + 1, :].broadcast_to([128, D]), [], ["tmpf"], "g_rep")
                  STT(a1, a1, 1.0, tmpf, ALU.add, ALU.mult, ["a1", "tmpf"], ["a1"])
                  xt = [A32.alloc(D) for _ in range(2)]
                  hb = [A16.alloc(D) for _ in range(4)]
                  hT = [A16.alloc(16 * 512).rearrange("p (k t) -> p k t", k=16) for _ in range(1)]
                  stg = [A16.alloc(16 * 512).rearrange("p (j t) -> p j t", j=16) for _ in range(1)]
                  sgt = [A32.alloc(512) for _ in range(2)]
                  ss = A32.alloc(8)
                  hTt = hT[0]
                  hk = ("hT", 0)

                  def p1_norm(ti):
                      for sub in range(4):
                          it = ti * 4 + sub
                          b2 = it % 2
                          r0 = T0 + it * 128
                          xk = ("xt", b2)
                          DMA("sync", xt[b2], cur_x[r0:r0 + 128, :], [("xcur", r0 // 128)], [xk], "xt%d" % b2)
                          ssk = U("ss")
                          hbk = ("hb", sub)
                          ACT(hb[sub], xt[b2], AF.Square, [xk], [hbk, ssk], accum_out=ss[:, 0:1])
                          ACT(ss[:, 1:2], ss[:, 0:1], AF.Sqrt, [ssk, "eps_t"], [ssk], scale=1.0 / D, bias=eps_t[:, 0:1])
                          P.op("vector", lambda e, ss=ss: e.reciprocal(out=ss[:, 2:3], in_=ss[:, 1:2]), [ssk], [ssk])
                          STT(tmpf, xt[b2], ss[:, 2:3], a1, ALU.mult, ALU.mult, [xk, ssk, "a1"], ["tmpf"])
                          TT("gpsimd", hb[sub], tmpf, sh1, ALU.add, ["tmpf", "sh1"], [hbk])

                  def p1_tr(ti):
                      for sub in range(4):
                          hbk = ("hb", sub)
                          for kc in range(16):
                              TR(pt[:, kc * 128:(kc + 1) * 128], hb[sub].rearrange("p (c k) -> p k c", k=16)[:, kc, :],
                                 ident_b[:], [hbk, "ident_b"], [PT])
                          CP("scalar" if sub % 2 else "vector", hTt[:, :, sub * 128:(sub + 1) * 128],
                             pt[:, :].rearrange("p (k t) -> p k t", k=16), [PT], [hk])

                  def p1_mm(ti):
                      st = stg[0]
                      sk = ("stg", 0)
                      c0 = ti * 512

                      def mmgrp(j, pi):
                          for kc in range(16):
                              MM(pm[pi][:, :], wi[:, kc, j * 128:(j + 1) * 128], hTt[:, kc, :], kc == 0, kc == 15,
                                 ["wi", hk], [PM[pi]])

                      pi = 0
                      for j in range(4):
                          mmgrp(j, pi)
                          CP("scalar", st[:, j, :], pm[pi][:, :], [PM[pi]], [sk])
                          pi = (pi + 1) % 4
                      for j in range(8):
                          pa = pi
                          mmgrp(4 + j, pa)
                          pb = (pi + 1) % 4
                          mmgrp(12 + j, pb)
                          sg = sgt[j % 2]
                          sgk = ("sg", j % 2)
                          ACT(sg, pm[pb][:, :], AF.Sigmoid, [PM[pb]], [sgk])
                          TT("vector", st[:, 4 + j, :], pm[pa][:, :], sg, ALU.mult, [PM[pa], sgk], [sk])
                          pi = (pi + 2) % 4
                      for j in range(4):
                          mmgrp(20 + j, pi)
                          CP("scalar", st[:, 12 + j, :], pm[pi][:, :], [PM[pi]], [sk])
                          pi = (pi + 1) % 4
                      DMA(STQ, zaT[q][:, c0:c0 + 512].rearrange("(j p) t -> p j t", p=128), st[:, 0:4, :],
                          [sk], [("zaT", q, ti)], "stg0")
                      DMA(STQ, vT[q][:, c0:c0 + 512].rearrange("(j p) t -> p j t", p=128), st[:, 4:12, :],
                          [sk], [("vT", q, ti)], "stg0")
                      DMA(STQ, zcT[q][:, c0:c0 + 512].rearrange("(j p) t -> p j t", p=128), st[:, 12:16, :],
                          [sk], [("zcT", q, ti)], "stg0")

                  p1_norm(0)
                  p1_tr(0)
                  for ti in range(NT5):
                      if ti + 1 < NT5:
                          p1_norm(ti + 1)
                      p1_mm(ti)
                      if ti + 1 < NT5:
                          p1_tr(ti + 1)

                  stop_here('P1')
                  P.barrier()
                  A16.reset(); A32.reset()
                  pwf = A32.alloc(4 * 128).rearrange("p (g e) -> p g e", g=4)
                  pwb = A16.alloc(4 * 128).rearrange("p (g e) -> p g e", g=4)
                  DMA("sync", pwf, pool_w[l].rearrange("g c e -> c g e"), [], ["pwf"], "pwf")
                  CP("vector", pwb, pwf, ["pwf"], ["pwb"])
                  psc = A32.alloc(4)
                  pscr = A32.alloc(4 * 128).rearrange("p (g e) -> p g e", g=4)[0:4]
                  DMA("sync", pscr[:, 0, :], pool_scale[l].rearrange("(g e) -> g e", g=4), [], ["pscr"], "pscr")
                  TR(pr[0][:, 0:4], pscr[:, 0, :], ident_f[0:4, 0:4], ["pscr", "ident_f"], [PR[0]])
                  CP("vector", psc, pr[0][:, 0:4], [PR[0]], ["psc"])
                  pcst = A32.alloc(64).rearrange("p (g j) -> p g j", g=4)
                  DMA("sync", pcst, k_poolc, [], ["pcst"], "pcst")
                  ub = [A16.alloc(528) for _ in range(2)]
                  sa = [A32.alloc(528) for _ in range(2)]
                  sb_ = [A32.alloc(528) for _ in range(2)]
                  dmt = [A16.alloc(512) for _ in range(2)]
                  pst2 = [A16.alloc(4 * 512).rearrange("p (g t) -> p g t", g=4) for _ in range(2)]
                  itc2 = [0]

                  def p2_tile(ti):
                      c0 = ti * 512
                      st = pst2[ti % 2]
                      sk = ("pst2", ti % 2)
                      for g, w in enumerate((2, 4, 8, 16)):
                          b2 = itc2[0] % 2
                          itc2[0] += 1
                          u = ub[b2]
                          uk = ("ub", b2)
                          lo = max(c0 - 8, 0)
                          hi = min(c0 + 520, S)
                          rd = [("zaT", q, tj) for tj in range(max(ti - 1, 0), min(ti + 2, NT5))]
                          if lo > c0 - 8:
                              MEMSET("gpsimd", u[:, 0:8], 0.0, [uk])
                          if hi < c0 + 520:
                              MEMSET("gpsimd", u[:, 520:528], 0.0, [uk])
                          DMA("sync", u[:, lo - (c0 - 8):hi - (c0 - 8)], zaT[q][g * 128:(g + 1) * 128, lo:hi], rd, [uk], "ub%d" % b2)
                          s_a, s_b = sa[b2], sb_[b2]
                          ka, kb = ("sa", b2), ("sb", b2)
                          TT("vector", s_a[:, 1:528], u[:, 0:527], u[:, 1:528], ALU.add, [uk], [ka])
                          cur, curk, oth, othk = s_a, ka, s_b, kb
                          lo_v = 1
                          hi_v = 528
                          step = 1
                          ww = 2
                          while ww < w:
                              nlo = lo_v + step
                              nhi = hi_v - step
                              TT("vector", oth[:, nlo:nhi], cur[:, nlo - step:nhi - step], cur[:, nlo + step:nhi + step],
                                 ALU.add, [curk], [othk])
                              cur, curk, oth, othk = oth, othk, cur, curk
                              lo_v, hi_v = nlo, nhi
                              step *= 2
                              ww *= 2
                          dm = dmt[b2]
                          dk_ = ("dm", b2)
                          STT(dm, cur[:, 8:520], 1.0 / w, u[:, 8:520], ALU.mult, ALU.subtract, [curk, uk], [dk_])
                          if ti == 0:
                              TT("vector", oth[:, 8:16], cur[:, 8:16], pcst[:, g, 0:8], ALU.mult, [curk, "pcst"], [othk])
                              TT("vector", dm[:, 0:8], oth[:, 8:16], u[:, 8:16], ALU.subtract, [othk, uk], [dk_])
                          if ti == NT5 - 1:
                              TT("vector", oth[:, 512:520], cur[:, 512:520], pcst[:, g, 8:16], ALU.mult, [curk, "pcst"], [othk])
                              TT("vector", dm[:, 504:512], oth[:, 512:520], u[:, 512:520], ALU.subtract, [othk, uk], [dk_])
                          pi = g
                          MM(pm[pi][:, :], pwb[:, g, :], dm, True, True, ["pwb", dk_], [PM[pi]])
                          ACT(st[:, g, :], pm[pi][:, :], AF.Copy, [PM[pi], "psc"], [sk], scale=psc[:, g:g + 1])
                      DMA(STQ, mixT[q][0:512, c0:c0 + 512].rearrange("(g p) t -> p g t", p=128), st, [sk],
                          [("mixT", q, ti, 0)], "pst2%d" % (ti % 2))

                  pww = A16.alloc(8 * 1024).rearrange("p (k n) -> p k n", k=8)
                  DMA("gpsimd", pww, conv_pw_w[l].rearrange("(k p) n -> p k n", p=128), [], ["pww"], "pww")
                  dwr = A32.alloc(1024)
                  DMA("sync", dwr[0:CONV_K, :], conv_dw_w[l], [], ["dwr"], "dwr")
                  dwT = A32.alloc(8 * 32).rearrange("p (j k) -> p j k", j=8)
                  for j in range(8):
                      TR(pr[0][:, j * 32:j * 32 + CONV_K], dwr[0:CONV_K, j * 128:(j + 1) * 128],
                         ident_f[0:CONV_K, 0:CONV_K], ["dwr", "ident_f"], [PR[0]])
                  CP("vector", dwT[:, :, 0:CONV_K], pr[0][:, 0:256].rearrange("p (j k) -> p j k", j=8)[:, :, 0:CONV_K],
                     [PR[0]], ["dwT"])
                  vr = A32.alloc(4 * 1024).rearrange("p (v n) -> p v n", v=4)
                  vecs = A32.alloc(32).rearrange("p (v j) -> p v j", v=4)
                  for vi, src in enumerate((conv_dw_b, conv_ln_g, conv_ln_b, conv_pw_b)):
                      DMA("sync", vr[0:8, vi, 0:128], src[l].rearrange("(j p) -> j p", p=128), [], [("vr", vi)], "vr%d" % vi)
                      TR(pr[1][:, vi * 8:vi * 8 + 8], vr[0:8, vi, 0:128], ident_f[0:8, 0:8], [("vr", vi), "ident_f"], [PR[1]])
                  CP("vector", vecs, pr[1][:, 0:32].rearrange("p (v j) -> p v j", v=4), [PR[1]], ["vecs"])
                  acc = [A32.alloc(512) for _ in range(8)]
                  cbf = [A16.alloc(512) for _ in range(2)]
                  sqb = [A16.alloc(512) for _ in range(2)]
                  sT = [A16.alloc(512) for _ in range(8)]
                  mean = A32.alloc(512)
                  var = A32.alloc(512)
                  rstd = A32.alloc(512)
                  xn = [A32.alloc(512) for _ in range(2)]
                  st3 = [A16.alloc(8 * 512).rearrange("p (j t) -> p j t", j=8) for _ in range(2)]
                  if CONV_PE:
                      vb = [A16.alloc(544) for _ in range(3)]
                      vb1 = [A16.alloc(544) for _ in range(3)]
                      dg = A16.alloc(8 * 32 * 128).rearrange("p (j k c) -> p j k c", j=8, k=32)
                      for j in range(8):
                          for k in range(CONV_K):
                              TS("vector" if (j * CONV_K + k) % 2 else "gpsimd", dg[:, j, k, :], ident_b[:, :], dwT[:, j, k:k + 1], None,
                                 ALU.mult, None, ["ident_b", "dwT"], [("dg", j)])
                  else:
                      vb = [A16.alloc(544) for _ in range(2)]
                  itc3 = [0]

                  def p3_tile(ti):
                      c0 = ti * 512
                      for j in range(8):
                          if CONV_PE:
                              b2 = itc3[0] % 2
                              b3 = itc3[0] % 3
                              itc3[0] += 1
                              v, vk = vb[b3], ("vb", b3)
                              v1, v1k = vb1[b3], ("vb1", b3)
                              lo = max(c0 - 15, 0)
                              hi = min(c0 + 527, S)
                              rd = [("vT", q, tj) for tj in range(max(ti - 1, 0), min(ti + 2, NT5))]
                              if lo > c0 - 15:
                                  MEMSET("gpsimd", v[:, 0:16], 0.0, [vk])
                                  MEMSET("gpsimd", v1[:, 0:16], 0.0, [v1k])
                              if hi < c0 + 527:
                                  MEMSET("gpsimd", v[:, 526:544], 0.0, [vk])
                                  MEMSET("gpsimd", v1[:, 526:544], 0.0, [v1k])
                              DMA("sync", v[:, lo - (c0 - 15):hi - (c0 - 15)], vT[q][j * 128:(j + 1) * 128, lo:hi], rd, [vk], "vb%d" % b3)
                              lo1 = max(c0 - 14, 0)
                              DMA("sync", v1[:, lo1 - (c0 - 14):hi - (c0 - 14)], vT[q][j * 128:(j + 1) * 128, lo1:hi], rd, [v1k], "vb1%d" % b3)
                              pi = itc3[0] % 4
                              for k in range(CONV_K):
                                  if k % 2 == 0:
                                      MM(pm[pi][:, :], dg[:, j, k, :], v[:, k:k + 512], k == 0, k == CONV_K - 1,
                                         [("dg", j), vk], [PM[pi]])
                                  else:
                                      MM(pm[pi][:, :], dg[:, j, k, :], v1[:, k - 1:k - 1 + 512], k == 0, k == CONV_K - 1,
                                         [("dg", j), v1k], [PM[pi]])
                              a = acc[j]
                              ak = ("acc", j)
                              ACT(a, pm[pi][:, :], AF.Identity, [PM[pi], "vecs"], [ak], bias=vecs[:, 0, j:j + 1])
                              cb_, ck = cbf[b2], ("cbf", b2)
                              sq_, sqk = sqb[b2], ("sqb", b2)
                              CP("vector", cb_, a, [ak], [ck])
                              ACT(sq_, a, AF.Square, [ak], [sqk])
                              MM(pr[0][:, :], ones_b[:], cb_, j == 0, j == 7, ["ones_b", ck], [PR[0]])
                              MM(pr[1][:, :], ones_b[:], sq_, j == 0, j == 7, ["ones_b", sqk], [PR[1]])
                          else:
                              b2 = itc3[0] % 2
                              itc3[0] += 1
                              v = vb[b2]
                              vk = ("vb", b2)
                              lo = max(c0 - 15, 0)
                              hi = min(c0 + 527, S)
                              rd = [("vT", q, tj) for tj in range(max(ti - 1, 0), min(ti + 2, NT5))]
                              if lo > c0 - 15:
                                  MEMSET("gpsimd", v[:, 0:15], 0.0, [vk])
                              if hi < c0 + 527:
                                  MEMSET("gpsimd", v[:, 527:542], 0.0, [vk])
                              DMA("sync", v[:, lo - (c0 - 15):hi - (c0 - 15)], vT[q][j * 128:(j + 1) * 128, lo:hi], rd, [vk], "vb%d" % b2)
                              a = acc[j]
                              ak = ("acc", j)
                              TS("vector", a, v[:, 0:512], dwT[:, j, 0:1], vecs[:, 0, j:j + 1], ALU.mult, ALU.add,
                                 [vk, "dwT", "vecs"], [ak])
                              for k in range(1, CONV_K):
                                  STT(a, v[:, k:k + 512], dwT[:, j, k:k + 1], a, ALU.mult, ALU.add, [vk, "dwT", ak], [ak])
                              cb_, ck = cbf[b2], ("cbf", b2)
                              sq_, sqk = sqb[b2], ("sqb", b2)
                              CP("gpsimd", cb_, a, [ak], [ck])
                              ACT(sq_, a, AF.Square, [ak], [sqk])
                              MM(pr[0][:, :], ones_b[:], cb_, j == 0, j == 7, ["ones_b", ck], [PR[0]])
                              MM(pr[1][:, :], ones_b[:], sq_, j == 0, j == 7, ["ones_b", sqk], [PR[1]])
                      TS("vector", mean, pr[0][:, :], 1.0 / 1024, None, ALU.mult, None, [PR[0]], ["mean"])
                      TS("vector", var, pr[1][:, :], 1.0 / 1024, None, ALU.mult, None, [PR[1]], ["var"])
                      TT("vector", rstd, mean, mean, ALU.mult, ["mean"], ["rstd"])
                      TT("vector", var, var, rstd, ALU.subtract, ["var", "rstd"], ["var"])
                      ACT(var, var, AF.Sqrt, ["var", "eps_t"], ["var"], bias=eps_t[:, 0:1], scale=1.0)
                      P.op("vector", lambda e, rstd=rstd, var=var: e.reciprocal(out=rstd, in_=var), ["var"], ["rstd"])
                      for j in range(8):
                          x_, xk_ = xn[j % 2], ("xn", j % 2)
                          TT("gpsimd", x_, acc[j], mean, ALU.subtract, [("acc", j), "mean"], [xk_])
                          TT("vector", x_, x_, rstd, ALU.mult, [xk_, "rstd"], [xk_])
                          ACT(sT[j], x_, AF.Silu, [xk_, "vecs"], [("sT", j)], scale=vecs[:, 1, j:j + 1], bias=vecs[:, 2, j:j + 1])
                      st = st3[ti % 2]
                      sk = ("st3", ti % 2)
                      for e_ in range(8):
                          pi = e_ % 4
                          for j in range(8):
                              MM(pm[pi][:, :], pww[:, j, e_ * 128:(e_ + 1) * 128], sT[j], j == 0, j == 7,
                                 ["pww", ("sT", j)], [PM[pi]])
                          ACT(st[:, e_, :], pm[pi][:, :], AF.Identity, [PM[pi], "vecs"], [sk], bias=vecs[:, 3, e_:e_ + 1])
                      DMA(STQ, mixT[q][512:1536, c0:c0 + 512].rearrange("(j p) t -> p j t", p=128), st, [sk],
                          [("mixT", q, ti, 1)], "st3%d" % (ti % 2))

                  p2_tile(0)
                  for ti in range(NT5):
                      if ti + 1 < NT5:
                          p2_tile(ti + 1)
                      p3_tile(ti)
                  stop_here('P3')
                  P.barrier()
                  A16.reset(); A32.reset()
                  cs = A16.alloc(2 * 192).rearrange("p (h n) -> p h n", h=2)
                  ccn = A16.alloc(2 * 128).rearrange("p (h n) -> p h n", h=2)
                  fcs = A16.alloc(2 * S).rearrange("p (h n) -> p h n", h=2)
                  DMA("sync", cs, k_cs128, [], ["cs"], "cs")
                  DMA("sync", ccn, k_ccn, [], ["ccn"], "ccn")
                  DMA("sync", fcs[0:S2], k_fcs[q], [], ["fcs"], "fcs")
                  fwf = A32.alloc(4 * 512).rearrange("p (h e) -> p h e", h=4)
                  fwb = A16.alloc(4 * 512).rearrange("p (h e) -> p h e", h=4)
                  DMA("sync", fwf, fourier_w[l].rearrange("(h m) e -> m h e", h=4), [], ["fwf"], "fwf")
                  CP("vector", fwb, fwf, ["fwf"], ["fwb"])
                  Ut = A16.alloc(128 * S2).rearrange("p (c t) -> p c t", c=128)
                  Ah = A16.alloc(128 * 192).rearrange("p (c n) -> p c n", c=128)
                  Xh = A16.alloc(2 * S).rearrange("p (h k) -> p h k", h=2)
                  rst = [A16.alloc(512) for _ in range(2)]
                  ALLZC = [("zcT", q, tj) for tj in range(NT5)]
                  nrm = 1.0 / math.sqrt(S * 128.0)
                  ri = 0
                  for h in range(4):
                      for cq in range(8):
                          DMA("sync", Ut[:, cq * 16:(cq + 1) * 16, :],
                              zcT[q][h * 128 + cq * 16:h * 128 + (cq + 1) * 16, :].rearrange("c (a b) -> a c b", b=S2),
                              ALLZC, ["Ut"], "Ut")
                      for half in range(2):
                          pmv = [pm[i] for i in range(4)]
                          for cg in range(16):
                              for ci in range(8):
                                  c_ = cg * 8 + ci
                                  bank = ci // 2
                                  col = (ci % 2) * 192
                                  MM(pmv[bank][0:S2, col:col + 192], Ut[:, c_, :], cs[:, half, :], True, True,
                                     ["Ut", "cs"], [PM[bank]])
                              for bank in range(4):
                                  eng = "vector" if bank % 2 == 0 else "scalar"
                                  CP(eng, Ah[0:S2, cg * 8 + bank * 2:cg * 8 + bank * 2 + 2, :],
                                     pmv[bank][0:S2, 0:384].rearrange("p (c n) -> p c n", c=2), [PM[bank]], [("Ah", cg)])
                          AHK = [("Ah", cg) for cg in range(16)]
                          G = 512 // S2
                          G = min(G, 64)
                          for kg in range(64 // G):
                              pxr, pxi = pr[0], pr[1]
                              for gi in range(G):
                                  k1l = kg * G + gi
                                  k1 = half * 64 + k1l
                                  fc_ = fcs[0:S2, 0, :].rearrange("p (b a) -> p a b", a=128)[:, k1, :]
                                  fs_ = fcs[0:S2, 1, :].rearrange("p (b a) -> p a b", a=128)[:, k1, :]
                                  ar = Ah[0:S2, :, k1l]
                                  ai = Ah[0:S2, :, 64 + k1l]
                                  an = Ah[0:S2, :, 128 + k1l]
                                  o = gi * S2
                                  MM(pxr[:, o:o + S2], ar, fc_, True, False, AHK + ["fcs"], [PR[0]])
                                  MM(pxr[:, o:o + S2], an, fs_, False, True, AHK + ["fcs"], [PR[0]])
                                  MM(pxi[:, o:o + S2], ar, fs_, True, False, AHK + ["fcs"], [PR[1]])
                                  MM(pxi[:, o:o + S2], ai, fc_, False, True, AHK + ["fcs"], [PR[1]])
                              k1b = half * 64 + kg * G
                              for ri_, px in enumerate((pxr, pxi)):
                                  dst = Xh[:, ri_, :].rearrange("p (b a) -> p a b", a=128)[:, k1b:k1b + G, :]
                                  src = px[:, 0:G * S2].rearrange("p (g b) -> p g b", g=G)
                                  CP("vector" if ri_ == 0 else "scalar", dst, src, [PR[ri_]], [("Xh", half, kg)])
                      XHK = [("Xh", hf, kg) for hf in range(2) for kg in range(64 // G)]
                      for kt in range(NT5):
                          pi = kt % 4
                          MM(pm[pi][:, :], ccn[:, 0, :], Xh[:, 0, kt * 512:(kt + 1) * 512], True, False, ["ccn"] + XHK, [PM[pi]])
                          MM(pm[pi][:, :], ccn[:, 1, :], Xh[:, 1, kt * 512:(kt + 1) * 512], False, True, ["ccn"] + XHK, [PM[pi]])
                          r_ = rst[ri % 2]
                          rk = ("rst", ri % 2)
                          ACT(r_, pm[pi][:, :], AF.Copy, [PM[pi]], [rk], scale=nrm)
                          DMA(STQ, rzT[q][h * 128:(h + 1) * 128, kt * 512:(kt + 1) * 512], r_, [rk], [("rzT", q, h, kt)],
                              "rst%d" % (ri % 2))
                          ri += 1
                  rzb = [A16.alloc(4 * 512).rearrange("p (h t) -> p h t", h=4) for _ in range(2)]
                  st4 = [A16.alloc(4 * 512).rearrange("p (e t) -> p e t", e=4) for _ in range(2)]
                  for ti in range(NT5):
                      c0 = ti * 512
                      rb, rbk = rzb[ti % 2], ("rzb", ti % 2)
                      DMA("sync", rb, rzT[q][:, c0:c0 + 512].rearrange("(h m) t -> m h t", h=4),
                          [("rzT", q, h, ti) for h in range(4)], [rbk], "rzb%d" % (ti % 2))
                      st, sk = st4[ti % 2], ("st4", ti % 2)
                      for e_ in range(4):
                          pi = e_
                          for h in range(4):
                              MM(pm[pi][:, :], fwb[:, h, e_ * 128:(e_ + 1) * 128], rb[:, h, :], h == 0, h == 3,
                                 ["fwb", rbk], [PM[pi]])
                          CP("scalar" if e_ % 2 else "vector", st[:, e_, :], pm[pi][:, :], [PM[pi]], [sk])
                      DMA(STQ, mixT[q][1536:2048, c0:c0 + 512].rearrange("(e p) t -> p e t", p=128), st, [sk],
                          [("mixT", q, ti, 2)], "st4%d" % (ti % 2))

                  stop_here('P4')
                  P.barrier()
                  A16.reset(); A32.reset()
                  wo = A16.alloc(16 * 2048).rearrange("p (k n) -> p k n", k=16)
                  DMA("gpsimd", wo, w_out[l].rearrange("(k p) n -> p k n", p=128), [], ["wo"], "wo")
                  g1 = A32.alloc(D)
                  a2 = A32.alloc(D)
                  sh2 = A32.alloc(D)
                  tmpf = A32.alloc(D)
                  TFK = [("tmpf", i) for i in range(4)]
                  DMA("sync", g1, mod_rep(q, 2), MODK, ["g1"], "g1")
                  DMA("sync", a2, mod_rep(q, 4), MODK, ["a2"], "a2")
                  DMA("sync", sh2, mod_rep(q, 3), MODK, ["sh2"], "sh2")
                  DMA("sync", tmpf, norm2_g[l:l + 1, :].broadcast_to([128, D]), [], TFK, "g_rep")
                  STT(a2, a2, 1.0, tmpf, ALU.add, ALU.mult, ["a2"] + TFK, ["a2"])
                  wr = A32.alloc(16 * 36).rearrange("p (k n) -> p k n", k=16)
                  DMA("sync", wr[:, :, 0:4], rc_w[l].rearrange("(p k) g -> p k g", k=16), [], [("wr", 0)], "wr")
                  for g in range(4):
                      DMA("sync", wr[:, :, 4 + 8 * g:12 + 8 * g], rf_w[l, g].rearrange("(p k) e -> p k e", k=16), [],
                          [("wr", 1 + g)], "wr")
                  WRK = [("wr", i) for i in range(5)]
                  rb_ = A32.alloc(36)
                  DMA("sync", rb_[:, 0:4], rc_b[l:l + 1, :].broadcast_to([128, 4]), [], [("rb", 0)], "rb")
                  DMA("sync", rb_[:, 4:36], rf_b[l:l + 1].rearrange("o g e -> o (g e)").broadcast_to([128, 32]), [], [("rb", 1)], "rb")
                  RBK = [("rb", 0), ("rb", 1)]
                  mx = [A16.alloc(16 * 512).rearrange("p (k t) -> p k t", k=16) for _ in range(2)]
                  xt = [A32.alloc(D) for _ in range(1)]
                  x1t = [A32.alloc(D) for _ in range(2)]
                  h2f2 = [A32.alloc(D) for _ in range(2)]
                  h2b = [A16.alloc(D) for _ in range(2)]
                  h2T = A32.alloc(16 * 128).rearrange("p (k t) -> p k t", k=16)
                  sm2 = [A32.alloc(256) for _ in range(3)]
                  ssn = A32.alloc(8)
                  if q == 0:
                      MEMSET("vector", cnt_run[:], 0.0, ["cnt_run"])

                  def p5_main(it):
                      ti, sub = it // 4, it % 4
                      c0 = ti * 512
                      m_, mk_ = mx[ti % 2], ("mx", ti % 2)
                      if sub == 0:
                          DMA("sync", m_, mixT[q][:, c0:c0 + 512].rearrange("(k p) t -> p k t", p=128),
                              [("mixT", q, ti, i) for i in range(3)], [mk_], "mx%d" % (ti % 2))
                      b2 = it % 2
                      r0 = T0 + it * 128
                      xk = ("xt", 0)
                      DMA("sync", xt[0], cur_x[r0:r0 + 128, :], [("xcur", r0 // 128)], [xk], "xt0")
                      x1, x1k = x1t[b2], ("x1t", b2)
                      for cbk in range(4):
                          pi = cbk
                          for kc in range(16):
                              MM(pm[pi][:, :], m_[:, kc, sub * 128:(sub + 1) * 128], wo[:, kc, cbk * 512:(cbk + 1) * 512],
                                 kc == 0, kc == 15, [mk_, "wo"], [PM[pi]])
                          sl = slice(cbk * 512, (cbk + 1) * 512)
                          TT("vector", tmpf[:, sl], pm[pi][:, :], g1[:, sl], ALU.mult, [PM[pi], "g1"], [("tmpf", cbk)])
                          TT("gpsimd", x1[:, sl], tmpf[:, sl], xt[0][:, sl], ALU.add, [("tmpf", cbk), xk], [x1k])
                      DMA(STQ, x1_d[r0:r0 + 128, :], x1, [x1k], [("x1", r0 // 128)], "x1t%d" % b2)
                      ssk = U("ss")
                      ss = ssn[:, 0:4]
                      hb_, hbk = h2b[b2], ("h2b", b2)
                      h2f, h2fk = h2f2[b2], ("h2f", b2)
                      ACT(hb_, x1, AF.Square, [x1k], [hbk, ssk], accum_out=ss[:, 0:1])
                      ACT(ss[:, 1:2], ss[:, 0:1], AF.Sqrt, [ssk, "eps_t"], [ssk], scale=1.0 / D, bias=eps_t[:, 0:1])
                      P.op("vector", lambda e, ss=ss: e.reciprocal(out=ss[:, 2:3], in_=ss[:, 1:2]), [ssk], [ssk])
                      STT(tmpf, x1, ss[:, 2:3], a2, ALU.mult, ALU.mult, [x1k, ssk, "a2"], [("tmpf", i) for i in range(4)])
                      TT("gpsimd", h2f, tmpf, sh2, ALU.add, [("tmpf", i) for i in range(4)] + ["sh2"], [h2fk])
                      CP("scalar", hb_, h2f, [h2fk], [hbk])
                      DMA(STQ, h2_d[r0:r0 + 128, :], hb_, [hbk], [("h2", r0 // 128)], "h2b%d" % b2)

                  def p5_router(it):
                      git = (T0 // 128) + it
                      b2 = it % 2
                      h2f, h2fk = h2f2[b2], ("h2f", b2)
                      for kc in range(16):
                          pj = pr[(kc // 4) % 2]
                          TR(pj[:, (kc % 4) * 128:(kc % 4 + 1) * 128], h2f.rearrange("p (c k) -> p k c", k=16)[:, kc, :],
                             ident_f[:], [h2fk, "ident_f"], [PR[(kc // 4) % 2]])
                          if kc % 4 == 3:
                              g4 = kc // 4
                              CP("scalar" if g4 % 2 else "vector", h2T[:, g4 * 4:g4 * 4 + 4, :],
                                 pj[:, :].rearrange("p (k t) -> p k t", k=4), [PR[(kc // 4) % 2]], [("h2T", g4)])
                      for kc in range(16):
                          MM(pr[0][:, 0:36], h2T[:, kc, :], wr[:, kc, :], kc == 0, kc == 15,
                             [("h2T", kc // 4)] + WRK, [PR[0]])
                      rkeys[it] = U("rt")
                      route_tile(P, nc, sm2[it % 3], pr, PR, rb_, RBK, r_E, r_f, cnt_run, utinc, ones_b, git,
                                 TT, TS, STT, ACT, CP, RED, MM, U, part="A", rk=rkeys[it])

                  def p5_routerB(it):
                      git = (T0 // 128) + it
                      route_tile(P, nc, sm2[it % 3], pr, PR, rb_, RBK, r_E, r_f, cnt_run, utinc, ones_b, git,
                                 TT, TS, STT, ACT, CP, RED, MM, U, part="B", rk=rkeys[it])

                  rkeys = {}
                  NSUB = NT5 * 4
                  for it in range(NSUB):
                      p5_main(it)
                      if it > 0:
                          p5_router(it - 1)
                      if it > 1:
                          p5_routerB(it - 2)
                  p5_router(NSUB - 1)
                  if NSUB > 1:
                      p5_routerB(NSUB - 2)
                  p5_routerB(NSUB - 1)

              stop_here('P5')
              P.barrier()
              A16.reset(); A32.reset()
              fin = A32.alloc(8 * 32).rearrange("p (a e) -> p a e", a=8)
              RALL = ["cnt_run"]
              TS("vector", fin[:, 0, :], cnt_run[:], 127.0, None, ALU.add, None, ["cnt_run"], ["fin"])
              fin_i = A32.alloc(32).bitcast(I32)
              CP("vector", fin_i, fin[:, 0, :], ["fin"], ["fin_i"])
              TS("vector", fin_i, fin_i, 7, 7, ALU.arith_shift_right, ALU.logical_shift_left, ["fin_i"], ["fin_i"])
              CP("vector", fin[:, 2, :], fin_i, ["fin_i"], ["fin"])
              MEMSET("vector", fin[:, 3, :], 1.0, ["fin"])
              P.op("vector", lambda e: e.tensor_tensor_scan(out=fin[:, 4, :], data0=fin[:, 3, :], data1=fin[:, 2, :],
                                                            initial=0.0, op0=ALU.mult, op1=ALU.add), ["fin"], ["fin"])
              TT("vector", fin[:, 5, :], fin[:, 4, :], fin[:, 2, :], ALU.subtract, ["fin"], ["fin"])
              big = A32.alloc(NT * 32).rearrange("p (t e) -> p t e", e=32)
              slf = A32.alloc(NT * 2).rearrange("p (t k) -> p t k", k=2)
              for k in range(2):
                  TT("vector", big, r_E[:, :, k * 32:(k + 1) * 32], fin[:, 5:6, :].to_broadcast([128, NT, 32]), ALU.mult,
                     ["fin", "r_E"], ["big"])
                  RED(slf[:, :, k], big, ALU.add, ["big"], ["slf"])
                  TT("vector", slf[:, :, k], slf[:, :, k], r_f[:, :, k], ALU.add, ["slf", "r_f"], ["slf"])
              CP("vector", r_slot[:], slf, ["slf"], ["r_slot"])
              bst = A32.alloc(NB)
              DMA("sync", bst, k_bstart, [], ["bst"], "bst")
              ebf = A32.alloc(NB)
              CH = 32
              bigb = A32.alloc(CH * 32).rearrange("p (b e) -> p b e", e=32)
              for b0 in range(0, NB, CH):
                  nb_ = min(CH, NB - b0)
                  TT("vector", bigb[:, 0:nb_, :], fin[:, 4:5, :].to_broadcast([128, nb_, 32]),
                     bst[:, b0:b0 + nb_].unsqueeze(2).to_broadcast([128, nb_, 32]), ALU.is_le, ["fin", "bst"], ["bigb"])
                  RED(ebf[:, b0:b0 + nb_], bigb[:, 0:nb_, :], ALU.add, ["bigb"], ["ebf"])
              TS("vector", ebf, ebf, 31.0, None, ALU.min, None, ["ebf"], ["ebf"])
              sam = A32.alloc(NB)
              MEMSET("vector", sam[:, 0:1], 0.0, ["sam"])
              TT("vector", sam[:, 1:NB], ebf[:, 1:NB], ebf[:, 0:NB - 1], ALU.is_equal, ["ebf"], ["sam"])
              wif = A32.alloc(NB)
              TS("vector", wif, ebf, 128.0, iotap[:, 0:1], ALU.mult, ALU.add, ["ebf", "iotap"], ["wif"])
              STT(wif, sam, 1.0e6, wif, ALU.mult, ALU.add, ["sam", "wif"], ["wif"])
              if l > 0:
                  TS("vector", wif, wif, float(l * NE * 128), None, ALU.add, None, ["wif"], ["wif"])
              CP("vector", widx[:], wif, ["wif"], ["widx"])
              hl = [A16.alloc(D) for _ in range(3)]
              for it in range(NT):
                  b3 = it % 3
                  hk_ = ("hl", b3)
                  DMA("sync", hl[b3], h2_d[it * 128:(it + 1) * 128, :], [("h2", it)], [hk_], "hl%d" % b3)
                  for k in range(2):
                      off = r_slot[:, it, k:k + 1]
                      P.dma("gpsimd", (lambda e, off=off, src=hl[b3]: e.indirect_dma_start(
                          out=xs_d, out_offset=bass.IndirectOffsetOnAxis(ap=off, axis=0), in_=src, in_offset=None,
                          bounds_check=BR(e, L - 1), oob_is_err=False)), [hk_, "r_slot"], [("xs", it, k)], "xs_sc")
              XSK = [("xs", it, k) for it in range(NT) for k in range(2)]

              P.barrier()
              A16.reset(); A32.reset()
              W1 = A16.alloc(16 * 512)
              W3 = A16.alloc(16 * 512)
              W2 = A16.alloc(4 * 2048)
              xb = [A16.alloc(D) for _ in range(3)]
              xbT = [A16.alloc(16 * 128).rearrange("p (k t) -> p k t", k=16) for _ in range(2)]

              def xload(b):
                  DMA("sync", xb[b % 3], xs_d[b * 128:(b + 1) * 128, :], XSK, [("xb", b % 3)], "xb%d" % (b % 3))

              sgf = [A32.alloc(512) for _ in range(2)]
              hid = [A16.alloc(512) for _ in range(2)]
              hidT = [A16.alloc(4 * 128).rearrange("p (k t) -> p k t", k=4) for _ in range(2)]
              yb = [A32.alloc(D) for _ in range(2)]
              w1v = e_w1.rearrange("l e (p k) n -> (l e p) (k n)", k=16)
              w3v = e_w3.rearrange("l e (p k) n -> (l e p) (k n)", k=16)
              w2v = e_w2.rearrange("l e (p k) n -> (l e p) (k n)", k=4)
              bnd_l = (l + 1) * NE * 128 - 1

              def wgather(b, Wt, wv, wk):
                  off = widx[:, b:b + 1]
                  P.dma("gpsimd", (lambda e, off=off, Wt=Wt, wv=wv, bnd=bnd_l: e.indirect_dma_start(
                      out=Wt, out_offset=None, in_=wv, in_offset=bass.IndirectOffsetOnAxis(ap=off, axis=0),
                      bounds_check=BR(e, bnd), oob_is_err=False)), ["widx"], [wk], wk)

              def stageA(b):
                  b2 = b % 2
                  wgather(b, W1, w1v, "W1")
                  wgather(b, W3, w3v, "W3")
                  xk = ("xb", b % 3)
                  if b + 1 < NB:
                      xload(b + 1)
                  for kc in range(16):
                      TR(pt[:, kc * 128:(kc + 1) * 128], xb[b % 3].rearrange("p (c k) -> p k c", k=16)[:, kc, :], ident_b[:],
                         [xk, "ident_b"], [PT])
                  xT, xTk = xbT[b2], ("xbT", b2)
                  CP("scalar", xT[:, 0:8, :], pt[:, 0:1024].rearrange("p (k t) -> p k t", k=8), [PT], [xTk])
                  CP("vector", xT[:, 8:16, :], pt[:, 1024:2048].rearrange("p (k t) -> p k t", k=8), [PT], [xTk])
                  p1, p3 = 2 * b2, 2 * b2 + 1
                  for kc in range(16):
                      MM(pm[p1][:, :], xT[:, kc, :], W1[:, kc * 512:(kc + 1) * 512], kc == 0, kc == 15, [xTk, "W1"], [PM[p1]])
                  for kc in range(16):
                      MM(pm[p3][:, :], xT[:, kc, :], W3[:, kc * 512:(kc + 1) * 512], kc == 0, kc == 15, [xTk, "W3"], [PM[p3]])
                  ACT(sgf[b2], pm[p1][:, :], AF.Silu, [PM[p1]], [("sgf", b2)])
                  TT("vector", hid[b2], pm[p3][:, :], sgf[b2], ALU.mult, [PM[p3], ("sgf", b2)], [("hid", b2)])

              def stageB(b):
                  b2 = b % 2
                  wgather(b, W2, w2v, "W2")
                  for fc in range(4):
                      TR(pt[:, fc * 128:(fc + 1) * 128], hid[b2].rearrange("p (c k) -> p k c", k=4)[:, fc, :], ident_b[:],
                         [("hid", b2), "ident_b"], [PT])
                  CP("vector", hidT[b2], pt[:, 0:512].rearrange("p (k t) -> p k t", k=4), [PT], [("hidT", b2)])
                  y_, yk = yb[b2], ("yb", b2)
                  for cbk in range(4):
                      pi = cbk % 2
                      for fc in range(4):
                          MM(pr[pi][:, :], hidT[b2][:, fc, :], W2[:, fc * 2048 + cbk * 512:fc * 2048 + (cbk + 1) * 512],
                             fc == 0, fc == 3, [("hidT", b2), "W2"], [PR[pi]])
                      if cbk % 2:
                          CP("scalar", y_[:, cbk * 512:(cbk + 1) * 512], pr[pi][:, :], [PR[pi]], [yk])
                      else:
                          CP("vector", y_[:, cbk * 512:(cbk + 1) * 512], pr[pi][:, :], [PR[pi]], [yk])
                  DMA(STQ, ys_d[b * 128:(b + 1) * 128, :], y_, [yk], [("ys", b)], "yb%d" % b2)

              xload(0)
              stageA(0)
              for b in range(NB):
                  if b + 1 < NB:
                      stageA(b + 1)
                  stageB(b)
              YSK = [("ys", b) for b in range(NB)]

              stop_here('P7')
              P.barrier()
              A16.reset(); A32.reset()
              g2 = [A32.alloc(D) for _ in range(2)]
              for q in range(2):
                  DMA("sync", g2[q], mod_rep(q, 5), MODK, [("g2", q)], "g2%d" % q)
              if last:
                  fg = A32.alloc(D)
                  DMA("sync", fg, final_g.rearrange("(o n) -> o n", o=1).broadcast_to([128, D]), [], ["fg"], "fg")
              ya = [A32.alloc(D) for _ in range(2)]
              ybb = [A32.alloc(D) for _ in range(2)]
              x1t = [A32.alloc(D) for _ in range(2)]
              junk = A16.alloc(D)
              ss = A32.alloc(8)
              dst_x = y_out if last else x2_d
              for it in range(NT):
                  b2 = it % 2
                  q = 0 if it * 128 < OFFS[1] else 1
                  for k, yt in enumerate((ya, ybb)):
                      off = r_slot[:, it, k:k + 1]
                      P.dma("gpsimd", (lambda e, off=off, dst=yt[b2]: e.indirect_dma_start(
                          out=dst, out_offset=None, in_=ys_d, in_offset=bass.IndirectOffsetOnAxis(ap=off, axis=0),
                          bounds_check=BR(e, L - 1), oob_is_err=False)), YSK + ["r_slot"], [("yg", k, b2)], "yg%d%d" % (k, b2))
                  xk = ("x1t", b2)
                  DMA("sync", x1t[b2], x1_d[it * 128:(it + 1) * 128, :], [("x1", it)], [xk], "x1l%d" % b2)
                  A_, B_ = ya[b2], ybb[b2]
                  TS("vector", A_, A_, r_f[:, it, 2:3], None, ALU.mult, None, [("yg", 0, b2), "r_f"], [("yg", 0, b2)])
                  STT(A_, B_, r_f[:, it, 3:4], A_, ALU.mult, ALU.add, [("yg", 1, b2), ("yg", 0, b2), "r_f"], [("yg", 0, b2)])
                  TT("gpsimd", A_, A_, g2[q], ALU.mult, [("yg", 0, b2), ("g2", q)], [("yg", 0, b2)])
                  TT("vector", B_, A_, x1t[b2], ALU.add, [("yg", 0, b2), xk], [("yg", 1, b2)])
                  if not last:
                      DMA(STQ, dst_x[it * 128:(it + 1) * 128, :], B_, [("yg", 1, b2)], [("xcur", it)], "xo%d" % b2)
                  else:
                      ssk = U("ss")
                      ACT(junk, B_, AF.Square, [("yg", 1, b2)], ["junk", ssk], accum_out=ss[:, 0:1])
                      ACT(ss[:, 1:2], ss[:, 0:1], AF.Sqrt, [ssk, "eps_t"], [ssk], scale=1.0 / D, bias=eps_t[:, 0:1])
                      P.op("vector", lambda e, ss=ss: e.reciprocal(out=ss[:, 2:3], in_=ss[:, 1:2]), [ssk], [ssk])
                      STT(A_, B_, ss[:, 2:3], fg, ALU.mult, ALU.mult, [("yg", 1, b2), ssk, "fg"], [("yg", 0, b2)])
                      DMA(STQ, dst_x[it * 128:(it + 1) * 128, :], A_, [("yg", 0, b2)], [("yout", it)], "xo%d" % b2)
              cur_x = x2_d

          P.emit()
        except _Stop:
            pass
    return nc


def route_tile(P, nc, sm, pr, PR, rb_, RBK, r_E, r_f, cnt_run, utinc, ones_b, git,
               TT, TS, STT, ACT, CP, RED, MM, U, part="AB", rk=None):
    if "A" not in part:
        Ef = sm[:, 96:160]
        Mb = sm[:, 160:192]
        Mbb = sm[:, 192:224].bitcast(BF16)[:, 0:32]
        return route_tile_b(P, sm, pr, PR, r_f, cnt_run, utinc, ones_b, git, TT, RED, MM, rk, Ef, Mb, Mbb)
    rk = rk if rk is not None else U("rt")
    Lg = sm[:, 8:44]
    TT("vector", Lg, pr[0][:, 0:36], rb_, ALU.add, [PR[0]] + RBK, [rk])
    m = sm[:, 44:45]
    RED(m, Lg[:, 0:4], ALU.max, [rk], [rk])
    oh = sm[:, 48:52]
    TS("vector", oh, Lg[:, 0:4], m, None, ALU.is_equal, None, [rk], [rk])
    negm = sm[:, 45:46]
    TS("vector", negm, m, -1.0, None, ALU.mult, None, [rk], [rk])
    ex = sm[:, 52:56]
    se = sm[:, 46:47]
    ACT(ex, Lg[:, 0:4], AF.Exp, [rk], [rk], bias=negm, scale=1.0, accum_out=se)
    pg = sm[:, 47:48]
    P.op("vector", lambda e: e.reciprocal(out=pg, in_=se), [rk], [rk])
    lf = sm[:, 56:64]
    TS("vector", lf, Lg[:, 4:12], oh[:, 0:1], None, ALU.mult, None, [rk], [rk])
    for g in range(1, 4):
        STT(lf, Lg[:, 4 + 8 * g:12 + 8 * g], oh[:, g:g + 1], lf, ALU.mult, ALU.add, [rk], [rk])
    top = sm[:, 64:72]
    P.op("vector", lambda e: e.max(out=top, in_=lf), [rk], [rk])
    s1 = sm[:, 72:80]
    s2 = sm[:, 80:88]
    TS("vector", s1, lf, top[:, 0:1], None, ALU.is_equal, None, [rk], [rk])
    TS("vector", s2, lf, top[:, 1:2], None, ALU.is_equal, None, [rk], [rk])
    dv = sm[:, 88:89]
    TT("vector", dv, top[:, 0:1], top[:, 1:2], ALU.subtract, [rk], [rk])
    sg = sm[:, 89:90]
    ACT(sg, dv, AF.Sigmoid, [rk], [rk])
    TT("vector", r_f[:, git, 2:3], pg, sg, ALU.mult, [rk], ["r_f"])
    TT("vector", r_f[:, git, 3:4], pg, r_f[:, git, 2:3], ALU.subtract, [rk, "r_f"], ["r_f"])
    Ef = sm[:, 96:160]
    for g in range(4):
        TS("vector", Ef[:, 8 * g:8 * g + 8], s1, oh[:, g:g + 1], None, ALU.mult, None, [rk], [rk])
        TS("vector", Ef[:, 32 + 8 * g:40 + 8 * g], s2, oh[:, g:g + 1], None, ALU.mult, None, [rk], [rk])
    CP("vector", r_E[:, git, :], Ef, [rk], ["r_E"])
    Mb = sm[:, 160:192]
    TT("vector", Mb, Ef[:, 0:32], Ef[:, 32:64], ALU.add, [rk], [rk])
    Mbb = sm[:, 192:224].bitcast(BF16)[:, 0:32]
    CP("vector", Mbb, Mb, [rk], [rk])
    if "B" in part:
        route_tile_b(P, sm, pr, PR, r_f, cnt_run, utinc, ones_b, git, TT, RED, MM, rk, Ef, Mb, Mbb)


def route_tile_b(P, sm, pr, PR, r_f, cnt_run, utinc, ones_b, git, TT, RED, MM, rk, Ef, Mb, Mbb):
    MM(pr[1][:, 0:32], utinc[:], Mbb, True, True, ["utinc", rk], [PR[1]])
    MM(pr[1][:, 32:64], ones_b[:], Mbb, True, True, ["ones_b", rk], [PR[1]])
    rank = sm[:, 224:256]
    TT("vector", rank, pr[1][:, 0:32], Mb, ALU.subtract, [PR[1], rk], [rk])
    TT("vector", rank, rank, cnt_run[:], ALU.add, [rk, "cnt_run"], [rk])
    TT("vector", cnt_run[:], cnt_run[:], pr[1][:, 32:64], ALU.add, [PR[1], "cnt_run"], ["cnt_run"])
    tmp = sm[:, 160:192]
    for k in range(2):
        TT("vector", tmp, Ef[:, 32 * k:32 * k + 32], rank, ALU.mult, [rk], [rk])
        RED(r_f[:, git, k:k + 1], tmp, ALU.add, [rk], ["r_f"])


_WNAMES = ["w_ada", "b_ada", "norm1_g", "w_in", "pool_w", "pool_scale", "conv_dw_w", "conv_dw_b",
           "conv_ln_g", "conv_ln_b", "conv_pw_w", "conv_pw_b", "fourier_w", "w_out", "norm2_g",
           "router_coarse_w", "router_coarse_b", "router_fine_w", "router_fine_b",
           "expert_w1", "expert_w3", "expert_w2", "final_g"]


def kernel(**inputs):
    xs_ = np.asarray(inputs["x_sample"], np.float32)
    xp_ = np.asarray(inputs["x_prompt"], np.float32)
    cs_ = np.asarray(inputs["c_sample"], np.float32)
    cp_ = np.asarray(inputs["c_prompt"], np.float32)
    S0, S1 = xs_.shape[1], xp_.shape[1]
    depth = inputs["w_ada"].shape[0]
    nc = build((S0, S1), depth)
    N = S0 + S1
    NB = -(-(2 * N + NE * 127) // 128)
    consts = make_consts((S0, S1), NB)
    wts = {k: np.ascontiguousarray(np.asarray(inputs[k], np.float32)) for k in _WNAMES}
    in_maps = []
    for core in range(8):
        b = core % 4
        m = {"x": np.ascontiguousarray(np.concatenate([xs_[b], xp_[b]], axis=0)),
             "c": np.ascontiguousarray(np.stack([cs_[b], cp_[b]], axis=0))}
        m.update(wts)
        m.update(consts)
        in_maps.append(m)
    res = run_bass_kernel_spmd(nc, in_maps, core_ids=list(range(8)))
    y_s = np.stack([res.results[b]["y"][:S0] for b in range(4)], axis=0)
    y_p = np.stack([res.results[b]["y"][S0:] for b in range(4)], axis=0)
    return (y_p.astype(np.float32), y_s.astype(np.float32))
```

```python
import math
import os
from contextlib import ExitStack

import numpy as np
import ml_dtypes
import concourse.bass as bass
import concourse.mybir as mybir
from concourse.bass_utils import run_bass_kernel_spmd

F32 = mybir.dt.float32
BF16 = mybir.dt.bfloat16
I32 = mybir.dt.int32
AF = mybir.ActivationFunctionType
ALU = mybir.AluOpType
AX = mybir.AxisListType

D = 2048
NG, EPG, NE = 4, 8, 32
DE = 512
CONV_K = 31
EPS = 1e-6
SAME_ENGINE_SYNC = True
CONV_PE = os.environ.get('KCONV', 'pe') == 'pe'
NOSYNC = set(os.environ.get('KNOSYNC', 'tensor').split(','))


class _Op:
    __slots__ = ("q", "fn", "deps", "sig", "sigval", "sem", "is_dma", "idx", "bar")


class Prog:
    def __init__(self, nc):
        self.nc = nc
        self.ops = []
        self.last_w = {}
        self.readers = {}
        self.dma_cnt = {}
        self.last_eng = {}
        self.bar = None
        self.bar_done = set()

    def barrier(self):
        deps = [(o, None) for o in self.last_eng.values()]
        dmas = dict(self.dma_cnt)
        self.bar = (deps, dmas)
        self.bar_done = set()

    def _add(self, q, fn, reads, writes, is_dma, semkey):
        o = _Op()
        o.q = q
        o.fn = fn
        o.is_dma = is_dma
        o.sig = False
        o.sigval = 0
        o.idx = len(self.ops)
        deps = {}
        for r in reads:
            w = self.last_w.get(r)
            if w is not None:
                deps[w.idx] = w
        for w_ in writes:
            w = self.last_w.get(w_)
            if w is not None:
                deps[w.idx] = w
            for rd in self.readers.get(w_, ()):
                deps[rd.idx] = rd
        o.deps = []
        for d in deps.values():
            if d.is_dma:
                o.deps.append((d, self.dma_cnt[d.sem]))
            else:
                o.deps.append((d, None))
        o.bar = None
        if self.bar is not None and q not in self.bar_done:
            self.bar_done.add(q)
            bdeps, bdmas = self.bar
            o.deps.extend(bdeps)
            o.bar = bdmas
        if is_dma:
            o.sem = ("dma", semkey)
            self.dma_cnt[o.sem] = self.dma_cnt.get(o.sem, 0) + 1
            o.sigval = self.dma_cnt[o.sem]
        else:
            o.sem = ("eng", q)
            self.last_eng[q] = o
        for w_ in writes:
            self.last_w[w_] = o
            self.readers[w_] = []
        for r in reads:
            if r not in writes:
                self.readers.setdefault(r, []).append(o)
        self.ops.append(o)
        return o

    def op(self, eng, fn, reads=(), writes=()):
        return self._add(eng, fn, tuple(reads), tuple(writes), False, None)

    def dma(self, q, fn, reads=(), writes=(), semkey=None):
        assert semkey is not None
        return self._add(q, fn, tuple(reads), tuple(writes), True, semkey)

    def emit(self):
        nc = self.nc
        ops = self.ops
        for o in ops:
            for d, _ in o.deps:
                if d.is_dma:
                    continue
                if d.q != o.q or (SAME_ENGINE_SYNC and d.q not in NOSYNC):
                    d.sig = True
        cnt = {}
        for o in ops:
            if not o.is_dma and o.sig:
                cnt[o.q] = cnt.get(o.q, 0) + 1
                o.sigval = cnt[o.q]
        semkeys = []
        seen = set()
        for o in ops:
            if (o.is_dma or o.sig) and o.sem not in seen:
                seen.add(o.sem)
                semkeys.append(o.sem)
        self.n_sems = len(semkeys)
        with ExitStack() as es:
            sems = {}
            for i, k in enumerate(semkeys):
                sems[k] = es.enter_context(nc.semaphore("s%d" % i))
            block = es.enter_context(nc.Block())
            queues = {}
            for o in ops:
                queues.setdefault(o.q, []).append(o)
            totals = dict(self.dma_cnt)

            def run_queue(qname, eng):
                waited = {}
                for o in queues.get(qname, ()):
                    need = {}
                    for d, n in o.deps:
                        if d.is_dma:
                            v = 16 * n
                        else:
                            if d.q == o.q and (d.q in NOSYNC or not SAME_ENGINE_SYNC):
                                continue
                            v = d.sigval
                        if v > need.get(d.sem, 0):
                            need[d.sem] = v
                    if o.bar is not None:
                        for k, n in o.bar.items():
                            if 16 * n > need.get(k, 0):
                                need[k] = 16 * n
                    for k, v in need.items():
                        if waited.get(k, 0) < v:
                            eng.wait_ge(sems[k], v)
                            waited[k] = v
                    ins = o.fn(eng)
                    if o.is_dma:
                        ins.then_inc(sems[o.sem], 16)
                    elif o.sig:
                        ins.then_inc(sems[o.sem], 1)
                if qname == "sync":
                    for k, n in totals.items():
                        if waited.get(k, 0) < 16 * n:
                            eng.wait_ge(sems[k], 16 * n)

            @block.sync
            def _(e):
                run_queue("sync", e)

            @block.scalar
            def _(e):
                run_queue("scalar", e)

            @block.gpsimd
            def _(e):
                run_queue("gpsimd", e)

            @block.vector
            def _(e):
                run_queue("vector", e)

            @block.tensor
            def _(e):
                run_queue("tensor", e)


def _bf(a):
    return np.ascontiguousarray(a.astype(ml_dtypes.bfloat16))


def make_consts(SEQS, NB):
    c = {}
    t = np.arange(128)
    ang = 2 * np.pi * np.outer(t, t) / 128.0
    cs = np.zeros((128, 2, 192), np.float64)
    for h in range(2):
        sl = slice(h * 64, h * 64 + 64)
        cs[:, h, 0:64] = np.cos(ang[:, sl])
        cs[:, h, 64:128] = np.sin(ang[:, sl])
        cs[:, h, 128:192] = -np.sin(ang[:, sl])
    c["cs128"] = _bf(cs)
    ccn = np.zeros((128, 2, 128), np.float64)
    ccn[:, 0] = np.cos(ang)
    ccn[:, 1] = -np.sin(ang)
    c["ccn"] = _bf(ccn)
    for q, S in enumerate(SEQS):
        S2 = S // 128
        a = 2 * np.pi * (np.outer(np.arange(S2), np.arange(S)) % S) / S
        f = np.zeros((S2, 2, S), np.float64)
        f[:, 0] = np.cos(a)
        f[:, 1] = np.sin(a)
        c["fcs%d" % q] = _bf(f)
    pc = np.zeros((128, 4, 16), np.float32)
    for g, w in enumerate((2, 4, 8, 16)):
        for j in range(8):
            cnt = min(j + w // 2, 10 ** 9) - max(j - w // 2, 0)
            pc[:, g, j] = 1.0 / cnt
        for j in range(8):
            tt = -8 + j
            hi = min(tt + w // 2, 0)
            lo = tt - w // 2
            pc[:, g, 8 + j] = 1.0 / (hi - lo)
    c["poolc"] = pc
    ut = (np.arange(128)[:, None] <= np.arange(128)[None, :]).astype(np.float32)
    c["utinc"] = _bf(ut)
    c["iotap"] = np.arange(128, dtype=np.float32).reshape(128, 1)
    c["bstart"] = np.tile((128.0 * np.arange(NB, dtype=np.float32))[None, :], (128, 1))
    c["iota32"] = np.tile(np.arange(32, dtype=np.float32)[None, :], (128, 1))
    return c


class Arena:
    def __init__(self, t, size):
        self.t = t
        self.size = size
        self.off = 0

    def reset(self):
        self.off = 0

    def alloc(self, n, align=16):
        self.off = (self.off + align - 1) // align * align
        o = self.off
        self.off += n
        assert self.off <= self.size, ("arena overflow", self.off, self.size)
        return self.t[:, o:o + n]


def build(SEQS=(8192, 2048), DEPTH=2, dbg=False, NL=None):
    NL = DEPTH if NL is None else NL
    N = sum(SEQS)
    NT = N // 128
    NB = -(-(2 * N + NE * 127) // 128)
    L = NB * 128
    OFFS = [0]
    for S in SEQS:
        OFFS.append(OFFS[-1] + S)
    nc = bass.Bass("TRN2", target_bir_lowering=False)

    def din(name, shape, dt=F32):
        return nc.dram_tensor(name, list(shape), dt, kind="ExternalInput").ap()

    def dscr(name, shape, dt):
        kind = "ExternalOutput" if dbg else "Internal"
        return nc.dram_tensor(name, list(shape), dt, kind=kind).ap()

    x_in = din("x", [N, D])
    c_in = din("c", [2, D])
    w_ada = din("w_ada", [DEPTH, D, 6 * D])
    b_ada = din("b_ada", [DEPTH, 6 * D])
    norm1_g = din("norm1_g", [DEPTH, D])
    w_in = din("w_in", [DEPTH, D, 3072])
    pool_w = din("pool_w", [DEPTH, 4, 128, 128])
    pool_scale = din("pool_scale", [DEPTH, 512])
    conv_dw_w = din("conv_dw_w", [DEPTH, CONV_K, 1024])
    conv_dw_b = din("conv_dw_b", [DEPTH, 1024])
    conv_ln_g = din("conv_ln_g", [DEPTH, 1024])
    conv_ln_b = din("conv_ln_b", [DEPTH, 1024])
    conv_pw_w = din("conv_pw_w", [DEPTH, 1024, 1024])
    conv_pw_b = din("conv_pw_b", [DEPTH, 1024])
    fourier_w = din("fourier_w", [DEPTH, 512, 512])
    w_out = din("w_out", [DEPTH, D, D])
    norm2_g = din("norm2_g", [DEPTH, D])
    rc_w = din("router_coarse_w", [DEPTH, D, NG])
    rc_b = din("router_coarse_b", [DEPTH, NG])
    rf_w = din("router_fine_w", [DEPTH, NG, D, EPG])
    rf_b = din("router_fine_b", [DEPTH, NG, EPG])
    e_w1 = din("expert_w1", [DEPTH, NE, D, DE])
    e_w3 = din("expert_w3", [DEPTH, NE, D, DE])
    e_w2 = din("expert_w2", [DEPTH, NE, DE, D])
    final_g = din("final_g", [D])
    k_cs128 = din("cs128", [128, 2, 192], BF16)
    k_ccn = din("ccn", [128, 2, 128], BF16)
    k_fcs = [din("fcs%d" % q, [S // 128, 2, S], BF16) for q, S in enumerate(SEQS)]
    k_poolc = din("poolc", [128, 4, 16])
    k_utinc = din("utinc", [128, 128], BF16)
    k_iotap = din("iotap", [128, 1])
    k_bstart = din("bstart", [128, NB])
    k_iota32 = din("iota32", [128, 32])

    y_out = nc.dram_tensor("y", [N, D], F32, kind="ExternalOutput").ap()

    x1_d = dscr("x1", [N, D], F32)
    x2_d = dscr("x2", [N, D], F32)
    h2_d = dscr("h2", [N, D], BF16)
    xs_d = dscr("xs", [L, D], BF16)
    ys_d = dscr("ys", [L, D], F32)
    mod_d = dscr("modrow", [2, 6 * D], F32)
    zaT = [dscr("zaT%d" % q, [512, S], BF16) for q, S in enumerate(SEQS)]
    vT = [dscr("vT%d" % q, [1024, S], BF16) for q, S in enumerate(SEQS)]
    zcT = [dscr("zcT%d" % q, [512, S], BF16) for q, S in enumerate(SEQS)]
    rzT = [dscr("rzT%d" % q, [512, S], BF16) for q, S in enumerate(SEQS)]
    mixT = [dscr("mixT%d" % q, [2048, S], BF16) for q, S in enumerate(SEQS)]

    P = Prog(nc)
    es = ExitStack()
    with es:
        AR_SZ = 96 * 1024
        art = es.enter_context(nc.sbuf_tensor("arena", [128, AR_SZ], BF16))
        AR = Arena(art, AR_SZ)

        class _A16:
            def alloc(self, n):
                return AR.alloc(n)

            def reset(self):
                AR.reset()

        class _A32:
            def alloc(self, n):
                return AR.alloc(2 * n).bitcast(F32)

            def reset(self):
                pass

        A16 = _A16()
        A32 = _A32()
        ident_f = es.enter_context(nc.sbuf_tensor("ident_f", [128, 128], F32))
        ident_b = es.enter_context(nc.sbuf_tensor("ident_b", [128, 128], BF16))
        ones_b = es.enter_context(nc.sbuf_tensor("ones_b", [128, 128], BF16))
        utinc = es.enter_context(nc.sbuf_tensor("utinc_s", [128, 128], BF16))
        iotap = es.enter_context(nc.sbuf_tensor("iotap_s", [128, 1], F32))
        iota32 = es.enter_context(nc.sbuf_tensor("iota32_s", [128, 32], F32))
        eps_t = es.enter_context(nc.sbuf_tensor("eps_t", [128, 1], F32))
        r_E = es.enter_context(nc.sbuf_tensor("r_E", [128, NT, 64], BF16))
        r_f = es.enter_context(nc.sbuf_tensor("r_f", [128, NT, 4], F32))
        r_slot = es.enter_context(nc.sbuf_tensor("r_slot", [128, NT, 2], I32))
        cnt_run = es.enter_context(nc.sbuf_tensor("cnt_run", [128, 32], F32))
        widx = es.enter_context(nc.sbuf_tensor("widx", [128, NB], I32))
        pm = [es.enter_context(nc.psum_tensor("pm%d" % i, [128, 512], F32)) for i in range(4)]
        pt = es.enter_context(nc.psum_tensor("pt", [128, 2048], BF16))
        pr = [es.enter_context(nc.psum_tensor("pr%d" % i, [128, 512], F32)) for i in range(2)]
        PM = [("pm", i) for i in range(4)]
        PT = "pt"
        PR = [("pr", i) for i in range(2)]

        STQ = os.environ.get('KSTQ', 'sync')
        uid = [0]
        _bregs = {}

        def BR(e, val):
            if val not in _bregs:
                _bregs[val] = e.to_reg(val)
            return _bregs[val]

        def U(name):
            uid[0] += 1
            return (name, uid[0])

        def t16(n, shape=None):
            v = A16.alloc(n)
            return v

        def DMA(q, out, in_, reads, writes, semkey, **kw):
            P.dma(q, lambda e: e.dma_start(out=out, in_=in_, **kw), reads, writes, semkey)

        def MM(out, lhsT, rhs, start, stop, reads, writes):
            P.op("tensor", lambda e: e.matmul(out, lhsT=lhsT, rhs=rhs, start=start, stop=stop), reads, writes)

        def TR(out, in_, ident, reads, writes):
            P.op("tensor", lambda e: e.transpose(out=out, in_=in_, identity=ident), reads, writes)

        def ACT(out, in_, func, reads, writes, bias=None, scale=None, accum_out=None, eng="scalar"):
            kw = {}
            if bias is not None:
                kw["bias"] = bias
            if scale is not None:
                kw["scale"] = scale
            if accum_out is not None:
                kw["accum_out"] = accum_out
            P.op("scalar", lambda e: e.activation(out=out, in_=in_, func=func, **kw), reads, writes)

        def TT(eng, out, in0, in1, op, reads, writes):
            P.op(eng, lambda e: e.tensor_tensor(out=out, in0=in0, in1=in1, op=op), reads, writes)

        def TS(eng, out, in0, s1, s2, op0, op1, reads, writes, accum_out=None):
            if op1 is None:
                P.op(eng, lambda e: e.tensor_scalar(out=out, in0=in0, scalar1=s1, scalar2=None, op0=op0), reads, writes)
            elif accum_out is not None:
                P.op(eng, lambda e: e.tensor_scalar(out=out, in0=in0, scalar1=s1, scalar2=s2, op0=op0, op1=op1, accum_out=accum_out), reads, writes)
            else:
                P.op(eng, lambda e: e.tensor_scalar(out=out, in0=in0, scalar1=s1, scalar2=s2, op0=op0, op1=op1), reads, writes)

        def STT(out, in0, scalar, in1, op0, op1, reads, writes):
            P.op("vector", lambda e: e.scalar_tensor_tensor(out=out, in0=in0, scalar=scalar, in1=in1, op0=op0, op1=op1), reads, writes)

        def CP(eng, out, in_, reads, writes):
            if eng == "scalar":
                P.op("scalar", lambda e: e.copy(out=out, in_=in_), reads, writes)
            else:
                P.op(eng, lambda e: e.tensor_copy(out=out, in_=in_), reads, writes)

        def MEMSET(eng, ap, val, writes):
            P.op(eng, lambda e: e.memset(ap, val), (), writes)

        def RED(out, in_, op, reads, writes, axis=AX.X):
            P.op("vector", lambda e: e.tensor_reduce(out=out, in_=in_, axis=axis, op=op), reads, writes)

        def bcast_row(dram_row_ap, n):
            return dram_row_ap.partition_broadcast(128)

        MEMSET("gpsimd", ident_f[:], 0.0, ["ident_f"])
        P.op("gpsimd", lambda e: e.affine_select(out=ident_f[:], in_=ident_f[:], pattern=[[-1, 128]],
                                                 compare_op=ALU.not_equal, fill=1.0, base=0, channel_multiplier=1),
             ["ident_f"], ["ident_f"])
        CP("vector", ident_b[:], ident_f[:], ["ident_f"], ["ident_b"])
        MEMSET("gpsimd", ones_b[:], 1.0, ["ones_b"])
        MEMSET("gpsimd", eps_t[:], EPS, ["eps_t"])
        DMA("sync", utinc[:], k_utinc, [], ["utinc"], "c_utinc")
        DMA("sync", iotap[:], k_iotap, [], ["iotap"], "c_iotap")
        DMA("sync", iota32[:], k_iota32, [], ["iota32"], "c_iota32")

        class _Stop(Exception):
            pass

        def stop_here(tag):
            if os.environ.get("KSTOP") == tag:
                P.emit()
                raise _Stop()

        cur_x = x_in
        try:
          for l in range(NL):
              last = (l == NL - 1)
              nxt_x = y_out if False else x2_d
              P.barrier()
              A16.reset(); A32.reset()
              cst = A32.alloc(2 * 16).rearrange("p (q k) -> p q k", q=2)
              csb = A16.alloc(16 * 2).rearrange("p (k q) -> p k q", q=2)
              brow = [A32.alloc(512) for _ in range(2)]
              mrow = A32.alloc(512)
              DMA("sync", cst, c_in.rearrange("q (p k) -> p q k", k=16), [], ["cst"], "cst")
              for q in range(2):
                  ACT(csb[:, :, q], cst[:, q, :], AF.Silu, ["cst"], [("csb", q)])
              wa = [A16.alloc(16 * 512).rearrange("p (k n) -> p k n", k=16) for _ in range(2)]
              for cb in range(24):
                  wbuf = wa[cb % 2]
                  wk = ("wa", cb % 2)
                  DMA("gpsimd", wbuf, w_ada[l, :, cb * 512:(cb + 1) * 512].rearrange("(p k) n -> p k n", k=16),
                      [], [wk], "wa%d" % (cb % 2))
                  pk = PM[cb % 2]
                  pst = pm[cb % 2]
                  for kc in range(16):
                      MM(pst[0:2, :], csb[:, kc, :], wbuf[:, kc, :], kc == 0, kc == 15,
                         [wk, ("csb", 0), ("csb", 1)], [pk])
                  mk = U("mrow")
                  bk = ("brow", cb % 2)
                  DMA("sync", brow[cb % 2][0:2, :], b_ada[l:l + 1, cb * 512:(cb + 1) * 512].broadcast_to([2, 512]),
                      [], [bk], "brow%d" % (cb % 2))
                  TT("vector", mrow[0:2, :], pst[0:2, :], brow[cb % 2][0:2, :], ALU.add,
                     [pk, bk], ["mrow"])
                  DMA(STQ, mod_d[:, cb * 512:(cb + 1) * 512], mrow[0:2, :], ["mrow"], [("mod", cb)], "mrow")
              MODK = [("mod", cb) for cb in range(24)]

              def mod_rep(q, i):
                  return mod_d[q:q + 1, i * D:(i + 1) * D].broadcast_to([128, D])

              for q, S in enumerate(SEQS):
                  T0 = OFFS[q]
                  NT5 = S // 512
                  S2 = S // 128
                  P.barrier()
                  A16.reset(); A32.reset()
                  wi = A16.alloc(16 * 3072).rearrange("p (k n) -> p k n", k=16)
                  DMA("gpsimd", wi, w_in[l].rearrange("(p k) n -> p k n", k=16), [], ["wi"], "wi")
                  a1 = A32.alloc(D)
                  sh1 = A32.alloc(D)
                  tmpf = A32.alloc(D)
                  DMA("sync", a1, mod_rep(q, 1), MODK, ["a1"], "a1")
                  DMA("sync", sh1, mod_rep(q, 0), MODK, ["sh1"], "sh1")
                  DMA("sync", tmpf, norm1_g[l:l + 1, :].broadcast_to([128, D]), [], ["tmpf"], "g_rep")
                  STT(a1, a1, 1.0, tmpf, ALU.add, ALU.mult, ["a1", "tmpf"], ["a1"])
                  xt = [A32.alloc(D) for _ in range(2)]
                  hb = [A16.alloc(D) for _ in range(4)]
                  hT = [A16.alloc(16 * 512).rearrange("p (k t) -> p k t", k=16) for _ in range(1)]
                  stg = [A16.alloc(16 * 512).rearrange("p (j t) -> p j t", j=16) for _ in range(1)]
                  sgt = [A32.alloc(512) for _ in range(2)]
                  ss = A32.alloc(8)
                  hTt = hT[0]
                  hk = ("hT", 0)

                  def p1_norm(ti):
                      for sub in range(4):
                          it = ti * 4 + sub
                          b2 = it % 2
                          r0 = T0 + it * 128
                          xk = ("xt", b2)
                          DMA("sync", xt[b2], cur_x[r0:r0 + 128, :], [("xcur", r0 // 128)], [xk], "xt%d" % b2)
                          ssk = U("ss")
                          hbk = ("hb", sub)
                          ACT(hb[sub], xt[b2], AF.Square, [xk], [hbk, ssk], accum_out=ss[:, 0:1])
                          ACT(ss[:, 1:2], ss[:, 0:1], AF.Sqrt, [ssk, "eps_t"], [ssk], scale=1.0 / D, bias=eps_t[:, 0:1])
                          P.op("vector", lambda e, ss=ss: e.reciprocal(out=ss[:, 2:3], in_=ss[:, 1:2]), [ssk], [ssk])
                          STT(tmpf, xt[b2], ss[:, 2:3], a1, ALU.mult, ALU.mult, [xk, ssk, "a1"], ["tmpf"])
                          TT("gpsimd", hb[sub], tmpf, sh1, ALU.add, ["tmpf", "sh1"], [hbk])

                  def p1_tr(ti):
                      for sub in range(4):
                          hbk = ("hb", sub)
                          for kc in range(16):
                              TR(pt[:, kc * 128:(kc + 1) * 128], hb[sub].rearrange("p (c k) -> p k c", k=16)[:, kc, :],
                                 ident_b[:], [hbk, "ident_b"], [PT])
                          CP("scalar" if sub % 2 else "vector", hTt[:, :, sub * 128:(sub + 1) * 128],
                             pt[:, :].rearrange("p (k t) -> p k t", k=16), [PT], [hk])

                  def p1_mm(ti):
                      st = stg[0]
                      sk = ("stg", 0)
                      c0 = ti * 512

                      def mmgrp(j, pi):
                          for kc in range(16):
                              MM(pm[pi][:, :], wi[:, kc, j * 128:(j + 1) * 128], hTt[:, kc, :], kc == 0, kc == 15,
                                 ["wi", hk], [PM[pi]])

                      pi = 0
                      for j in range(4):
                          mmgrp(j, pi)
                          CP("scalar", st[:, j, :], pm[pi][:, :], [PM[pi]], [sk])
                          pi = (pi + 1) % 4
                      for j in range(8):
                          pa = pi
                          mmgrp(4 + j, pa)
                          pb = (pi + 1) % 4
                          mmgrp(12 + j, pb)
                          sg = sgt[j % 2]
                          sgk = ("sg", j % 2)
                          ACT(sg, pm[pb][:, :], AF.Sigmoid, [PM[pb]], [sgk])
                          TT("vector", st[:, 4 + j, :], pm[pa][:, :], sg, ALU.mult, [PM[pa], sgk], [sk])
                          pi = (pi + 2) % 4
                      for j in range(4):
                          mmgrp(20 + j, pi)
                          CP("scalar", st[:, 12 + j, :], pm[pi][:, :], [PM[pi]], [sk])
                          pi = (pi + 1) % 4
                      DMA(STQ, zaT[q][:, c0:c0 + 512].rearrange("(j p) t -> p j t", p=128), st[:, 0:4, :],
                          [sk], [("zaT", q, ti)], "stg0")
                      DMA(STQ, vT[q][:, c0:c0 + 512].rearrange("(j p) t -> p j t", p=128), st[:, 4:12, :],
                          [sk], [("vT", q, ti)], "stg0")
                      DMA(STQ, zcT[q][:, c0:c0 + 512].rearrange("(j p) t -> p j t", p=128), st[:, 12:16, :],
                          [sk], [("zcT", q, ti)], "stg0")

                  p1_norm(0)
                  p1_tr(0)
                  for ti in range(NT5):
                      if ti + 1 < NT5:
                          p1_norm(ti + 1)
                      p1_mm(ti)
                      if ti + 1 < NT5:
                          p1_tr(ti + 1)

                  stop_here('P1')
                  P.barrier()
                  A16.reset(); A32.reset()
                  pwf = A32.alloc(4 * 128).rearrange("p (g e) -> p g e", g=4)
                  pwb = A16.alloc(4 * 128).rearrange("p (g e) -> p g e", g=4)
                  DMA("sync", pwf, pool_w[l].rearrange("g c e -> c g e"), [], ["pwf"], "pwf")
                  CP("vector", pwb, pwf, ["pwf"], ["pwb"])
                  psc = A32.alloc(4)
                  pscr = A32.alloc(4 * 128).rearrange("p (g e) -> p g e", g=4)[0:4]
                  DMA("sync", pscr[:, 0, :], pool_scale[l].rearrange("(g e) -> g e", g=4), [], ["pscr"], "pscr")
                  TR(pr[0][:, 0:4], pscr[:, 0, :], ident_f[0:4, 0:4], ["pscr", "ident_f"], [PR[0]])
                  CP("vector", psc, pr[0][:, 0:4], [PR[0]], ["psc"])
                  pcst = A32.alloc(64).rearrange("p (g j) -> p g j", g=4)
                  DMA("sync", pcst, k_poolc, [], ["pcst"], "pcst")
                  ub = [A16.alloc(528) for _ in range(2)]
                  sa = [A32.alloc(528) for _ in range(2)]
                  sb_ = [A32.alloc(528) for _ in range(2)]
                  dmt = [A16.alloc(512) for _ in range(2)]
                  pst2 = [A16.alloc(4 * 512).rearrange("p (g t) -> p g t", g=4) for _ in range(2)]
                  itc2 = [0]

                  def p2_tile(ti):
                      c0 = ti * 512
                      st = pst2[ti % 2]
                      sk = ("pst2", ti % 2)
                      for g, w in enumerate((2, 4, 8, 16)):
                          b2 = itc2[0] % 2
                          itc2[0] += 1
                          u = ub[b2]
                          uk = ("ub", b2)
                          lo = max(c0 - 8, 0)
                          hi = min(c0 + 520, S)
                          rd = [("zaT", q, tj) for tj in range(max(ti - 1, 0), min(ti + 2, NT5))]
                          if lo > c0 - 8:
                              MEMSET("gpsimd", u[:, 0:8], 0.0, [uk])
                          if hi < c0 + 520:
                              MEMSET("gpsimd", u[:, 520:528], 0.0, [uk])
                          DMA("sync", u[:, lo - (c0 - 8):hi - (c0 - 8)], zaT[q][g * 128:(g + 1) * 128, lo:hi], rd, [uk], "ub%d" % b2)
                          s_a, s_b = sa[b2], sb_[b2]
                          ka, kb = ("sa", b2), ("sb", b2)
                          TT("vector", s_a[:, 1:528], u[:, 0:527], u[:, 1:528], ALU.add, [uk], [ka])
                          cur, curk, oth, othk = s_a, ka, s_b, kb
                          lo_v = 1
                          hi_v = 528
                          step = 1
                          ww = 2
                          while ww < w:
                              nlo = lo_v + step
                              nhi = hi_v - step
                              TT("vector", oth[:, nlo:nhi], cur[:, nlo - step:nhi - step], cur[:, nlo + step:nhi + step],
                                 ALU.add, [curk], [othk])
                              cur, curk, oth, othk = oth, othk, cur, curk
                              lo_v, hi_v = nlo, nhi
                              step *= 2
                              ww *= 2
                          dm = dmt[b2]
                          dk_ = ("dm", b2)
                          STT(dm, cur[:, 8:520], 1.0 / w, u[:, 8:520], ALU.mult, ALU.subtract, [curk, uk], [dk_])
                          if ti == 0:
                              TT("vector", oth[:, 8:16], cur[:, 8:16], pcst[:, g, 0:8], ALU.mult, [curk, "pcst"], [othk])
                              TT("vector", dm[:, 0:8], oth[:, 8:16], u[:, 8:16], ALU.subtract, [othk, uk], [dk_])
                          if ti == NT5 - 1:
                              TT("vector", oth[:, 512:520], cur[:, 512:520], pcst[:, g, 8:16], ALU.mult, [curk, "pcst"], [othk])
                              TT("vector", dm[:, 504:512], oth[:, 512:520], u[:, 512:520], ALU.subtract, [othk, uk], [dk_])
                          pi = g
                          MM(pm[pi][:, :], pwb[:, g, :], dm, True, True, ["pwb", dk_], [PM[pi]])
                          ACT(st[:, g, :], pm[pi][:, :], AF.Copy, [PM[pi], "psc"], [sk], scale=psc[:, g:g + 1])
                      DMA(STQ, mixT[q][0:512, c0:c0 + 512].rearrange("(g p) t -> p g t", p=128), st, [sk],
                          [("mixT", q, ti, 0)], "pst2%d" % (ti % 2))

                  pww = A16.alloc(8 * 1024).rearrange("p (k n) -> p k n", k=8)
                  DMA("gpsimd", pww, conv_pw_w[l].rearrange("(k p) n -> p k n", p=128), [], ["pww"], "pww")
                  dwr = A32.alloc(1024)
                  DMA("sync", dwr[0:CONV_K, :], conv_dw_w[l], [], ["dwr"], "dwr")
                  dwT = A32.alloc(8 * 32).rearrange("p (j k) -> p j k", j=8)
                  for j in range(8):
                      TR(pr[0][:, j * 32:j * 32 + CONV_K], dwr[0:CONV_K, j * 128:(j + 1) * 128],
                         ident_f[0:CONV_K, 0:CONV_K], ["dwr", "ident_f"], [PR[0]])
                  CP("vector", dwT[:, :, 0:CONV_K], pr[0][:, 0:256].rearrange("p (j k) -> p j k", j=8)[:, :, 0:CONV_K],
                     [PR[0]], ["dwT"])
                  vr = A32.alloc(4 * 1024).rearrange("p (v n) -> p v n", v=4)
                  vecs = A32.alloc(32).rearrange("p (v j) -> p v j", v=4)
                  for vi, src in enumerate((conv_dw_b, conv_ln_g, conv_ln_b, conv_pw_b)):
                      DMA("sync", vr[0:8, vi, 0:128], src[l].rearrange("(j p) -> j p", p=128), [], [("vr", vi)], "vr%d" % vi)
                      TR(pr[1][:, vi * 8:vi * 8 + 8], vr[0:8, vi, 0:128], ident_f[0:8, 0:8], [("vr", vi), "ident_f"], [PR[1]])
                  CP("vector", vecs, pr[1][:, 0:32].rearrange("p (v j) -> p v j", v=4), [PR[1]], ["vecs"])
                  acc = [A32.alloc(512) for _ in range(8)]
                  cbf = [A16.alloc(512) for _ in range(2)]
                  sqb = [A16.alloc(512) for _ in range(2)]
                  sT = [A16.alloc(512) for _ in range(8)]
                  mean = A32.alloc(512)
                  var = A32.alloc(512)
                  rstd = A32.alloc(512)
                  xn = [A32.alloc(512) for _ in range(2)]
                  st3 = [A16.alloc(8 * 512).rearrange("p (j t) -> p j t", j=8) for _ in range(2)]
                  if CONV_PE:
                      vb = [A16.alloc(544) for _ in range(3)]
                      vb1 = [A16.alloc(544) for _ in range(3)]
                      dg = A16.alloc(8 * 32 * 128).rearrange("p (j k c) -> p j k c", j=8, k=32)
                      for j in range(8):
                          for k in range(CONV_K):
                              TS("vector" if (j * CONV_K + k) % 2 else "gpsimd", dg[:, j, k, :], ident_b[:, :], dwT[:, j, k:k + 1], None,
                                 ALU.mult, None, ["ident_b", "dwT"], [("dg", j)])
                  else:
                      vb = [A16.alloc(544) for _ in range(2)]
                  itc3 = [0]

                  def p3_tile(ti):
                      c0 = ti * 512
                      for j in range(8):
                          if CONV_PE:
                              b2 = itc3[0] % 2
                              b3 = itc3[0] % 3
                              itc3[0] += 1
                              v, vk = vb[b3], ("vb", b3)
                              v1, v1k = vb1[b3], ("vb1", b3)
                              lo = max(c0 - 15, 0)
                              hi = min(c0 + 527, S)
                              rd = [("vT", q, tj) for tj in range(max(ti - 1, 0), min(ti + 2, NT5))]
                              if lo > c0 - 15:
                                  MEMSET("gpsimd", v[:, 0:16], 0.0, [vk])
                                  MEMSET("gpsimd", v1[:, 0:16], 0.0, [v1k])
                              if hi < c0 + 527:
                                  MEMSET("gpsimd", v[:, 526:544], 0.0, [vk])
                                  MEMSET("gpsimd", v1[:, 526:544], 0.0, [v1k])
                              DMA("sync", v[:, lo - (c0 - 15):hi - (c0 - 15)], vT[q][j * 128:(j + 1) * 128, lo:hi], rd, [vk], "vb%d" % b3)
                              lo1 = max(c0 - 14, 0)
                              DMA("sync", v1[:, lo1 - (c0 - 14):hi - (c0 - 14)], vT[q][j * 128:(j + 1) * 128, lo1:hi], rd, [v1k], "vb1%d" % b3)
                              pi = itc3[0] % 4
                              for k in range(CONV_K):
                                  if k % 2 == 0:
                                      MM(pm[pi][:, :], dg[:, j, k, :], v[:, k:k + 512], k == 0, k == CONV_K - 1,
                                         [("dg", j), vk], [PM[pi]])
                                  else:
                                      MM(pm[pi][:, :], dg[:, j, k, :], v1[:, k - 1:k - 1 + 512], k == 0, k == CONV_K - 1,
                                         [("dg", j), v1k], [PM[pi]])
                              a = acc[j]
                              ak = ("acc", j)
                              ACT(a, pm[pi][:, :], AF.Identity, [PM[pi], "vecs"], [ak], bias=vecs[:, 0, j:j + 1])
                              cb_, ck = cbf[b2], ("cbf", b2)
                              sq_, sqk = sqb[b2], ("sqb", b2)
                              CP("vector", cb_, a, [ak], [ck])
                              ACT(sq_, a, AF.Square, [ak], [sqk])
                              MM(pr[0][:, :], ones_b[:], cb_, j == 0, j == 7, ["ones_b", ck], [PR[0]])
                              MM(pr[1][:, :], ones_b[:], sq_, j == 0, j == 7, ["ones_b", sqk], [PR[1]])
                          else:
                              b2 = itc3[0] % 2
                              itc3[0] += 1
                              v = vb[b2]
                              vk = ("vb", b2)
                              lo = max(c0 - 15, 0)
                              hi = min(c0 + 527, S)
                              rd = [("vT", q, tj) for tj in range(max(ti - 1, 0), min(ti + 2, NT5))]
                              if lo > c0 - 15:
                                  MEMSET("gpsimd", v[:, 0:15], 0.0, [vk])
                              if hi < c0 + 527:
                                  MEMSET("gpsimd", v[:, 527:542], 0.0, [vk])
                              DMA("sync", v[:, lo - (c0 - 15):hi - (c0 - 15)], vT[q][j * 128:(j + 1) * 128, lo:hi], rd, [vk], "vb%d" % b2)
                              a = acc[j]
                              ak = ("acc", j)
                              TS("vector", a, v[:, 0:512], dwT[:, j, 0:1], vecs[:, 0, j:j + 1], ALU.mult, ALU.add,
                                 [vk, "dwT", "vecs"], [ak])
                              for k in range(1, CONV_K):
                                  STT(a, v[:, k:k + 512], dwT[:, j, k:k + 1], a, ALU.mult, ALU.add, [vk, "dwT", ak], [ak])
                              cb_, ck = cbf[b2], ("cbf", b2)
                              sq_, sqk = sqb[b2], ("sqb", b2)
                              CP("gpsimd", cb_, a, [ak], [ck])
                              ACT(sq_, a, AF.Square, [ak], [sqk])
                              MM(pr[0][:, :], ones_b[:], cb_, j == 0, j == 7, ["ones_b", ck], [PR[0]])
                              MM(pr[1][:, :], ones_b[:], sq_, j == 0, j == 7, ["ones_b", sqk], [PR[1]])
                      TS("vector", mean, pr[0][:, :], 1.0 / 1024, None, ALU.mult, None, [PR[0]], ["mean"])
                      TS("vector", var, pr[1][:, :], 1.0 / 1024, None, ALU.mult, None, [PR[1]], ["var"])
                      TT("vector", rstd, mean, mean, ALU.mult, ["mean"], ["rstd"])
                      TT("vector", var, var, rstd, ALU.subtract, ["var", "rstd"], ["var"])
                      ACT(var, var, AF.Sqrt, ["var", "eps_t"], ["var"], bias=eps_t[:, 0:1], scale=1.0)
                      P.op("vector", lambda e, rstd=rstd, var=var: e.reciprocal(out=rstd, in_=var), ["var"], ["rstd"])
                      for j in range(8):
                          x_, xk_ = xn[j % 2], ("xn", j % 2)
                          TT("gpsimd", x_, acc[j], mean, ALU.subtract, [("acc", j), "mean"], [xk_])
                          TT("vector", x_, x_, rstd, ALU.mult, [xk_, "rstd"], [xk_])
                          ACT(sT[j], x_, AF.Silu, [xk_, "vecs"], [("sT", j)], scale=vecs[:, 1, j:j + 1], bias=vecs[:, 2, j:j + 1])
                      st = st3[ti % 2]
                      sk = ("st3", ti % 2)
                      for e_ in range(8):
                          pi = e_ % 4
                          for j in range(8):
                              MM(pm[pi][:, :], pww[:, j, e_ * 128:(e_ + 1) * 128], sT[j], j == 0, j == 7,
                                 ["pww", ("sT", j)], [PM[pi]])
                          ACT(st[:, e_, :], pm[pi][:, :], AF.Identity, [PM[pi], "vecs"], [sk], bias=vecs[:, 3, e_:e_ + 1])
                      DMA(STQ, mixT[q][512:1536, c0:c0 + 512].rearrange("(j p) t -> p j t", p=128), st, [sk],
                          [("mixT", q, ti, 1)], "st3%d" % (ti % 2))

                  p2_tile(0)
                  for ti in range(NT5):
                      if ti + 1 < NT5:
                          p2_tile(ti + 1)
                      p3_tile(ti)
                  stop_here('P3')
                  P.barrier()
                  A16.reset(); A32.reset()
                  cs = A16.alloc(2 * 192).rearrange("p (h n) -> p h n", h=2)
                  ccn = A16.alloc(2 * 128).rearrange("p (h n) -> p h n", h=2)
                  fcs = A16.alloc(2 * S).rearrange("p (h n) -> p h n", h=2)
                  DMA("sync", cs, k_cs128, [], ["cs"], "cs")
                  DMA("sync", ccn, k_ccn, [], ["ccn"], "ccn")
                  DMA("sync", fcs[0:S2], k_fcs[q], [], ["fcs"], "fcs")
                  fwf = A32.alloc(4 * 512).rearrange("p (h e) -> p h e", h=4)
                  fwb = A16.alloc(4 * 512).rearrange("p (h e) -> p h e", h=4)
                  DMA("sync", fwf, fourier_w[l].rearrange("(h m) e -> m h e", h=4), [], ["fwf"], "fwf")
                  CP("vector", fwb, fwf, ["fwf"], ["fwb"])
                  Ut = A16.alloc(128 * S2).rearrange("p (c t) -> p c t", c=128)
                  Ah = A16.alloc(128 * 192).rearrange("p (c n) -> p c n", c=128)
                  Xh = A16.alloc(2 * S).rearrange("p (h k) -> p h k", h=2)
                  rst = [A16.alloc(512) for _ in range(2)]
                  ALLZC = [("zcT", q, tj) for tj in range(NT5)]
                  nrm = 1.0 / math.sqrt(S * 128.0)
                  ri = 0
                  for h in range(4):
                      for cq in range(8):
                          DMA("sync", Ut[:, cq * 16:(cq + 1) * 16, :],
                              zcT[q][h * 128 + cq * 16:h * 128 + (cq + 1) * 16, :].rearrange("c (a b) -> a c b", b=S2),
                              ALLZC, ["Ut"], "Ut")
                      for half in range(2):
                          pmv = [pm[i] for i in range(4)]
                          for cg in range(16):
                              for ci in range(8):
                                  c_ = cg * 8 + ci
                                  bank = ci // 2
                                  col = (ci % 2) * 192
                                  MM(pmv[bank][0:S2, col:col + 192], Ut[:, c_, :], cs[:, half, :], True, True,
                                     ["Ut", "cs"], [PM[bank]])
                              for bank in range(4):
                                  eng = "vector" if bank % 2 == 0 else "scalar"
                                  CP(eng, Ah[0:S2, cg * 8 + bank * 2:cg * 8 + bank * 2 + 2, :],
                                     pmv[bank][0:S2, 0:384].rearrange("p (c n) -> p c n", c=2), [PM[bank]], [("Ah", cg)])
                          AHK = [("Ah", cg) for cg in range(16)]
                          G = 512 // S2
                          G = min(G, 64)
                          for kg in range(64 // G):
                              pxr, pxi = pr[0], pr[1]
                              for gi in range(G):
                                  k1l = kg * G + gi
                                  k1 = half * 64 + k1l
                                  fc_ = fcs[0:S2, 0, :].rearrange("p (b a) -> p a b", a=128)[:, k1, :]
                                  fs_ = fcs[0:S2, 1, :].rearrange("p (b a) -> p a b", a=128)[:, k1, :]
                                  ar = Ah[0:S2, :, k1l]
                                  ai = Ah[0:S2, :, 64 + k1l]
                                  an = Ah[0:S2, :, 128 + k1l]
                                  o = gi * S2
                                  MM(pxr[:, o:o + S2], ar, fc_, True, False, AHK + ["fcs"], [PR[0]])
                                  MM(pxr[:, o:o + S2], an, fs_, False, True, AHK + ["fcs"], [PR[0]])
                                  MM(pxi[:, o:o + S2], ar, fs_, True, False, AHK + ["fcs"], [PR[1]])
                                  MM(pxi[:, o:o + S2], ai, fc_, False, True, AHK + ["fcs"], [PR[1]])
                              k1b = half * 64 + kg * G
                              for ri_, px in enumerate((pxr, pxi)):
                                  dst = Xh[:, ri_, :].rearrange("p (b a) -> p a b", a=128)[:, k1b:k1b + G, :]
                                  src = px[:, 0:G * S2].rearrange("p (g b) -> p g b", g=G)
                                  CP("vector" if ri_ == 0 else "scalar", dst, src, [PR[ri_]], [("Xh", half, kg)])
                      XHK = [("Xh", hf, kg) for hf in range(2) for kg in range(64 // G)]
                      for kt in range(NT5):
                          pi = kt % 4
                          MM(pm[pi][:, :], ccn[:, 0, :], Xh[:, 0, kt * 512:(kt + 1) * 512], True, False, ["ccn"] + XHK, [PM[pi]])
                          MM(pm[pi][:, :], ccn[:, 1, :], Xh[:, 1, kt * 512:(kt + 1) * 512], False, True, ["ccn"] + XHK, [PM[pi]])
                          r_ = rst[ri % 2]
                          rk = ("rst", ri % 2)
                          ACT(r_, pm[pi][:, :], AF.Copy, [PM[pi]], [rk], scale=nrm)
                          DMA(STQ, rzT[q][h * 128:(h + 1) * 128, kt * 512:(kt + 1) * 512], r_, [rk], [("rzT", q, h, kt)],
                              "rst%d" % (ri % 2))
                          ri += 1
                  rzb = [A16.alloc(4 * 512).rearrange("p (h t) -> p h t", h=4) for _ in range(2)]
                  st4 = [A16.alloc(4 * 512).rearrange("p (e t) -> p e t", e=4) for _ in range(2)]
                  for ti in range(NT5):
                      c0 = ti * 512
                      rb, rbk = rzb[ti % 2], ("rzb", ti % 2)
                      DMA("sync", rb, rzT[q][:, c0:c0 + 512].rearrange("(h m) t -> m h t", h=4),
                          [("rzT", q, h, ti) for h in range(4)], [rbk], "rzb%d" % (ti % 2))
                      st, sk = st4[ti % 2], ("st4", ti % 2)
                      for e_ in range(4):
                          pi = e_
                          for h in range(4):
                              MM(pm[pi][:, :], fwb[:, h, e_ * 128:(e_ + 1) * 128], rb[:, h, :], h == 0, h == 3,
                                 ["fwb", rbk], [PM[pi]])
                          CP("scalar" if e_ % 2 else "vector", st[:, e_, :], pm[pi][:, :], [PM[pi]], [sk])
                      DMA(STQ, mixT[q][1536:2048, c0:c0 + 512].rearrange("(e p) t -> p e t", p=128), st, [sk],
                          [("mixT", q, ti, 2)], "st4%d" % (ti % 2))

                  stop_here('P4')
                  P.barrier()
                  A16.reset(); A32.reset()
                  wo = A16.alloc(16 * 2048).rearrange("p (k n) -> p k n", k=16)
                  DMA("gpsimd", wo, w_out[l].rearrange("(k p) n -> p k n", p=128), [], ["wo"], "wo")
                  g1 = A32.alloc(D)
                  a2 = A32.alloc(D)
                  sh2 = A32.alloc(D)
                  tmpf = A32.alloc(D)
                  TFK = [("tmpf", i) for i in range(4)]
                  DMA("sync", g1, mod_rep(q, 2), MODK, ["g1"], "g1")
                  DMA("sync", a2, mod_rep(q, 4), MODK, ["a2"], "a2")
                  DMA("sync", sh2, mod_rep(q, 3), MODK, ["sh2"], "sh2")
                  DMA("sync", tmpf, norm2_g[l:l + 1, :].broadcast_to([128, D]), [], TFK, "g_rep")
                  STT(a2, a2, 1.0, tmpf, ALU.add, ALU.mult, ["a2"] + TFK, ["a2"])
                  wr = A32.alloc(16 * 36).rearrange("p (k n) -> p k n", k=16)
                  DMA("sync", wr[:, :, 0:4], rc_w[l].rearrange("(p k) g -> p k g", k=16), [], [("wr", 0)], "wr")
                  for g in range(4):
                      DMA("sync", wr[:, :, 4 + 8 * g:12 + 8 * g], rf_w[l, g].rearrange("(p k) e -> p k e", k=16), [],
                          [("wr", 1 + g)], "wr")
                  WRK = [("wr", i) for i in range(5)]
                  rb_ = A32.alloc(36)
                  DMA("sync", rb_[:, 0:4], rc_b[l:l + 1, :].broadcast_to([128, 4]), [], [("rb", 0)], "rb")
                  DMA("sync", rb_[:, 4:36], rf_b[l:l + 1].rearrange("o g e -> o (g e)").broadcast_to([128, 32]), [], [("rb", 1)], "rb")
                  RBK = [("rb", 0), ("rb", 1)]
                  mx = [A16.alloc(16 * 512).rearrange("p (k t) -> p k t", k=16) for _ in range(2)]
                  xt = [A32.alloc(D) for _ in range(1)]
                  x1t = [A32.alloc(D) for _ in range(2)]
                  h2f2 = [A32.alloc(D) for _ in range(2)]
                  h2b = [A16.alloc(D) for _ in range(2)]
                  h2T = A32.alloc(16 * 128).rearrange("p (k t) -> p k t", k=16)
                  sm2 = [A32.alloc(256) for _ in range(3)]
                  ssn = A32.alloc(8)
                  if q == 0:
                      MEMSET("vector", cnt_run[:], 0.0, ["cnt_run"])

                  def p5_main(it):
                      ti, sub = it // 4, it % 4
                      c0 = ti * 512
                      m_, mk_ = mx[ti % 2], ("mx", ti % 2)
                      if sub == 0:
                          DMA("sync", m_, mixT[q][:, c0:c0 + 512].rearrange("(k p) t -> p k t", p=128),
                              [("mixT", q, ti, i) for i in range(3)], [mk_], "mx%d" % (ti % 2))
                      b2 = it % 2
                      r0 = T0 + it * 128
                      xk = ("xt", 0)
                      DMA("sync", xt[0], cur_x[r0:r0 + 128, :], [("xcur", r0 // 128)], [xk], "xt0")
                      x1, x1k = x1t[b2], ("x1t", b2)
                      for cbk in range(4):
                          pi = cbk
                          for kc in range(16):
                              MM(pm[pi][:, :], m_[:, kc, sub * 128:(sub + 1) * 128], wo[:, kc, cbk * 512:(cbk + 1) * 512],
                                 kc == 0, kc == 15, [mk_, "wo"], [PM[pi]])
                          sl = slice(cbk * 512, (cbk + 1) * 512)
                          TT("vector", tmpf[:, sl], pm[pi][:, :], g1[:, sl], ALU.mult, [PM[pi], "g1"], [("tmpf", cbk)])
                          TT("gpsimd", x1[:, sl], tmpf[:, sl], xt[0][:, sl], ALU.add, [("tmpf", cbk), xk], [x1k])
                      DMA(STQ, x1_d[r0:r0 + 128, :], x1, [x1k], [("x1", r0 // 128)], "x1t%d" % b2)
                      ssk = U("ss")
                      ss = ssn[:, 0:4]
                      hb_, hbk = h2b[b2], ("h2b", b2)
                      h2f, h2fk = h2f2[b2], ("h2f", b2)
                      ACT(hb_, x1, AF.Square, [x1k], [hbk, ssk], accum_out=ss[:, 0:1])
                      ACT(ss[:, 1:2], ss[:, 0:1], AF.Sqrt, [ssk, "eps_t"], [ssk], scale=1.0 / D, bias=eps_t[:, 0:1])
                      P.op("vector", lambda e, ss=ss: e.reciprocal(out=ss[:, 2:3], in_=ss[:, 1:2]), [ssk], [ssk])
                      STT(tmpf, x1, ss[:, 2:3], a2, ALU.mult, ALU.mult, [x1k, ssk, "a2"], [("tmpf", i) for i in range(4)])
                      TT("gpsimd", h2f, tmpf, sh2, ALU.add, [("tmpf", i) for i in range(4)] + ["sh2"], [h2fk])
                      CP("scalar", hb_, h2f, [h2fk], [hbk])
                      DMA(STQ, h2_d[r0:r0 + 128, :], hb_, [hbk], [("h2", r0 // 128)], "h2b%d" % b2)

                  def p5_router(it):
                      git = (T0 // 128) + it
                      b2 = it % 2
                      h2f, h2fk = h2f2[b2], ("h2f", b2)
                      for kc in range(16):
                          pj = pr[(kc // 4) % 2]
                          TR(pj[:, (kc % 4) * 128:(kc % 4 + 1) * 128], h2f.rearrange("p (c k) -> p k c", k=16)[:, kc, :],
                             ident_f[:], [h2fk, "ident_f"], [PR[(kc // 4) % 2]])
                          if kc % 4 == 3:
                              g4 = kc // 4
                              CP("scalar" if g4 % 2 else "vector", h2T[:, g4 * 4:g4 * 4 + 4, :],
                                 pj[:, :].rearrange("p (k t) -> p k t", k=4), [PR[(kc // 4) % 2]], [("h2T", g4)])
                      for kc in range(16):
                          MM(pr[0][:, 0:36], h2T[:, kc, :], wr[:, kc, :], kc == 0, kc == 15,
                             [("h2T", kc // 4)] + WRK, [PR[0]])
                      rkeys[it] = U("rt")
                      route_tile(P, nc, sm2[it % 3], pr, PR, rb_, RBK, r_E, r_f, cnt_run, utinc, ones_b, git,
                                 TT, TS, STT, ACT, CP, RED, MM, U, part="A", rk=rkeys[it])

                  def p5_routerB(it):
                      git = (T0 // 128) + it
                      route_tile(P, nc, sm2[it % 3], pr, PR, rb_, RBK, r_E, r_f, cnt_run, utinc, ones_b, git,
                                 TT, TS, STT, ACT, CP, RED, MM, U, part="B", rk=rkeys[it])

                  rkeys = {}
                  NSUB = NT5 * 4
                  for it in range(NSUB):
                      p5_main(it)
                      if it > 0:
                          p5_router(it - 1)
                      if it > 1:
                          p5_routerB(it - 2)
                  p5_router(NSUB - 1)
                  if NSUB > 1:
                      p5_routerB(NSUB - 2)
                  p5_routerB(NSUB - 1)

              stop_here('P5')
              P.barrier()
              A16.reset(); A32.reset()
              fin = A32.alloc(8 * 32).rearrange("p (a e) -> p a e", a=8)
              RALL = ["cnt_run"]
              TS("vector", fin[:, 0, :], cnt_run[:], 127.0, None, ALU.add, None, ["cnt_run"], ["fin"])
              fin_i = A32.alloc(32).bitcast(I32)
              CP("vector", fin_i, fin[:, 0, :], ["fin"], ["fin_i"])
              TS("vector", fin_i, fin_i, 7, 7, ALU.arith_shift_right, ALU.logical_shift_left, ["fin_i"], ["fin_i"])
              CP("vector", fin[:, 2, :], fin_i, ["fin_i"], ["fin"])
              MEMSET("vector", fin[:, 3, :], 1.0, ["fin"])
              P.op("vector", lambda e: e.tensor_tensor_scan(out=fin[:, 4, :], data0=fin[:, 3, :], data1=fin[:, 2, :],
                                                            initial=0.0, op0=ALU.mult, op1=ALU.add), ["fin"], ["fin"])
              TT("vector", fin[:, 5, :], fin[:, 4, :], fin[:, 2, :], ALU.subtract, ["fin"], ["fin"])
              big = A32.alloc(NT * 32).rearrange("p (t e) -> p t e", e=32)
              slf = A32.alloc(NT * 2).rearrange("p (t k) -> p t k", k=2)
              for k in range(2):
                  TT("vector", big, r_E[:, :, k * 32:(k + 1) * 32], fin[:, 5:6, :].to_broadcast([128, NT, 32]), ALU.mult,
                     ["fin", "r_E"], ["big"])
                  RED(slf[:, :, k], big, ALU.add, ["big"], ["slf"])
                  TT("vector", slf[:, :, k], slf[:, :, k], r_f[:, :, k], ALU.add, ["slf", "r_f"], ["slf"])
              CP("vector", r_slot[:], slf, ["slf"], ["r_slot"])
              bst = A32.alloc(NB)
              DMA("sync", bst, k_bstart, [], ["bst"], "bst")
              ebf = A32.alloc(NB)
              CH = 32
              bigb = A32.alloc(CH * 32).rearrange("p (b e) -> p b e", e=32)
              for b0 in range(0, NB, CH):
                  nb_ = min(CH, NB - b0)
                  TT("vector", bigb[:, 0:nb_, :], fin[:, 4:5, :].to_broadcast([128, nb_, 32]),
                     bst[:, b0:b0 + nb_].unsqueeze(2).to_broadcast([128, nb_, 32]), ALU.is_le, ["fin", "bst"], ["bigb"])
                  RED(ebf[:, b0:b0 + nb_], bigb[:, 0:nb_, :], ALU.add, ["bigb"], ["ebf"])
              TS("vector", ebf, ebf, 31.0, None, ALU.min, None, ["ebf"], ["ebf"])
              sam = A32.alloc(NB)
              MEMSET("vector", sam[:, 0:1], 0.0, ["sam"])
              TT("vector", sam[:, 1:NB], ebf[:, 1:NB], ebf[:, 0:NB - 1], ALU.is_equal, ["ebf"], ["sam"])
              wif = A32.alloc(NB)
              TS("vector", wif, ebf, 128.0, iotap[:, 0:1], ALU.mult, ALU.add, ["ebf", "iotap"], ["wif"])
              STT(wif, sam, 1.0e6, wif, ALU.mult, ALU.add, ["sam", "wif"], ["wif"])
              if l > 0:
                  TS("vector", wif, wif, float(l * NE * 128), None, ALU.add, None, ["wif"], ["wif"])
              CP("vector", widx[:], wif, ["wif"], ["widx"])
              hl = [A16.alloc(D) for _ in range(3)]
              for it in range(NT):
                  b3 = it % 3
                  hk_ = ("hl", b3)
                  DMA("sync", hl[b3], h2_d[it * 128:(it + 1) * 128, :], [("h2", it)], [hk_], "hl%d" % b3)
                  for k in range(2):
                      off = r_slot[:, it, k:k + 1]
                      P.dma("gpsimd", (lambda e, off=off, src=hl[b3]: e.indirect_dma_start(
                          out=xs_d, out_offset=bass.IndirectOffsetOnAxis(ap=off, axis=0), in_=src, in_offset=None,
                          bounds_check=BR(e, L - 1), oob_is_err=False)), [hk_, "r_slot"], [("xs", it, k)], "xs_sc")
              XSK = [("xs", it, k) for it in range(NT) for k in range(2)]

              P.barrier()
              A16.reset(); A32.reset()
              W1 = A16.alloc(16 * 512)
              W3 = A16.alloc(16 * 512)
              W2 = A16.alloc(4 * 2048)
              xb = [A16.alloc(D) for _ in range(3)]
              xbT = [A16.alloc(16 * 128).rearrange("p (k t) -> p k t", k=16) for _ in range(2)]

              def xload(b):
                  DMA("sync", xb[b % 3], xs_d[b * 128:(b + 1) * 128, :], XSK, [("xb", b % 3)], "xb%d" % (b % 3))

              sgf = [A32.alloc(512) for _ in range(2)]
              hid = [A16.alloc(512) for _ in range(2)]
              hidT = [A16.alloc(4 * 128).rearrange("p (k t) -> p k t", k=4) for _ in range(2)]
              yb = [A32.alloc(D) for _ in range(2)]
              w1v = e_w1.rearrange("l e (p k) n -> (l e p) (k n)", k=16)
              w3v = e_w3.rearrange("l e (p k) n -> (l e p) (k n)", k=16)
              w2v = e_w2.rearrange("l e (p k) n -> (l e p) (k n)", k=4)
              bnd_l = (l + 1) * NE * 128 - 1

              def wgather(b, Wt, wv, wk):
                  off = widx[:, b:b + 1]
                  P.dma("gpsimd", (lambda e, off=off, Wt=Wt, wv=wv, bnd=bnd_l: e.indirect_dma_start(
                      out=Wt, out_offset=None, in_=wv, in_offset=bass.IndirectOffsetOnAxis(ap=off, axis=0),
                      bounds_check=BR(e, bnd), oob_is_err=False)), ["widx"], [wk], wk)

              def stageA(b):
                  b2 = b % 2
                  wgather(b, W1, w1v, "W1")
                  wgather(b, W3, w3v, "W3")
                  xk = ("xb", b % 3)
                  if b + 1 < NB:
                      xload(b + 1)
                  for kc in range(16):
                      TR(pt[:, kc * 128:(kc + 1) * 128], xb[b % 3].rearrange("p (c k) -> p k c", k=16)[:, kc, :], ident_b[:],
                         [xk, "ident_b"], [PT])
                  xT, xTk = xbT[b2], ("xbT", b2)
                  CP("scalar", xT[:, 0:8, :], pt[:, 0:1024].rearrange("p (k t) -> p k t", k=8), [PT], [xTk])
                  CP("vector", xT[:, 8:16, :], pt[:, 1024:2048].rearrange("p (k t) -> p k t", k=8), [PT], [xTk])
                  p1, p3 = 2 * b2, 2 * b2 + 1
                  for kc in range(16):
                      MM(pm[p1][:, :], xT[:, kc, :], W1[:, kc * 512:(kc + 1) * 512], kc == 0, kc == 15, [xTk, "W1"], [PM[p1]])
                  for kc in range(16):
                      MM(pm[p3][:, :], xT[:, kc, :], W3[:, kc * 512:(kc + 1) * 512], kc == 0, kc == 15, [xTk, "W3"], [PM[p3]])
                  ACT(sgf[b2], pm[p1][:, :], AF.Silu, [PM[p1]], [("sgf", b2)])
                  TT("vector", hid[b2], pm[p3][:, :], sgf[b2], ALU.mult, [PM[p3], ("sgf", b2)], [("hid", b2)])

              def stageB(b):
                  b2 = b % 2
                  wgather(b, W2, w2v, "W2")
                  for fc in range(4):
                      TR(pt[:, fc * 128:(fc + 1) * 128], hid[b2].rearrange("p (c k) -> p k c", k=4)[:, fc, :], ident_b[:],
                         [("hid", b2), "ident_b"], [PT])
                  CP("vector", hidT[b2], pt[:, 0:512].rearrange("p (k t) -> p k t", k=4), [PT], [("hidT", b2)])
                  y_, yk = yb[b2], ("yb", b2)
                  for cbk in range(4):
                      pi = cbk % 2
                      for fc in range(4):
                          MM(pr[pi][:, :], hidT[b2][:, fc, :], W2[:, fc * 2048 + cbk * 512:fc * 2048 + (cbk + 1) * 512],
                             fc == 0, fc == 3, [("hidT", b2), "W2"], [PR[pi]])
                      if cbk % 2:
                          CP("scalar", y_[:, cbk * 512:(cbk + 1) * 512], pr[pi][:, :], [PR[pi]], [yk])
                      else:
                          CP("vector", y_[:, cbk * 512:(cbk + 1) * 512], pr[pi][:, :], [PR[pi]], [yk])
                  DMA(STQ, ys_d[b * 128:(b + 1) * 128, :], y_, [yk], [("ys", b)], "yb%d" % b2)

              xload(0)
              stageA(0)
              for b in range(NB):
                  if b + 1 < NB:
                      stageA(b + 1)
                  stageB(b)
              YSK = [("ys", b) for b in range(NB)]

              stop_here('P7')
              P.barrier()
              A16.reset(); A32.reset()
              g2 = [A32.alloc(D) for _ in range(2)]
              for q in range(2):
                  DMA("sync", g2[q], mod_rep(q, 5), MODK, [("g2", q)], "g2%d" % q)
              if last:
                  fg = A32.alloc(D)
                  DMA("sync", fg, final_g.rearrange("(o n) -> o n", o=1).broadcast_to([128, D]), [], ["fg"], "fg")
              ya = [A32.alloc(D) for _ in range(2)]
              ybb = [A32.alloc(D) for _ in range(2)]
              x1t = [A32.alloc(D) for _ in range(2)]
              junk = A16.alloc(D)
              ss = A32.alloc(8)
              dst_x = y_out if last else x2_d
              for it in range(NT):
                  b2 = it % 2
                  q = 0 if it * 128 < OFFS[1] else 1
                  for k, yt in enumerate((ya, ybb)):
                      off = r_slot[:, it, k:k + 1]
                      P.dma("gpsimd", (lambda e, off=off, dst=yt[b2]: e.indirect_dma_start(
                          out=dst, out_offset=None, in_=ys_d, in_offset=bass.IndirectOffsetOnAxis(ap=off, axis=0),
                          bounds_check=BR(e, L - 1), oob_is_err=False)), YSK + ["r_slot"], [("yg", k, b2)], "yg%d%d" % (k, b2))
                  xk = ("x1t", b2)
                  DMA("sync", x1t[b2], x1_d[it * 128:(it + 1) * 128, :], [("x1", it)], [xk], "x1l%d" % b2)
                  A_, B_ = ya[b2], ybb[b2]
                  TS("vector", A_, A_, r_f[:, it, 2:3], None, ALU.mult, None, [("yg", 0, b2), "r_f"], [("yg", 0, b2)])
                  STT(A_, B_, r_f[:, it, 3:4], A_, ALU.mult, ALU.add, [("yg", 1, b2), ("yg", 0, b2), "r_f"], [("yg", 0, b2)])
                  TT("vector", A_, A_, g2[q], ALU.mult, [("yg", 0, b2), ("g2", q)], [("yg", 0, b2)])
                  TT("vector", B_, A_, x1t[b2], ALU.add, [("yg", 0, b2), xk], [("yg", 1, b2)])
                  if not last:
                      DMA(STQ, dst_x[it * 128:(it + 1) * 128, :], B_, [("yg", 1, b2)], [("xcur", it)], "xo%d" % b2)
                  else:
                      ssk = U("ss")
                      ACT(junk, B_, AF.Square, [("yg", 1, b2)], ["junk", ssk], accum_out=ss[:, 0:1])
                      ACT(ss[:, 1:2], ss[:, 0:1], AF.Sqrt, [ssk, "eps_t"], [ssk], scale=1.0 / D, bias=eps_t[:, 0:1])
                      P.op("vector", lambda e, ss=ss: e.reciprocal(out=ss[:, 2:3], in_=ss[:, 1:2]), [ssk], [ssk])
                      STT(A_, B_, ss[:, 2:3], fg, ALU.mult, ALU.mult, [("yg", 1, b2), ssk, "fg"], [("yg", 0, b2)])
                      DMA(STQ, dst_x[it * 128:(it + 1) * 128, :], A_, [("yg", 0, b2)], [("yout", it)], "xo%d" % b2)
              cur_x = x2_d

          P.emit()
        except _Stop:
            pass
    return nc


def route_tile(P, nc, sm, pr, PR, rb_, RBK, r_E, r_f, cnt_run, utinc, ones_b, git,
               TT, TS, STT, ACT, CP, RED, MM, U, part="AB", rk=None):
    if "A" not in part:
        Ef = sm[:, 96:160]
        Mb = sm[:, 160:192]
        Mbb = sm[:, 192:224].bitcast(BF16)[:, 0:32]
        return route_tile_b(P, sm, pr, PR, r_f, cnt_run, utinc, ones_b, git, TT, RED, MM, rk, Ef, Mb, Mbb)
    rk = rk if rk is not None else U("rt")
    Lg = sm[:, 8:44]
    TT("vector", Lg, pr[0][:, 0:36], rb_, ALU.add, [PR[0]] + RBK, [rk])
    m = sm[:, 44:45]
    RED(m, Lg[:, 0:4], ALU.max, [rk], [rk])
    oh = sm[:, 48:52]
    TS("vector", oh, Lg[:, 0:4], m, None, ALU.is_equal, None, [rk], [rk])
    negm = sm[:, 45:46]
    TS("vector", negm, m, -1.0, None, ALU.mult, None, [rk], [rk])
    ex = sm[:, 52:56]
    se = sm[:, 46:47]
    ACT(ex, Lg[:, 0:4], AF.Exp, [rk], [rk], bias=negm, scale=1.0, accum_out=se)
    pg = sm[:, 47:48]
    P.op("vector", lambda e: e.reciprocal(out=pg, in_=se), [rk], [rk])
    lf = sm[:, 56:64]
    TS("vector", lf, Lg[:, 4:12], oh[:, 0:1], None, ALU.mult, None, [rk], [rk])
    for g in range(1, 4):
        STT(lf, Lg[:, 4 + 8 * g:12 + 8 * g], oh[:, g:g + 1], lf, ALU.mult, ALU.add, [rk], [rk])
    top = sm[:, 64:72]
    P.op("vector", lambda e: e.max(out=top, in_=lf), [rk], [rk])
    s1 = sm[:, 72:80]
    s2 = sm[:, 80:88]
    TS("vector", s1, lf, top[:, 0:1], None, ALU.is_equal, None, [rk], [rk])
    TS("vector", s2, lf, top[:, 1:2], None, ALU.is_equal, None, [rk], [rk])
    dv = sm[:, 88:89]
    TT("vector", dv, top[:, 0:1], top[:, 1:2], ALU.subtract, [rk], [rk])
    sg = sm[:, 89:90]
    ACT(sg, dv, AF.Sigmoid, [rk], [rk])
    TT("vector", r_f[:, git, 2:3], pg, sg, ALU.mult, [rk], ["r_f"])
    TT("vector", r_f[:, git, 3:4], pg, r_f[:, git, 2:3], ALU.subtract, [rk, "r_f"], ["r_f"])
    Ef = sm[:, 96:160]
    for g in range(4):
        TS("vector", Ef[:, 8 * g:8 * g + 8], s1, oh[:, g:g + 1], None, ALU.mult, None, [rk], [rk])
        TS("vector", Ef[:, 32 + 8 * g:40 + 8 * g], s2, oh[:, g:g + 1], None, ALU.mult, None, [rk], [rk])
    CP("vector", r_E[:, git, :], Ef, [rk], ["r_E"])
    Mb = sm[:, 160:192]
    TT("vector", Mb, Ef[:, 0:32], Ef[:, 32:64], ALU.add, [rk], [rk])
    Mbb = sm[:, 192:224].bitcast(BF16)[:, 0:32]
    CP("vector", Mbb, Mb, [rk], [rk])
    if "B" in part:
        route_tile_b(P, sm, pr, PR, r_f, cnt_run, utinc, ones_b, git, TT, RED, MM, rk, Ef, Mb, Mbb)


def route_tile_b(P, sm, pr, PR, r_f, cnt_run, utinc, ones_b, git, TT, RED, MM, rk, Ef, Mb, Mbb):
    MM(pr[1][:, 0:32], utinc[:], Mbb, True, True, ["utinc", rk], [PR[1]])
    MM(pr[1][:, 32:64], ones_b[:], Mbb, True, True, ["ones_b", rk], [PR[1]])
    rank = sm[:, 224:256]
    TT("vector", rank, pr[1][:, 0:32], Mb, ALU.subtract, [PR[1], rk], [rk])
    TT("vector", rank, rank, cnt_run[:], ALU.add, [rk, "cnt_run"], [rk])
    TT("vector", cnt_run[:], cnt_run[:], pr[1][:, 32:64], ALU.add, [PR[1], "cnt_run"], ["cnt_run"])
    tmp = sm[:, 160:192]
    for k in range(2):
        TT("vector", tmp, Ef[:, 32 * k:32 * k + 32], rank, ALU.mult, [rk], [rk])
        RED(r_f[:, git, k:k + 1], tmp, ALU.add, [rk], ["r_f"])


_WNAMES = ["w_ada", "b_ada", "norm1_g", "w_in", "pool_w", "pool_scale", "conv_dw_w", "conv_dw_b",
           "conv_ln_g", "conv_ln_b", "conv_pw_w", "conv_pw_b", "fourier_w", "w_out", "norm2_g",
           "router_coarse_w", "router_coarse_b", "router_fine_w", "router_fine_b",
           "expert_w1", "expert_w3", "expert_w2", "final_g"]


def kernel(**inputs):
    xs_ = np.asarray(inputs["x_sample"], np.float32)
    xp_ = np.asarray(inputs["x_prompt"], np.float32)
    cs_ = np.asarray(inputs["c_sample"], np.float32)
    cp_ = np.asarray(inputs["c_prompt"], np.float32)
    S0, S1 = xs_.shape[1], xp_.shape[1]
    depth = inputs["w_ada"].shape[0]
    nc = build((S0, S1), depth)
    N = S0 + S1
    NB = -(-(2 * N + NE * 127) // 128)
    consts = make_consts((S0, S1), NB)
    wts = {k: np.ascontiguousarray(np.asarray(inputs[k], np.float32)) for k in _WNAMES}
    in_maps = []
    for core in range(8):
        b = core % 4
        m = {"x": np.ascontiguousarray(np.concatenate([xs_[b], xp_[b]], axis=0)),
             "c": np.ascontiguousarray(np.stack([cs_[b], cp_[b]], axis=0))}
        m.update(wts)
        m.update(consts)
        in_maps.append(m)
    res = run_bass_kernel_spmd(nc, in_maps, core_ids=list(range(8)))
    y_s = np.stack([res.results[b]["y"][:S0] for b in range(4)], axis=0)
    y_p = np.stack([res.results[b]["y"][S0:] for b in range(4)], axis=0)
    return (y_p.astype(np.float32), y_s.astype(np.float32))
```
